# Optimizing a Trainium2 kernel written in Bass

```python
import math
import jax
import jax.numpy as jnp
from jax import lax
import numpy as np

D_MODEL = 1024
BATCH = 1
SEQ = 16384
DEPTH = 2

CTX_LEN = 256
GRID_W = 64
BRANCH_W = 256
N_BRANCH = 4
CONF_K = 31
SCONV_K = 3
DIFF_HEADS = 4
DIFF_DK = 32
DIFF_DV = 2 * DIFF_DK
RET_HEADS = 4
RET_DK = 32
RET_DV = 2 * RET_DK
RET_CHUNK = 128
Q_BLOCK = 128
ROPE_BASE = 10000.0
N_EXPERTS = 16
N_GROUPS = 4
EXPERTS_PER_GROUP = N_EXPERTS // N_GROUPS
TOP_K = 2
D_FF_EXPERT = 512
MOE_BLOCK = 256
EPS = 1e-6
IN_SPLITS = (2 * BRANCH_W,
             3 * BRANCH_W,
             DIFF_HEADS * 2 * DIFF_DK,
             DIFF_HEADS * 2 * DIFF_DK,
             DIFF_HEADS * DIFF_DV,
             RET_HEADS * RET_DK,
             RET_HEADS * RET_DK,
             RET_HEADS * RET_DV,
             BRANCH_W)
IN_COLS = sum(IN_SPLITS)

kernel_name = 'hybrid_gated_branch_dit_moe'


def rms_norm(x, g):
    xf = x.astype(jnp.float32)
    y = xf * lax.rsqrt(jnp.mean(xf * xf, axis=-1, keepdims=True) + EPS)
    return (y * g.astype(jnp.float32)).astype(x.dtype)


def layer_norm(x, g=None, b=None):
    xf = x.astype(jnp.float32)
    mu = jnp.mean(xf, axis=-1, keepdims=True)
    var = jnp.mean(jnp.square(xf - mu), axis=-1, keepdims=True)
    y = (xf - mu) * lax.rsqrt(var + EPS)
    if g is not None:
        y = y * g.astype(jnp.float32) + b.astype(jnp.float32)
    return y.astype(x.dtype)


def adaln(cv, w, b):
    return jnp.split(jax.nn.silu(cv) @ w + b, 6, axis=-1)


def modulate(h, shift, scale):
    return h * (1.0 + scale) + shift


def split_cols(z):
    points = np.cumsum(np.array(IN_SPLITS))[:-1].tolist()
    return jnp.split(z, points, axis=-1)


def axial_rope(length, dim):
    rows = length // GRID_W
    row = jnp.repeat(jnp.arange(rows, dtype=jnp.float32), GRID_W)
    col = jnp.tile(jnp.arange(GRID_W, dtype=jnp.float32), rows)
    nf = dim // 4
    inv = ROPE_BASE ** (-jnp.arange(nf, dtype=jnp.float32) / nf)
    ang = jnp.concatenate([row[:, None] * inv, col[:, None] * inv], axis=-1)
    return jnp.cos(ang), jnp.sin(ang)


def apply_rope(x, cos, sin):
    x1, x2 = jnp.split(x.astype(jnp.float32), 2, axis=-1)
    c = cos[None, :, None, :]
    s = sin[None, :, None, :]
    return jnp.concatenate([x1 * c - x2 * s, x1 * s + x2 * c], axis=-1).astype(x.dtype)


def depthwise_conv(x, w):
    k = w.shape[0]
    pad = (k - 1) // 2
    return lax.conv_general_dilated(x, w[:, None, :].astype(x.dtype), window_strides=(1,),
                                    padding=[(pad, pad)], dimension_numbers=('NWC', 'WIO', 'NWC'),
                                    feature_group_count=x.shape[-1])


def conformer_conv(z, w_dw, b_dw, g_n, b_n):
    a, gt = jnp.split(z, 2, axis=-1)
    u = a * jax.nn.sigmoid(gt)
    u = depthwise_conv(u, w_dw) + b_dw
    return jax.nn.silu(layer_norm(u, g_n, b_n))


def short_conv(z, w_dw):
    bg, cg, xv = jnp.split(z, 3, axis=-1)
    return bg * depthwise_conv(cg * xv, w_dw)


def split_maps(z, rope):
    b, l, _ = z.shape
    z = z.reshape(b, l, DIFF_HEADS, 2, DIFF_DK)
    z1, z2 = z[..., 0, :], z[..., 1, :]
    if rope is not None:
        z1 = apply_rope(z1, *rope)
        z2 = apply_rope(z2, *rope)
    return z1, z2


def two_map_attention(q1, q2, k1, k2, v, lam):
    scale = DIFF_DK ** -0.5
    s1 = jnp.einsum('bqhd,bkhd->bhqk', q1, k1).astype(jnp.float32) * scale
    s2 = jnp.einsum('bqhd,bkhd->bhqk', q2, k2).astype(jnp.float32) * scale
    a = jax.nn.softmax(s1, axis=-1) - lam * jax.nn.softmax(s2, axis=-1)
    return jnp.einsum('bhqk,bkhe->bqhe', a.astype(v.dtype), v)


def diff_attention_latent(q1, q2, k1, k2, v, lam):
    b, l, h, dk = q1.shape
    nb = l // Q_BLOCK
    qb1 = jnp.moveaxis(q1.reshape(b, nb, Q_BLOCK, h, dk), 1, 0)
    qb2 = jnp.moveaxis(q2.reshape(b, nb, Q_BLOCK, h, dk), 1, 0)
    o = lax.map(lambda qs: two_map_attention(qs[0], qs[1], k1, k2, v, lam), (qb1, qb2))
    return jnp.moveaxis(o, 0, 1).reshape(b, l, h, DIFF_DV)


def diff_lambda(lq1, lk1, lq2, lk2, lam_init):
    f = jnp.float32
    return (jnp.exp(jnp.sum(lq1.astype(f) * lk1.astype(f))) -
            jnp.exp(jnp.sum(lq2.astype(f) * lk2.astype(f))) + lam_init)


def diff_out(o, g, lam_init):
    b, l = o.shape[:2]
    return (rms_norm(o, g) * (1.0 - lam_init)).reshape(b, l, DIFF_HEADS * DIFF_DV)


def retention_chunked(q, k, v, log_gamma, s0):
    f = jnp.float32
    b, l, h, dk = q.shape
    dv = v.shape[-1]
    n = l // RET_CHUNK
    qc = q.reshape(b, n, RET_CHUNK, h, dk)
    kc = k.reshape(b, n, RET_CHUNK, h, dk)
    vc = v.reshape(b, n, RET_CHUNK, h, dv)
    lg = log_gamma.astype(f)
    pos = jnp.arange(RET_CHUNK, dtype=f)
    diff = pos[:, None] - pos[None, :]
    dmask = jnp.exp(jnp.where(diff[None] >= 0, diff[None] * lg[:, None, None], -jnp.inf))
    scores = jnp.einsum('bnihd,bnjhd->bnhij', qc, kc).astype(f) * dmask
    o_intra = jnp.einsum('bnhij,bnjhe->bnihe', scores, vc.astype(f))
    q_dec = jnp.exp((pos + 1.0)[:, None] * lg[None, :])
    k_dec = jnp.exp((RET_CHUNK - 1.0 - pos)[:, None] * lg[None, :])
    kv = jnp.einsum('bnjhd,bnjhe->bnhde', kc.astype(f) * k_dec[:, :, None], vc.astype(f))
    chunk_dec = jnp.exp(RET_CHUNK * lg)[None, :, None, None]

    def step(s, kv_n):
        return chunk_dec * s + kv_n, s

    s_final, s_start = lax.scan(step, s0.astype(f), jnp.moveaxis(kv, 1, 0))
    o_cross = jnp.einsum('bnihd,nbhde->bnihe', qc.astype(f) * q_dec[:, :, None], s_start)
    o = (o_intra + o_cross).reshape(b, l, h, dv)
    return o.astype(v.dtype), s_final


def bidir_retention(q, k, v, ld_f, ld_b, s0_f, s0_b):
    o_f, s_f = retention_chunked(q, k, v, ld_f, s0_f)
    o_b, s_b = retention_chunked(jnp.flip(q, 1), jnp.flip(k, 1), jnp.flip(v, 1), ld_b, s0_b)
    return o_f + jnp.flip(o_b, 1), s_f, s_b


def ret_qkv(z, rope):
    b, l, _ = z[5].shape
    q = z[5].reshape(b, l, RET_HEADS, RET_DK)
    k = z[6].reshape(b, l, RET_HEADS, RET_DK) * (RET_DK ** -0.5)
    v = z[7].reshape(b, l, RET_HEADS, RET_DV)
    if rope is not None:
        q = apply_rope(q, *rope)
        k = apply_rope(k, *rope)
    return q, k, v


def retention_out(o, g):
    b, l = o.shape[:2]
    return jax.nn.silu(g) * layer_norm(o).reshape(b, l, RET_HEADS * RET_DV)


def merge_branches(h, ys, w_gate, b_gate, w_branch, w_o):
    gates = jax.nn.sigmoid((h @ w_gate + b_gate).astype(jnp.float32)).astype(h.dtype)
    gates = gates.reshape(h.shape[:-1] + (N_BRANCH, D_MODEL))
    m = gates[..., 0, :] * (ys[0] @ w_branch[0])
    for i in range(1, N_BRANCH):
        m = m + gates[..., i, :] * (ys[i] @ w_branch[i])
    return m @ w_o


def token_mixers(hl, hc, p, lam_init, last):
    b, l, _ = hl.shape
    zl = split_cols(hl @ p['w_in'])
    zc = split_cols(hc @ p['w_in'])
    rope = axial_rope(l, DIFF_DK)
    lam = diff_lambda(p['lam_q1'], p['lam_k1'], p['lam_q2'], p['lam_k2'], lam_init)
    kc1, kc2 = split_maps(zc[3], None)
    vc = zc[4].reshape(b, -1, DIFF_HEADS, DIFF_DV)
    q1, q2 = split_maps(zl[2], rope)
    k1, k2 = split_maps(zl[3], rope)
    vl = zl[4].reshape(b, l, DIFF_HEADS, DIFF_DV)
    o_attn = diff_attention_latent(q1, q2, jnp.concatenate([kc1, k1], 1), jnp.concatenate([kc2, k2], 1),
                                   jnp.concatenate([vc, vl], 1), lam)
    qrc, krc, vrc = ret_qkv(zc, None)
    qrl, krl, vrl = ret_qkv(zl, rope)
    s0 = jnp.zeros((b, RET_HEADS, RET_DK, RET_DV), jnp.float32)
    orc, s_f, s_b = bidir_retention(qrc, krc, vrc, p['ret_ld_f'], p['ret_ld_b'], s0, s0)
    orl, _, _ = bidir_retention(qrl, krl, vrl, p['ret_ld_f'], p['ret_ld_b'], s_f, s_b)
    ys_l = [conformer_conv(zl[0], p['conv_a_w'], p['conv_a_b'], p['conv_a_g'], p['conv_a_beta']),
            short_conv(zl[1], p['conv_b_w']),
            diff_out(o_attn, p['diff_g'], lam_init),
            retention_out(orl, zl[8])]
    yl = merge_branches(hl, ys_l, p['w_gate'], p['b_gate'], p['w_branch'], p['w_o'])
    if last:
        return yl, None
    qc1, qc2 = split_maps(zc[2], None)
    ys_c = [conformer_conv(zc[0], p['conv_a_w'], p['conv_a_b'], p['conv_a_g'], p['conv_a_beta']),
            short_conv(zc[1], p['conv_b_w']),
            diff_out(two_map_attention(qc1, qc2, kc1, kc2, vc, lam), p['diff_g'], lam_init),
            retention_out(orc, zc[8])]
    yc = merge_branches(hc, ys_c, p['w_gate'], p['b_gate'], p['w_branch'], p['w_o'])
    return yl, yc


def grouped_moe(h, w_router, b_router, w1, w3, w2):
    f = jnp.float32
    t = h.shape[0]
    s = jax.nn.sigmoid((h @ w_router).astype(f))
    sb = s + b_router.astype(f)
    gscore = lax.top_k(sb.reshape(t, N_GROUPS, EXPERTS_PER_GROUP), 2)[0].sum(-1)
    gsel = jnp.argmax(gscore, axis=-1)
    egroup = jnp.arange(N_EXPERTS) // EXPERTS_PER_GROUP
    sb_m = jnp.where(egroup[None, :] == gsel[:, None], sb, -jnp.inf)
    _, eidx = lax.top_k(sb_m, TOP_K)
    wsel = jnp.take_along_axis(s, eidx, axis=-1)
    wsel = wsel / jnp.sum(wsel, axis=-1, keepdims=True)
    a = t * TOP_K
    e_flat = eidx.reshape(a)
    w_flat = wsel.reshape(a)
    tok_flat = jnp.repeat(jnp.arange(t, dtype=jnp.int32), TOP_K)
    order = jnp.argsort(e_flat)
    e_sorted = e_flat[order]
    counts = jnp.bincount(e_flat, length=N_EXPERTS)
    start = jnp.cumsum(counts) - counts
    padded = (counts + MOE_BLOCK - 1) // MOE_BLOCK * MOE_BLOCK
    pend = jnp.cumsum(padded)
    pstart = pend - padded
    pos = pstart[e_sorted] + jnp.arange(a) - start[e_sorted]
    cap = (a + N_EXPERTS * MOE_BLOCK + MOE_BLOCK - 1) // MOE_BLOCK * MOE_BLOCK
    nb = cap // MOE_BLOCK
    buf_tok = jnp.zeros((cap,), jnp.int32).at[pos].set(tok_flat[order])
    buf_w = jnp.zeros((cap,), f).at[pos].set(w_flat[order])
    blk_e = jnp.minimum(jnp.searchsorted(pend, jnp.arange(nb) * MOE_BLOCK, side='right'), N_EXPERTS - 1)

    def expert_block(args):
        idx, wt, e = args
        xb = h[idx]
        u = jax.nn.silu(xb @ w1[e]) * (xb @ w3[e])
        return (u @ w2[e]) * wt[:, None].astype(h.dtype)

    y = lax.map(expert_block, (buf_tok.reshape(nb, MOE_BLOCK), buf_w.reshape(nb, MOE_BLOCK), blk_e))
    return jnp.zeros_like(h).at[buf_tok].add(y.reshape(cap, D_MODEL))


def setup_inputs(seed: int = 0) -> dict:
    key = jax.random.key(seed)
    ks = jax.random.split(key, 40)
    D = D_MODEL

    def nrm(k, shape, scale):
        return jax.random.normal(k, shape, jnp.float32) * scale

    base_ld = jnp.log(1.0 - 2.0 ** (-5.0 - jnp.arange(RET_HEADS, dtype=jnp.float32)))
    return {
        'x': nrm(ks[0], (BATCH, SEQ, D), 1.0),
        'c': nrm(ks[1], (BATCH, D), 1.0),
        'ctx': nrm(ks[2], (BATCH, CTX_LEN, D), 1.0),
        'c_ctx': nrm(ks[3], (D,), 1.0),
        'w_mod': nrm(ks[4], (DEPTH, D, 6 * D), 0.3 * D ** -0.5),
        'b_mod': nrm(ks[5], (DEPTH, 6 * D), 0.02),
        'g_norm1': 1.0 + nrm(ks[6], (DEPTH, D), 0.02),
        'g_norm2': 1.0 + nrm(ks[7], (DEPTH, D), 0.02),
        'w_in': nrm(ks[8], (DEPTH, D, IN_COLS), D ** -0.5),
        'conv_a_w': nrm(ks[9], (DEPTH, CONF_K, BRANCH_W), CONF_K ** -0.5),
        'conv_a_b': nrm(ks[10], (DEPTH, BRANCH_W), 0.02),
        'conv_a_g': 1.0 + nrm(ks[11], (DEPTH, BRANCH_W), 0.02),
        'conv_a_beta': nrm(ks[12], (DEPTH, BRANCH_W), 0.02),
        'conv_b_w': nrm(ks[13], (DEPTH, SCONV_K, BRANCH_W), SCONV_K ** -0.5),
        'lam_q1': nrm(ks[14], (DEPTH, DIFF_DK), 0.1),
        'lam_k1': nrm(ks[15], (DEPTH, DIFF_DK), 0.1),
        'lam_q2': nrm(ks[16], (DEPTH, DIFF_DK), 0.1),
        'lam_k2': nrm(ks[17], (DEPTH, DIFF_DK), 0.1),
        'diff_g': 1.0 + nrm(ks[18], (DEPTH, DIFF_DV), 0.02),
        'ret_ld_f': base_ld[None, :] * (1.0 + nrm(ks[19], (DEPTH, RET_HEADS), 0.1)),
        'ret_ld_b': base_ld[None, :] * (1.0 + nrm(ks[20], (DEPTH, RET_HEADS), 0.1)),
        'w_gate': nrm(ks[21], (DEPTH, D, N_BRANCH * D), D ** -0.5),
        'b_gate': nrm(ks[22], (DEPTH, N_BRANCH * D), 0.02),
        'w_branch': nrm(ks[23], (DEPTH, N_BRANCH, BRANCH_W, D), BRANCH_W ** -0.5),
        'w_o': nrm(ks[24], (DEPTH, D, D), D ** -0.5),
        'w_router': nrm(ks[25], (D, N_EXPERTS), D ** -0.5),
        'b_router': nrm(ks[26], (N_EXPERTS,), 0.01),
        'w1_e': nrm(ks[27], (DEPTH, N_EXPERTS, D, D_FF_EXPERT), D ** -0.5),
        'w3_e': nrm(ks[28], (DEPTH, N_EXPERTS, D, D_FF_EXPERT), D ** -0.5),
        'w2_e': nrm(ks[29], (DEPTH, N_EXPERTS, D_FF_EXPERT, D), D_FF_EXPERT ** -0.5),
        'g_final': 1.0 + nrm(ks[30], (D,), 0.02),
    }


def reference(x, c, ctx, c_ctx, w_mod, b_mod, g_norm1, g_norm2, w_in, conv_a_w, conv_a_b, conv_a_g,
              conv_a_beta, conv_b_w, lam_q1, lam_k1, lam_q2, lam_k2, diff_g, ret_ld_f, ret_ld_b,
              w_gate, b_gate, w_branch, w_o, w_router, b_router, w1_e, w3_e, w2_e, g_final):
    xl, xc = x, ctx
    for l in range(DEPTH):
        last = l == DEPTH - 1
        lam_init = 0.8 - 0.6 * math.exp(-0.3 * l)
        p = {'w_in': w_in[l], 'conv_a_w': conv_a_w[l], 'conv_a_b': conv_a_b[l], 'conv_a_g': conv_a_g[l],
             'conv_a_beta': conv_a_beta[l], 'conv_b_w': conv_b_w[l], 'lam_q1': lam_q1[l], 'lam_k1': lam_k1[l],
             'lam_q2': lam_q2[l], 'lam_k2': lam_k2[l], 'diff_g': diff_g[l], 'ret_ld_f': ret_ld_f[l],
             'ret_ld_b': ret_ld_b[l], 'w_gate': w_gate[l], 'b_gate': b_gate[l], 'w_branch': w_branch[l],
             'w_o': w_o[l]}
        mod_l = [m[:, None, :] for m in adaln(c, w_mod[l], b_mod[l])]
        mod_c = adaln(c_ctx, w_mod[l], b_mod[l])
        hl = modulate(rms_norm(xl, g_norm1[l]), mod_l[0], mod_l[1])
        hc = modulate(rms_norm(xc, g_norm1[l]), mod_c[0], mod_c[1])
        yl, yc = token_mixers(hl, hc, p, lam_init, last)
        xl = xl + mod_l[2] * yl
        hl2 = modulate(rms_norm(xl, g_norm2[l]), mod_l[3], mod_l[4])
        n_lat = hl2.shape[0] * hl2.shape[1]
        if last:
            ml = grouped_moe(hl2.reshape(n_lat, D_MODEL), w_router, b_router, w1_e[l], w3_e[l], w2_e[l])
            xl = xl + mod_l[5] * ml.reshape(xl.shape)
        else:
            xc = xc + mod_c[2] * yc
            hc2 = modulate(rms_norm(xc, g_norm2[l]), mod_c[3], mod_c[4])
            tokens = jnp.concatenate([hl2.reshape(n_lat, D_MODEL), hc2.reshape(-1, D_MODEL)], axis=0)
            m = grouped_moe(tokens, w_router, b_router, w1_e[l], w3_e[l], w2_e[l])
            xl = xl + mod_l[5] * m[:n_lat].reshape(xl.shape)
            xc = xc + mod_c[5] * m[n_lat:].reshape(xc.shape)
    return rms_norm(xl, g_final)
```

```python
import math
from contextlib import ExitStack
import numpy as np
import ml_dtypes
import concourse.bass as bass
import concourse.mybir as mybir
from concourse.bass_utils import run_bass_kernel_spmd

F32 = mybir.dt.float32
BF16 = mybir.dt.bfloat16
AF = mybir.ActivationFunctionType
ALU = mybir.AluOpType
AX = mybir.AxisListType
NPBF = ml_dtypes.bfloat16

NCORE = 8
D = 1024
SEQ = 16384
TL = SEQ // NCORE
TC = 256
DEPTH = 2
INC = 2816
EPS = 1e-6
NEXP = 16
DFF = 512
QSCALE = 32 ** -0.5
import os
ATT_ROW = os.environ.get("ATT_ROW", "0") == "1"
MOE_CAP = 768
NSLOT = NEXP * MOE_CAP


class Op:
    __slots__ = ("id", "eng", "fn", "deps", "dma", "n", "signal", "val", "cc")


class Sched:
    ENGS = (("sp", "sync"), ("act", "scalar"), ("dve", "vector"), ("pool", "gpsimd"), ("pe", "tensor"))

    def __init__(self, nc, stack):
        self.nc = nc
        self.ops = []
        self.phase_start = 0
        self.lastw = {}
        self.readers = {}
        self.K = dict(sp=12, pool=8, act=4)
        self.dma_list = {e: [] for e in self.K}
        self.sems = {e: stack.enter_context(nc.semaphore("sm_" + e)) for e in ("pe", "act", "dve", "pool")}
        self.dsems = {e: [stack.enter_context(nc.semaphore("sd_%s%d" % (e, i))) for i in range(k)]
                      for e, k in self.K.items()}
        self.ccsems = [stack.enter_context(nc.semaphore("sc_%d" % i)) for i in range(12)]
        self.ncc = 0
        self.cc_list = []
        self.cnt = {e: 0 for e in self.sems}
        self.waited = {e: {} for e, _ in self.ENGS}

    def add(self, eng, fn, r=(), w=(), dma=False, cc=False):
        i = len(self.ops)
        deps = {}
        for k in r:
            j = self.lastw.get(k)
            if j is not None:
                deps[j] = True
        for k in w:
            j = self.lastw.get(k)
            if j is not None and j not in deps:
                deps[j] = False
            rd = self.readers.get(k)
            if rd:
                for j in rd.values():
                    if isinstance(j, list):
                        for jj in j:
                            deps.setdefault(jj, False)
                    else:
                        deps.setdefault(j, False)
        n = None
        if dma:
            lst = self.dma_list[eng]
            n = len(lst)
            if n >= self.K[eng]:
                deps.setdefault(lst[n - self.K[eng]], False)
            lst.append(i)
        op = Op()
        op.id, op.eng, op.fn, op.deps, op.dma, op.n, op.signal, op.val = i, eng, fn, deps, dma, n, False, 0
        op.cc = None
        if cc:
            op.dma = True
            op.cc = self.ncc
            self.ncc += 1
            self.cc_list.append(i)
            dma = True
        self.ops.append(op)
        for k in r:
            rd = self.readers.setdefault(k, {})
            if dma:
                rd.setdefault("dma", []).append(i)
            else:
                rd[eng] = i
        for k in w:
            self.lastw[k] = i
            self.readers[k] = {}
        return i

    def _needed(self, op, dj, raw):
        if dj.dma:
            return True
        if dj.eng == op.eng:
            if op.dma:
                return True
            return raw and op.eng != "pe"
        return True

    def end_phase(self, name=None):
        nc = self.nc
        ps = self.phase_start
        for e in self.K:
            lst = [i for i in self.dma_list[e][-self.K[e]:] if i >= ps]
            if e == "pool":
                lst = lst + [i for i in self.cc_list if i >= ps]
            if lst:
                i = self.add(e, lambda h: h.nop())
                for j in lst:
                    self.ops[i].deps[j] = True
        ops = self.ops
        for op in ops[ps:]:
            latest = {}
            for j, raw in op.deps.items():
                if j < ps:
                    continue
                dj = ops[j]
                if not dj.dma and self._needed(op, dj, raw):
                    if latest.get(dj.eng, -1) < j:
                        latest[dj.eng] = j
            for j in latest.values():
                ops[j].signal = True
        for op in ops[ps:]:
            if op.signal and not op.dma:
                self.cnt[op.eng] += 1
                op.val = self.cnt[op.eng]
        with nc.Block() as block:
            for e, bn in self.ENGS:
                ops_e = [op for op in ops[ps:] if op.eng == e]
                if not ops_e:
                    continue

                def body(h, ops_e=ops_e, e=e):
                    self._emit(e, h, ops_e, ps)
                getattr(block, bn)(body)
        self.phase_start = len(ops)
        self.lastw = {k: v for k, v in self.lastw.items() if isinstance(k, str) and k.startswith("D:")}
        self.readers = {k: {} for k in self.lastw}
        for op in ops[:self.phase_start]:
            op.fn = None

    def _emit(self, e, h, ops_e, ps):
        ops = self.ops
        waited = self.waited[e]
        for op in ops_e:
            want = {}
            for j, raw in op.deps.items():
                if j < ps:
                    continue
                dj = ops[j]
                if not self._needed(op, dj, raw):
                    continue
                if dj.cc is not None:
                    key = ("cc", dj.cc)
                    sem = self.ccsems[dj.cc]
                    val = 1
                elif dj.dma:
                    K = self.K[dj.eng]
                    key = (dj.eng, dj.n % K)
                    sem = self.dsems[dj.eng][dj.n % K]
                    val = 16 * (dj.n // K + 1)
                else:
                    key = dj.eng
                    sem = self.sems[dj.eng]
                    if key in want and want[key][2] > j:
                        continue
                    want[key] = (sem, dj.val, j)
                    continue
                if key not in want or want[key][1] < val:
                    want[key] = (sem, val, j)
            for key, (sem, val, _j) in want.items():
                if waited.get(key, 0) >= val:
                    continue
                h.wait_ge(sem, val)
                waited[key] = val
            inst = op.fn(h)
            if op.cc is not None:
                inst.then_inc(self.ccsems[op.cc])
            elif op.dma:
                inst.then_inc(self.dsems[e][op.n % self.K[e]], 16)
            elif op.signal:
                inst.then_inc(self.sems[e], 1)


class Ctx:
    def __init__(self, nc, S):
        self.nc = nc
        self.S = S
        self.stack = None
        self.uid = 0

    def psum(self, name, shape, dt=F32):
        self.uid += 1
        return self.stack.enter_context(self.nc.psum_tensor("%s_%d" % (name, self.uid), list(shape), dt))

    def begin(self, nf=8, nb=0):
        self.stack = ExitStack()
        self.ps = [self.stack.enter_context(self.nc.psum_tensor("ps%d_%d" % (i, self.uid), [128, 512], F32))
                   for i in range(nf)]
        self.psb = [self.stack.enter_context(self.nc.psum_tensor("psb%d_%d" % (i, self.uid), [128, 1024], BF16))
                    for i in range(nb)]
        self.uid += 1

    def end(self):
        self.S.end_phase()
        self.stack.close()
        self.stack = None

    def sb(self, name, shape, dt=F32):
        self.uid += 1
        return self.stack.enter_context(self.nc.sbuf_tensor("%s_%d" % (name, self.uid), list(shape), dt))

    def dma(self, eng, out, in_, r=(), w=(), **kw):
        return self.S.add(eng, lambda h: h.dma_start(out=out, in_=in_, **kw), r=r, w=w, dma=True)

    def mm(self, out, lhsT, rhs, start, stop, r=(), w=(), **kw):
        return self.S.add("pe", lambda h: h.matmul(out, lhsT, rhs, start=start, stop=stop, **kw), r=r, w=w)

    def tr(self, out, in_, ident, r=(), w=()):
        return self.S.add("pe", lambda h: h.transpose(out, in_, ident), r=r, w=w)

    def act(self, out, in_, func, r=(), w=(), eng="act", **kw):
        return self.S.add(eng, lambda h: h.activation(out=out, in_=in_, func=func, **kw), r=r, w=w)

    def tt(self, out, in0, in1, op, r=(), w=(), eng="dve"):
        return self.S.add(eng, lambda h: h.tensor_tensor(out=out, in0=in0, in1=in1, op=op), r=r, w=w)

    def ts(self, out, in0, s1, op0, s2=None, op1=None, r=(), w=(), eng="dve", **kw):
        if op1 is None:
            return self.S.add(eng, lambda h: h.tensor_scalar(out=out, in0=in0, scalar1=s1, scalar2=None, op0=op0, **kw),
                              r=r, w=w)
        return self.S.add(eng, lambda h: h.tensor_scalar(out=out, in0=in0, scalar1=s1, scalar2=s2, op0=op0, op1=op1, **kw),
                          r=r, w=w)

    def stt(self, out, in0, scalar, in1, op0, op1, r=(), w=()):
        return self.S.add("dve", lambda h: h.scalar_tensor_tensor(out=out, in0=in0, scalar=scalar, in1=in1,
                                                                    op0=op0, op1=op1), r=r, w=w)

    def copy(self, out, in_, r=(), w=(), eng="dve"):
        if eng == "act":
            return self.S.add("act", lambda h: h.activation(out=out, in_=in_, func=AF.Copy), r=r, w=w)
        return self.S.add(eng, lambda h: h.tensor_copy(out=out, in_=in_), r=r, w=w)

    def memset(self, ap, val, w=(), eng="dve"):
        return self.S.add(eng, lambda h: h.memset(ap, val), w=w)

    def red(self, out, in_, op, r=(), w=(), axis=None):
        ax = AX.X if axis is None else axis
        return self.S.add("dve", lambda h: h.tensor_reduce(out=out, in_=in_, axis=ax, op=op), r=r, w=w)

    def recip(self, out, in_, r=(), w=()):
        return self.S.add("dve", lambda h: h.reciprocal(out=out, in_=in_), r=r, w=w)


def phase_mods(cx, l, dr):
    cx.begin()
    cv = cx.sb("cv", [128, 8, 2])
    sc = cx.sb("sc", [128, 8, 2])
    acc = cx.sb("macc", [2, 6144])
    bm = cx.sb("mbm", [2, 6144])
    g1b = cx.sb("g1b", [2, 1024])
    g2b = cx.sb("g2b", [2, 1024])
    mv = cx.sb("mv", [2, 6, 1024])
    wm = [cx.sb("wm%d" % i, [128, 6144]) for i in range(2)]
    for s, src in enumerate((dr["c"], dr["c_ctx"])):
        for kc in range(8):
            cx.dma("sp", cv[:, kc, s:s + 1], src[0:1, kc * 128:(kc + 1) * 128].rearrange("o p -> p o"), w=["cv"])
    cx.dma("sp", bm[:, :], dr["b_mod"][l, :].partition_broadcast(2), w=["bm"])
    cx.dma("sp", g1b[:, :], dr["g_norm1"][l, :].partition_broadcast(2), w=["g1b"])
    cx.dma("sp", g2b[:, :], dr["g_norm2"][l, :].partition_broadcast(2), w=["g2b"])
    cx.act(sc[:, :, :], cv[:, :, :], AF.Silu, r=["cv"], w=["sc"])
    for kc in range(8):
        b = kc % 2
        cx.dma("sp", wm[b][:, :], dr["w_mod"][l, kc * 128:(kc + 1) * 128, :], w=["wm%d" % b])
        for n in range(12):
            p = cx.ps[n % 4]
            cx.mm(p[0:2, :], sc[:, kc, :], wm[b][:, n * 512:(n + 1) * 512], True, True,
                  r=["sc", "wm%d" % b], w=["ps%d" % (n % 4)])
            a = acc[:, n * 512:(n + 1) * 512]
            if kc == 0:
                cx.tt(a, p[0:2, :], bm[:, n * 512:(n + 1) * 512], ALU.add, r=["ps%d" % (n % 4), "bm"], w=["macc%d" % n])
            else:
                cx.tt(a, p[0:2, :], a, ALU.add, r=["ps%d" % (n % 4), "macc%d" % n], w=["macc%d" % n])
    allacc = ["macc%d" % n for n in range(12)]
    cx.stt(mv[:, 0, :], acc[:, 1024:2048], 1.0, g1b[:, :], ALU.add, ALU.mult, r=allacc + ["g1b"], w=["mv0"])
    cx.copy(mv[:, 1, :], acc[:, 0:1024], r=allacc, w=["mv1"])
    cx.copy(mv[:, 2, :], acc[:, 2048:3072], r=allacc, w=["mv2"])
    cx.stt(mv[:, 3, :], acc[:, 4096:5120], 1.0, g2b[:, :], ALU.add, ALU.mult, r=allacc + ["g2b"], w=["mv3"])
    cx.copy(mv[:, 4, :], acc[:, 3072:4096], r=allacc, w=["mv4"])
    cx.copy(mv[:, 5, :], acc[:, 5120:6144], r=allacc, w=["mv5"])
    cx.dma("sp", dr["modv"][l, :, :, :], mv[:, :, :], r=["mv%d" % i for i in range(6)], w=["D:modv%d" % l])
    cx.end()


CST = {}
_off = 0
for _n, _w in (("c127mj", 1), ("cj", 1), ("ef_l", 16), ("eb_l", 16), ("ef_c", 2), ("eb_c", 2), ("ip1", 128),
               ("m128i", 128), ("D1", 128), ("D2", 128), ("U", 128), ("Lo", 128), ("I2", 128), ("ones", 128), ("I1", 128), ("hm", 4), ("bm8", 8), ("Ltri", 128), ("eoff", 16)):
    CST[_n] = (_off, _off + _w)
    _off += _w
CSTW = _off


def make_cst():
    c = np.zeros((128, CSTW), np.float32)
    p = np.arange(128, dtype=np.float32)
    i = np.arange(128, dtype=np.float32)

    def put(n, v):
        a, b = CST[n]
        c[:, a:b] = v
    put("c127mj", (127 - p)[:, None])
    put("cj", p[:, None])
    put("ef_l", (128.0 * (15 - np.arange(16)))[None, :])
    put("eb_l", (128.0 * np.arange(16))[None, :])
    put("ef_c", (128.0 * (1 - np.arange(2)))[None, :])
    put("eb_c", (128.0 * np.arange(2))[None, :])
    put("ip1", (i + 1)[None, :])
    put("m128i", (128 - i)[None, :])
    dd = i[None, :] - p[:, None]
    put("D1", np.maximum(dd, 0))
    put("D2", np.maximum(-dd, 0))
    put("U", (dd > 0).astype(np.float32))
    put("Lo", (dd < 0).astype(np.float32))
    put("I2", 2.0 * (dd == 0))
    put("ones", 1.0)
    put("I1", (dd == 0).astype(np.float32))
    put("hm", (p[:, None] // 32 == np.arange(4)[None, :]).astype(np.float32))
    put("bm8", np.tile((p[:, None] // 32 == np.arange(4)[None, :]).astype(np.float32), (1, 2)))
    put("Ltri", (dd > 0).astype(np.float32))
    put("eoff", (float(MOE_CAP) * np.arange(16))[None, :])
    return c


def rope_tables(core):
    t = np.arange(core * TL, (core + 1) * TL)
    row = (t // 64).astype(np.float32)
    col = (t % 64).astype(np.float32)
    inv = (np.float32(10000.0) ** (-np.arange(8, dtype=np.float32) / np.float32(8))).astype(np.float32)
    ang = np.concatenate([row[:, None] * inv[None, :], col[:, None] * inv[None, :]], axis=1).astype(np.float32)
    cos = np.cos(ang).astype(np.float32).T
    sin = np.sin(ang).astype(np.float32).T
    return np.tile(cos, (8, 1)).copy(), np.tile(sin, (8, 1)).copy()


def cs(cst, name):
    a, b = CST[name]
    return cst[:, a:b]


def phase_a(cx, l, dr, segs, wl=None):
    wl = l if wl is None else wl
    cx.begin(nf=6, nb=2)
    ps, psb = cx.ps, cx.psb
    W = cx.sb("W", [128, 8, INC], BF16)
    WR = cx.sb("WR", [128, 8, 768], BF16)
    cst = cx.sb("cst", [128, CSTW])
    ident = cx.sb("ident", [128, 128], BF16)
    cx.dma("sp", cst[:, :], dr["cst"][:, :], w=["cst"])
    cx.dma("sp", ident[:, :], dr["ident"][:, :], w=["ident"])
    for kc in range(8):
        cx.dma("pool", W[:, kc, :], dr["w_in"][wl, kc * 128:(kc + 1) * 128, :], w=["W%d" % kc])
    for kc in range(8):
        cx.ts(W[:, kc, 2176:2304], W[:, kc, 2176:2304], QSCALE, ALU.mult, r=["W%d" % kc], w=["W%d" % kc], eng="pool")
        for (s0, n, o0) in ((1280, 512, 0), (2048, 256, 512)):
            src = W[:, kc, s0:s0 + n].rearrange("p (b t d) -> p b t d", t=2, d=16)
            dst = WR[:, kc, o0:o0 + n].rearrange("p (b t d) -> p b t d", t=2, d=16)
            cx.ts(dst[:, :, 0, :], src[:, :, 1, :], -1.0, ALU.mult, r=["W%d" % kc], w=["WR%d" % kc], eng="pool")
            cx.copy(dst[:, :, 1, :], src[:, :, 0, :], r=["W%d" % kc], w=["WR%d" % kc], eng="pool")
    Wk = ["W%d" % kc for kc in range(8)]
    WRk = ["WR%d" % kc for kc in range(8)]
    lgf = cx.sb("lgf", [128, 4]); lgb = cx.sb("lgb", [128, 4])
    lgfc = cx.sb("lgfc", [128, 1]); lgbc = cx.sb("lgbc", [128, 1])
    cx.dma("sp", lgf[:, :], dr["ret_ld_f"][l, :].partition_broadcast(128), w=["lgf"])
    cx.dma("sp", lgb[:, :], dr["ret_ld_b"][l, :].partition_broadcast(128), w=["lgb"])
    for h in range(4):
        cx.dma("sp", lgfc[32 * h:32 * h + 32, :], dr["ret_ld_f"][l, h:h + 1].partition_broadcast(32), w=["lgfc"])
        cx.dma("sp", lgbc[32 * h:32 * h + 32, :], dr["ret_ld_b"][l, h:h + 1].partition_broadcast(32), w=["lgbc"])
    kdf = cx.sb("kdf", [128, 4]); kdb = cx.sb("kdb", [128, 4])
    KDF = cx.sb("KDF", [128, 128]); KDB = cx.sb("KDB", [128, 128])
    cx.act(kdf[:, :], lgf[:, :], AF.Exp, scale=cs(cst, "c127mj"), r=["lgf", "cst"], w=["kdf"])
    cx.act(kdb[:, :], lgb[:, :], AF.Exp, scale=cs(cst, "cj"), r=["lgb", "cst"], w=["kdb"])
    for h in range(4):
        cx.ts(KDF[:, 32 * h:32 * h + 32], cs(cst, "ones")[:, 0:32], kdf[:, h:h + 1], ALU.mult, r=["kdf", "cst"], w=["KDF"])
        cx.ts(KDB[:, 32 * h:32 * h + 32], cs(cst, "ones")[:, 0:32], kdb[:, h:h + 1], ALU.mult, r=["kdb", "cst"], w=["KDB"])
    pw = {}
    for tag, nch in (("l", 16), ("c", 2)):
        pf = cx.sb("pwf" + tag, [128, nch]); pb = cx.sb("pwb" + tag, [128, nch])
        cx.act(pf[:, :], cs(cst, "ef_" + tag), AF.Exp, scale=lgfc[:, 0:1], r=["lgfc", "cst"], w=["pwf" + tag])
        cx.act(pb[:, :], cs(cst, "eb_" + tag), AF.Exp, scale=lgbc[:, 0:1], r=["lgbc", "cst"], w=["pwb" + tag])
        pw[tag] = (pf, pb)
    Cl = cx.sb("Cl", [128, TL]); Sl = cx.sb("Sl", [128, TL])
    cx.dma("sp", Cl[:, :], dr["cosT"][:, :], w=["Cl"])
    cx.dma("sp", Sl[:, :], dr["sinT"][:, :], w=["Sl"])
    xt = [cx.sb("xt%d" % i, [128, D]) for i in range(2)]
    junk = cx.sb("junk", [128, D], BF16)
    t1 = [cx.sb("t1_%d" % i, [128, D]) for i in range(2)]
    hb = [cx.sb("hb%d" % i, [128, D], BF16) for i in range(2)]
    ssq = cx.sb("ssq", [128, 4]); rstd = cx.sb("rstd", [128, 4])
    hT = cx.sb("hT", [128, 8, 512], BF16)
    gsb = cx.sb("gsb", [128, D]); shb = cx.sb("shb", [128, D])
    ev = [cx.sb("ev%d" % i, [128, 512]) for i in range(4)]
    evb = [cx.sb("evb%d" % i, [128, 512], BF16) for i in range(4)]
    rk_sb = cx.sb("rk_sb", [128, 512], BF16)
    vt = [cx.sb("vt%d" % i, [128, 512], BF16) for i in range(2)]
    rgt = [cx.sb("rgt%d" % i, [128, 256], BF16) for i in range(2)]
    kfb = [cx.sb("kfb%d" % i, [128, 256], BF16) for i in range(2)]
    Tst = cx.sb("Tst", [128, 2, 256])
    evi = [0]

    def nxt():
        evi[0] = (evi[0] + 1) % 4
        return evi[0]

    xi = 0
    for (tag, T, xd) in segs:
        sfx = "_" + tag
        seg = 0 if tag == "l" else 1
        cx.dma("sp", gsb[:, :], dr["modv"][l, seg, 0, :].partition_broadcast(128), r=["D:modv%d" % l], w=["gsb"])
        cx.dma("sp", shb[:, :], dr["modv"][l, seg, 1, :].partition_broadcast(128), r=["D:modv%d" % l], w=["shb"])
        G = min(512, T)
        for g in range(T // G):
            t0 = g * G
            nt = G // 128
            for j in range(nt):
                b = xi % 2
                xi += 1
                kx = "xt%d" % b
                cx.dma("sp", xt[b][:, :], xd[t0 + j * 128:t0 + (j + 1) * 128, :], w=[kx])
                cx.act(junk[:, :], xt[b][:, :], AF.Square, accum_out=ssq[:, j:j + 1], r=[kx], w=["junk", "ssq%d" % j])
                cx.act(rstd[:, j:j + 1], ssq[:, j:j + 1], AF.Sqrt, scale=1.0 / D, bias=EPS, r=["ssq%d" % j], w=["rs%d" % j])
                cx.recip(rstd[:, j:j + 1], rstd[:, j:j + 1], r=["rs%d" % j], w=["rs%d" % j])
                cx.stt(t1[b][:, :], xt[b][:, :], rstd[:, j:j + 1], gsb[:, :], ALU.mult, ALU.mult,
                       r=[kx, "rs%d" % j, "gsb"], w=["t1_%d" % b])
                cx.tt(hb[b][:, :], t1[b][:, :], shb[:, :], ALU.add, r=["t1_%d" % b, "shb"], w=["hb%d" % b], eng="pool")
                for kc in range(8):
                    cx.tr(psb[0][:, kc * 128:(kc + 1) * 128], hb[b][:, kc * 128:(kc + 1) * 128], ident[:, :],
                          r=["hb%d" % b, "ident"], w=["psb0"])
                cx.copy(hT[:, :, j * 128:(j + 1) * 128], psb[0][:, :].rearrange("p (k t) -> p k t", k=8),
                        r=["psb0"], w=["hT"], eng="act")
            cx.dma("sp", dr["hT" + sfx][:, :, t0:t0 + G], hT[:, :, 0:G], r=["hT"], w=["D:hT" + sfx])

            def proj(cc, bank, rot=False):
                Wt = WR if rot else W
                for kc in range(8):
                    cx.mm(ps[bank][:, 0:G], Wt[:, kc, cc * 128:(cc + 1) * 128], hT[:, kc, 0:G], kc == 0, kc == 7,
                          r=["hT", (WRk if rot else Wk)[kc]], w=["ps%d" % bank])

            Cg = Cl[:, t0:t0 + G] if tag == "l" else None
            Sg = Sl[:, t0:t0 + G] if tag == "l" else None
            for c2 in range(2):
                e = nxt()
                proj(2 + c2, 0)
                cx.act(ev[e][:, 0:G], ps[0][:, 0:G], AF.Sigmoid, r=["ps0"], w=["ev%d" % e])
                proj(0 + c2, 1)
                cx.tt(ev[e][:, 0:G], ps[1][:, 0:G], ev[e][:, 0:G], ALU.mult, r=["ps1", "ev%d" % e], w=["ev%d" % e])
                cx.dma("sp", dr["uT" + sfx][c2, :, t0:t0 + G], ev[e][:, 0:G], r=["ev%d" % e], w=["D:uT" + sfx])
            for c2 in range(2):
                e = nxt()
                proj(4 + c2, 0)
                cx.copy(evb[e][:, 0:G], ps[0][:, 0:G], r=["ps0"], w=["evb%d" % e], eng="act")
                cx.dma("sp", dr["bgT" + sfx][c2, :, t0:t0 + G], evb[e][:, 0:G], r=["evb%d" % e], w=["D:bgT" + sfx])
                proj(6 + c2, 1)
                cx.copy(ev[e][:, 0:G], ps[1][:, 0:G], r=["ps1"], w=["ev%d" % e], eng="act")
                proj(8 + c2, 0)
                cx.tt(ev[e][:, 0:G], ps[0][:, 0:G], ev[e][:, 0:G], ALU.mult, r=["ps0", "ev%d" % e], w=["ev%d" % e])
                cx.dma("sp", dr["tT" + sfx][c2, :, t0:t0 + G], ev[e][:, 0:G], r=["ev%d" % e], w=["D:tT" + sfx])
            for (cc, ro, dst, keep) in ((10, 0, dr["qT" + sfx][0], None), (11, 1, dr["qT" + sfx][1], None),
                                        (12, 2, dr["kT" + sfx][0], None), (13, 3, dr["kT" + sfx][1], None),
                                        (16, 4, dr["rqT" + sfx], None), (17, 5, dr["rkT" + sfx], rk_sb)):
                e = nxt()
                ob = keep if keep is not None else evb[e]
                okey = "rk_sb" if keep is not None else "evb%d" % e
                proj(cc, 0)
                if tag == "l":
                    proj(ro, 2, rot=True)
                    e2 = nxt()
                    cx.tt(ev[e][:, 0:G], ps[0][:, 0:G], Cg, ALU.mult, r=["ps0", "Cl"], w=["ev%d" % e])
                    cx.tt(ev[e2][:, 0:G], ps[2][:, 0:G], Sg, ALU.mult, r=["ps2", "Sl"], w=["ev%d" % e2])
                    cx.tt(ob[:, 0:G], ev[e][:, 0:G], ev[e2][:, 0:G], ALU.add, r=["ev%d" % e, "ev%d" % e2], w=[okey], eng="pool")
                else:
                    cx.copy(ob[:, 0:G], ps[0][:, 0:G], r=["ps0"], w=[okey], eng="act")
                cx.dma("sp", dst[:, t0:t0 + G], ob[:, 0:G], r=[okey], w=["D:rope" + sfx + str(cc)])
            pf, pb = pw[tag]
            for j in range(nt):
                n = (t0 // 128) + j
                b = j % 2
                tsl = slice(j * 128, (j + 1) * 128)
                for kc in range(8):
                    cx.mm(ps[3][:, 0:256], hT[:, kc, tsl], W[:, kc, 1792:2048], kc == 0, kc == 7, r=["hT", Wk[kc]], w=["ps3"])
                for kc in range(8):
                    cx.mm(ps[3][:, 256:512], hT[:, kc, tsl], W[:, kc, 2304:2560], kc == 0, kc == 7, r=["hT", Wk[kc]], w=["ps3"])
                for kc in range(8):
                    cx.mm(ps[4][:, 0:256], hT[:, kc, tsl], W[:, kc, 2560:2816], kc == 0, kc == 7, r=["hT", Wk[kc]], w=["ps4"])
                cx.copy(vt[b][:, :], ps[3][:, :], r=["ps3", "ps3"], w=["vt%d" % b])
                cx.act(rgt[b][:, :], ps[4][:, 0:256], AF.Silu, r=["ps4"], w=["rgt%d" % b])
                rows = slice(t0 + j * 128, t0 + (j + 1) * 128)
                cx.dma("sp", dr["V" + sfx][rows, :], vt[b][:, 0:256], r=["vt%d" % b], w=["D:V" + sfx])
                cx.dma("sp", dr["rv" + sfx][rows, :], vt[b][:, 256:512], r=["vt%d" % b], w=["D:rv" + sfx])
                cx.dma("sp", dr["rg" + sfx][rows, :], rgt[b][:, :], r=["rgt%d" % b], w=["D:rg" + sfx])
                cx.tr(psb[1][:, 0:128], rk_sb[:, tsl], ident[:, :], r=["rk_sb", "ident"], w=["psb1"])
                cx.tt(kfb[b][:, 0:128], psb[1][:, 0:128], KDF[:, :], ALU.mult, r=["psb1", "KDF"], w=["kfb%d" % b])
                cx.tt(kfb[b][:, 128:256], psb[1][:, 0:128], KDB[:, :], ALU.mult, r=["psb1", "KDB"], w=["kfb%d" % b])
                cx.mm(ps[5][:, 0:256], kfb[b][:, 0:128], vt[b][:, 256:512], True, True, r=["kfb%d" % b, "vt%d" % b], w=["ps5"])
                cx.mm(ps[5][:, 256:512], kfb[b][:, 128:256], vt[b][:, 256:512], True, True, r=["kfb%d" % b, "vt%d" % b], w=["ps5"])
                if n == 0:
                    cx.ts(Tst[:, 0, :], ps[5][:, 0:256], pf[:, n:n + 1], ALU.mult, r=["ps5", "pwf" + tag], w=["Tf"])
                    cx.ts(Tst[:, 1, :], ps[5][:, 256:512], pb[:, n:n + 1], ALU.mult, r=["ps5", "pwb" + tag], w=["Tb"])
                else:
                    cx.stt(Tst[:, 0, :], ps[5][:, 0:256], pf[:, n:n + 1], Tst[:, 0, :], ALU.mult, ALU.add,
                           r=["ps5", "pwf" + tag, "Tf"], w=["Tf"])
                    cx.stt(Tst[:, 1, :], ps[5][:, 256:512], pb[:, n:n + 1], Tst[:, 1, :], ALU.mult, ALU.add,
                           r=["ps5", "pwb" + tag, "Tb"], w=["Tb"])
        cx.dma("sp", dr["Tst" + sfx].rearrange("a p e -> p a e"), Tst[:, :, :], r=["Tf", "Tb"], w=["D:Tst" + sfx])
    cx.end()


def phase_conv(cx, l, dr, segs):
    cx.begin(nf=8, nb=0)
    ps = cx.ps
    cst = cx.sb("cst", [128, CSTW])
    cx.dma("sp", cst[:, :], dr["cst"][:, :], w=["cst"])
    praw = cx.sb("praw", [40, 256])
    cx.memset(praw[:, :], 0.0, w=["praw"])
    cx.dma("sp", praw[0:31, :], dr["conv_a_w"][l, :, :], w=["praw"])
    cx.dma("sp", praw[31:32, :], dr["conv_a_b"][l:l + 1, :], w=["praw"])
    cx.dma("sp", praw[32:33, :], dr["conv_a_g"][l:l + 1, :], w=["praw"])
    cx.dma("sp", praw[33:34, :], dr["conv_a_beta"][l:l + 1, :], w=["praw"])
    cx.dma("sp", praw[34:37, :], dr["conv_b_w"][l, :, :], w=["praw"])
    par = cx.sb("par", [128, 2, 40])
    for c2 in range(2):
        cx.tr(ps[7][:, 0:40], praw[0:40, c2 * 128:(c2 + 1) * 128], cs(cst, "I1")[0:40, 0:40], r=["praw", "cst"], w=["ps7"])
        cx.copy(par[:, c2, :], ps[7][:, 0:40], r=["ps7"], w=["par"])
    onesm = cx.sb("onesm", [128, 128])
    cx.memset(onesm[:, :], 1.0 / 256.0, w=["onesm"])
    HG = cx.sb("HG", [128, NCORE, 4, 32]); selt = cx.sb("selt", [128, 2, NCORE])
    cx.dma("sp", HG[:, :, :, :], dr["hg"].rearrange("(r a p) w -> p r a w", r=NCORE, a=4), w=["HG"])
    cx.dma("sp", selt[:, :, :], dr["sel"][:, :, :], w=["selt"])
    for (tag, T) in segs:
        sfx = "_" + tag
        ue = cx.sb("ue" + tag, [128, 2, T + 32])
        te = cx.sb("te" + tag, [128, 2, T + 32])
        bg = cx.sb("bg" + tag, [128, 2, T], BF16)
        acc = cx.sb("acc" + tag, [128, 2, T])
        sq = cx.sb("sq" + tag, [128, 2, T])
        yb = cx.sb("yb" + tag, [128, 2, T], BF16)
        for c2 in range(2):
            cx.dma("sp", ue[:, c2, 16:16 + T], dr["uT" + sfx][c2, :, :], r=["D:uT" + sfx], w=["ue%d" % c2])
            cx.dma("sp", te[:, c2, 16:16 + T], dr["tT" + sfx][c2, :, :], r=["D:tT" + sfx], w=["te%d" % c2])
            cx.dma("sp", bg[:, c2, :], dr["bgT" + sfx][c2, :, :], r=["D:bgT" + sfx], w=["bg%d" % c2])
            if tag == "l":
                for a, (buf, k) in enumerate(((ue, "ue%d" % c2), (te, "te%d" % c2))):
                    ai = a * 2 + c2
                    for side, dst, src in ((0, slice(0, 16), slice(16, 32)), (1, slice(16 + T, 32 + T), slice(0, 16))):
                        for r_ in range(NCORE):
                            if r_ == 0:
                                cx.ts(buf[:, c2, dst], HG[:, r_, ai, src], selt[:, side, r_:r_ + 1], ALU.mult,
                                      r=["HG", "selt"], w=[k])
                            else:
                                cx.stt(buf[:, c2, dst], HG[:, r_, ai, src], selt[:, side, r_:r_ + 1], buf[:, c2, dst],
                                       ALU.mult, ALU.add, r=["HG", "selt", k], w=[k])
            else:
                for buf, k in ((ue, "ue%d" % c2), (te, "te%d" % c2)):
                    cx.memset(buf[:, c2, 0:16], 0.0, w=[k], eng="pool")
                    cx.memset(buf[:, c2, 16 + T:32 + T], 0.0, w=[k], eng="pool")
        for c2 in range(2):
            ka = "acc%d" % c2
            cx.ts(acc[:, c2, :], ue[:, c2, 1:1 + T], par[:, c2, 0:1], ALU.mult, s2=par[:, c2, 31:32], op1=ALU.add,
                  r=["ue%d" % c2, "par"], w=[ka])
            for k in range(1, 31):
                cx.stt(acc[:, c2, :], ue[:, c2, k + 1:k + 1 + T], par[:, c2, k:k + 1], acc[:, c2, :], ALU.mult, ALU.add,
                       r=["ue%d" % c2, "par", ka], w=[ka])
            cx.tt(sq[:, c2, :], acc[:, c2, :], acc[:, c2, :], ALU.mult, r=[ka], w=["sq%d" % c2], eng="pool")
        G = min(512, T)
        mm2 = cx.sb("m2" + tag, [128, G]); var = cx.sb("var" + tag, [128, G]); dd = cx.sb("dd" + tag, [128, G])
        for g in range(T // G):
            gs = slice(g * G, (g + 1) * G)
            for c2 in range(2):
                cx.mm(ps[0][:, 0:G], onesm[:, :], acc[:, c2, gs], c2 == 0, c2 == 1, r=["onesm", "acc%d" % c2], w=["ps0"])
            for c2 in range(2):
                cx.mm(ps[1][:, 0:G], onesm[:, :], sq[:, c2, gs], c2 == 0, c2 == 1, r=["onesm", "sq%d" % c2], w=["ps1"])
            cx.act(mm2[:, :], ps[0][:, 0:G], AF.Square, r=["ps0"], w=["mm2"])
            cx.tt(var[:, :], ps[1][:, 0:G], mm2[:, :], ALU.subtract, r=["ps1", "mm2"], w=["var"])
            cx.act(var[:, :], var[:, :], AF.Ln, bias=EPS, r=["var"], w=["var"])
            cx.act(var[:, :], var[:, :], AF.Exp, scale=-0.5, r=["var"], w=["var"])
            for c2 in range(2):
                cx.tt(dd[:, :], acc[:, c2, gs], ps[0][:, 0:G], ALU.subtract, r=["acc%d" % c2, "ps0"], w=["dd"])
                cx.tt(dd[:, :], dd[:, :], var[:, :], ALU.mult, r=["dd", "var"], w=["dd"])
                cx.act(yb[:, c2, gs], dd[:, :], AF.Silu, scale=par[:, c2, 32:33], bias=par[:, c2, 33:34],
                       r=["dd", "par"], w=["yb%d" % c2])
        for c2 in range(2):
            cx.dma("sp", dr["ysT" + sfx][0 + c2, :, :], yb[:, c2, :], r=["yb%d" % c2], w=["D:ys0" + sfx])
        for c2 in range(2):
            ka = "acc%d" % c2
            cx.ts(acc[:, c2, :], te[:, c2, 15:15 + T], par[:, c2, 34:35], ALU.mult, r=["te%d" % c2, "par"], w=[ka])
            for k in (1, 2):
                cx.stt(acc[:, c2, :], te[:, c2, 15 + k:15 + k + T], par[:, c2, 34 + k:35 + k], acc[:, c2, :], ALU.mult, ALU.add,
                       r=["te%d" % c2, "par", ka], w=[ka])
            cx.tt(yb[:, c2, :], acc[:, c2, :], bg[:, c2, :], ALU.mult, r=[ka, "bg%d" % c2], w=["yb%d" % c2])
            cx.dma("sp", dr["ysT" + sfx][2 + c2, :, :], yb[:, c2, :], r=["yb%d" % c2], w=["D:ys1" + sfx])
    cx.end()


def phase_ret(cx, l, dr, segs):
    cx.begin(nf=6, nb=2)
    ps, psb = cx.ps, cx.psb
    cst = cx.sb("cst", [128, CSTW])
    ident = cx.sb("ident", [128, 128], BF16)
    cx.dma("sp", cst[:, :], dr["cst"][:, :], w=["cst"])
    cx.dma("sp", ident[:, :], dr["ident"][:, :], w=["ident"])
    lgf = cx.sb("lgf", [128, 4]); lgb = cx.sb("lgb", [128, 4])
    lgfc = cx.sb("lgfc", [128, 1]); lgbc = cx.sb("lgbc", [128, 1])
    cx.dma("sp", lgf[:, :], dr["ret_ld_f"][l, :].partition_broadcast(128), w=["lgf"])
    cx.dma("sp", lgb[:, :], dr["ret_ld_b"][l, :].partition_broadcast(128), w=["lgb"])
    for h in range(4):
        cx.dma("sp", lgfc[32 * h:32 * h + 32, :], dr["ret_ld_f"][l, h:h + 1].partition_broadcast(32), w=["lgfc"])
        cx.dma("sp", lgbc[32 * h:32 * h + 32, :], dr["ret_ld_b"][l, h:h + 1].partition_broadcast(32), w=["lgbc"])
    kdf = cx.sb("kdf", [128, 4]); kdb = cx.sb("kdb", [128, 4])
    KDF = cx.sb("KDF", [128, 128]); KDB = cx.sb("KDB", [128, 128])
    cx.act(kdf[:, :], lgf[:, :], AF.Exp, scale=cs(cst, "c127mj"), r=["lgf", "cst"], w=["kdf"])
    cx.act(kdb[:, :], lgb[:, :], AF.Exp, scale=cs(cst, "cj"), r=["lgb", "cst"], w=["kdb"])
    for h in range(4):
        cx.ts(KDF[:, 32 * h:32 * h + 32], cs(cst, "ones")[:, 0:32], kdf[:, h:h + 1], ALU.mult, r=["kdf", "cst"], w=["KDF"])
        cx.ts(KDB[:, 32 * h:32 * h + 32], cs(cst, "ones")[:, 0:32], kdb[:, h:h + 1], ALU.mult, r=["kdb", "cst"], w=["KDB"])
    cdf = cx.sb("cdf", [128, 1]); cdb = cx.sb("cdb", [128, 1])
    cx.act(cdf[:, :], lgfc[:, :], AF.Exp, scale=128.0, r=["lgfc"], w=["cdf"])
    cx.act(cdb[:, :], lgbc[:, :], AF.Exp, scale=128.0, r=["lgbc"], w=["cdb"])
    qdf4 = cx.sb("qdf4", [128, 4, 128]); qdb4 = cx.sb("qdb4", [128, 4, 128])
    for c in range(4):
        cx.act(qdf4[:, c, :], cs(cst, "ip1"), AF.Exp, scale=lgfc[:, 0:1], r=["lgfc", "cst"], w=["qdf4"])
        cx.act(qdb4[:, c, :], cs(cst, "m128i"), AF.Exp, scale=lgbc[:, 0:1], r=["lgbc", "cst"], w=["qdb4"])
    maskT = cx.sb("maskT", [128, 4, 128]); mtmp = cx.sb("mtmp", [128, 128])
    for h in range(4):
        cx.act(mtmp[:, :], cs(cst, "D1"), AF.Exp, scale=lgf[:, h:h + 1], r=["lgf", "cst"], w=["mtmp"])
        cx.tt(maskT[:, h, :], mtmp[:, :], cs(cst, "U"), ALU.mult, r=["mtmp", "cst"], w=["maskT"])
        cx.tt(maskT[:, h, :], maskT[:, h, :], cs(cst, "I2"), ALU.add, r=["maskT", "cst"], w=["maskT"])
        cx.act(mtmp[:, :], cs(cst, "D2"), AF.Exp, scale=lgb[:, h:h + 1], r=["lgb", "cst"], w=["mtmp"])
        cx.tt(mtmp[:, :], mtmp[:, :], cs(cst, "Lo"), ALU.mult, r=["mtmp", "cst"], w=["mtmp"])
        cx.tt(maskT[:, h, :], maskT[:, h, :], mtmp[:, :], ALU.add, r=["maskT", "mtmp"], w=["maskT"])
    for (tag, T) in segs:
        sfx = "_" + tag
        NCH = T // 128
        rq = cx.sb("rq" + tag, [128, T], BF16); rk = cx.sb("rk" + tag, [128, T], BF16)
        rqh = cx.sb("rqh" + tag, [128, 4, T], BF16)
        rv = cx.sb("rv" + tag, [128, NCH, 256], BF16); rg = cx.sb("rg" + tag, [128, NCH, 256], BF16)
        cx.dma("sp", rq[:, :], dr["rqT" + sfx][:, :], r=["D:rope" + sfx + "16"], w=["rq"])
        cx.dma("sp", rk[:, :], dr["rkT" + sfx][:, :], r=["D:rope" + sfx + "17"], w=["rk"])
        cx.dma("sp", rv[:, :, :], dr["rv" + sfx].rearrange("(n p) e -> p n e", p=128), r=["D:rv" + sfx], w=["rv"])
        cx.dma("sp", rg[:, :, :], dr["rg" + sfx].rearrange("(n p) e -> p n e", p=128), r=["D:rg" + sfx], w=["rg"])
        for h in range(4):
            cx.ts(rqh[:, h, :], rq[:, :], cs(cst, "hm")[:, h:h + 1], ALU.mult, r=["rq", "cst"], w=["rqh"], eng="pool")
        SF = cx.sb("SF" + tag, [128, NCH, 256]); SB = cx.sb("SB" + tag, [128, NCH, 256])
        SFb = cx.sb("SFb" + tag, [128, NCH, 256], BF16); SBb = cx.sb("SBb" + tag, [128, NCH, 256], BF16)
        KV = cx.sb("KV" + tag, [128, NCH, 2, 256])
        if tag == "l":
            Tall = cx.sb("Tall", [128, 9, 2, 256]); expo = cx.sb("expo", [128, 2, 9]); coef = cx.sb("coef", [128, 2, 9])
            cx.dma("sp", Tall[:, 0:8, :, :], dr["gt"].rearrange("(s a p) e -> p s a e", s=NCORE, a=2), w=["Tall"])
            cx.dma("sp", Tall[:, 8, :, :], dr["Tst_c"].rearrange("a p e -> p a e"), w=["Tall"])
            cx.dma("sp", expo[:, :, :], dr["expo"][:, :, :], w=["expo"])
            cx.act(coef[:, 0, :], expo[:, 0, :], AF.Exp, scale=lgfc[:, 0:1], r=["expo", "lgfc"], w=["coef"])
            cx.act(coef[:, 1, :], expo[:, 1, :], AF.Exp, scale=lgbc[:, 0:1], r=["expo", "lgbc"], w=["coef"])
            for a, (St, n0, key) in enumerate(((SF, 0, "SF0"), (SB, NCH - 1, "SB%d" % (NCH - 1)))):
                cx.ts(St[:, n0, :], Tall[:, 0, a, :], coef[:, a, 0:1], ALU.mult, r=["Tall", "coef"], w=[key])
                for s in range(1, 9):
                    cx.stt(St[:, n0, :], Tall[:, s, a, :], coef[:, a, s:s + 1], St[:, n0, :], ALU.mult, ALU.add,
                           r=["Tall", "coef", key], w=[key])
        else:
            cx.memset(SF[:, 0, :], 0.0, w=["SF0"])
            cx.memset(SB[:, NCH - 1, :], 0.0, w=["SB%d" % (NCH - 1)])
        kfb = [cx.sb("kfb%d" % i + tag, [128, 256], BF16) for i in range(2)]
        for n in range(NCH):
            b = n % 2
            csl = slice(n * 128, (n + 1) * 128)
            cx.tr(psb[0][:, 0:128], rk[:, csl], ident[:, :], r=["rk", "ident"], w=["psb0"])
            cx.tt(kfb[b][:, 0:128], psb[0][:, 0:128], KDF[:, :], ALU.mult, r=["psb0", "KDF"], w=["kfb%d" % b])
            cx.tt(kfb[b][:, 128:256], psb[0][:, 0:128], KDB[:, :], ALU.mult, r=["psb0", "KDB"], w=["kfb%d" % b])
            cx.mm(ps[0][:, 0:256], kfb[b][:, 0:128], rv[:, n, :], True, True, r=["kfb%d" % b, "rv"], w=["ps0"])
            cx.mm(ps[0][:, 256:512], kfb[b][:, 128:256], rv[:, n, :], True, True, r=["kfb%d" % b, "rv"], w=["ps0"])
            cx.copy(KV[:, n, :, :], ps[0][:, :].rearrange("p (a e) -> p a e", a=2), r=["ps0", "ps0"], w=["KV%d" % n], eng="act")
        for n in range(NCH - 1):
            cx.stt(SF[:, n + 1, :], SF[:, n, :], cdf[:, 0:1], KV[:, n, 0, :], ALU.mult, ALU.add,
                   r=["SF%d" % n, "cdf", "KV%d" % n], w=["SF%d" % (n + 1)])
        for n in range(NCH - 1, 0, -1):
            cx.stt(SB[:, n - 1, :], SB[:, n, :], cdb[:, 0:1], KV[:, n, 1, :], ALU.mult, ALU.add,
                   r=["SB%d" % n, "cdb", "KV%d" % n], w=["SB%d" % (n - 1)])
        allSF = ["SF%d" % n for n in range(NCH)]; allSB = ["SB%d" % n for n in range(NCH)]
        cx.copy(SFb[:, :, :], SF[:, :, :], r=allSF, w=["SFb"], eng="pool")
        cx.copy(SBb[:, :, :], SB[:, :, :], r=allSB, w=["SBb"], eng="pool")
        sT = [cx.sb("sT%d" % i + tag, [128, 4, 128], BF16) for i in range(2)]
        Qf = [cx.sb("Qf%d" % i + tag, [128, 4, 128], BF16) for i in range(2)]
        Qb = [cx.sb("Qb%d" % i + tag, [128, 4, 128], BF16) for i in range(2)]
        osb = cx.sb("osb" + tag, [128, 4, 64]); osq = cx.sb("osq" + tag, [128, 4, 64])
        st = cx.sb("st" + tag, [128, 4, 4])
        ysb = [cx.sb("ysb%d" % i + tag, [128, 256], BF16) for i in range(2)]
        yT = cx.sb("yT" + tag, [128, 2, T], BF16)
        for n in range(NCH):
            b = n % 2
            csl = slice(n * 128, (n + 1) * 128)
            for h in range(4):
                cx.mm(ps[1][:, h * 128:(h + 1) * 128], rk[:, csl], rqh[:, h, csl], True, True, r=["rk", "rqh"], w=["ps1"])
            cx.tt(sT[b][:, :, :], ps[1][:, :].rearrange("p (h i) -> p h i", h=4), maskT[:, :, :], ALU.mult,
                  r=["ps1", "maskT"], w=["sT%d" % b])
            cx.tt(Qf[b][:, :, :], rqh[:, :, csl], qdf4[:, :, :], ALU.mult, r=["rqh", "qdf4"], w=["Qf%d" % b], eng="pool")
            cx.tt(Qb[b][:, :, :], rqh[:, :, csl], qdb4[:, :, :], ALU.mult, r=["rqh", "qdb4"], w=["Qb%d" % b], eng="pool")
            for h in range(4):
                o = ps[2][:, h * 64:(h + 1) * 64]
                es = slice(h * 64, (h + 1) * 64)
                cx.mm(o, sT[b][:, h, :], rv[:, n, es], True, False, r=["sT%d" % b, "rv"], w=["ps2"])
                cx.mm(o, Qf[b][:, h, :], SFb[:, n, es], False, False, r=["Qf%d" % b, "SFb"], w=["ps2"])
                cx.mm(o, Qb[b][:, h, :], SBb[:, n, es], False, True, r=["Qb%d" % b, "SBb"], w=["ps2"])
            p2 = ["ps2"]
            cx.copy(osb[:, :, :], ps[2][:, 0:256].rearrange("p (h e) -> p h e", h=4), r=p2, w=["osb"], eng="act")
            cx.red(st[:, 0, :], osb[:, :, :], ALU.add, r=["osb"], w=["st0"])
            cx.tt(osq[:, :, :], osb[:, :, :], osb[:, :, :], ALU.mult, r=["osb"], w=["osq"], eng="pool")
            cx.red(st[:, 1, :], osq[:, :, :], ALU.add, r=["osq"], w=["st1"])
            cx.ts(st[:, 2, :], st[:, 0, :], 1.0 / 64, ALU.mult, r=["st0"], w=["st2"])
            cx.tt(st[:, 3, :], st[:, 2, :], st[:, 2, :], ALU.mult, r=["st2"], w=["st3"])
            cx.stt(st[:, 3, :], st[:, 1, :], 1.0 / 64, st[:, 3, :], ALU.mult, ALU.subtract, r=["st1", "st3"], w=["st3"])
            cx.act(st[:, 3, :], st[:, 3, :], AF.Sqrt, bias=EPS, r=["st3"], w=["st3"])
            cx.recip(st[:, 3, :], st[:, 3, :], r=["st3"], w=["st3"])
            for h in range(4):
                cx.ts(osb[:, h, :], osb[:, h, :], st[:, 2, h:h + 1], ALU.subtract, s2=st[:, 3, h:h + 1], op1=ALU.mult,
                      r=["osb", "st2", "st3"], w=["osb"])
            cx.tt(ysb[b][:, :], osb[:, :, :].rearrange("p h e -> p (h e)"), rg[:, n, :], ALU.mult, r=["osb", "rg"], w=["ysb%d" % b])
            for c2 in range(2):
                cx.tr(psb[1][:, c2 * 128:(c2 + 1) * 128], ysb[b][:, c2 * 128:(c2 + 1) * 128], ident[:, :],
                      r=["ysb%d" % b, "ident"], w=["psb1"])
            cx.copy(yT[:, :, csl], psb[1][:, 0:256].rearrange("p (c t) -> p c t", c=2), r=["psb1"], w=["yT"], eng="act")
        for c2 in range(2):
            cx.dma("sp", dr["ysT" + sfx][6 + c2, :, :], yT[:, c2, :], r=["yT"], w=["D:ys3" + sfx])
    cx.end()


def phase_merge(cx, l, dr, segs, wl=None):
    wl = l if wl is None else wl
    cx.begin(nf=8, nb=0)
    ps = cx.ps
    cst = cx.sb("cst", [128, CSTW])
    cx.dma("sp", cst[:, :], dr["cst"][:, :], w=["cst"])
    WG = cx.sb("WG", [128, 8, 4096], BF16)
    WB = cx.sb("WB", [128, 8, 1024], BF16)
    WO = cx.sb("WO", [128, 8, 1024], BF16)
    for kc in range(8):
        cx.dma("pool", WG[:, kc, :], dr["w_gate"][wl, kc * 128:(kc + 1) * 128, :], w=["WG%d" % kc])
        cx.dma("pool", WB[:, kc, :], dr["w_branch"][wl, kc // 2, (kc % 2) * 128:(kc % 2 + 1) * 128, :], w=["WB%d" % kc])
        cx.dma("pool", WO[:, kc, :], dr["w_o"][wl, kc * 128:(kc + 1) * 128, :], w=["WO%d" % kc])
    braw = cx.sb("braw", [32, 128]); bgt = cx.sb("bgt", [128, 32])
    cx.dma("sp", braw[:, :], dr["b_gate"][l, :].rearrange("(a p) -> a p", p=128), w=["braw"])
    cx.tr(ps[7][:, 0:32], braw[:, :], cs(cst, "I1")[0:32, 0:32], r=["braw", "cst"], w=["ps7"])
    cx.copy(bgt[:, :], ps[7][:, 0:32], r=["ps7"], w=["bgt"])
    g1b = cx.sb("g1b", [128, D])
    hT = [cx.sb("mhT%d" % i, [128, 8, 512], BF16) for i in range(2)]
    yT = [cx.sb("myT%d" % i, [128, 8, 512], BF16) for i in range(2)]
    mT = cx.sb("mT", [128, 8, 512], BF16)
    sg = [cx.sb("sg%d" % i, [128, 512]) for i in range(2)]
    macc = cx.sb("macc", [128, 512]); mtmp = cx.sb("mtmp", [128, 512])
    xt = [cx.sb("mxt%d" % i, [128, D]) for i in range(2)]
    gi = 0
    xi = 0
    for (tag, T, xin, xout) in segs:
        sfx = "_" + tag
        seg = 0 if tag == "l" else 1
        cx.dma("sp", g1b[:, :], dr["modv"][l, seg, 2, :].partition_broadcast(128), r=["D:modv%d" % l], w=["g1b"])
        G = min(512, T)
        for g in range(T // G):
            t0 = g * G
            b = gi % 2
            gi += 1
            cx.dma("sp", hT[b][:, :, 0:G], dr["hT" + sfx][:, :, t0:t0 + G], r=["D:hT" + sfx], w=["mhT%d" % b])
            cx.dma("sp", yT[b][:, :, 0:G], dr["ysT" + sfx][:, :, t0:t0 + G].rearrange("a p t -> p a t"),
                   r=["D:ys0" + sfx, "D:ys1" + sfx, "D:ys2" + sfx, "D:ys3" + sfx], w=["myT%d" % b])
            for nn in range(8):
                for i in range(4):
                    pa = ps[(i % 2) * 2]
                    pb = ps[(i % 2) * 2 + 1]
                    ka = "ps%d" % ((i % 2) * 2)
                    kb = "ps%d" % ((i % 2) * 2 + 1)
                    col = i * 1024 + nn * 128
                    for kc in range(8):
                        cx.mm(pa[:, 0:G], WG[:, kc, col:col + 128], hT[b][:, kc, 0:G], kc == 0, kc == 7,
                              r=["WG%d" % kc, "mhT%d" % b], w=[ka])
                    for c2 in range(2):
                        cx.mm(pb[:, 0:G], WB[:, i * 2 + c2, nn * 128:(nn + 1) * 128], yT[b][:, i * 2 + c2, 0:G], c2 == 0, c2 == 1,
                              r=["WB%d" % (i * 2 + c2), "myT%d" % b], w=[kb])
                    s = sg[i % 2]
                    ks = "sg%d" % (i % 2)
                    cx.act(s[:, 0:G], pa[:, 0:G], AF.Sigmoid, bias=bgt[:, i * 8 + nn:i * 8 + nn + 1], r=[ka, "bgt"], w=[ks])
                    if i == 0:
                        cx.tt(macc[:, 0:G], pb[:, 0:G], s[:, 0:G], ALU.mult, r=[kb, ks], w=["macc"])
                    elif i < 3:
                        cx.tt(mtmp[:, 0:G], pb[:, 0:G], s[:, 0:G], ALU.mult, r=[kb, ks], w=["mtmp"])
                        cx.tt(macc[:, 0:G], macc[:, 0:G], mtmp[:, 0:G], ALU.add, r=["macc", "mtmp"], w=["macc"], eng="pool")
                    else:
                        cx.tt(mtmp[:, 0:G], pb[:, 0:G], s[:, 0:G], ALU.mult, r=[kb, ks], w=["mtmp"])
                        cx.tt(mT[:, nn, 0:G], macc[:, 0:G], mtmp[:, 0:G], ALU.add, r=["macc", "mtmp"], w=["mT"], eng="pool")
            for j in range(G // 128):
                xb = xi % 2
                xi += 1
                rows = slice(t0 + j * 128, t0 + (j + 1) * 128)
                cx.dma("sp", xt[xb][:, :], xin[rows, :], w=["mxt%d" % xb])
                for nh in range(2):
                    po = ps[4 + nh]
                    for kc in range(8):
                        cx.mm(po[:, :], mT[:, kc, j * 128:(j + 1) * 128], WO[:, kc, nh * 512:(nh + 1) * 512], kc == 0, kc == 7,
                              r=["mT", "WO%d" % kc], w=["ps%d" % (4 + nh)])
                    hs = slice(nh * 512, (nh + 1) * 512)
                    cx.tt(mtmp[:, :], po[:, :], g1b[:, hs], ALU.mult, r=["ps%d" % (4 + nh), "g1b"], w=["mtmp"])
                    cx.tt(xt[xb][:, hs], xt[xb][:, hs], mtmp[:, :], ALU.add, r=["mxt%d" % xb, "mtmp"], w=["mxt%d" % xb], eng="pool")
                cx.dma("sp", xout[rows, :], xt[xb][:, :], r=["mxt%d" % xb], w=["D:xmid" + sfx])
    cx.end()


def phase_attn(cx, l, dr, segs, lam_init):
    NB = 2
    cx.begin(nf=0, nb=1)
    psb = cx.psb
    psS = [cx.psum("psS%d" % i, [128, NB * 512]) for i in range(2)]
    psO = [cx.psum("psO%d" % i, [128, 512]) for i in range(2)]
    ps6 = cx.psum("ps6", [128, 512])
    NKT = max(s[2] for s in segs)
    cst = cx.sb("cst", [128, CSTW])
    ident = cx.sb("ident", [128, 128], BF16)
    cx.dma("sp", cst[:, :], dr["cst"][:, :], w=["cst"])
    cx.dma("sp", ident[:, :], dr["ident"][:, :], w=["ident"])
    kT = cx.sb("kTall", [128, 2, NKT * 128], BF16)
    Va = cx.sb("Vaug", [128, NKT, 4, 65], BF16)
    vst = [cx.sb("vst%d" % i, [128, 10, 256], BF16) for i in range(2)]
    kTk = []
    for c in range(2):
        cx.dma("sp", kT[:, c, 0:TC], dr["kT_c"][c, :, :], w=["kTc%d" % c])
        kTk.append("kTc%d" % c)
        if NKT > TC // 128:
            for r_ in range(NCORE):
                cx.dma("sp" if (r_ % 2 == 0) else "act", kT[:, c, TC + r_ * TL:TC + (r_ + 1) * TL],
                       dr["gk"][(r_ * 2 + c) * 128:(r_ * 2 + c + 1) * 128, :], w=["kT%d_%d" % (c, r_)])
                kTk.append("kT%d_%d" % (c, r_))
    cx.memset(Va[:, :, :, 64:65], 1.0, w=["Vones"], eng="pool")
    chunks = [(0, TC // 128, dr["V_c"], 0)]
    k0 = TC // 128
    while k0 < NKT:
        k1 = min(NKT, k0 + 10)
        chunks.append((k0, k1, dr["gv"], (k0 - TC // 128) * 128))
        k0 = k1
    nst = len(chunks)
    for i, (k0, k1, src, row0) in enumerate(chunks):
        b = i % 2
        cx.dma("sp", vst[b][:, 0:k1 - k0, :], src[row0:row0 + (k1 - k0) * 128, :].rearrange("(k p) e -> p k e", p=128), w=["vst%d" % b])
        cx.copy(Va[:, k0:k1, :, 0:64], vst[b][:, 0:k1 - k0, :].rearrange("p k (h e) -> p k h e", h=4), r=["vst%d" % b],
                w=["Va%d" % i], eng=("pool" if i % 2 == 0 else "dve"))
    Vak = ["Va%d" % i for i in range(nst)] + ["Vones"]
    lq = cx.sb("lq", [128, 4, 32]); lp = cx.sb("lp", [128, 2, 32]); ls = cx.sb("ls", [128, 4])
    for i, nm in enumerate(("lam_q1", "lam_k1", "lam_q2", "lam_k2")):
        cx.dma("sp", lq[:, i, :], dr[nm][l, :].partition_broadcast(128), w=["lq"])
    cx.tt(lp[:, 0, :], lq[:, 0, :], lq[:, 1, :], ALU.mult, r=["lq"], w=["lp"])
    cx.tt(lp[:, 1, :], lq[:, 2, :], lq[:, 3, :], ALU.mult, r=["lq"], w=["lp"])
    cx.red(ls[:, 0:2], lp[:, :, :], ALU.add, r=["lp"], w=["ls"])
    cx.act(ls[:, 0:2], ls[:, 0:2], AF.Exp, r=["ls"], w=["ls"])
    cx.tt(ls[:, 2:3], ls[:, 1:2], ls[:, 0:1], ALU.subtract, r=["ls"], w=["ls2"])
    cx.ts(ls[:, 3:4], ls[:, 2:3], -lam_init, ALU.add, r=["ls2"], w=["nlam"])
    nlam = ls[:, 3:4]
    dgb = cx.sb("dgb", [128, 4, 64])
    for h in range(4):
        cx.dma("sp", dgb[:, h, :], dr["diff_g"][l, :].partition_broadcast(128), w=["dgb"])
    cx.ts(dgb[:, :, :], dgb[:, :, :], 1.0 - lam_init, ALU.mult, r=["dgb"], w=["dgb"])
    qg = [cx.sb("qg%d" % i, [128, 2, 512], BF16) for i in range(2)]
    qm = [cx.sb("qm%d" % i, [128, 8, 512], BF16) for i in range(2)]
    pT = [cx.sb("pT%d" % i, [128, NB, 512], BF16) for i in range(2)]
    oT = cx.sb("oT", [65, 2, 512])
    oatt = cx.sb("oatt", [128, 4, 4, 64]); osq = cx.sb("aosq", [128, 4, 64])
    rr = cx.sb("rr", [128, 4]); ast = cx.sb("ast", [128, 2, 4])
    ysb = [cx.sb("aysb%d" % i, [128, 256], BF16) for i in range(2)]
    yT = cx.sb("ayT", [128, 2, 512], BF16)
    gi = 0
    for (tag, T, nkt) in segs:
        sfx = "_" + tag
        G = min(512, T)
        nt = G // 128
        assert nkt % NB == 0
        for g in range(T // G):
            t0 = g * G
            b = gi % 2
            gi += 1
            cx.dma("sp", qg[b][:, :, 0:G], dr["qT" + sfx][:, :, t0:t0 + G].rearrange("c p t -> p c t"),
                   r=["D:rope" + sfx + "10", "D:rope" + sfx + "11"], w=["qg%d" % b])
            qb_ = b
            for c in range(2):
                for bl in range(4):
                    cx.ts(qm[qb_][:, c * 4 + bl, 0:G], qg[b][:, c, 0:G], cs(cst, "bm8")[:, bl:bl + 1], ALU.mult,
                          r=["qg%d" % b, "cst"], w=["qm%d_%d" % (qb_, c * 4 + bl)])
            items = [(h, m, kb) for h in range(4) for m in range(2) for kb in range(nkt // NB)]
            LA = 1

            def emit_S(i):
                h, m, kb = items[i]
                c = h // 2
                qi = c * 4 + (h % 2) * 2 + m
                sb_ = i % 2
                for j in range(NB):
                    kt = kb * NB + j
                    kk = "kTc%d" % c if kt < TC // 128 else "kT%d_%d" % (c, (kt * 128 - TC) // TL)
                    cx.mm(psS[sb_][:, j * 512:j * 512 + G], kT[:, c, kt * 128:(kt + 1) * 128], qm[qb_][:, qi, 0:G], True, True,
                          r=[kk, "qm%d_%d" % (qb_, qi)], w=["psS%d" % sb_])
                cx.act(pT[sb_][:, :, 0:G], psS[sb_][:, :].rearrange("p (j n) -> p j n", j=NB)[:, :, 0:G], AF.Exp, scale=QSCALE,
                       r=["psS%d" % sb_], w=["pT%d" % sb_])

            def emit_O(i):
                h, m, kb = items[i]
                sb_ = i % 2
                for j in range(NB):
                    kt = kb * NB + j
                    vk = "Va0" if kt < TC // 128 else "Va%d" % (1 + (kt - TC // 128) // 10)
                    cx.mm(psO[m][0:65, 0:G], Va[:, kt, h, :], pT[sb_][:, j, 0:G], kt == 0, kt == nkt - 1,
                          r=[vk, "Vones", "pT%d" % sb_], w=["psO%d" % m])
                if kb == nkt // NB - 1:
                    cx.copy(oT[:, m, 0:G], psO[m][0:65, 0:G], r=["psO%d" % m], w=["oT%d" % m])
                    if m == 1:
                        head_epilogue(h)

            def head_epilogue(h):
                for j in range(nt):
                    for m in range(2):
                        cx.tr(ps6[:, m * 65:m * 65 + 65], oT[0:65, m, j * 128:(j + 1) * 128], cs(cst, "I1")[0:65, 0:65],
                              r=["oT%d" % m, "cst"], w=["ps6"])
                    cx.recip(rr[:, 0:1], ps6[:, 64:65], r=["ps6"], w=["rr0"])
                    cx.recip(rr[:, 1:2], ps6[:, 129:130], r=["ps6"], w=["rr1"])
                    cx.tt(rr[:, 2:3], rr[:, 1:2], nlam, ALU.mult, r=["rr1", "nlam"], w=["rr2"])
                    cx.ts(oatt[:, j, h, :], ps6[:, 0:64], rr[:, 0:1], ALU.mult, r=["ps6", "rr0"], w=["oatt%d" % j])
                    cx.stt(oatt[:, j, h, :], ps6[:, 65:129], rr[:, 2:3], oatt[:, j, h, :], ALU.mult, ALU.add,
                           r=["ps6", "rr2", "oatt%d" % j], w=["oatt%d" % j])

            if ATT_ROW:
                items = [(h, kt) for h in range(4) for kt in range(nkt)]

                def emit_S(i):
                    h, kt = items[i]
                    c = h // 2
                    sb_ = i % 2
                    kk = "kTc%d" % c if kt < TC // 128 else "kT%d_%d" % (c, (kt * 128 - TC) // TL)
                    for m in range(2):
                        blk = (h % 2) * 2 + m
                        rs = slice(32 * blk, 32 * blk + 32)
                        cx.mm(psS[sb_][:, m * 512:m * 512 + G], kT[rs, c, kt * 128:(kt + 1) * 128], qg[b][rs, c, 0:G], True, True,
                              r=[kk, "qg%d" % b], w=["psS%d" % sb_], tile_position=(32 * blk, 0))
                    cx.act(pT[sb_][:, :, 0:G], psS[sb_][:, :].rearrange("p (j n) -> p j n", j=NB)[:, :, 0:G], AF.Exp, scale=QSCALE,
                           r=["psS%d" % sb_], w=["pT%d" % sb_])

                def emit_O(i):
                    h, kt = items[i]
                    sb_ = i % 2
                    vk = "Va0" if kt < TC // 128 else "Va%d" % (1 + (kt - TC // 128) // 10)
                    for m in range(2):
                        cx.mm(psO[m][0:65, 0:G], Va[:, kt, h, :], pT[sb_][:, m, 0:G], kt == 0, kt == nkt - 1,
                              r=[vk, "Vones", "pT%d" % sb_], w=["psO%d" % m])
                    if kt == nkt - 1:
                        for m in range(2):
                            cx.copy(oT[:, m, 0:G], psO[m][0:65, 0:G], r=["psO%d" % m], w=["oT%d" % m])
                        head_epilogue(h)
            n_it = len(items)
            for i in range(n_it + LA):
                if i < n_it:
                    emit_S(i)
                if i >= LA:
                    emit_O(i - LA)
            for j in range(nt):
                yb = j % 2
                cx.tt(osq[:, :, :], oatt[:, j, :, :], oatt[:, j, :, :], ALU.mult, r=["oatt%d" % j], w=["aosq"], eng="pool")
                cx.red(ast[:, 0, :], osq[:, :, :], ALU.add, r=["aosq"], w=["ast0"])
                cx.act(ast[:, 1, :], ast[:, 0, :], AF.Sqrt, scale=1.0 / 64, bias=EPS, r=["ast0"], w=["ast1"])
                cx.recip(ast[:, 1, :], ast[:, 1, :], r=["ast1"], w=["ast1"])
                for h in range(4):
                    cx.stt(oatt[:, j, h, :], oatt[:, j, h, :], ast[:, 1, h:h + 1], dgb[:, h, :], ALU.mult, ALU.mult,
                           r=["oatt%d" % j, "ast1", "dgb"], w=["oatt%d" % j])
                cx.copy(ysb[yb][:, :], oatt[:, j, :, :].rearrange("p h e -> p (h e)"), r=["oatt%d" % j], w=["aysb%d" % yb], eng="pool")
                for c2 in range(2):
                    cx.tr(psb[0][:, c2 * 128:(c2 + 1) * 128], ysb[yb][:, c2 * 128:(c2 + 1) * 128], ident[:, :],
                          r=["aysb%d" % yb, "ident"], w=["psb0"])
                cx.copy(yT[:, :, j * 128:(j + 1) * 128], psb[0][:, 0:256].rearrange("p (c t) -> p c t", c=2), r=["psb0"], w=["ayT"])
            for c2 in range(2):
                cx.dma("sp", dr["ysT" + sfx][4 + c2, :, t0:t0 + G], yT[:, c2, 0:G], r=["ayT"], w=["D:ys2" + sfx])
    cx.end()


def phase_moe(cx, l, dr, segs, final, wl=None):
    wl = l if wl is None else wl
    cx.begin(nf=6, nb=2)
    ps, psb = cx.ps, cx.psb
    NT = sum(s[1] for s in segs) // 128
    TT = NT * 128
    cst = cx.sb("cst", [128, CSTW])
    ident = cx.sb("ident", [128, 128], BF16)
    cx.dma("sp", cst[:, :], dr["cst"][:, :], w=["cst"])
    cx.dma("sp", ident[:, :], dr["ident"][:, :], w=["ident"])
    h2T = cx.sb("h2T", [128, 8, TT], BF16)
    acc = cx.sb("eacc", [128, NT, D])
    wt = cx.sb("wt", [128, NT, 16])
    WR = cx.sb("WRt", [128, 8, 16], BF16)
    cx.dma("pool", WR[:, :, :], dr["w_router"].rearrange("(k p) e -> p k e", p=128), w=["WRt"])
    brb = cx.sb("brb", [128, 16])
    cx.dma("sp", brb[:, :], dr["b_router"][0, :].partition_broadcast(128), w=["brb"])
    gsb = cx.sb("gsb2", [128, D]); shb = cx.sb("shb2", [128, D]); g2b = cx.sb("g2b2", [128, D])
    xt = [cx.sb("ext%d" % i, [128, D]) for i in range(2)]
    junk = cx.sb("ejunk", [128, D], BF16)
    t1 = cx.sb("et1", [128, D])
    hb = [cx.sb("ehb%d" % i, [128, D], BF16) for i in range(2)]
    ssq = cx.sb("essq", [128, 2]); rstd = cx.sb("erstd", [128, 2])
    rt = cx.sb("rt", [128, 8, 16])
    W1 = [cx.sb("W1_%d" % i, [128, 8, DFF], BF16) for i in range(2)]
    W3 = [cx.sb("W3_%d" % i, [128, 8, DFF], BF16) for i in range(2)]
    W2 = [cx.sb("W2_%d" % i, [128, 4, D], BF16) for i in range(2)]

    def load_expert(e):
        b = e % 2
        cx.dma("pool", W1[b][:, :, :], dr["w1_e"][wl, e].rearrange("(k p) f -> p k f", p=128), w=["W1_%d" % b])
        cx.dma("pool", W3[b][:, :, :], dr["w3_e"][wl, e].rearrange("(k p) f -> p k f", p=128), w=["W3_%d" % b])
        cx.dma("pool", W2[b][:, :, :], dr["w2_e"][wl, e].rearrange("(k p) n -> p k n", p=128), w=["W2_%d" % b])
    load_expert(0)
    load_expert(1)
    ti = 0
    tiles = []
    for (tag, T, xmid, xout) in segs:
        seg = 0 if tag == "l" else 1
        cx.dma("sp", gsb[:, :], dr["modv"][l, seg, 3, :].partition_broadcast(128), r=["D:modv%d" % l], w=["gsb2"])
        cx.dma("sp", shb[:, :], dr["modv"][l, seg, 4, :].partition_broadcast(128), r=["D:modv%d" % l], w=["shb2"])
        for j in range(T // 128):
            b = ti % 2
            kx = "ext%d" % b
            rows = slice(j * 128, (j + 1) * 128)
            tiles.append((tag, seg, xmid, xout, rows))
            cx.dma("sp", xt[b][:, :], xmid[rows, :], r=["D:xmid_" + tag], w=[kx])
            cx.act(junk[:, :], xt[b][:, :], AF.Square, accum_out=ssq[:, b:b + 1], r=[kx], w=["ejunk", "essq%d" % b])
            cx.act(rstd[:, b:b + 1], ssq[:, b:b + 1], AF.Sqrt, scale=1.0 / D, bias=EPS, r=["essq%d" % b], w=["ers%d" % b])
            cx.recip(rstd[:, b:b + 1], rstd[:, b:b + 1], r=["ers%d" % b], w=["ers%d" % b])
            cx.stt(t1[:, :], xt[b][:, :], rstd[:, b:b + 1], gsb[:, :], ALU.mult, ALU.mult, r=[kx, "ers%d" % b, "gsb2"], w=["et1"])
            cx.tt(hb[b][:, :], t1[:, :], shb[:, :], ALU.add, r=["et1", "shb2"], w=["ehb%d" % b], eng="pool")
            for kc in range(8):
                cx.tr(psb[0][:, kc * 128:(kc + 1) * 128], hb[b][:, kc * 128:(kc + 1) * 128], ident[:, :],
                      r=["ehb%d" % b, "ident"], w=["psb0"])
            cx.copy(h2T[:, :, ti * 128:(ti + 1) * 128], psb[0][:, :].rearrange("p (k t) -> p k t", k=8),
                    r=["psb0"], w=["h2T%d" % ti], eng="act")
            for kc in range(8):
                cx.mm(ps[0][:, 0:16], h2T[:, kc, ti * 128:(ti + 1) * 128], WR[:, kc, :], kc == 0, kc == 7,
                      r=["h2T%d" % ti, "WRt"], w=["ps0"])
            s_ = rt[:, 0, :]; sbv = rt[:, 1, :]; tmp = rt[:, 2, :]; sb2 = rt[:, 3, :]; sbm = rt[:, 4, :]
            msk = rt[:, 5, :]; sel = rt[:, 6, :]
            g4 = rt[:, 7, 0:4]; g4b = rt[:, 7, 4:8]; gm = rt[:, 7, 8:12]; e1 = rt[:, 7, 12:13]; e2 = rt[:, 7, 13:14]
            den = rt[:, 7, 14:15]
            cx.act(s_, ps[0][:, 0:16], AF.Sigmoid, r=["ps0"], w=["r_s"])
            cx.tt(sbv, s_, brb[:, :], ALU.add, r=["r_s", "brb"], w=["r_sb"])
            v4 = lambda a: a.rearrange("p (g e) -> p g e", g=4)
            cx.red(g4, v4(sbv), ALU.max, r=["r_sb"], w=["r_g4"])
            for g_ in range(4):
                cx.ts(tmp[:, g_ * 4:(g_ + 1) * 4], sbv[:, g_ * 4:(g_ + 1) * 4], g4[:, g_:g_ + 1], ALU.is_equal,
                      r=["r_sb", "r_g4"], w=["r_tmp"])
            cx.stt(sb2, tmp, -1.0e9, sbv, ALU.mult, ALU.add, r=["r_tmp", "r_sb"], w=["r_sb2"])
            cx.red(g4b, v4(sb2), ALU.max, r=["r_sb2"], w=["r_g4b"])
            cx.tt(g4, g4, g4b, ALU.add, r=["r_g4", "r_g4b"], w=["r_g4"])
            cx.red(e1, g4, ALU.max, r=["r_g4"], w=["r_e1"])
            cx.ts(gm, g4, e1, ALU.is_equal, s2=-1.0, op1=ALU.add, r=["r_g4", "r_e1"], w=["r_gm"])
            for g_ in range(4):
                cx.ts(tmp[:, g_ * 4:(g_ + 1) * 4], cs(cst, "ones")[:, 0:4], gm[:, g_:g_ + 1], ALU.mult,
                      r=["r_gm", "cst"], w=["r_tmp"])
            cx.stt(sbm, tmp, 1.0e9, sbv, ALU.mult, ALU.add, r=["r_tmp", "r_sb"], w=["r_sbm"])
            cx.red(e1, sbm, ALU.max, r=["r_sbm"], w=["r_e1"])
            cx.ts(msk, sbm, e1, ALU.is_equal, r=["r_sbm", "r_e1"], w=["r_msk"])
            cx.stt(sb2, msk, -1.0e9, sbm, ALU.mult, ALU.add, r=["r_msk", "r_sbm"], w=["r_sb2"])
            cx.red(e2, sb2, ALU.max, r=["r_sb2"], w=["r_e2"])
            cx.ts(sel, sb2, e2, ALU.is_equal, r=["r_sb2", "r_e2"], w=["r_sel"])
            cx.tt(sel, sel, msk, ALU.add, r=["r_sel", "r_msk"], w=["r_sel"])
            cx.tt(sel, sel, s_, ALU.mult, r=["r_sel", "r_s"], w=["r_sel"])
            cx.red(den, sel, ALU.add, r=["r_sel"], w=["r_den"])
            cx.recip(den, den, r=["r_den"], w=["r_den"])
            cx.ts(wt[:, ti, :], sel, den, ALU.mult, r=["r_sel", "r_den"], w=["wt%d" % ti])
            ti += 1
    uT = [cx.sb("uT%d" % i, [128, 4, 512], BF16) for i in range(2)]
    s1 = [cx.sb("s1_%d" % i, [128, 512]) for i in range(2)]
    groups = []
    t = 0
    while t < NT:
        n = min(4, NT - t)
        groups.append((t, n))
        t += n
    ui = 0
    for e in range(NEXP):
        b = e % 2
        if e >= 2:
            load_expert(e)
        for (tg, ntl) in groups:
            G = ntl * 128
            gsl = slice(tg * 128, tg * 128 + G)
            hk = ["h2T%d" % i for i in range(tg, tg + ntl)]
            ub = ui % 2
            ui += 1
            for fc in range(4):
                fs = slice(fc * 128, (fc + 1) * 128)
                pa = ps[(fc % 2) * 2]; pb = ps[(fc % 2) * 2 + 1]
                ka = "ps%d" % ((fc % 2) * 2); kb = "ps%d" % ((fc % 2) * 2 + 1)
                for kc in range(8):
                    cx.mm(pa[:, 0:G], W1[b][:, kc, fs], h2T[:, kc, gsl], kc == 0, kc == 7, r=hk + ["W1_%d" % b], w=[ka])
                for kc in range(8):
                    cx.mm(pb[:, 0:G], W3[b][:, kc, fs], h2T[:, kc, gsl], kc == 0, kc == 7, r=hk + ["W3_%d" % b], w=[kb])
                sb_ = s1[fc % 2]
                cx.act(sb_[:, 0:G], pa[:, 0:G], AF.Silu, r=[ka], w=["s1_%d" % (fc % 2)])
                cx.tt(uT[ub][:, fc, 0:G], pb[:, 0:G], sb_[:, 0:G], ALU.mult, r=[kb, "s1_%d" % (fc % 2)], w=["uT%d" % ub])
            for j in range(ntl):
                tix = tg + j
                for nh in range(2):
                    po = ps[4 + nh]
                    for fc in range(4):
                        cx.mm(po[:, :], uT[ub][:, fc, j * 128:(j + 1) * 128], W2[b][:, fc, nh * 512:(nh + 1) * 512], fc == 0, fc == 3,
                              r=["uT%d" % ub, "W2_%d" % b], w=["ps%d" % (4 + nh)])
                    a = acc[:, tix, nh * 512:(nh + 1) * 512]
                    ka2 = "eacc%d_%d" % (tix, nh)
                    if e == 0:
                        cx.ts(a, po[:, :], wt[:, tix, e:e + 1], ALU.mult, r=["ps%d" % (4 + nh), "wt%d" % tix], w=[ka2])
                    else:
                        cx.stt(a, po[:, :], wt[:, tix, e:e + 1], a, ALU.mult, ALU.add, r=["ps%d" % (4 + nh), "wt%d" % tix, ka2], w=[ka2])
    if final:
        gfb = cx.sb("gfb", [128, D])
        cx.dma("sp", gfb[:, :], dr["g_final"][0, :].partition_broadcast(128), w=["gfb"])
    cur = None
    for ti, (tag, seg, xmid, xout, rows) in enumerate(tiles):
        if cur != seg:
            cx.dma("sp", g2b[:, :], dr["modv"][l, seg, 5, :].partition_broadcast(128), r=["D:modv%d" % l], w=["g2b2"])
            cur = seg
        b = ti % 2
        kx = "ext%d" % b
        cx.dma("sp", xt[b][:, :], xmid[rows, :], r=["D:xmid_" + tag], w=[kx])
        cx.tt(t1[:, :], acc[:, ti, :], g2b[:, :], ALU.mult, r=["eacc%d_0" % ti, "eacc%d_1" % ti, "g2b2"], w=["et1"], eng="pool")
        cx.tt(xt[b][:, :], xt[b][:, :], t1[:, :], ALU.add, r=[kx, "et1"], w=[kx])
        if final:
            cx.act(junk[:, :], xt[b][:, :], AF.Square, accum_out=ssq[:, b:b + 1], r=[kx], w=["ejunk", "essq%d" % b])
            cx.act(rstd[:, b:b + 1], ssq[:, b:b + 1], AF.Sqrt, scale=1.0 / D, bias=EPS, r=["essq%d" % b], w=["ers%d" % b])
            cx.recip(rstd[:, b:b + 1], rstd[:, b:b + 1], r=["ers%d" % b], w=["ers%d" % b])
            cx.stt(xt[b][:, :], xt[b][:, :], rstd[:, b:b + 1], gfb[:, :], ALU.mult, ALU.mult, r=[kx, "ers%d" % b, "gfb"], w=[kx])
        cx.dma("sp", xout[rows, :], xt[b][:, :], r=[kx], w=["D:xout_" + tag])
    cx.end()


def phase_moe_sparse(cx, l, dr, segs, final, wl=None):
    wl = l if wl is None else wl
    C = MOE_CAP
    cx.begin(nf=6, nb=2)
    ps, psb = cx.ps, cx.psb
    NT = sum(s[1] for s in segs) // 128
    cst = cx.sb("cst", [128, CSTW])
    ident = cx.sb("ident", [128, 128], BF16)
    cx.dma("sp", cst[:, :], dr["cst"][:, :], w=["cst"])
    cx.dma("sp", ident[:, :], dr["ident"][:, :], w=["ident"])
    Xg = dr["Xg"]
    Yg = dr["Yg"]
    bcreg = {}

    def bc(h):
        if "r" not in bcreg:
            bcreg["r"] = h.to_reg(NSLOT - 1)
        return bcreg["r"]
    W1 = [cx.sb("W1_%d" % i, [128, 8, DFF], BF16) for i in range(2)]
    W3 = [cx.sb("W3_%d" % i, [128, 8, DFF], BF16) for i in range(2)]
    W2 = [cx.sb("W2_%d" % i, [128, 4, D], BF16) for i in range(2)]

    def load_expert(e):
        b = e % 2
        cx.dma("pool", W1[b][:, :, :], dr["w1_e"][wl, e].rearrange("(k p) f -> p k f", p=128), w=["W1_%d" % b])
        cx.dma("pool", W3[b][:, :, :], dr["w3_e"][wl, e].rearrange("(k p) f -> p k f", p=128), w=["W3_%d" % b])
        cx.dma("pool", W2[b][:, :, :], dr["w2_e"][wl, e].rearrange("(k p) n -> p k n", p=128), w=["W2_%d" % b])
    load_expert(0)
    load_expert(1)
    zt = cx.sb("zt", [128, 4, D], BF16)
    cx.memset(zt[:, :, :], 0.0, w=["zt"], eng="pool")
    for i in range(NSLOT // 512):
        cx.dma("sp", Xg[i * 512:(i + 1) * 512, :].rearrange("(b p) d -> p b d", p=128), zt[:, :, :], r=["zt"], w=["D:XgZ%d" % i])
    WR = cx.sb("WRt", [128, 8, 16], BF16)
    cx.dma("pool", WR[:, :, :], dr["w_router"].rearrange("(k p) e -> p k e", p=128), w=["WRt"])
    brb = cx.sb("brb", [128, 16])
    cx.dma("sp", brb[:, :], dr["b_router"][0, :].partition_broadcast(128), w=["brb"])
    gsb = cx.sb("gsb2", [128, D]); shb = cx.sb("shb2", [128, D]); g2b = cx.sb("g2b2", [128, D])
    xt = [cx.sb("ext%d" % i, [128, D]) for i in range(2)]
    junk = cx.sb("ejunk", [128, D], BF16)
    t1 = cx.sb("et1", [128, D])
    hb = [cx.sb("ehb%d" % i, [128, D], BF16) for i in range(2)]
    hTt = [cx.sb("ehT%d" % i, [128, 8, 128], BF16) for i in range(2)]
    ssq = cx.sb("essq", [128, 2]); rstd = cx.sb("erstd", [128, 2])
    rt = cx.sb("rt", [128, 10, 16])
    off = cx.sb("roff", [128, 16])
    slf = cx.sb("slf", [128, NT, 2]); sli = cx.sb("sli", [128, NT, 2], mybir.dt.int32); wts = cx.sb("wts", [128, NT, 2])
    cx.memset(off[:, :], 0.0, w=["roff"])
    ti = 0
    tiles = []
    for (tag, T, xmid, xout) in segs:
        seg = 0 if tag == "l" else 1
        cx.dma("sp", gsb[:, :], dr["modv"][l, seg, 3, :].partition_broadcast(128), r=["D:modv%d" % l], w=["gsb2"])
        cx.dma("sp", shb[:, :], dr["modv"][l, seg, 4, :].partition_broadcast(128), r=["D:modv%d" % l], w=["shb2"])
        for j in range(T // 128):
            b = ti % 2
            kx = "ext%d" % b
            rows = slice(j * 128, (j + 1) * 128)
            tiles.append((tag, seg, xmid, xout, rows))
            cx.dma("sp", xt[b][:, :], xmid[rows, :], r=["D:xmid_" + tag], w=[kx])
            cx.act(junk[:, :], xt[b][:, :], AF.Square, accum_out=ssq[:, b:b + 1], r=[kx], w=["ejunk", "essq%d" % b])
            cx.act(rstd[:, b:b + 1], ssq[:, b:b + 1], AF.Sqrt, scale=1.0 / D, bias=EPS, r=["essq%d" % b], w=["ers%d" % b])
            cx.recip(rstd[:, b:b + 1], rstd[:, b:b + 1], r=["ers%d" % b], w=["ers%d" % b])
            cx.stt(t1[:, :], xt[b][:, :], rstd[:, b:b + 1], gsb[:, :], ALU.mult, ALU.mult, r=[kx, "ers%d" % b, "gsb2"], w=["et1"])
            cx.tt(hb[b][:, :], t1[:, :], shb[:, :], ALU.add, r=["et1", "shb2"], w=["ehb%d" % b], eng="pool")
            for kc in range(8):
                cx.tr(psb[0][:, kc * 128:(kc + 1) * 128], hb[b][:, kc * 128:(kc + 1) * 128], ident[:, :],
                      r=["ehb%d" % b, "ident"], w=["psb0"])
            cx.copy(hTt[b][:, :, :], psb[0][:, :].rearrange("p (k t) -> p k t", k=8), r=["psb0"], w=["ehT%d" % b], eng="act")
            for kc in range(8):
                cx.mm(ps[0][:, 0:16], hTt[b][:, kc, :], WR[:, kc, :], kc == 0, kc == 7, r=["ehT%d" % b, "WRt"], w=["ps0"])
            s_ = rt[:, 0, :]; sbv = rt[:, 1, :]; tmp = rt[:, 2, :]; sb2 = rt[:, 3, :]; sbm = rt[:, 4, :]
            msk = rt[:, 5, :]; sel = rt[:, 6, :]; pos = rt[:, 8, :]; m2 = rt[:, 9, :]
            g4 = rt[:, 7, 0:4]; g4b = rt[:, 7, 4:8]; gm = rt[:, 7, 8:12]; e1 = rt[:, 7, 12:13]; e2 = rt[:, 7, 13:14]
            den = rt[:, 7, 14:15]
            cx.act(s_, ps[0][:, 0:16], AF.Sigmoid, r=["ps0"], w=["r_s"])
            cx.tt(sbv, s_, brb[:, :], ALU.add, r=["r_s", "brb"], w=["r_sb"])
            v4 = lambda a: a.rearrange("p (g e) -> p g e", g=4)
            cx.red(g4, v4(sbv), ALU.max, r=["r_sb"], w=["r_g4"])
            for g_ in range(4):
                cx.ts(tmp[:, g_ * 4:(g_ + 1) * 4], sbv[:, g_ * 4:(g_ + 1) * 4], g4[:, g_:g_ + 1], ALU.is_equal,
                      r=["r_sb", "r_g4"], w=["r_tmp"])
            cx.stt(sb2, tmp, -1.0e9, sbv, ALU.mult, ALU.add, r=["r_tmp", "r_sb"], w=["r_sb2"])
            cx.red(g4b, v4(sb2), ALU.max, r=["r_sb2"], w=["r_g4b"])
            cx.tt(g4, g4, g4b, ALU.add, r=["r_g4", "r_g4b"], w=["r_g4"])
            cx.red(e1, g4, ALU.max, r=["r_g4"], w=["r_e1"])
            cx.ts(gm, g4, e1, ALU.is_equal, s2=-1.0, op1=ALU.add, r=["r_g4", "r_e1"], w=["r_gm"])
            for g_ in range(4):
                cx.ts(tmp[:, g_ * 4:(g_ + 1) * 4], cs(cst, "ones")[:, 0:4], gm[:, g_:g_ + 1], ALU.mult,
                      r=["r_gm", "cst"], w=["r_tmp"])
            cx.stt(sbm, tmp, 1.0e9, sbv, ALU.mult, ALU.add, r=["r_tmp", "r_sb"], w=["r_sbm"])
            cx.red(e1, sbm, ALU.max, r=["r_sbm"], w=["r_e1"])
            cx.ts(msk, sbm, e1, ALU.is_equal, r=["r_sbm", "r_e1"], w=["r_msk"])
            cx.stt(sb2, msk, -1.0e9, sbm, ALU.mult, ALU.add, r=["r_msk", "r_sbm"], w=["r_sb2"])
            cx.red(e2, sb2, ALU.max, r=["r_sb2"], w=["r_e2"])
            cx.ts(m2, sb2, e2, ALU.is_equal, r=["r_sb2", "r_e2"], w=["r_m2"])
            cx.tt(sel, m2, msk, ALU.add, r=["r_m2", "r_msk"], w=["r_sel"])
            cx.mm(ps[1][:, 0:16], cs(cst, "Ltri"), sel, True, True, r=["cst", "r_sel"], w=["ps1"])
            cx.mm(ps[1][:, 16:32], cs(cst, "ones"), sel, True, True, r=["cst", "r_sel"], w=["ps1"])
            cx.tt(pos, ps[1][:, 0:16], off[:, :], ALU.add, r=["ps1", "roff"], w=["r_pos"])
            cx.tt(off[:, :], ps[1][:, 16:32], off[:, :], ALU.add, r=["ps1", "roff"], w=["roff"])
            cx.ts(tmp, pos, float(C) - 0.5, ALU.is_lt, r=["r_pos"], w=["r_tmp"])
            cx.tt(pos, pos, cs(cst, "eoff"), ALU.add, r=["r_pos", "cst"], w=["r_pos"])
            cx.stt(pos, tmp, -1.0e6, pos, ALU.mult, ALU.add, r=["r_tmp", "r_pos"], w=["r_pos"])
            cx.ts(pos, pos, 1.0e6, ALU.add, r=["r_pos"], w=["r_pos"])
            cx.tt(sb2, sel, s_, ALU.mult, r=["r_sel", "r_s"], w=["r_sb2"])
            cx.red(den, sb2, ALU.add, r=["r_sb2"], w=["r_den"])
            cx.recip(den, den, r=["r_den"], w=["r_den"])
            cx.ts(sb2, sb2, den, ALU.mult, r=["r_sb2", "r_den"], w=["r_sb2"])
            cx.tt(sb2, sb2, tmp, ALU.mult, r=["r_sb2", "r_tmp"], w=["r_sb2"])
            for q, mk, kk in ((0, msk, "r_msk"), (1, m2, "r_m2")):
                cx.tt(sbm, mk, pos, ALU.mult, r=[kk, "r_pos"], w=["r_sbm"])
                cx.red(slf[:, ti, q:q + 1], sbm, ALU.add, r=["r_sbm"], w=["slf%d_%d" % (ti, q)])
                cx.tt(sbm, mk, sb2, ALU.mult, r=[kk, "r_sb2"], w=["r_sbm"])
                cx.red(wts[:, ti, q:q + 1], sbm, ALU.add, r=["r_sbm"], w=["wts%d_%d" % (ti, q)])
            cx.copy(sli[:, ti, :], slf[:, ti, :], r=["slf%d_0" % ti, "slf%d_1" % ti], w=["sli%d" % ti])
            for q in range(2):
                idx = sli[:, ti, q:q + 1]
                src = hb[b][:, :]
                cx.S.add("pool", lambda h, idx=idx, src=src: h.indirect_dma_start(
                    out=Xg[:, :], out_offset=bass.IndirectOffsetOnAxis(ap=idx, axis=0), in_=src, in_offset=None,
                    bounds_check=bc(h), oob_is_err=False), r=["sli%d" % ti, "ehb%d" % b] + ["D:XgZ%d" % i_ for i_ in range(NSLOT // 512)], w=["D:Xg%d_%d" % (ti, q)], dma=True)
            ti += 1
    dummy = cx.sb("dummy", [128, 4])
    cx.memset(dummy[:, 0:1], 0.0, w=["XgAll"], eng="pool")
    cx.S.ops[-1].deps.update({cx.S.lastw[k]: True for k in ["D:Xg%d_%d" % (t_, q) for t_ in range(NT) for q in range(2)]})
    NBLK = C // 128
    NPC = (C + 511) // 512
    PW = C // NPC
    xg = [cx.sb("xg%d" % i, [128, NBLK, D], BF16) for i in range(2)]
    xT = [cx.sb("xTe%d" % i, [128, 8, C], BF16) for i in range(2)]
    uT = cx.sb("uTe", [128, 4, C], BF16)
    s1 = [cx.sb("s1_%d" % i, [128, 512]) for i in range(2)]
    yb = [cx.sb("ybe%d" % i, [128, D]) for i in range(2)]
    yi = 0

    def load_xg(e):
        cx.dma("sp", xg[e % 2][:, :, :], Xg[e * C:(e + 1) * C, :].rearrange("(j p) d -> p j d", p=128), r=["XgAll"], w=["xg%d" % (e % 2)])
    load_xg(0)
    for e in range(NEXP):
        b = e % 2
        if e >= 2:
            load_expert(e)
        if e + 1 < NEXP:
            load_xg(e + 1)
        for j in range(NBLK):
            pbk = psb[j % 2]
            kp = "psb%d" % (j % 2)
            for kc in range(8):
                cx.tr(pbk[:, kc * 128:(kc + 1) * 128], xg[b][:, j, kc * 128:(kc + 1) * 128], ident[:, :],
                      r=["xg%d" % b, "ident"], w=[kp])
            cx.copy(xT[b][:, :, j * 128:(j + 1) * 128], pbk[:, :].rearrange("p (k t) -> p k t", k=8), r=[kp], w=["xTe%d" % b],
                    eng=("act" if j % 2 == 0 else "dve"))
        it = 0
        for fc in range(4):
            fs = slice(fc * 128, (fc + 1) * 128)
            for pc in range(NPC):
                cs_ = slice(pc * PW, (pc + 1) * PW)
                pa = ps[(it % 2) * 2]; pb = ps[(it % 2) * 2 + 1]
                ka = "ps%d" % ((it % 2) * 2); kb = "ps%d" % ((it % 2) * 2 + 1)
                for kc in range(8):
                    cx.mm(pa[:, 0:PW], W1[b][:, kc, fs], xT[b][:, kc, cs_], kc == 0, kc == 7, r=["xTe%d" % b, "W1_%d" % b], w=[ka])
                for kc in range(8):
                    cx.mm(pb[:, 0:PW], W3[b][:, kc, fs], xT[b][:, kc, cs_], kc == 0, kc == 7, r=["xTe%d" % b, "W3_%d" % b], w=[kb])
                sb_ = s1[it % 2]
                cx.act(sb_[:, 0:PW], pa[:, 0:PW], AF.Silu, r=[ka], w=["s1_%d" % (it % 2)])
                cx.tt(uT[:, fc, cs_], pb[:, 0:PW], sb_[:, 0:PW], ALU.mult, r=[kb, "s1_%d" % (it % 2)], w=["uTe"])
                it += 1
        for j in range(NBLK):
            y = yb[yi % 2]
            ky = "ybe%d" % (yi % 2)
            yi += 1
            for nh in range(2):
                po = ps[4 + nh]
                for fc in range(4):
                    cx.mm(po[:, :], uT[:, fc, j * 128:(j + 1) * 128], W2[b][:, fc, nh * 512:(nh + 1) * 512], fc == 0, fc == 3,
                          r=["uTe", "W2_%d" % b], w=["ps%d" % (4 + nh)])
                cx.copy(y[:, nh * 512:(nh + 1) * 512], po[:, :], r=["ps%d" % (4 + nh)], w=[ky], eng=("act" if nh == 0 else "dve"))
            cx.dma("sp", Yg[e * C + j * 128:e * C + (j + 1) * 128, :], y[:, :], r=[ky], w=["D:Yg%d_%d" % (e, j)])
    if final:
        gfb = cx.sb("gfb", [128, D])
        cx.dma("sp", gfb[:, :], dr["g_final"][0, :].partition_broadcast(128), w=["gfb"])
    cx.memset(dummy[:, 1:2], 0.0, w=["YgAll"], eng="pool")
    cx.S.ops[-1].deps.update({cx.S.lastw[k]: True for k in ["D:Yg%d_%d" % (e_, j_) for e_ in range(NEXP) for j_ in range(C // 128)]})
    yg = [[cx.sb("yg%d_%d" % (i, q), [128, D]) for q in range(2)] for i in range(2)]
    cur = None
    for ti, (tag, seg, xmid, xout, rows) in enumerate(tiles):
        if cur != seg:
            cx.dma("sp", g2b[:, :], dr["modv"][l, seg, 5, :].partition_broadcast(128), r=["D:modv%d" % l], w=["g2b2"])
            cur = seg
        b = ti % 2
        kx = "ext%d" % b
        cx.dma("sp", xt[b][:, :], xmid[rows, :], r=["D:xmid_" + tag], w=[kx])
        for q in range(2):
            dst = yg[b][q][:, :]
            idx = sli[:, ti, q:q + 1]
            cx.memset(dst, 0.0, w=["yg%d_%d" % (b, q)], eng="pool")
            cx.S.add("pool", lambda h, idx=idx, dst=dst: h.indirect_dma_start(
                out=dst, out_offset=None, in_=Yg[:, :], in_offset=bass.IndirectOffsetOnAxis(ap=idx, axis=0),
                bounds_check=bc(h), oob_is_err=False), r=["sli%d" % ti, "YgAll"], w=["yg%d_%d" % (b, q)], dma=True)
        cx.ts(t1[:, :], yg[b][0][:, :], wts[:, ti, 0:1], ALU.mult, r=["yg%d_0" % b, "wts%d_0" % ti], w=["et1"])
        cx.stt(t1[:, :], yg[b][1][:, :], wts[:, ti, 1:2], t1[:, :], ALU.mult, ALU.add, r=["yg%d_1" % b, "wts%d_1" % ti, "et1"], w=["et1"])
        cx.tt(t1[:, :], t1[:, :], g2b[:, :], ALU.mult, r=["et1", "g2b2"], w=["et1"], eng="pool")
        cx.tt(xt[b][:, :], xt[b][:, :], t1[:, :], ALU.add, r=[kx, "et1"], w=[kx])
        if final:
            cx.act(junk[:, :], xt[b][:, :], AF.Square, accum_out=ssq[:, b:b + 1], r=[kx], w=["ejunk", "essq%d" % b])
            cx.act(rstd[:, b:b + 1], ssq[:, b:b + 1], AF.Sqrt, scale=1.0 / D, bias=EPS, r=["essq%d" % b], w=["ers%d" % b])
            cx.recip(rstd[:, b:b + 1], rstd[:, b:b + 1], r=["ers%d" % b], w=["ers%d" % b])
            cx.stt(xt[b][:, :], xt[b][:, :], rstd[:, b:b + 1], gfb[:, :], ALU.mult, ALU.mult, r=[kx, "ers%d" % b, "gfb"], w=[kx])
        cx.dma("sp", xout[rows, :], xt[b][:, :], r=[kx], w=["D:xout_" + tag])
    cx.end()


def make_expo(core):
    BIG = 1.0e7
    e = np.full((2, 9), BIG, np.float32)
    for c2 in range(NCORE):
        if c2 < core:
            e[0, c2] = TL * (core - 1 - c2)
        if c2 > core:
            e[1, c2] = TL * (c2 - core - 1)
    e[0, 8] = TL * core
    e[1, 8] = TL * (NCORE - 1 - core)
    return np.broadcast_to(e[None], (128, 2, 9)).copy()


def make_expo(core):
    BIG = 1.0e7
    e = np.full((2, 9), BIG, np.float32)
    for c2 in range(NCORE):
        if c2 < core:
            e[0, c2] = TL * (core - 1 - c2)
        if c2 > core:
            e[1, c2] = TL * (c2 - core - 1)
    e[0, 8] = TL * core
    e[1, 8] = TL * (NCORE - 1 - core)
    return np.broadcast_to(e[None], (128, 2, 9)).copy()


def make_sel(core):
    s = np.zeros((128, 2, NCORE), np.float32)
    if core > 0:
        s[:, 0, core - 1] = 1.0
    if core < NCORE - 1:
        s[:, 1, core + 1] = 1.0
    return s


def phase_exchange(cx, l, dr):
    cx.begin(nf=0, nb=0)
    hin = dr["hin"]
    for a, (src, c2) in enumerate(((dr["uT_l"], 0), (dr["uT_l"], 1), (dr["tT_l"], 0), (dr["tT_l"], 1))):
        cx.dma("sp", hin[a * 128:(a + 1) * 128, 0:16], src[c2, :, 0:16], r=["D:uT_l", "D:tT_l"], w=["D:hin"])
        cx.dma("sp", hin[a * 128:(a + 1) * 128, 16:32], src[c2, :, TL - 16:TL], r=["D:uT_l", "D:tT_l"], w=["D:hin"])
    grp = [list(range(NCORE))]

    def cc(src, dst, rk, wk):
        cx.S.add("pool", lambda h: h.collective_compute("AllGather", ALU.bypass, replica_groups=grp, ins=[src], outs=[dst]),
                 r=rk, w=wk, cc=True)
    cc(dr["kT_l"].rearrange("c p t -> (c p) t").opt(), dr["gk"].opt(), ["D:rope_l12", "D:rope_l13"], ["D:gk"])
    cc(dr["V_l"].opt(), dr["gv"].opt(), ["D:V_l"], ["D:gv"])
    cc(dr["Tst_l"].rearrange("a p e -> (a p) e").opt(), dr["gt"].opt(), ["D:Tst_l"], ["D:gt"])
    cc(hin.opt(), dr["hg"].opt(), ["D:hin"], ["D:hg"])
    cx.end()


A_OUT = (("hT", lambda T: [128, 8, T], BF16), ("uT", lambda T: [2, 128, T], F32), ("tT", lambda T: [2, 128, T], F32),
         ("bgT", lambda T: [2, 128, T], BF16), ("qT", lambda T: [2, 128, T], BF16), ("kT", lambda T: [2, 128, T], BF16),
         ("rqT", lambda T: [128, T], BF16), ("rkT", lambda T: [128, T], BF16), ("V", lambda T: [T, 256], BF16),
         ("rv", lambda T: [T, 256], BF16), ("rg", lambda T: [T, 256], BF16), ("Tst", lambda T: [2, 128, 256], F32))
SEGT = (("l", TL), ("c", TC))
NKALL = (SEQ + TC) // 128
EXT_IN = (("c", [1, D]), ("c_ctx", [1, D]), ("w_mod", [2, D, 6 * D]), ("b_mod", [2, 6 * D]), ("g_norm1", [2, D]), ("g_norm2", [2, D]),
          ("w_in", [2, D, INC]), ("conv_a_w", [2, 31, 256]), ("conv_a_b", [2, 256]), ("conv_a_g", [2, 256]),
          ("conv_a_beta", [2, 256]), ("conv_b_w", [2, 3, 256]), ("lam_q1", [2, 32]), ("lam_k1", [2, 32]), ("lam_q2", [2, 32]),
          ("lam_k2", [2, 32]), ("diff_g", [2, 64]), ("ret_ld_f", [2, 4]), ("ret_ld_b", [2, 4]), ("w_gate", [2, D, 4096]),
          ("b_gate", [2, 4096]), ("w_branch", [2, 4, 256, D]), ("w_o", [2, D, D]), ("w_router", [D, 16]), ("b_router", [1, 16]),
          ("w1_e", [2, NEXP, D, DFF]), ("w3_e", [2, NEXP, D, DFF]), ("w2_e", [2, NEXP, DFF, D]), ("g_final", [1, D]))


SPARSE_MOE = True


def moe_phase(cx, l, dr, segs, final, wl=None):
    if SPARSE_MOE:
        return phase_moe_sparse(cx, l, dr, segs, final, wl=wl)
    return phase_moe(cx, l, dr, segs, final, wl=wl)


def lam_init_of(l):
    return 0.8 - 0.6 * math.exp(-0.3 * l)


class Launch:
    def __init__(self):
        self.nc = bass.Bass("TRN2", target_bir_lowering=False)
        self.dr = {}
        self.ins = []
        self.outs = []

    def t(self, name, shape, dt=F32, kind=None):
        if kind is None:
            self.dr[name] = self.nc.dram_tensor(name, list(shape), dt).ap()
        else:
            self.dr[name] = self.nc.dram_tensor(name, list(shape), dt, kind=kind).ap()
        if kind == "ExternalInput":
            self.ins.append(name)
        elif kind == "ExternalOutput":
            self.outs.append(name)


def build_fused():
    L = Launch()
    L.t("cst", [128, CSTW], F32, "ExternalInput")
    L.t("ident", [128, 128], BF16, "ExternalInput")
    for n_, s_ in EXT_IN:
        L.t(n_, s_, F32, "ExternalInput")
    for n_, s_ in (("cosT", [128, TL]), ("sinT", [128, TL]), ("x_l", [TL, D]), ("x_c", [TC, D]), ("expo", [128, 2, 9]),
                   ("sel", [128, 2, NCORE])):
        L.t(n_, s_, F32, "ExternalInput")
    L.t("out", [TL, D], F32, "ExternalOutput")
    L.t("modv", [2, 2, 6, D])
    L.t("x1_l", [TL, D])
    L.t("x1_c", [TC, D])
    L.t("Xg", [NSLOT, D], BF16)
    L.t("Yg", [NSLOT, D], F32)
    drl = []
    for l in range(DEPTH):
        d_ = dict(L.dr)
        for tag, T in SEGT:
            for nm, shp, dt in A_OUT:
                L.t("%s_%s%d" % (nm, tag, l), shp(T), dt)
                d_[nm + "_" + tag] = L.dr["%s_%s%d" % (nm, tag, l)]
            L.t("ysT_%s%d" % (tag, l), [8, 128, T], BF16)
            L.t("xmid_%s%d" % (tag, l), [T, D])
            d_["ysT_" + tag] = L.dr["ysT_%s%d" % (tag, l)]
            d_["xmid_" + tag] = L.dr["xmid_%s%d" % (tag, l)]
        for nm, shp, dt in (("gk", [NCORE * 256, TL], BF16), ("gv", [NCORE * TL, 256], BF16), ("gt", [NCORE * 256, 256], F32),
                            ("hg", [NCORE * 512, 32], F32), ("hin", [512, 32], F32)):
            L.t("%s%d" % (nm, l), shp, dt)
            d_[nm] = L.dr["%s%d" % (nm, l)]
        drl.append(d_)
    for d_ in drl:
        for k in ("modv", "x1_l", "x1_c", "Xg", "Yg"):
            d_[k] = L.dr[k]
    with ExitStack() as st:
        S = Sched(L.nc, st)
        cx = Ctx(L.nc, S)
        phase_mods(cx, 0, drl[0])
        phase_mods(cx, 1, drl[0])
        d0 = drl[0]
        phase_a(cx, 0, d0, [("l", TL, L.dr["x_l"]), ("c", TC, L.dr["x_c"])])
        phase_exchange(cx, 0, d0)
        phase_conv(cx, 0, d0, [("l", TL), ("c", TC)])
        phase_attn(cx, 0, d0, [("l", TL, NKALL), ("c", TC, TC // 128)], lam_init_of(0))
        phase_ret(cx, 0, d0, [("l", TL), ("c", TC)])
        phase_merge(cx, 0, d0, [("l", TL, L.dr["x_l"], d0["xmid_l"]), ("c", TC, L.dr["x_c"], d0["xmid_c"])])
        moe_phase(cx, 0, d0, [("l", TL, d0["xmid_l"], L.dr["x1_l"]), ("c", TC, d0["xmid_c"], L.dr["x1_c"])], False)
        d1 = drl[1]
        phase_a(cx, 1, d1, [("l", TL, L.dr["x1_l"]), ("c", TC, L.dr["x1_c"])])
        phase_exchange(cx, 1, d1)
        phase_conv(cx, 1, d1, [("l", TL)])
        phase_attn(cx, 1, d1, [("l", TL, NKALL)], lam_init_of(1))
        phase_ret(cx, 1, d1, [("l", TL)])
        phase_merge(cx, 1, d1, [("l", TL, L.dr["x1_l"], d1["xmid_l"])])
        moe_phase(cx, 1, d1, [("l", TL, d1["xmid_l"], L.dr["out"])], True)
    return L


def kernel_fused(**inp):
    f32 = lambda a: np.ascontiguousarray(np.asarray(a, dtype=np.float32))
    x = f32(inp["x"])[0]
    ctx = f32(inp["ctx"])[0]
    base = dict(cst=make_cst(), ident=np.eye(128, dtype=np.float32).astype(NPBF), x_c=ctx)
    for n_, s_ in EXT_IN:
        base[n_] = f32(inp[n_]).reshape(s_)
    L = build_fused()
    maps = []
    for c in range(NCORE):
        m = dict(base)
        cosT, sinT = rope_tables(c)
        m.update(cosT=cosT, sinT=sinT, x_l=x[c * TL:(c + 1) * TL], expo=make_expo(c), sel=make_sel(c))
        maps.append({k: m[k] for k in L.ins})
    res = run_bass_kernel_spmd(L.nc, maps, core_ids=list(range(NCORE)))
    out = np.concatenate([np.asarray(res.results[c]["out"]) for c in range(NCORE)], axis=0)
    return out.reshape(1, SEQ, D).astype(np.float32)


WSLICE = ("w_in", "w_gate", "w_branch", "w_o", "w1_e", "w3_e", "w2_e")
GATH = (("gk", [NCORE * 256, TL], BF16), ("gv", [NCORE * TL, 256], BF16), ("gt", [NCORE * 256, 256], F32),
        ("hg", [NCORE * 512, 32], F32))


def build_stage(stage):
    L = Launch()
    L.t("cst", [128, CSTW], F32, "ExternalInput")
    L.t("ident", [128, 128], BF16, "ExternalInput")
    for n_, s_ in EXT_IN:
        if stage > 1 and n_ in ("w_mod", "b_mod", "c", "c_ctx"):
            continue
        if stage == 1 and n_ in ("w_gate", "w_branch", "w_o", "w1_e", "w3_e", "w2_e"):
            continue
        if stage == 3 and n_ == "w_in":
            continue
        shp = [1] + list(s_[1:]) if n_ in WSLICE else s_
        L.t(n_, shp, F32, "ExternalInput")
    for n_, s_ in (("cosT", [128, TL]), ("sinT", [128, TL]), ("expo", [128, 2, 9]), ("sel", [128, 2, NCORE])):
        L.t(n_, s_, F32, "ExternalInput")
    io = "ExternalInput"
    if stage == 1:
        L.t("x_l", [TL, D], F32, io)
        L.t("x_c", [TC, D], F32, io)
        L.t("modv", [2, 2, 6, D], F32, "ExternalOutput")
    else:
        L.t("modv", [2, 2, 6, D], F32, io)

    def a_tensors(prefix, kind, tags):
        d_ = {}
        for tag, T in SEGT:
            if tag not in tags:
                continue
            for nm, shp, dt in A_OUT:
                L.t(prefix + nm + "_" + tag, shp(T), dt, kind)
                d_[nm + "_" + tag] = L.dr[prefix + nm + "_" + tag]
        return d_
    with ExitStack() as st:
        S = Sched(L.nc, st)
        cx = Ctx(L.nc, S)
        if stage == 1:
            dA = dict(L.dr)
            dA.update(a_tensors("", "ExternalOutput", ("l", "c")))
            phase_mods(cx, 0, dA)
            phase_mods(cx, 1, dA)
            phase_a(cx, 0, dA, [("l", TL, L.dr["x_l"]), ("c", TC, L.dr["x_c"])], wl=0)
        else:
            l = stage - 2
            tags = ("l", "c")
            for nm, shp, dt in GATH:
                L.t(nm, shp, dt, io)
            L.t("Xg", [NSLOT, D], BF16)
            L.t("Yg", [NSLOT, D], F32)
            dB = dict(L.dr)
            dB.update(a_tensors("b_", io, tags))
            segs = [("l", TL), ("c", TC)] if l == 0 else [("l", TL)]
            for tag, T in segs:
                L.t("ysT_" + tag, [8, 128, T], BF16)
                L.t("xmid_" + tag, [T, D])
                dB["ysT_" + tag] = L.dr["ysT_" + tag]
                dB["xmid_" + tag] = L.dr["xmid_" + tag]
            if l == 0:
                L.t("x_l", [TL, D], F32, io)
                L.t("x_c", [TC, D], F32, io)
                L.t("x1_l", [TL, D], F32, "ExternalOutput")
                L.t("x1_c", [TC, D])
                xin_l, xin_c, xo_l, xo_c = L.dr["x_l"], L.dr["x_c"], L.dr["x1_l"], L.dr["x1_c"]
            else:
                L.t("x1_l", [TL, D], F32, io)
                L.t("out", [TL, D], F32, "ExternalOutput")
                xin_l, xo_l = L.dr["x1_l"], L.dr["out"]
            phase_conv(cx, l, dB, segs)
            phase_attn(cx, l, dB, [("l", TL, NKALL)] + ([("c", TC, TC // 128)] if l == 0 else []), lam_init_of(l))
            phase_ret(cx, l, dB, segs)
            if l == 0:
                phase_merge(cx, l, dB, [("l", TL, xin_l, dB["xmid_l"]), ("c", TC, xin_c, dB["xmid_c"])], wl=0)
                moe_phase(cx, l, dB, [("l", TL, dB["xmid_l"], xo_l), ("c", TC, dB["xmid_c"], xo_c)], False, wl=0)
                dA = dict(L.dr)
                dA.update(a_tensors("", "ExternalOutput", ("l", "c")))
                phase_a(cx, 1, dA, [("l", TL, xo_l), ("c", TC, xo_c)], wl=0)
            else:
                phase_merge(cx, l, dB, [("l", TL, xin_l, dB["xmid_l"])], wl=0)
                moe_phase(cx, l, dB, [("l", TL, dB["xmid_l"], xo_l)], True, wl=0)
    return L


def host_gather(oA):
    gk = np.concatenate([np.asarray(o["kT_l"]).reshape(256, TL) for o in oA], axis=0)
    gv = np.concatenate([np.asarray(o["V_l"]) for o in oA], axis=0)
    gt = np.concatenate([np.asarray(o["Tst_l"]).reshape(256, 256) for o in oA], axis=0)
    hs = []
    for o in oA:
        u = np.asarray(o["uT_l"])
        t = np.asarray(o["tT_l"])
        h = np.concatenate([np.concatenate([a[c2][:, 0:16], a[c2][:, TL - 16:TL]], axis=1) for a in (u, t) for c2 in range(2)], axis=0)
        hs.append(h)
    hg = np.concatenate(hs, axis=0).astype(np.float32)
    return dict(gk=gk, gv=gv, gt=gt, hg=hg)


def kernel_unfused(**inp):
    f32 = lambda a: np.ascontiguousarray(np.asarray(a, dtype=np.float32))
    x = f32(inp["x"])[0]
    ctx = f32(inp["ctx"])[0]
    ropes = [rope_tables(c) for c in range(NCORE)]
    full = {n_: f32(inp[n_]).reshape(s_) for n_, s_ in EXT_IN}
    cst = make_cst()
    ident = np.eye(128, dtype=np.float32).astype(NPBF)

    def run(L, extra, wl):
        maps = []
        for c in range(NCORE):
            m = dict(cst=cst, ident=ident, cosT=ropes[c][0], sinT=ropes[c][1], expo=make_expo(c), sel=make_sel(c),
                     x_l=x[c * TL:(c + 1) * TL], x_c=ctx)
            for k, v in full.items():
                m[k] = v[wl[k]:wl[k] + 1] if k in WSLICE else v
            m.update(extra[c])
            maps.append({k: m[k] for k in L.ins})
        res = run_bass_kernel_spmd(L.nc, maps, core_ids=list(range(NCORE)))
        return [{k: np.asarray(r[k]) for k in L.outs} for r in res.results]

    o1 = run(build_stage(1), [dict() for _ in range(NCORE)], dict.fromkeys(WSLICE, 0))
    modv = o1[0]["modv"]

    def b_extra(oA):
        g = host_gather(oA)
        ex = []
        for c in range(NCORE):
            e = dict(g)
            e["modv"] = modv
            for k, v in oA[c].items():
                if k != "modv" and k != "x1_l":
                    e["b_" + k] = v
            ex.append(e)
        return ex
    wl2 = dict.fromkeys(WSLICE, 0)
    wl2["w_in"] = 1
    o2 = run(build_stage(2), b_extra(o1), wl2)
    ex3 = b_extra(o2)
    for c in range(NCORE):
        ex3[c]["x1_l"] = o2[c]["x1_l"]
    o3 = run(build_stage(3), ex3, dict.fromkeys(WSLICE, 1))
    out = np.concatenate([o3[c]["out"] for c in range(NCORE)], axis=0)
    return out.reshape(1, SEQ, D).astype(np.float32)


FUSED = False


def kernel(**inp):
    return kernel_fused(**inp) if FUSED else kernel_unfused(**inp)
```

```python
import math
from contextlib import ExitStack
import numpy as np
import ml_dtypes
import concourse.bass as bass
import concourse.mybir as mybir
from concourse.bass_utils import run_bass_kernel_spmd

F32 = mybir.dt.float32
BF16 = mybir.dt.bfloat16
AF = mybir.ActivationFunctionType
ALU = mybir.AluOpType
AX = mybir.AxisListType
NPBF = ml_dtypes.bfloat16

NCORE = 8
D = 1024
SEQ = 16384
TL = SEQ // NCORE
TC = 256
DEPTH = 2
INC = 2816
EPS = 1e-6
NEXP = 16
DFF = 512
QSCALE = 32 ** -0.5
ATT_ROW = False
MOE_CAP = 768
NSLOT = NEXP * MOE_CAP


class Op:
    __slots__ = ("id", "eng", "fn", "deps", "dma", "n", "signal", "val", "cc")


class Sched:
    ENGS = (("sp", "sync"), ("act", "scalar"), ("dve", "vector"), ("pool", "gpsimd"), ("pe", "tensor"))

    def __init__(self, nc, stack):
        self.nc = nc
        self.ops = []
        self.phase_start = 0
        self.lastw = {}
        self.readers = {}
        self.K = dict(sp=12, pool=8, act=4)
        self.dma_list = {e: [] for e in self.K}
        self.sems = {e: stack.enter_context(nc.semaphore("sm_" + e)) for e in ("pe", "act", "dve", "pool")}
        self.dsems = {e: [stack.enter_context(nc.semaphore("sd_%s%d" % (e, i))) for i in range(k)]
                      for e, k in self.K.items()}
        self.ccsems = [stack.enter_context(nc.semaphore("sc_%d" % i)) for i in range(12)]
        self.ncc = 0
        self.cc_list = []
        self.cnt = {e: 0 for e in self.sems}
        self.waited = {e: {} for e, _ in self.ENGS}

    def add(self, eng, fn, r=(), w=(), dma=False, cc=False):
        i = len(self.ops)
        deps = {}
        for k in r:
            j = self.lastw.get(k)
            if j is not None:
                deps[j] = True
        for k in w:
            j = self.lastw.get(k)
            if j is not None and j not in deps:
                deps[j] = False
            rd = self.readers.get(k)
            if rd:
                for j in rd.values():
                    if isinstance(j, list):
                        for jj in j:
                            deps.setdefault(jj, False)
                    else:
                        deps.setdefault(j, False)
        n = None
        if dma:
            lst = self.dma_list[eng]
            n = len(lst)
            if n >= self.K[eng]:
                deps.setdefault(lst[n - self.K[eng]], False)
            lst.append(i)
        op = Op()
        op.id, op.eng, op.fn, op.deps, op.dma, op.n, op.signal, op.val = i, eng, fn, deps, dma, n, False, 0
        op.cc = None
        if cc:
            op.dma = True
            op.cc = self.ncc
            self.ncc += 1
            self.cc_list.append(i)
            dma = True
        self.ops.append(op)
        for k in r:
            rd = self.readers.setdefault(k, {})
            if dma:
                rd.setdefault("dma", []).append(i)
            else:
                rd[eng] = i
        for k in w:
            self.lastw[k] = i
            self.readers[k] = {}
        return i

    def _needed(self, op, dj, raw):
        if dj.dma:
            return True
        if dj.eng == op.eng:
            if op.dma:
                return True
            return raw and op.eng != "pe"
        return True

    def end_phase(self, name=None):
        nc = self.nc
        ps = self.phase_start
        for e in self.K:
            lst = [i for i in self.dma_list[e][-self.K[e]:] if i >= ps]
            if e == "pool":
                lst = lst + [i for i in self.cc_list if i >= ps]
            if lst:
                i = self.add(e, lambda h: h.nop())
                for j in lst:
                    self.ops[i].deps[j] = True
        ops = self.ops
        for op in ops[ps:]:
            latest = {}
            for j, raw in op.deps.items():
                if j < ps:
                    continue
                dj = ops[j]
                if not dj.dma and self._needed(op, dj, raw):
                    if latest.get(dj.eng, -1) < j:
                        latest[dj.eng] = j
            for j in latest.values():
                ops[j].signal = True
        for op in ops[ps:]:
            if op.signal and not op.dma:
                self.cnt[op.eng] += 1
                op.val = self.cnt[op.eng]
        with nc.Block() as block:
            for e, bn in self.ENGS:
                ops_e = [op for op in ops[ps:] if op.eng == e]
                if not ops_e:
                    continue

                def body(h, ops_e=ops_e, e=e):
                    self._emit(e, h, ops_e, ps)
                getattr(block, bn)(body)
        self.phase_start = len(ops)
        self.lastw = {k: v for k, v in self.lastw.items() if isinstance(k, str) and k.startswith("D:")}
        self.readers = {k: {} for k in self.lastw}
        for op in ops[:self.phase_start]:
            op.fn = None

    def _emit(self, e, h, ops_e, ps):
        ops = self.ops
        waited = self.waited[e]
        for op in ops_e:
            want = {}
            for j, raw in op.deps.items():
                if j < ps:
                    continue
                dj = ops[j]
                if not self._needed(op, dj, raw):
                    continue
                if dj.cc is not None:
                    key = ("cc", dj.cc)
                    sem = self.ccsems[dj.cc]
                    val = 1
                elif dj.dma:
                    K = self.K[dj.eng]
                    key = (dj.eng, dj.n % K)
                    sem = self.dsems[dj.eng][dj.n % K]
                    val = 16 * (dj.n // K + 1)
                else:
                    key = dj.eng
                    sem = self.sems[dj.eng]
                    if key in want and want[key][2] > j:
                        continue
                    want[key] = (sem, dj.val, j)
                    continue
                if key not in want or want[key][1] < val:
                    want[key] = (sem, val, j)
            for key, (sem, val, _j) in want.items():
                if waited.get(key, 0) >= val:
                    continue
                h.wait_ge(sem, val)
                waited[key] = val
            inst = op.fn(h)
            if op.cc is not None:
                inst.then_inc(self.ccsems[op.cc])
            elif op.dma:
                inst.then_inc(self.dsems[e][op.n % self.K[e]], 16)
            elif op.signal:
                inst.then_inc(self.sems[e], 1)


class Ctx:
    def __init__(self, nc, S):
        self.nc = nc
        self.S = S
        self.stack = None
        self.uid = 0

    def psum(self, name, shape, dt=F32):
        self.uid += 1
        return self.stack.enter_context(self.nc.psum_tensor("%s_%d" % (name, self.uid), list(shape), dt))

    def begin(self, nf=8, nb=0):
        self.stack = ExitStack()
        self.ps = [self.stack.enter_context(self.nc.psum_tensor("ps%d_%d" % (i, self.uid), [128, 512], F32))
                   for i in range(nf)]
        self.psb = [self.stack.enter_context(self.nc.psum_tensor("psb%d_%d" % (i, self.uid), [128, 1024], BF16))
                    for i in range(nb)]
        self.uid += 1

    def end(self):
        self.S.end_phase()
        self.stack.close()
        self.stack = None

    def sb(self, name, shape, dt=F32):
        self.uid += 1
        return self.stack.enter_context(self.nc.sbuf_tensor("%s_%d" % (name, self.uid), list(shape), dt))

    def dma(self, eng, out, in_, r=(), w=(), **kw):
        return self.S.add(eng, lambda h: h.dma_start(out=out, in_=in_, **kw), r=r, w=w, dma=True)

    def mm(self, out, lhsT, rhs, start, stop, r=(), w=(), **kw):
        return self.S.add("pe", lambda h: h.matmul(out, lhsT, rhs, start=start, stop=stop, **kw), r=r, w=w)

    def tr(self, out, in_, ident, r=(), w=()):
        return self.S.add("pe", lambda h: h.transpose(out, in_, ident), r=r, w=w)

    def act(self, out, in_, func, r=(), w=(), eng="act", **kw):
        return self.S.add(eng, lambda h: h.activation(out=out, in_=in_, func=func, **kw), r=r, w=w)

    def tt(self, out, in0, in1, op, r=(), w=(), eng="dve"):
        return self.S.add(eng, lambda h: h.tensor_tensor(out=out, in0=in0, in1=in1, op=op), r=r, w=w)

    def ts(self, out, in0, s1, op0, s2=None, op1=None, r=(), w=(), eng="dve", **kw):
        if op1 is None:
            return self.S.add(eng, lambda h: h.tensor_scalar(out=out, in0=in0, scalar1=s1, scalar2=None, op0=op0, **kw),
                              r=r, w=w)
        return self.S.add(eng, lambda h: h.tensor_scalar(out=out, in0=in0, scalar1=s1, scalar2=s2, op0=op0, op1=op1, **kw),
                          r=r, w=w)

    def stt(self, out, in0, scalar, in1, op0, op1, r=(), w=()):
        return self.S.add("dve", lambda h: h.scalar_tensor_tensor(out=out, in0=in0, scalar=scalar, in1=in1,
                                                                    op0=op0, op1=op1), r=r, w=w)

    def copy(self, out, in_, r=(), w=(), eng="dve"):
        if eng == "act":
            return self.S.add("act", lambda h: h.activation(out=out, in_=in_, func=AF.Copy), r=r, w=w)
        return self.S.add(eng, lambda h: h.tensor_copy(out=out, in_=in_), r=r, w=w)

    def memset(self, ap, val, w=(), eng="dve"):
        return self.S.add(eng, lambda h: h.memset(ap, val), w=w)

    def red(self, out, in_, op, r=(), w=(), axis=None):
        ax = AX.X if axis is None else axis
        return self.S.add("dve", lambda h: h.tensor_reduce(out=out, in_=in_, axis=ax, op=op), r=r, w=w)

    def recip(self, out, in_, r=(), w=()):
        return self.S.add("dve", lambda h: h.reciprocal(out=out, in_=in_), r=r, w=w)


def phase_mods(cx, l, dr):
    cx.begin()
    cv = cx.sb("cv", [128, 8, 2])
    sc = cx.sb("sc", [128, 8, 2])
    acc = cx.sb("macc", [2, 6144])
    bm = cx.sb("mbm", [2, 6144])
    g1b = cx.sb("g1b", [2, 1024])
    g2b = cx.sb("g2b", [2, 1024])
    mv = cx.sb("mv", [2, 6, 1024])
    wm = [cx.sb("wm%d" % i, [128, 6144]) for i in range(2)]
    for s, src in enumerate((dr["c"], dr["c_ctx"])):
        for kc in range(8):
            cx.dma("sp", cv[:, kc, s:s + 1], src[0:1, kc * 128:(kc + 1) * 128].rearrange("o p -> p o"), w=["cv"])
    cx.dma("sp", bm[:, :], dr["b_mod"][l, :].partition_broadcast(2), w=["bm"])
    cx.dma("sp", g1b[:, :], dr["g_norm1"][l, :].partition_broadcast(2), w=["g1b"])
    cx.dma("sp", g2b[:, :], dr["g_norm2"][l, :].partition_broadcast(2), w=["g2b"])
    cx.act(sc[:, :, :], cv[:, :, :], AF.Silu, r=["cv"], w=["sc"])
    for kc in range(8):
        b = kc % 2
        cx.dma("sp", wm[b][:, :], dr["w_mod"][l, kc * 128:(kc + 1) * 128, :], w=["wm%d" % b])
        for n in range(12):
            p = cx.ps[n % 4]
            cx.mm(p[0:2, :], sc[:, kc, :], wm[b][:, n * 512:(n + 1) * 512], True, True,
                  r=["sc", "wm%d" % b], w=["ps%d" % (n % 4)])
            a = acc[:, n * 512:(n + 1) * 512]
            if kc == 0:
                cx.tt(a, p[0:2, :], bm[:, n * 512:(n + 1) * 512], ALU.add, r=["ps%d" % (n % 4), "bm"], w=["macc%d" % n])
            else:
                cx.tt(a, p[0:2, :], a, ALU.add, r=["ps%d" % (n % 4), "macc%d" % n], w=["macc%d" % n])
    allacc = ["macc%d" % n for n in range(12)]
    cx.stt(mv[:, 0, :], acc[:, 1024:2048], 1.0, g1b[:, :], ALU.add, ALU.mult, r=allacc + ["g1b"], w=["mv0"])
    cx.copy(mv[:, 1, :], acc[:, 0:1024], r=allacc, w=["mv1"])
    cx.copy(mv[:, 2, :], acc[:, 2048:3072], r=allacc, w=["mv2"])
    cx.stt(mv[:, 3, :], acc[:, 4096:5120], 1.0, g2b[:, :], ALU.add, ALU.mult, r=allacc + ["g2b"], w=["mv3"])
    cx.copy(mv[:, 4, :], acc[:, 3072:4096], r=allacc, w=["mv4"])
    cx.copy(mv[:, 5, :], acc[:, 5120:6144], r=allacc, w=["mv5"])
    cx.dma("sp", dr["modv"][l, :, :, :], mv[:, :, :], r=["mv%d" % i for i in range(6)], w=["D:modv%d" % l])
    cx.end()


CST = {}
_off = 0
for _n, _w in (("c127mj", 1), ("cj", 1), ("ef_l", 16), ("eb_l", 16), ("ef_c", 2), ("eb_c", 2), ("ip1", 128),
               ("m128i", 128), ("D1", 128), ("D2", 128), ("U", 128), ("Lo", 128), ("I2", 128), ("ones", 128), ("I1", 128), ("hm", 4), ("bm8", 8), ("Ltri", 128), ("eoff", 16)):
    CST[_n] = (_off, _off + _w)
    _off += _w
CSTW = _off


def make_cst():
    c = np.zeros((128, CSTW), np.float32)
    p = np.arange(128, dtype=np.float32)
    i = np.arange(128, dtype=np.float32)

    def put(n, v):
        a, b = CST[n]
        c[:, a:b] = v
    put("c127mj", (127 - p)[:, None])
    put("cj", p[:, None])
    put("ef_l", (128.0 * (15 - np.arange(16)))[None, :])
    put("eb_l", (128.0 * np.arange(16))[None, :])
    put("ef_c", (128.0 * (1 - np.arange(2)))[None, :])
    put("eb_c", (128.0 * np.arange(2))[None, :])
    put("ip1", (i + 1)[None, :])
    put("m128i", (128 - i)[None, :])
    dd = i[None, :] - p[:, None]
    put("D1", np.maximum(dd, 0))
    put("D2", np.maximum(-dd, 0))
    put("U", (dd > 0).astype(np.float32))
    put("Lo", (dd < 0).astype(np.float32))
    put("I2", 2.0 * (dd == 0))
    put("ones", 1.0)
    put("I1", (dd == 0).astype(np.float32))
    put("hm", (p[:, None] // 32 == np.arange(4)[None, :]).astype(np.float32))
    put("bm8", np.tile((p[:, None] // 32 == np.arange(4)[None, :]).astype(np.float32), (1, 2)))
    put("Ltri", (dd > 0).astype(np.float32))
    put("eoff", (float(MOE_CAP) * np.arange(16))[None, :])
    return c


def rope_tables(core):
    t = np.arange(core * TL, (core + 1) * TL)
    row = (t // 64).astype(np.float32)
    col = (t % 64).astype(np.float32)
    inv = (np.float32(10000.0) ** (-np.arange(8, dtype=np.float32) / np.float32(8))).astype(np.float32)
    ang = np.concatenate([row[:, None] * inv[None, :], col[:, None] * inv[None, :]], axis=1).astype(np.float32)
    cos = np.cos(ang).astype(np.float32).T
    sin = np.sin(ang).astype(np.float32).T
    return np.tile(cos, (8, 1)).copy(), np.tile(sin, (8, 1)).copy()


def cs(cst, name):
    a, b = CST[name]
    return cst[:, a:b]


def phase_a(cx, l, dr, segs, wl=None):
    wl = l if wl is None else wl
    cx.begin(nf=6, nb=2)
    ps, psb = cx.ps, cx.psb
    W = cx.sb("W", [128, 8, INC], BF16)
    WR = cx.sb("WR", [128, 8, 768], BF16)
    cst = cx.sb("cst", [128, CSTW])
    ident = cx.sb("ident", [128, 128], BF16)
    cx.dma("sp", cst[:, :], dr["cst"][:, :], w=["cst"])
    cx.dma("sp", ident[:, :], dr["ident"][:, :], w=["ident"])
    for kc in range(8):
        cx.dma("pool", W[:, kc, :], dr["w_in"][wl, kc * 128:(kc + 1) * 128, :], w=["W%d" % kc])
    for kc in range(8):
        cx.ts(W[:, kc, 2176:2304], W[:, kc, 2176:2304], QSCALE, ALU.mult, r=["W%d" % kc], w=["W%d" % kc], eng="pool")
        for (s0, n, o0) in ((1280, 512, 0), (2048, 256, 512)):
            src = W[:, kc, s0:s0 + n].rearrange("p (b t d) -> p b t d", t=2, d=16)
            dst = WR[:, kc, o0:o0 + n].rearrange("p (b t d) -> p b t d", t=2, d=16)
            cx.ts(dst[:, :, 0, :], src[:, :, 1, :], -1.0, ALU.mult, r=["W%d" % kc], w=["WR%d" % kc], eng="pool")
            cx.copy(dst[:, :, 1, :], src[:, :, 0, :], r=["W%d" % kc], w=["WR%d" % kc], eng="pool")
    Wk = ["W%d" % kc for kc in range(8)]
    WRk = ["WR%d" % kc for kc in range(8)]
    lgf = cx.sb("lgf", [128, 4]); lgb = cx.sb("lgb", [128, 4])
    lgfc = cx.sb("lgfc", [128, 1]); lgbc = cx.sb("lgbc", [128, 1])
    cx.dma("sp", lgf[:, :], dr["ret_ld_f"][l, :].partition_broadcast(128), w=["lgf"])
    cx.dma("sp", lgb[:, :], dr["ret_ld_b"][l, :].partition_broadcast(128), w=["lgb"])
    for h in range(4):
        cx.dma("sp", lgfc[32 * h:32 * h + 32, :], dr["ret_ld_f"][l, h:h + 1].partition_broadcast(32), w=["lgfc"])
        cx.dma("sp", lgbc[32 * h:32 * h + 32, :], dr["ret_ld_b"][l, h:h + 1].partition_broadcast(32), w=["lgbc"])
    kdf = cx.sb("kdf", [128, 4]); kdb = cx.sb("kdb", [128, 4])
    KDF = cx.sb("KDF", [128, 128]); KDB = cx.sb("KDB", [128, 128])
    cx.act(kdf[:, :], lgf[:, :], AF.Exp, scale=cs(cst, "c127mj"), r=["lgf", "cst"], w=["kdf"])
    cx.act(kdb[:, :], lgb[:, :], AF.Exp, scale=cs(cst, "cj"), r=["lgb", "cst"], w=["kdb"])
    for h in range(4):
        cx.ts(KDF[:, 32 * h:32 * h + 32], cs(cst, "ones")[:, 0:32], kdf[:, h:h + 1], ALU.mult, r=["kdf", "cst"], w=["KDF"])
        cx.ts(KDB[:, 32 * h:32 * h + 32], cs(cst, "ones")[:, 0:32], kdb[:, h:h + 1], ALU.mult, r=["kdb", "cst"], w=["KDB"])
    pw = {}
    for tag, nch in (("l", 16), ("c", 2)):
        pf = cx.sb("pwf" + tag, [128, nch]); pb = cx.sb("pwb" + tag, [128, nch])
        cx.act(pf[:, :], cs(cst, "ef_" + tag), AF.Exp, scale=lgfc[:, 0:1], r=["lgfc", "cst"], w=["pwf" + tag])
        cx.act(pb[:, :], cs(cst, "eb_" + tag), AF.Exp, scale=lgbc[:, 0:1], r=["lgbc", "cst"], w=["pwb" + tag])
        pw[tag] = (pf, pb)
    Cl = cx.sb("Cl", [128, TL]); Sl = cx.sb("Sl", [128, TL])
    cx.dma("sp", Cl[:, :], dr["cosT"][:, :], w=["Cl"])
    cx.dma("sp", Sl[:, :], dr["sinT"][:, :], w=["Sl"])
    xt = [cx.sb("xt%d" % i, [128, D]) for i in range(2)]
    junk = cx.sb("junk", [128, D], BF16)
    t1 = [cx.sb("t1_%d" % i, [128, D]) for i in range(2)]
    hb = [cx.sb("hb%d" % i, [128, D], BF16) for i in range(2)]
    ssq = cx.sb("ssq", [128, 4]); rstd = cx.sb("rstd", [128, 4])
    hTs = [cx.sb("hT%d" % i, [128, 8, 512], BF16) for i in range(2)]
    gcount = [0]
    gsb = cx.sb("gsb", [128, D]); shb = cx.sb("shb", [128, D])
    ev = [cx.sb("ev%d" % i, [128, 512]) for i in range(4)]
    evb = [cx.sb("evb%d" % i, [128, 512], BF16) for i in range(4)]
    rk_sb = cx.sb("rk_sb", [128, 512], BF16)
    vt = [cx.sb("vt%d" % i, [128, 512], BF16) for i in range(2)]
    rgt = [cx.sb("rgt%d" % i, [128, 256], BF16) for i in range(2)]
    kfb = [cx.sb("kfb%d" % i, [128, 256], BF16) for i in range(2)]
    Tst = cx.sb("Tst", [128, 2, 256])
    evi = [0]

    def nxt():
        evi[0] = (evi[0] + 1) % 4
        return evi[0]

    xi = 0
    for (tag, T, xd) in segs:
        sfx = "_" + tag
        seg = 0 if tag == "l" else 1
        cx.dma("sp", gsb[:, :], dr["modv"][l, seg, 0, :].partition_broadcast(128), r=["D:modv%d" % l], w=["gsb"])
        cx.dma("sp", shb[:, :], dr["modv"][l, seg, 1, :].partition_broadcast(128), r=["D:modv%d" % l], w=["shb"])
        G = min(512, T)
        for g in range(T // G):
            t0 = g * G
            nt = G // 128
            hT = hTs[gcount[0] % 2]
            kh = "hT%d" % (gcount[0] % 2)
            gcount[0] += 1
            for j in range(nt):
                b = xi % 2
                xi += 1
                kx = "xt%d" % b
                cx.dma("sp", xt[b][:, :], xd[t0 + j * 128:t0 + (j + 1) * 128, :], w=[kx])
                cx.act(junk[:, :], xt[b][:, :], AF.Square, accum_out=ssq[:, j:j + 1], r=[kx], w=["junk", "ssq%d" % j])
                cx.act(rstd[:, j:j + 1], ssq[:, j:j + 1], AF.Sqrt, scale=1.0 / D, bias=EPS, r=["ssq%d" % j], w=["rs%d" % j])
                cx.recip(rstd[:, j:j + 1], rstd[:, j:j + 1], r=["rs%d" % j], w=["rs%d" % j])
                cx.stt(t1[b][:, :], xt[b][:, :], rstd[:, j:j + 1], gsb[:, :], ALU.mult, ALU.mult,
                       r=[kx, "rs%d" % j, "gsb"], w=["t1_%d" % b])
                cx.tt(hb[b][:, :], t1[b][:, :], shb[:, :], ALU.add, r=["t1_%d" % b, "shb"], w=["hb%d" % b], eng="pool")
                for kc in range(8):
                    cx.tr(psb[0][:, kc * 128:(kc + 1) * 128], hb[b][:, kc * 128:(kc + 1) * 128], ident[:, :],
                          r=["hb%d" % b, "ident"], w=["psb0"])
                cx.copy(hT[:, :, j * 128:(j + 1) * 128], psb[0][:, :].rearrange("p (k t) -> p k t", k=8),
                        r=["psb0"], w=[kh], eng="act")
            cx.dma("sp", dr["hT" + sfx][:, :, t0:t0 + G], hT[:, :, 0:G], r=[kh], w=["D:hT" + sfx])

            def proj(cc, bank, rot=False):
                Wt = WR if rot else W
                for kc in range(8):
                    cx.mm(ps[bank][:, 0:G], Wt[:, kc, cc * 128:(cc + 1) * 128], hT[:, kc, 0:G], kc == 0, kc == 7,
                          r=[kh, (WRk if rot else Wk)[kc]], w=["ps%d" % bank])

            Cg = Cl[:, t0:t0 + G] if tag == "l" else None
            Sg = Sl[:, t0:t0 + G] if tag == "l" else None
            for c2 in range(2):
                e = nxt()
                proj(2 + c2, 0)
                cx.act(ev[e][:, 0:G], ps[0][:, 0:G], AF.Sigmoid, r=["ps0"], w=["ev%d" % e])
                proj(0 + c2, 1)
                cx.tt(ev[e][:, 0:G], ps[1][:, 0:G], ev[e][:, 0:G], ALU.mult, r=["ps1", "ev%d" % e], w=["ev%d" % e])
                cx.dma("sp", dr["uT" + sfx][c2, :, t0:t0 + G], ev[e][:, 0:G], r=["ev%d" % e], w=["D:uT" + sfx])
            for c2 in range(2):
                e = nxt()
                proj(4 + c2, 0)
                cx.copy(evb[e][:, 0:G], ps[0][:, 0:G], r=["ps0"], w=["evb%d" % e], eng="act")
                cx.dma("sp", dr["bgT" + sfx][c2, :, t0:t0 + G], evb[e][:, 0:G], r=["evb%d" % e], w=["D:bgT" + sfx])
                proj(6 + c2, 1)
                cx.copy(ev[e][:, 0:G], ps[1][:, 0:G], r=["ps1"], w=["ev%d" % e], eng="act")
                proj(8 + c2, 0)
                cx.tt(ev[e][:, 0:G], ps[0][:, 0:G], ev[e][:, 0:G], ALU.mult, r=["ps0", "ev%d" % e], w=["ev%d" % e])
                cx.dma("sp", dr["tT" + sfx][c2, :, t0:t0 + G], ev[e][:, 0:G], r=["ev%d" % e], w=["D:tT" + sfx])
            for (cc, ro, dst, keep) in ((10, 0, dr["qT" + sfx][0], None), (11, 1, dr["qT" + sfx][1], None),
                                        (12, 2, dr["kT" + sfx][0], None), (13, 3, dr["kT" + sfx][1], None),
                                        (16, 4, dr["rqT" + sfx], None), (17, 5, dr["rkT" + sfx], rk_sb)):
                e = nxt()
                ob = keep if keep is not None else evb[e]
                okey = "rk_sb" if keep is not None else "evb%d" % e
                proj(cc, 0)
                if tag == "l":
                    proj(ro, 2, rot=True)
                    e2 = nxt()
                    cx.tt(ev[e][:, 0:G], ps[0][:, 0:G], Cg, ALU.mult, r=["ps0", "Cl"], w=["ev%d" % e])
                    cx.tt(ev[e2][:, 0:G], ps[2][:, 0:G], Sg, ALU.mult, r=["ps2", "Sl"], w=["ev%d" % e2])
                    cx.tt(ob[:, 0:G], ev[e][:, 0:G], ev[e2][:, 0:G], ALU.add, r=["ev%d" % e, "ev%d" % e2], w=[okey], eng="pool")
                else:
                    cx.copy(ob[:, 0:G], ps[0][:, 0:G], r=["ps0"], w=[okey], eng="act")
                cx.dma("sp", dst[:, t0:t0 + G], ob[:, 0:G], r=[okey], w=["D:rope" + sfx + str(cc)])
            pf, pb = pw[tag]
            for j in range(nt):
                n = (t0 // 128) + j
                b = j % 2
                tsl = slice(j * 128, (j + 1) * 128)
                for kc in range(8):
                    cx.mm(ps[3][:, 0:256], hT[:, kc, tsl], W[:, kc, 1792:2048], kc == 0, kc == 7, r=[kh, Wk[kc]], w=["ps3"])
                for kc in range(8):
                    cx.mm(ps[3][:, 256:512], hT[:, kc, tsl], W[:, kc, 2304:2560], kc == 0, kc == 7, r=[kh, Wk[kc]], w=["ps3"])
                for kc in range(8):
                    cx.mm(ps[4][:, 0:256], hT[:, kc, tsl], W[:, kc, 2560:2816], kc == 0, kc == 7, r=[kh, Wk[kc]], w=["ps4"])
                cx.copy(vt[b][:, :], ps[3][:, :], r=["ps3", "ps3"], w=["vt%d" % b])
                cx.act(rgt[b][:, :], ps[4][:, 0:256], AF.Silu, r=["ps4"], w=["rgt%d" % b])
                rows = slice(t0 + j * 128, t0 + (j + 1) * 128)
                cx.dma("sp", dr["V" + sfx][rows, :], vt[b][:, 0:256], r=["vt%d" % b], w=["D:V" + sfx])
                cx.dma("sp", dr["rv" + sfx][rows, :], vt[b][:, 256:512], r=["vt%d" % b], w=["D:rv" + sfx])
                cx.dma("sp", dr["rg" + sfx][rows, :], rgt[b][:, :], r=["rgt%d" % b], w=["D:rg" + sfx])
                cx.tr(psb[1][:, 0:128], rk_sb[:, tsl], ident[:, :], r=["rk_sb", "ident"], w=["psb1"])
                cx.tt(kfb[b][:, 0:128], psb[1][:, 0:128], KDF[:, :], ALU.mult, r=["psb1", "KDF"], w=["kfb%d" % b])
                cx.tt(kfb[b][:, 128:256], psb[1][:, 0:128], KDB[:, :], ALU.mult, r=["psb1", "KDB"], w=["kfb%d" % b])
                cx.mm(ps[5][:, 0:256], kfb[b][:, 0:128], vt[b][:, 256:512], True, True, r=["kfb%d" % b, "vt%d" % b], w=["ps5"])
                cx.mm(ps[5][:, 256:512], kfb[b][:, 128:256], vt[b][:, 256:512], True, True, r=["kfb%d" % b, "vt%d" % b], w=["ps5"])
                if n == 0:
                    cx.ts(Tst[:, 0, :], ps[5][:, 0:256], pf[:, n:n + 1], ALU.mult, r=["ps5", "pwf" + tag], w=["Tf"])
                    cx.ts(Tst[:, 1, :], ps[5][:, 256:512], pb[:, n:n + 1], ALU.mult, r=["ps5", "pwb" + tag], w=["Tb"])
                else:
                    cx.stt(Tst[:, 0, :], ps[5][:, 0:256], pf[:, n:n + 1], Tst[:, 0, :], ALU.mult, ALU.add,
                           r=["ps5", "pwf" + tag, "Tf"], w=["Tf"])
                    cx.stt(Tst[:, 1, :], ps[5][:, 256:512], pb[:, n:n + 1], Tst[:, 1, :], ALU.mult, ALU.add,
                           r=["ps5", "pwb" + tag, "Tb"], w=["Tb"])
        cx.dma("sp", dr["Tst" + sfx].rearrange("a p e -> p a e"), Tst[:, :, :], r=["Tf", "Tb"], w=["D:Tst" + sfx])
    cx.end()


def phase_conv(cx, l, dr, segs):
    cx.begin(nf=8, nb=0)
    ps = cx.ps
    cst = cx.sb("cst", [128, CSTW])
    cx.dma("sp", cst[:, :], dr["cst"][:, :], w=["cst"])
    praw = cx.sb("praw", [40, 256])
    cx.memset(praw[:, :], 0.0, w=["praw"])
    cx.dma("sp", praw[0:31, :], dr["conv_a_w"][l, :, :], w=["praw"])
    cx.dma("sp", praw[31:32, :], dr["conv_a_b"][l:l + 1, :], w=["praw"])
    cx.dma("sp", praw[32:33, :], dr["conv_a_g"][l:l + 1, :], w=["praw"])
    cx.dma("sp", praw[33:34, :], dr["conv_a_beta"][l:l + 1, :], w=["praw"])
    cx.dma("sp", praw[34:37, :], dr["conv_b_w"][l, :, :], w=["praw"])
    par = cx.sb("par", [128, 2, 40])
    for c2 in range(2):
        cx.tr(ps[7][:, 0:40], praw[0:40, c2 * 128:(c2 + 1) * 128], cs(cst, "I1")[0:40, 0:40], r=["praw", "cst"], w=["ps7"])
        cx.copy(par[:, c2, :], ps[7][:, 0:40], r=["ps7"], w=["par"])
    onesm = cx.sb("onesm", [128, 128])
    cx.memset(onesm[:, :], 1.0 / 256.0, w=["onesm"])
    HG = cx.sb("HG", [128, NCORE, 4, 32]); selt = cx.sb("selt", [128, 2, NCORE])
    cx.dma("sp", HG[:, :, :, :], dr["hg"].rearrange("(r a p) w -> p r a w", r=NCORE, a=4), w=["HG"])
    cx.dma("sp", selt[:, :, :], dr["sel"][:, :, :], w=["selt"])
    for (tag, T) in segs:
        sfx = "_" + tag
        ue = cx.sb("ue" + tag, [128, 2, T + 32])
        te = cx.sb("te" + tag, [128, 2, T + 32])
        bg = cx.sb("bg" + tag, [128, 2, T], BF16)
        acc = cx.sb("acc" + tag, [128, 2, T])
        sq = cx.sb("sq" + tag, [128, 2, T])
        yb = cx.sb("yb" + tag, [128, 2, T], BF16)
        for c2 in range(2):
            cx.dma("sp", ue[:, c2, 16:16 + T], dr["uT" + sfx][c2, :, :], r=["D:uT" + sfx], w=["ue%d" % c2])
            cx.dma("sp", te[:, c2, 16:16 + T], dr["tT" + sfx][c2, :, :], r=["D:tT" + sfx], w=["te%d" % c2])
            cx.dma("sp", bg[:, c2, :], dr["bgT" + sfx][c2, :, :], r=["D:bgT" + sfx], w=["bg%d" % c2])
            if tag == "l":
                for a, (buf, k) in enumerate(((ue, "ue%d" % c2), (te, "te%d" % c2))):
                    ai = a * 2 + c2
                    for side, dst, src in ((0, slice(0, 16), slice(16, 32)), (1, slice(16 + T, 32 + T), slice(0, 16))):
                        for r_ in range(NCORE):
                            if r_ == 0:
                                cx.ts(buf[:, c2, dst], HG[:, r_, ai, src], selt[:, side, r_:r_ + 1], ALU.mult,
                                      r=["HG", "selt"], w=[k])
                            else:
                                cx.stt(buf[:, c2, dst], HG[:, r_, ai, src], selt[:, side, r_:r_ + 1], buf[:, c2, dst],
                                       ALU.mult, ALU.add, r=["HG", "selt", k], w=[k])
            else:
                for buf, k in ((ue, "ue%d" % c2), (te, "te%d" % c2)):
                    cx.memset(buf[:, c2, 0:16], 0.0, w=[k], eng="pool")
                    cx.memset(buf[:, c2, 16 + T:32 + T], 0.0, w=[k], eng="pool")
        for c2 in range(2):
            ka = "acc%d" % c2
            cx.ts(acc[:, c2, :], ue[:, c2, 1:1 + T], par[:, c2, 0:1], ALU.mult, s2=par[:, c2, 31:32], op1=ALU.add,
                  r=["ue%d" % c2, "par"], w=[ka])
            for k in range(1, 31):
                cx.stt(acc[:, c2, :], ue[:, c2, k + 1:k + 1 + T], par[:, c2, k:k + 1], acc[:, c2, :], ALU.mult, ALU.add,
                       r=["ue%d" % c2, "par", ka], w=[ka])
            cx.tt(sq[:, c2, :], acc[:, c2, :], acc[:, c2, :], ALU.mult, r=[ka], w=["sq%d" % c2], eng="pool")
        G = min(512, T)
        mm2 = cx.sb("m2" + tag, [128, G]); var = cx.sb("var" + tag, [128, G]); dd = cx.sb("dd" + tag, [128, G])
        for g in range(T // G):
            gs = slice(g * G, (g + 1) * G)
            for c2 in range(2):
                cx.mm(ps[0][:, 0:G], onesm[:, :], acc[:, c2, gs], c2 == 0, c2 == 1, r=["onesm", "acc%d" % c2], w=["ps0"])
            for c2 in range(2):
                cx.mm(ps[1][:, 0:G], onesm[:, :], sq[:, c2, gs], c2 == 0, c2 == 1, r=["onesm", "sq%d" % c2], w=["ps1"])
            cx.act(mm2[:, :], ps[0][:, 0:G], AF.Square, r=["ps0"], w=["mm2"])
            cx.tt(var[:, :], ps[1][:, 0:G], mm2[:, :], ALU.subtract, r=["ps1", "mm2"], w=["var"])
            cx.act(var[:, :], var[:, :], AF.Ln, bias=EPS, r=["var"], w=["var"])
            cx.act(var[:, :], var[:, :], AF.Exp, scale=-0.5, r=["var"], w=["var"])
            for c2 in range(2):
                cx.tt(dd[:, :], acc[:, c2, gs], ps[0][:, 0:G], ALU.subtract, r=["acc%d" % c2, "ps0"], w=["dd"])
                cx.tt(dd[:, :], dd[:, :], var[:, :], ALU.mult, r=["dd", "var"], w=["dd"])
                cx.act(yb[:, c2, gs], dd[:, :], AF.Silu, scale=par[:, c2, 32:33], bias=par[:, c2, 33:34],
                       r=["dd", "par"], w=["yb%d" % c2])
        for c2 in range(2):
            cx.dma("sp", dr["ysT" + sfx][0 + c2, :, :], yb[:, c2, :], r=["yb%d" % c2], w=["D:ys0" + sfx])
        for c2 in range(2):
            ka = "acc%d" % c2
            cx.ts(acc[:, c2, :], te[:, c2, 15:15 + T], par[:, c2, 34:35], ALU.mult, r=["te%d" % c2, "par"], w=[ka])
            for k in (1, 2):
                cx.stt(acc[:, c2, :], te[:, c2, 15 + k:15 + k + T], par[:, c2, 34 + k:35 + k], acc[:, c2, :], ALU.mult, ALU.add,
                       r=["te%d" % c2, "par", ka], w=[ka])
            cx.tt(yb[:, c2, :], acc[:, c2, :], bg[:, c2, :], ALU.mult, r=[ka, "bg%d" % c2], w=["yb%d" % c2])
            cx.dma("sp", dr["ysT" + sfx][2 + c2, :, :], yb[:, c2, :], r=["yb%d" % c2], w=["D:ys1" + sfx])
    cx.end()


def phase_ret(cx, l, dr, segs):
    cx.begin(nf=6, nb=2)
    ps, psb = cx.ps, cx.psb
    cst = cx.sb("cst", [128, CSTW])
    ident = cx.sb("ident", [128, 128], BF16)
    cx.dma("sp", cst[:, :], dr["cst"][:, :], w=["cst"])
    cx.dma("sp", ident[:, :], dr["ident"][:, :], w=["ident"])
    lgf = cx.sb("lgf", [128, 4]); lgb = cx.sb("lgb", [128, 4])
    lgfc = cx.sb("lgfc", [128, 1]); lgbc = cx.sb("lgbc", [128, 1])
    cx.dma("sp", lgf[:, :], dr["ret_ld_f"][l, :].partition_broadcast(128), w=["lgf"])
    cx.dma("sp", lgb[:, :], dr["ret_ld_b"][l, :].partition_broadcast(128), w=["lgb"])
    for h in range(4):
        cx.dma("sp", lgfc[32 * h:32 * h + 32, :], dr["ret_ld_f"][l, h:h + 1].partition_broadcast(32), w=["lgfc"])
        cx.dma("sp", lgbc[32 * h:32 * h + 32, :], dr["ret_ld_b"][l, h:h + 1].partition_broadcast(32), w=["lgbc"])
    kdf = cx.sb("kdf", [128, 4]); kdb = cx.sb("kdb", [128, 4])
    KDF = cx.sb("KDF", [128, 128]); KDB = cx.sb("KDB", [128, 128])
    cx.act(kdf[:, :], lgf[:, :], AF.Exp, scale=cs(cst, "c127mj"), r=["lgf", "cst"], w=["kdf"])
    cx.act(kdb[:, :], lgb[:, :], AF.Exp, scale=cs(cst, "cj"), r=["lgb", "cst"], w=["kdb"])
    for h in range(4):
        cx.ts(KDF[:, 32 * h:32 * h + 32], cs(cst, "ones")[:, 0:32], kdf[:, h:h + 1], ALU.mult, r=["kdf", "cst"], w=["KDF"])
        cx.ts(KDB[:, 32 * h:32 * h + 32], cs(cst, "ones")[:, 0:32], kdb[:, h:h + 1], ALU.mult, r=["kdb", "cst"], w=["KDB"])
    cdf = cx.sb("cdf", [128, 1]); cdb = cx.sb("cdb", [128, 1])
    cx.act(cdf[:, :], lgfc[:, :], AF.Exp, scale=128.0, r=["lgfc"], w=["cdf"])
    cx.act(cdb[:, :], lgbc[:, :], AF.Exp, scale=128.0, r=["lgbc"], w=["cdb"])
    qdf4 = cx.sb("qdf4", [128, 4, 128]); qdb4 = cx.sb("qdb4", [128, 4, 128])
    for c in range(4):
        cx.act(qdf4[:, c, :], cs(cst, "ip1"), AF.Exp, scale=lgfc[:, 0:1], r=["lgfc", "cst"], w=["qdf4"])
        cx.act(qdb4[:, c, :], cs(cst, "m128i"), AF.Exp, scale=lgbc[:, 0:1], r=["lgbc", "cst"], w=["qdb4"])
    maskT = cx.sb("maskT", [128, 4, 128]); mtmp = cx.sb("mtmp", [128, 128])
    for h in range(4):
        cx.act(mtmp[:, :], cs(cst, "D1"), AF.Exp, scale=lgf[:, h:h + 1], r=["lgf", "cst"], w=["mtmp"])
        cx.tt(maskT[:, h, :], mtmp[:, :], cs(cst, "U"), ALU.mult, r=["mtmp", "cst"], w=["maskT"])
        cx.tt(maskT[:, h, :], maskT[:, h, :], cs(cst, "I2"), ALU.add, r=["maskT", "cst"], w=["maskT"])
        cx.act(mtmp[:, :], cs(cst, "D2"), AF.Exp, scale=lgb[:, h:h + 1], r=["lgb", "cst"], w=["mtmp"])
        cx.tt(mtmp[:, :], mtmp[:, :], cs(cst, "Lo"), ALU.mult, r=["mtmp", "cst"], w=["mtmp"])
        cx.tt(maskT[:, h, :], maskT[:, h, :], mtmp[:, :], ALU.add, r=["maskT", "mtmp"], w=["maskT"])
    for (tag, T) in segs:
        sfx = "_" + tag
        NCH = T // 128
        rq = cx.sb("rq" + tag, [128, T], BF16); rk = cx.sb("rk" + tag, [128, T], BF16)
        rqh = cx.sb("rqh" + tag, [128, 4, T], BF16)
        rv = cx.sb("rv" + tag, [128, NCH, 256], BF16); rg = cx.sb("rg" + tag, [128, NCH, 256], BF16)
        cx.dma("sp", rq[:, :], dr["rqT" + sfx][:, :], r=["D:rope" + sfx + "16"], w=["rq"])
        cx.dma("sp", rk[:, :], dr["rkT" + sfx][:, :], r=["D:rope" + sfx + "17"], w=["rk"])
        cx.dma("sp", rv[:, :, :], dr["rv" + sfx].rearrange("(n p) e -> p n e", p=128), r=["D:rv" + sfx], w=["rv"])
        cx.dma("sp", rg[:, :, :], dr["rg" + sfx].rearrange("(n p) e -> p n e", p=128), r=["D:rg" + sfx], w=["rg"])
        for h in range(4):
            cx.ts(rqh[:, h, :], rq[:, :], cs(cst, "hm")[:, h:h + 1], ALU.mult, r=["rq", "cst"], w=["rqh"], eng="pool")
        SF = cx.sb("SF" + tag, [128, NCH, 256]); SB = cx.sb("SB" + tag, [128, NCH, 256])
        SFb = cx.sb("SFb" + tag, [128, NCH, 256], BF16); SBb = cx.sb("SBb" + tag, [128, NCH, 256], BF16)
        KV = cx.sb("KV" + tag, [128, NCH, 2, 256])
        if tag == "l":
            Tall = cx.sb("Tall", [128, 9, 2, 256]); expo = cx.sb("expo", [128, 2, 9]); coef = cx.sb("coef", [128, 2, 9])
            cx.dma("sp", Tall[:, 0:8, :, :], dr["gt"].rearrange("(s a p) e -> p s a e", s=NCORE, a=2), w=["Tall"])
            cx.dma("sp", Tall[:, 8, :, :], dr["Tst_c"].rearrange("a p e -> p a e"), w=["Tall"])
            cx.dma("sp", expo[:, :, :], dr["expo"][:, :, :], w=["expo"])
            cx.act(coef[:, 0, :], expo[:, 0, :], AF.Exp, scale=lgfc[:, 0:1], r=["expo", "lgfc"], w=["coef"])
            cx.act(coef[:, 1, :], expo[:, 1, :], AF.Exp, scale=lgbc[:, 0:1], r=["expo", "lgbc"], w=["coef"])
            for a, (St, n0, key) in enumerate(((SF, 0, "SF0"), (SB, NCH - 1, "SB%d" % (NCH - 1)))):
                cx.ts(St[:, n0, :], Tall[:, 0, a, :], coef[:, a, 0:1], ALU.mult, r=["Tall", "coef"], w=[key])
                for s in range(1, 9):
                    cx.stt(St[:, n0, :], Tall[:, s, a, :], coef[:, a, s:s + 1], St[:, n0, :], ALU.mult, ALU.add,
                           r=["Tall", "coef", key], w=[key])
        else:
            cx.memset(SF[:, 0, :], 0.0, w=["SF0"])
            cx.memset(SB[:, NCH - 1, :], 0.0, w=["SB%d" % (NCH - 1)])
        kfb = [cx.sb("kfb%d" % i + tag, [128, 256], BF16) for i in range(2)]
        for n in range(NCH):
            b = n % 2
            csl = slice(n * 128, (n + 1) * 128)
            cx.tr(psb[0][:, 0:128], rk[:, csl], ident[:, :], r=["rk", "ident"], w=["psb0"])
            cx.tt(kfb[b][:, 0:128], psb[0][:, 0:128], KDF[:, :], ALU.mult, r=["psb0", "KDF"], w=["kfb%d" % b])
            cx.tt(kfb[b][:, 128:256], psb[0][:, 0:128], KDB[:, :], ALU.mult, r=["psb0", "KDB"], w=["kfb%d" % b])
            cx.mm(ps[0][:, 0:256], kfb[b][:, 0:128], rv[:, n, :], True, True, r=["kfb%d" % b, "rv"], w=["ps0"])
            cx.mm(ps[0][:, 256:512], kfb[b][:, 128:256], rv[:, n, :], True, True, r=["kfb%d" % b, "rv"], w=["ps0"])
            cx.copy(KV[:, n, :, :], ps[0][:, :].rearrange("p (a e) -> p a e", a=2), r=["ps0", "ps0"], w=["KV%d" % n], eng="act")
        for n in range(NCH - 1):
            cx.stt(SF[:, n + 1, :], SF[:, n, :], cdf[:, 0:1], KV[:, n, 0, :], ALU.mult, ALU.add,
                   r=["SF%d" % n, "cdf", "KV%d" % n], w=["SF%d" % (n + 1)])
        for n in range(NCH - 1, 0, -1):
            cx.stt(SB[:, n - 1, :], SB[:, n, :], cdb[:, 0:1], KV[:, n, 1, :], ALU.mult, ALU.add,
                   r=["SB%d" % n, "cdb", "KV%d" % n], w=["SB%d" % (n - 1)])
        allSF = ["SF%d" % n for n in range(NCH)]; allSB = ["SB%d" % n for n in range(NCH)]
        cx.copy(SFb[:, :, :], SF[:, :, :], r=allSF, w=["SFb"], eng="pool")
        cx.copy(SBb[:, :, :], SB[:, :, :], r=allSB, w=["SBb"], eng="pool")
        sT = [cx.sb("sT%d" % i + tag, [128, 4, 128], BF16) for i in range(2)]
        Qf = [cx.sb("Qf%d" % i + tag, [128, 4, 128], BF16) for i in range(2)]
        Qb = [cx.sb("Qb%d" % i + tag, [128, 4, 128], BF16) for i in range(2)]
        osb = cx.sb("osb" + tag, [128, 4, 64]); osq = cx.sb("osq" + tag, [128, 4, 64])
        st = cx.sb("st" + tag, [128, 4, 4])
        ysb = [cx.sb("ysb%d" % i + tag, [128, 256], BF16) for i in range(2)]
        yT = cx.sb("yT" + tag, [128, 2, T], BF16)
        for n in range(NCH):
            b = n % 2
            csl = slice(n * 128, (n + 1) * 128)
            for h in range(4):
                cx.mm(ps[1][:, h * 128:(h + 1) * 128], rk[:, csl], rqh[:, h, csl], True, True, r=["rk", "rqh"], w=["ps1"])
            cx.tt(sT[b][:, :, :], ps[1][:, :].rearrange("p (h i) -> p h i", h=4), maskT[:, :, :], ALU.mult,
                  r=["ps1", "maskT"], w=["sT%d" % b])
            cx.tt(Qf[b][:, :, :], rqh[:, :, csl], qdf4[:, :, :], ALU.mult, r=["rqh", "qdf4"], w=["Qf%d" % b], eng="pool")
            cx.tt(Qb[b][:, :, :], rqh[:, :, csl], qdb4[:, :, :], ALU.mult, r=["rqh", "qdb4"], w=["Qb%d" % b], eng="pool")
            for h in range(4):
                o = ps[2][:, h * 64:(h + 1) * 64]
                es = slice(h * 64, (h + 1) * 64)
                cx.mm(o, sT[b][:, h, :], rv[:, n, es], True, False, r=["sT%d" % b, "rv"], w=["ps2"])
                cx.mm(o, Qf[b][:, h, :], SFb[:, n, es], False, False, r=["Qf%d" % b, "SFb"], w=["ps2"])
                cx.mm(o, Qb[b][:, h, :], SBb[:, n, es], False, True, r=["Qb%d" % b, "SBb"], w=["ps2"])
            p2 = ["ps2"]
            cx.copy(osb[:, :, :], ps[2][:, 0:256].rearrange("p (h e) -> p h e", h=4), r=p2, w=["osb"], eng="act")
            cx.red(st[:, 0, :], osb[:, :, :], ALU.add, r=["osb"], w=["st0"])
            cx.tt(osq[:, :, :], osb[:, :, :], osb[:, :, :], ALU.mult, r=["osb"], w=["osq"], eng="pool")
            cx.red(st[:, 1, :], osq[:, :, :], ALU.add, r=["osq"], w=["st1"])
            cx.ts(st[:, 2, :], st[:, 0, :], 1.0 / 64, ALU.mult, r=["st0"], w=["st2"])
            cx.tt(st[:, 3, :], st[:, 2, :], st[:, 2, :], ALU.mult, r=["st2"], w=["st3"])
            cx.stt(st[:, 3, :], st[:, 1, :], 1.0 / 64, st[:, 3, :], ALU.mult, ALU.subtract, r=["st1", "st3"], w=["st3"])
            cx.act(st[:, 3, :], st[:, 3, :], AF.Sqrt, bias=EPS, r=["st3"], w=["st3"])
            cx.recip(st[:, 3, :], st[:, 3, :], r=["st3"], w=["st3"])
            for h in range(4):
                cx.ts(osb[:, h, :], osb[:, h, :], st[:, 2, h:h + 1], ALU.subtract, s2=st[:, 3, h:h + 1], op1=ALU.mult,
                      r=["osb", "st2", "st3"], w=["osb"])
            cx.tt(ysb[b][:, :], osb[:, :, :].rearrange("p h e -> p (h e)"), rg[:, n, :], ALU.mult, r=["osb", "rg"], w=["ysb%d" % b])
            for c2 in range(2):
                cx.tr(psb[1][:, c2 * 128:(c2 + 1) * 128], ysb[b][:, c2 * 128:(c2 + 1) * 128], ident[:, :],
                      r=["ysb%d" % b, "ident"], w=["psb1"])
            cx.copy(yT[:, :, csl], psb[1][:, 0:256].rearrange("p (c t) -> p c t", c=2), r=["psb1"], w=["yT"], eng="act")
        for c2 in range(2):
            cx.dma("sp", dr["ysT" + sfx][6 + c2, :, :], yT[:, c2, :], r=["yT"], w=["D:ys3" + sfx])
    cx.end()


def phase_merge(cx, l, dr, segs, wl=None):
    wl = l if wl is None else wl
    cx.begin(nf=8, nb=0)
    ps = cx.ps
    cst = cx.sb("cst", [128, CSTW])
    cx.dma("sp", cst[:, :], dr["cst"][:, :], w=["cst"])
    WG = cx.sb("WG", [128, 8, 4096], BF16)
    WB = cx.sb("WB", [128, 8, 1024], BF16)
    WO = cx.sb("WO", [128, 8, 1024], BF16)
    for kc in range(8):
        cx.dma("pool", WG[:, kc, :], dr["w_gate"][wl, kc * 128:(kc + 1) * 128, :], w=["WG%d" % kc])
        cx.dma("pool", WB[:, kc, :], dr["w_branch"][wl, kc // 2, (kc % 2) * 128:(kc % 2 + 1) * 128, :], w=["WB%d" % kc])
        cx.dma("pool", WO[:, kc, :], dr["w_o"][wl, kc * 128:(kc + 1) * 128, :], w=["WO%d" % kc])
    braw = cx.sb("braw", [32, 128]); bgt = cx.sb("bgt", [128, 32])
    cx.dma("sp", braw[:, :], dr["b_gate"][l, :].rearrange("(a p) -> a p", p=128), w=["braw"])
    cx.tr(ps[7][:, 0:32], braw[:, :], cs(cst, "I1")[0:32, 0:32], r=["braw", "cst"], w=["ps7"])
    cx.copy(bgt[:, :], ps[7][:, 0:32], r=["ps7"], w=["bgt"])
    g1b = cx.sb("g1b", [128, D])
    hT = [cx.sb("mhT%d" % i, [128, 8, 512], BF16) for i in range(2)]
    yT = [cx.sb("myT%d" % i, [128, 8, 512], BF16) for i in range(2)]
    mT = cx.sb("mT", [128, 8, 512], BF16)
    sg = [cx.sb("sg%d" % i, [128, 512]) for i in range(2)]
    macc = cx.sb("macc", [128, 512]); mtmp = cx.sb("mtmp", [128, 512])
    xt = [cx.sb("mxt%d" % i, [128, D]) for i in range(2)]
    gi = 0
    xi = 0
    for (tag, T, xin, xout) in segs:
        sfx = "_" + tag
        seg = 0 if tag == "l" else 1
        cx.dma("sp", g1b[:, :], dr["modv"][l, seg, 2, :].partition_broadcast(128), r=["D:modv%d" % l], w=["g1b"])
        G = min(512, T)
        for g in range(T // G):
            t0 = g * G
            b = gi % 2
            gi += 1
            cx.dma("sp", hT[b][:, :, 0:G], dr["hT" + sfx][:, :, t0:t0 + G], r=["D:hT" + sfx], w=["mhT%d" % b])
            cx.dma("sp", yT[b][:, :, 0:G], dr["ysT" + sfx][:, :, t0:t0 + G].rearrange("a p t -> p a t"),
                   r=["D:ys0" + sfx, "D:ys1" + sfx, "D:ys2" + sfx, "D:ys3" + sfx], w=["myT%d" % b])
            for nn in range(8):
                for i in range(4):
                    pa = ps[(i % 2) * 2]
                    pb = ps[(i % 2) * 2 + 1]
                    ka = "ps%d" % ((i % 2) * 2)
                    kb = "ps%d" % ((i % 2) * 2 + 1)
                    col = i * 1024 + nn * 128
                    for kc in range(8):
                        cx.mm(pa[:, 0:G], WG[:, kc, col:col + 128], hT[b][:, kc, 0:G], kc == 0, kc == 7,
                              r=["WG%d" % kc, "mhT%d" % b], w=[ka])
                    for c2 in range(2):
                        cx.mm(pb[:, 0:G], WB[:, i * 2 + c2, nn * 128:(nn + 1) * 128], yT[b][:, i * 2 + c2, 0:G], c2 == 0, c2 == 1,
                              r=["WB%d" % (i * 2 + c2), "myT%d" % b], w=[kb])
                    s = sg[i % 2]
                    ks = "sg%d" % (i % 2)
                    cx.act(s[:, 0:G], pa[:, 0:G], AF.Sigmoid, bias=bgt[:, i * 8 + nn:i * 8 + nn + 1], r=[ka, "bgt"], w=[ks])
                    if i == 0:
                        cx.tt(macc[:, 0:G], pb[:, 0:G], s[:, 0:G], ALU.mult, r=[kb, ks], w=["macc"])
                    elif i < 3:
                        cx.tt(mtmp[:, 0:G], pb[:, 0:G], s[:, 0:G], ALU.mult, r=[kb, ks], w=["mtmp"])
                        cx.tt(macc[:, 0:G], macc[:, 0:G], mtmp[:, 0:G], ALU.add, r=["macc", "mtmp"], w=["macc"], eng="pool")
                    else:
                        cx.tt(mtmp[:, 0:G], pb[:, 0:G], s[:, 0:G], ALU.mult, r=[kb, ks], w=["mtmp"])
                        cx.tt(mT[:, nn, 0:G], macc[:, 0:G], mtmp[:, 0:G], ALU.add, r=["macc", "mtmp"], w=["mT"], eng="pool")
            for j in range(G // 128):
                xb = xi % 2
                xi += 1
                rows = slice(t0 + j * 128, t0 + (j + 1) * 128)
                cx.dma("sp", xt[xb][:, :], xin[rows, :], w=["mxt%d" % xb])
                for nh in range(2):
                    po = ps[4 + nh]
                    for kc in range(8):
                        cx.mm(po[:, :], mT[:, kc, j * 128:(j + 1) * 128], WO[:, kc, nh * 512:(nh + 1) * 512], kc == 0, kc == 7,
                              r=["mT", "WO%d" % kc], w=["ps%d" % (4 + nh)])
                    hs = slice(nh * 512, (nh + 1) * 512)
                    cx.tt(mtmp[:, :], po[:, :], g1b[:, hs], ALU.mult, r=["ps%d" % (4 + nh), "g1b"], w=["mtmp"])
                    cx.tt(xt[xb][:, hs], xt[xb][:, hs], mtmp[:, :], ALU.add, r=["mxt%d" % xb, "mtmp"], w=["mxt%d" % xb], eng="pool")
                cx.dma("sp", xout[rows, :], xt[xb][:, :], r=["mxt%d" % xb], w=["D:xmid" + sfx])
    cx.end()


def phase_attn(cx, l, dr, segs, lam_init):
    NB = 2
    cx.begin(nf=0, nb=1)
    psb = cx.psb
    psS = [cx.psum("psS%d" % i, [128, NB * 512]) for i in range(2)]
    psO = [cx.psum("psO%d" % i, [128, 512]) for i in range(2)]
    ps6 = cx.psum("ps6", [128, 512])
    NKT = max(s[2] for s in segs)
    cst = cx.sb("cst", [128, CSTW])
    ident = cx.sb("ident", [128, 128], BF16)
    cx.dma("sp", cst[:, :], dr["cst"][:, :], w=["cst"])
    cx.dma("sp", ident[:, :], dr["ident"][:, :], w=["ident"])
    kT = cx.sb("kTall", [128, 2, NKT * 128], BF16)
    Va = cx.sb("Vaug", [128, NKT, 4, 65], BF16)
    vst = [cx.sb("vst%d" % i, [128, 10, 256], BF16) for i in range(2)]
    kTk = []
    for c in range(2):
        cx.dma("sp", kT[:, c, 0:TC], dr["kT_c"][c, :, :], w=["kTc%d" % c])
        kTk.append("kTc%d" % c)
        if NKT > TC // 128:
            for r_ in range(NCORE):
                cx.dma("sp" if (r_ % 2 == 0) else "act", kT[:, c, TC + r_ * TL:TC + (r_ + 1) * TL],
                       dr["gk"][(r_ * 2 + c) * 128:(r_ * 2 + c + 1) * 128, :], w=["kT%d_%d" % (c, r_)])
                kTk.append("kT%d_%d" % (c, r_))
    cx.memset(Va[:, :, :, 64:65], 1.0, w=["Vones"], eng="pool")
    chunks = [(0, TC // 128, dr["V_c"], 0)]
    k0 = TC // 128
    while k0 < NKT:
        k1 = min(NKT, k0 + 10)
        chunks.append((k0, k1, dr["gv"], (k0 - TC // 128) * 128))
        k0 = k1
    nst = len(chunks)
    for i, (k0, k1, src, row0) in enumerate(chunks):
        b = i % 2
        cx.dma("sp", vst[b][:, 0:k1 - k0, :], src[row0:row0 + (k1 - k0) * 128, :].rearrange("(k p) e -> p k e", p=128), w=["vst%d" % b])
        cx.copy(Va[:, k0:k1, :, 0:64], vst[b][:, 0:k1 - k0, :].rearrange("p k (h e) -> p k h e", h=4), r=["vst%d" % b],
                w=["Va%d" % i], eng=("pool" if i % 2 == 0 else "dve"))
    Vak = ["Va%d" % i for i in range(nst)] + ["Vones"]
    lq = cx.sb("lq", [128, 4, 32]); lp = cx.sb("lp", [128, 2, 32]); ls = cx.sb("ls", [128, 4])
    for i, nm in enumerate(("lam_q1", "lam_k1", "lam_q2", "lam_k2")):
        cx.dma("sp", lq[:, i, :], dr[nm][l, :].partition_broadcast(128), w=["lq"])
    cx.tt(lp[:, 0, :], lq[:, 0, :], lq[:, 1, :], ALU.mult, r=["lq"], w=["lp"])
    cx.tt(lp[:, 1, :], lq[:, 2, :], lq[:, 3, :], ALU.mult, r=["lq"], w=["lp"])
    cx.red(ls[:, 0:2], lp[:, :, :], ALU.add, r=["lp"], w=["ls"])
    cx.act(ls[:, 0:2], ls[:, 0:2], AF.Exp, r=["ls"], w=["ls"])
    cx.tt(ls[:, 2:3], ls[:, 1:2], ls[:, 0:1], ALU.subtract, r=["ls"], w=["ls2"])
    cx.ts(ls[:, 3:4], ls[:, 2:3], -lam_init, ALU.add, r=["ls2"], w=["nlam"])
    nlam = ls[:, 3:4]
    dgb = cx.sb("dgb", [128, 4, 64])
    for h in range(4):
        cx.dma("sp", dgb[:, h, :], dr["diff_g"][l, :].partition_broadcast(128), w=["dgb"])
    cx.ts(dgb[:, :, :], dgb[:, :, :], 1.0 - lam_init, ALU.mult, r=["dgb"], w=["dgb"])
    qg = [cx.sb("qg%d" % i, [128, 2, 512], BF16) for i in range(2)]
    qm = [cx.sb("qm%d" % i, [128, 8, 512], BF16) for i in range(2)]
    pT = [cx.sb("pT%d" % i, [128, NB, 512], BF16) for i in range(2)]
    oT = cx.sb("oT", [65, 2, 512])
    oatt = cx.sb("oatt", [128, 4, 4, 64]); osq = cx.sb("aosq", [128, 4, 64])
    rr = cx.sb("rr", [128, 4]); ast = cx.sb("ast", [128, 2, 4])
    ysb = [cx.sb("aysb%d" % i, [128, 256], BF16) for i in range(2)]
    yT = cx.sb("ayT", [128, 2, 512], BF16)
    gi = 0
    for (tag, T, nkt) in segs:
        sfx = "_" + tag
        G = min(512, T)
        nt = G // 128
        assert nkt % NB == 0
        for g in range(T // G):
            t0 = g * G
            b = gi % 2
            gi += 1
            cx.dma("sp", qg[b][:, :, 0:G], dr["qT" + sfx][:, :, t0:t0 + G].rearrange("c p t -> p c t"),
                   r=["D:rope" + sfx + "10", "D:rope" + sfx + "11"], w=["qg%d" % b])
            qb_ = b
            for c in range(2):
                for bl in range(4):
                    cx.ts(qm[qb_][:, c * 4 + bl, 0:G], qg[b][:, c, 0:G], cs(cst, "bm8")[:, bl:bl + 1], ALU.mult,
                          r=["qg%d" % b, "cst"], w=["qm%d_%d" % (qb_, c * 4 + bl)])
            items = [(h, m, kb) for h in range(4) for m in range(2) for kb in range(nkt // NB)]
            LA = 1

            def emit_S(i):
                h, m, kb = items[i]
                c = h // 2
                qi = c * 4 + (h % 2) * 2 + m
                sb_ = i % 2
                for j in range(NB):
                    kt = kb * NB + j
                    kk = "kTc%d" % c if kt < TC // 128 else "kT%d_%d" % (c, (kt * 128 - TC) // TL)
                    cx.mm(psS[sb_][:, j * 512:j * 512 + G], kT[:, c, kt * 128:(kt + 1) * 128], qm[qb_][:, qi, 0:G], True, True,
                          r=[kk, "qm%d_%d" % (qb_, qi)], w=["psS%d" % sb_])
                cx.act(pT[sb_][:, :, 0:G], psS[sb_][:, :].rearrange("p (j n) -> p j n", j=NB)[:, :, 0:G], AF.Exp, scale=QSCALE,
                       r=["psS%d" % sb_], w=["pT%d" % sb_])

            def emit_O(i):
                h, m, kb = items[i]
                sb_ = i % 2
                for j in range(NB):
                    kt = kb * NB + j
                    vk = "Va0" if kt < TC // 128 else "Va%d" % (1 + (kt - TC // 128) // 10)
                    cx.mm(psO[m][0:65, 0:G], Va[:, kt, h, :], pT[sb_][:, j, 0:G], kt == 0, kt == nkt - 1,
                          r=[vk, "Vones", "pT%d" % sb_], w=["psO%d" % m])
                if kb == nkt // NB - 1:
                    cx.copy(oT[:, m, 0:G], psO[m][0:65, 0:G], r=["psO%d" % m], w=["oT%d" % m])
                    if m == 1:
                        head_epilogue(h)

            def head_epilogue(h):
                for j in range(nt):
                    for m in range(2):
                        cx.tr(ps6[:, m * 65:m * 65 + 65], oT[0:65, m, j * 128:(j + 1) * 128], cs(cst, "I1")[0:65, 0:65],
                              r=["oT%d" % m, "cst"], w=["ps6"])
                    cx.recip(rr[:, 0:1], ps6[:, 64:65], r=["ps6"], w=["rr0"])
                    cx.recip(rr[:, 1:2], ps6[:, 129:130], r=["ps6"], w=["rr1"])
                    cx.tt(rr[:, 2:3], rr[:, 1:2], nlam, ALU.mult, r=["rr1", "nlam"], w=["rr2"])
                    cx.ts(oatt[:, j, h, :], ps6[:, 0:64], rr[:, 0:1], ALU.mult, r=["ps6", "rr0"], w=["oatt%d" % j])
                    cx.stt(oatt[:, j, h, :], ps6[:, 65:129], rr[:, 2:3], oatt[:, j, h, :], ALU.mult, ALU.add,
                           r=["ps6", "rr2", "oatt%d" % j], w=["oatt%d" % j])

            if ATT_ROW:
                items = [(h, kt) for h in range(4) for kt in range(nkt)]

                def emit_S(i):
                    h, kt = items[i]
                    c = h // 2
                    sb_ = i % 2
                    kk = "kTc%d" % c if kt < TC // 128 else "kT%d_%d" % (c, (kt * 128 - TC) // TL)
                    for m in range(2):
                        blk = (h % 2) * 2 + m
                        rs = slice(32 * blk, 32 * blk + 32)
                        cx.mm(psS[sb_][:, m * 512:m * 512 + G], kT[rs, c, kt * 128:(kt + 1) * 128], qg[b][rs, c, 0:G], True, True,
                              r=[kk, "qg%d" % b], w=["psS%d" % sb_], tile_position=(32 * blk, 0))
                    cx.act(pT[sb_][:, :, 0:G], psS[sb_][:, :].rearrange("p (j n) -> p j n", j=NB)[:, :, 0:G], AF.Exp, scale=QSCALE,
                           r=["psS%d" % sb_], w=["pT%d" % sb_])

                def emit_O(i):
                    h, kt = items[i]
                    sb_ = i % 2
                    vk = "Va0" if kt < TC // 128 else "Va%d" % (1 + (kt - TC // 128) // 10)
                    for m in range(2):
                        cx.mm(psO[m][0:65, 0:G], Va[:, kt, h, :], pT[sb_][:, m, 0:G], kt == 0, kt == nkt - 1,
                              r=[vk, "Vones", "pT%d" % sb_], w=["psO%d" % m])
                    if kt == nkt - 1:
                        for m in range(2):
                            cx.copy(oT[:, m, 0:G], psO[m][0:65, 0:G], r=["psO%d" % m], w=["oT%d" % m])
                        head_epilogue(h)
            n_it = len(items)
            for i in range(n_it + LA):
                if i < n_it:
                    emit_S(i)
                if i >= LA:
                    emit_O(i - LA)
            for j in range(nt):
                yb = j % 2
                cx.tt(osq[:, :, :], oatt[:, j, :, :], oatt[:, j, :, :], ALU.mult, r=["oatt%d" % j], w=["aosq"], eng="pool")
                cx.red(ast[:, 0, :], osq[:, :, :], ALU.add, r=["aosq"], w=["ast0"])
                cx.act(ast[:, 1, :], ast[:, 0, :], AF.Sqrt, scale=1.0 / 64, bias=EPS, r=["ast0"], w=["ast1"])
                cx.recip(ast[:, 1, :], ast[:, 1, :], r=["ast1"], w=["ast1"])
                for h in range(4):
                    cx.stt(oatt[:, j, h, :], oatt[:, j, h, :], ast[:, 1, h:h + 1], dgb[:, h, :], ALU.mult, ALU.mult,
                           r=["oatt%d" % j, "ast1", "dgb"], w=["oatt%d" % j])
                cx.copy(ysb[yb][:, :], oatt[:, j, :, :].rearrange("p h e -> p (h e)"), r=["oatt%d" % j], w=["aysb%d" % yb], eng="pool")
                for c2 in range(2):
                    cx.tr(psb[0][:, c2 * 128:(c2 + 1) * 128], ysb[yb][:, c2 * 128:(c2 + 1) * 128], ident[:, :],
                          r=["aysb%d" % yb, "ident"], w=["psb0"])
                cx.copy(yT[:, :, j * 128:(j + 1) * 128], psb[0][:, 0:256].rearrange("p (c t) -> p c t", c=2), r=["psb0"], w=["ayT"])
            for c2 in range(2):
                cx.dma("sp", dr["ysT" + sfx][4 + c2, :, t0:t0 + G], yT[:, c2, 0:G], r=["ayT"], w=["D:ys2" + sfx])
    cx.end()


def phase_moe(cx, l, dr, segs, final, wl=None):
    wl = l if wl is None else wl
    cx.begin(nf=6, nb=2)
    ps, psb = cx.ps, cx.psb
    NT = sum(s[1] for s in segs) // 128
    TT = NT * 128
    cst = cx.sb("cst", [128, CSTW])
    ident = cx.sb("ident", [128, 128], BF16)
    cx.dma("sp", cst[:, :], dr["cst"][:, :], w=["cst"])
    cx.dma("sp", ident[:, :], dr["ident"][:, :], w=["ident"])
    h2T = cx.sb("h2T", [128, 8, TT], BF16)
    acc = cx.sb("eacc", [128, NT, D])
    wt = cx.sb("wt", [128, NT, 16])
    WR = cx.sb("WRt", [128, 8, 16], BF16)
    cx.dma("pool", WR[:, :, :], dr["w_router"].rearrange("(k p) e -> p k e", p=128), w=["WRt"])
    brb = cx.sb("brb", [128, 16])
    cx.dma("sp", brb[:, :], dr["b_router"][0, :].partition_broadcast(128), w=["brb"])
    gsb = cx.sb("gsb2", [128, D]); shb = cx.sb("shb2", [128, D]); g2b = cx.sb("g2b2", [128, D])
    xt = [cx.sb("ext%d" % i, [128, D]) for i in range(2)]
    junk = cx.sb("ejunk", [128, D], BF16)
    t1 = cx.sb("et1", [128, D])
    hb = [cx.sb("ehb%d" % i, [128, D], BF16) for i in range(2)]
    ssq = cx.sb("essq", [128, 2]); rstd = cx.sb("erstd", [128, 2])
    rt = cx.sb("rt", [128, 8, 16])
    W1 = [cx.sb("W1_%d" % i, [128, 8, DFF], BF16) for i in range(2)]
    W3 = [cx.sb("W3_%d" % i, [128, 8, DFF], BF16) for i in range(2)]
    W2 = [cx.sb("W2_%d" % i, [128, 4, D], BF16) for i in range(2)]

    def load_expert(e):
        b = e % 2
        cx.dma("pool", W1[b][:, :, :], dr["w1_e"][wl, e].rearrange("(k p) f -> p k f", p=128), w=["W1_%d" % b])
        cx.dma("pool", W3[b][:, :, :], dr["w3_e"][wl, e].rearrange("(k p) f -> p k f", p=128), w=["W3_%d" % b])
        cx.dma("pool", W2[b][:, :, :], dr["w2_e"][wl, e].rearrange("(k p) n -> p k n", p=128), w=["W2_%d" % b])
    load_expert(0)
    load_expert(1)
    ti = 0
    tiles = []
    for (tag, T, xmid, xout) in segs:
        seg = 0 if tag == "l" else 1
        cx.dma("sp", gsb[:, :], dr["modv"][l, seg, 3, :].partition_broadcast(128), r=["D:modv%d" % l], w=["gsb2"])
        cx.dma("sp", shb[:, :], dr["modv"][l, seg, 4, :].partition_broadcast(128), r=["D:modv%d" % l], w=["shb2"])
        for j in range(T // 128):
            b = ti % 2
            kx = "ext%d" % b
            rows = slice(j * 128, (j + 1) * 128)
            tiles.append((tag, seg, xmid, xout, rows))
            cx.dma("sp", xt[b][:, :], xmid[rows, :], r=["D:xmid_" + tag], w=[kx])
            cx.act(junk[:, :], xt[b][:, :], AF.Square, accum_out=ssq[:, b:b + 1], r=[kx], w=["ejunk", "essq%d" % b])
            cx.act(rstd[:, b:b + 1], ssq[:, b:b + 1], AF.Sqrt, scale=1.0 / D, bias=EPS, r=["essq%d" % b], w=["ers%d" % b])
            cx.recip(rstd[:, b:b + 1], rstd[:, b:b + 1], r=["ers%d" % b], w=["ers%d" % b])
            cx.stt(t1[:, :], xt[b][:, :], rstd[:, b:b + 1], gsb[:, :], ALU.mult, ALU.mult, r=[kx, "ers%d" % b, "gsb2"], w=["et1"])
            cx.tt(hb[b][:, :], t1[:, :], shb[:, :], ALU.add, r=["et1", "shb2"], w=["ehb%d" % b], eng="pool")
            for kc in range(8):
                cx.tr(psb[0][:, kc * 128:(kc + 1) * 128], hb[b][:, kc * 128:(kc + 1) * 128], ident[:, :],
                      r=["ehb%d" % b, "ident"], w=["psb0"])
            cx.copy(h2T[:, :, ti * 128:(ti + 1) * 128], psb[0][:, :].rearrange("p (k t) -> p k t", k=8),
                    r=["psb0"], w=["h2T%d" % ti], eng="act")
            for kc in range(8):
                cx.mm(ps[0][:, 0:16], h2T[:, kc, ti * 128:(ti + 1) * 128], WR[:, kc, :], kc == 0, kc == 7,
                      r=["h2T%d" % ti, "WRt"], w=["ps0"])
            s_ = rt[:, 0, :]; sbv = rt[:, 1, :]; tmp = rt[:, 2, :]; sb2 = rt[:, 3, :]; sbm = rt[:, 4, :]
            msk = rt[:, 5, :]; sel = rt[:, 6, :]
            g4 = rt[:, 7, 0:4]; g4b = rt[:, 7, 4:8]; gm = rt[:, 7, 8:12]; e1 = rt[:, 7, 12:13]; e2 = rt[:, 7, 13:14]
            den = rt[:, 7, 14:15]
            cx.act(s_, ps[0][:, 0:16], AF.Sigmoid, r=["ps0"], w=["r_s"])
            cx.tt(sbv, s_, brb[:, :], ALU.add, r=["r_s", "brb"], w=["r_sb"])
            v4 = lambda a: a.rearrange("p (g e) -> p g e", g=4)
            cx.red(g4, v4(sbv), ALU.max, r=["r_sb"], w=["r_g4"])
            for g_ in range(4):
                cx.ts(tmp[:, g_ * 4:(g_ + 1) * 4], sbv[:, g_ * 4:(g_ + 1) * 4], g4[:, g_:g_ + 1], ALU.is_equal,
                      r=["r_sb", "r_g4"], w=["r_tmp"])
            cx.stt(sb2, tmp, -1.0e9, sbv, ALU.mult, ALU.add, r=["r_tmp", "r_sb"], w=["r_sb2"])
            cx.red(g4b, v4(sb2), ALU.max, r=["r_sb2"], w=["r_g4b"])
            cx.tt(g4, g4, g4b, ALU.add, r=["r_g4", "r_g4b"], w=["r_g4"])
            cx.red(e1, g4, ALU.max, r=["r_g4"], w=["r_e1"])
            cx.ts(gm, g4, e1, ALU.is_equal, s2=-1.0, op1=ALU.add, r=["r_g4", "r_e1"], w=["r_gm"])
            for g_ in range(4):
                cx.ts(tmp[:, g_ * 4:(g_ + 1) * 4], cs(cst, "ones")[:, 0:4], gm[:, g_:g_ + 1], ALU.mult,
                      r=["r_gm", "cst"], w=["r_tmp"])
            cx.stt(sbm, tmp, 1.0e9, sbv, ALU.mult, ALU.add, r=["r_tmp", "r_sb"], w=["r_sbm"])
            cx.red(e1, sbm, ALU.max, r=["r_sbm"], w=["r_e1"])
            cx.ts(msk, sbm, e1, ALU.is_equal, r=["r_sbm", "r_e1"], w=["r_msk"])
            cx.stt(sb2, msk, -1.0e9, sbm, ALU.mult, ALU.add, r=["r_msk", "r_sbm"], w=["r_sb2"])
            cx.red(e2, sb2, ALU.max, r=["r_sb2"], w=["r_e2"])
            cx.ts(sel, sb2, e2, ALU.is_equal, r=["r_sb2", "r_e2"], w=["r_sel"])
            cx.tt(sel, sel, msk, ALU.add, r=["r_sel", "r_msk"], w=["r_sel"])
            cx.tt(sel, sel, s_, ALU.mult, r=["r_sel", "r_s"], w=["r_sel"])
            cx.red(den, sel, ALU.add, r=["r_sel"], w=["r_den"])
            cx.recip(den, den, r=["r_den"], w=["r_den"])
            cx.ts(wt[:, ti, :], sel, den, ALU.mult, r=["r_sel", "r_den"], w=["wt%d" % ti])
            ti += 1
    uT = [cx.sb("uT%d" % i, [128, 4, 512], BF16) for i in range(2)]
    s1 = [cx.sb("s1_%d" % i, [128, 512]) for i in range(2)]
    groups = []
    t = 0
    while t < NT:
        n = min(4, NT - t)
        groups.append((t, n))
        t += n
    ui = 0
    for e in range(NEXP):
        b = e % 2
        if e >= 2:
            load_expert(e)
        for (tg, ntl) in groups:
            G = ntl * 128
            gsl = slice(tg * 128, tg * 128 + G)
            hk = ["h2T%d" % i for i in range(tg, tg + ntl)]
            ub = ui % 2
            ui += 1
            for fc in range(4):
                fs = slice(fc * 128, (fc + 1) * 128)
                pa = ps[(fc % 2) * 2]; pb = ps[(fc % 2) * 2 + 1]
                ka = "ps%d" % ((fc % 2) * 2); kb = "ps%d" % ((fc % 2) * 2 + 1)
                for kc in range(8):
                    cx.mm(pa[:, 0:G], W1[b][:, kc, fs], h2T[:, kc, gsl], kc == 0, kc == 7, r=hk + ["W1_%d" % b], w=[ka])
                for kc in range(8):
                    cx.mm(pb[:, 0:G], W3[b][:, kc, fs], h2T[:, kc, gsl], kc == 0, kc == 7, r=hk + ["W3_%d" % b], w=[kb])
                sb_ = s1[fc % 2]
                cx.act(sb_[:, 0:G], pa[:, 0:G], AF.Silu, r=[ka], w=["s1_%d" % (fc % 2)])
                cx.tt(uT[ub][:, fc, 0:G], pb[:, 0:G], sb_[:, 0:G], ALU.mult, r=[kb, "s1_%d" % (fc % 2)], w=["uT%d" % ub])
            for j in range(ntl):
                tix = tg + j
                for nh in range(2):
                    po = ps[4 + nh]
                    for fc in range(4):
                        cx.mm(po[:, :], uT[ub][:, fc, j * 128:(j + 1) * 128], W2[b][:, fc, nh * 512:(nh + 1) * 512], fc == 0, fc == 3,
                              r=["uT%d" % ub, "W2_%d" % b], w=["ps%d" % (4 + nh)])
                    a = acc[:, tix, nh * 512:(nh + 1) * 512]
                    ka2 = "eacc%d_%d" % (tix, nh)
                    if e == 0:
                        cx.ts(a, po[:, :], wt[:, tix, e:e + 1], ALU.mult, r=["ps%d" % (4 + nh), "wt%d" % tix], w=[ka2])
                    else:
                        cx.stt(a, po[:, :], wt[:, tix, e:e + 1], a, ALU.mult, ALU.add, r=["ps%d" % (4 + nh), "wt%d" % tix, ka2], w=[ka2])
    if final:
        gfb = cx.sb("gfb", [128, D])
        cx.dma("sp", gfb[:, :], dr["g_final"][0, :].partition_broadcast(128), w=["gfb"])
    cur = None
    for ti, (tag, seg, xmid, xout, rows) in enumerate(tiles):
        if cur != seg:
            cx.dma("sp", g2b[:, :], dr["modv"][l, seg, 5, :].partition_broadcast(128), r=["D:modv%d" % l], w=["g2b2"])
            cur = seg
        b = ti % 2
        kx = "ext%d" % b
        cx.dma("sp", xt[b][:, :], xmid[rows, :], r=["D:xmid_" + tag], w=[kx])
        cx.tt(t1[:, :], acc[:, ti, :], g2b[:, :], ALU.mult, r=["eacc%d_0" % ti, "eacc%d_1" % ti, "g2b2"], w=["et1"], eng="pool")
        cx.tt(xt[b][:, :], xt[b][:, :], t1[:, :], ALU.add, r=[kx, "et1"], w=[kx])
        if final:
            cx.act(junk[:, :], xt[b][:, :], AF.Square, accum_out=ssq[:, b:b + 1], r=[kx], w=["ejunk", "essq%d" % b])
            cx.act(rstd[:, b:b + 1], ssq[:, b:b + 1], AF.Sqrt, scale=1.0 / D, bias=EPS, r=["essq%d" % b], w=["ers%d" % b])
            cx.recip(rstd[:, b:b + 1], rstd[:, b:b + 1], r=["ers%d" % b], w=["ers%d" % b])
            cx.stt(xt[b][:, :], xt[b][:, :], rstd[:, b:b + 1], gfb[:, :], ALU.mult, ALU.mult, r=[kx, "ers%d" % b, "gfb"], w=[kx])
        cx.dma("sp", xout[rows, :], xt[b][:, :], r=[kx], w=["D:xout_" + tag])
    cx.end()


def phase_moe_sparse(cx, l, dr, segs, final, wl=None):
    wl = l if wl is None else wl
    C = MOE_CAP
    cx.begin(nf=6, nb=2)
    ps, psb = cx.ps, cx.psb
    NT = sum(s[1] for s in segs) // 128
    cst = cx.sb("cst", [128, CSTW])
    ident = cx.sb("ident", [128, 128], BF16)
    cx.dma("sp", cst[:, :], dr["cst"][:, :], w=["cst"])
    cx.dma("sp", ident[:, :], dr["ident"][:, :], w=["ident"])
    Xg = dr["Xg"]
    Yg = dr["Yg"]
    bcreg = {}

    def bc(h):
        if "r" not in bcreg:
            bcreg["r"] = h.to_reg(NSLOT - 1)
        return bcreg["r"]
    W1 = [cx.sb("W1_%d" % i, [128, 8, DFF], BF16) for i in range(2)]
    W3 = [cx.sb("W3_%d" % i, [128, 8, DFF], BF16) for i in range(2)]
    W2 = [cx.sb("W2_%d" % i, [128, 4, D], BF16) for i in range(2)]

    def load_expert(e):
        b = e % 2
        cx.dma("pool", W1[b][:, :, :], dr["w1_e"][wl, e].rearrange("(k p) f -> p k f", p=128), w=["W1_%d" % b])
        cx.dma("pool", W3[b][:, :, :], dr["w3_e"][wl, e].rearrange("(k p) f -> p k f", p=128), w=["W3_%d" % b])
        cx.dma("pool", W2[b][:, :, :], dr["w2_e"][wl, e].rearrange("(k p) n -> p k n", p=128), w=["W2_%d" % b])
    load_expert(0)
    load_expert(1)
    zt = cx.sb("zt", [128, 4, D], BF16)
    cx.memset(zt[:, :, :], 0.0, w=["zt"], eng="pool")
    for i in range(NSLOT // 512):
        cx.dma("sp", Xg[i * 512:(i + 1) * 512, :].rearrange("(b p) d -> p b d", p=128), zt[:, :, :], r=["zt"], w=["D:XgZ%d" % i])
    WR = cx.sb("WRt", [128, 8, 16], BF16)
    cx.dma("pool", WR[:, :, :], dr["w_router"].rearrange("(k p) e -> p k e", p=128), w=["WRt"])
    brb = cx.sb("brb", [128, 16])
    cx.dma("sp", brb[:, :], dr["b_router"][0, :].partition_broadcast(128), w=["brb"])
    gsb = cx.sb("gsb2", [128, D]); shb = cx.sb("shb2", [128, D]); g2b = cx.sb("g2b2", [128, D])
    xt = [cx.sb("ext%d" % i, [128, D]) for i in range(2)]
    junk = cx.sb("ejunk", [128, D], BF16)
    t1s = [cx.sb("et1_%d" % i, [128, D]) for i in range(2)]
    hb = [cx.sb("ehb%d" % i, [128, D], BF16) for i in range(2)]
    hTt = [cx.sb("ehT%d" % i, [128, 8, 128], BF16) for i in range(2)]
    ssq = cx.sb("essq", [128, 2]); rstd = cx.sb("erstd", [128, 2])
    rts = [cx.sb("rt%d" % i, [128, 10, 16]) for i in range(2)]
    off = cx.sb("roff", [128, 16])
    slf = cx.sb("slf", [128, NT, 2]); sli = cx.sb("sli", [128, NT, 2], mybir.dt.int32); wts = cx.sb("wts", [128, NT, 2])
    cx.memset(off[:, :], 0.0, w=["roff"])
    ti = 0
    tiles = []
    for (tag, T, xmid, xout) in segs:
        seg = 0 if tag == "l" else 1
        cx.dma("sp", gsb[:, :], dr["modv"][l, seg, 3, :].partition_broadcast(128), r=["D:modv%d" % l], w=["gsb2"])
        cx.dma("sp", shb[:, :], dr["modv"][l, seg, 4, :].partition_broadcast(128), r=["D:modv%d" % l], w=["shb2"])
        for j in range(T // 128):
            b = ti % 2
            kx = "ext%d" % b
            rt = rts[b]
            t1 = t1s[b]
            K_ = lambda n: "%s_%d" % (n, b)
            rows = slice(j * 128, (j + 1) * 128)
            tiles.append((tag, seg, xmid, xout, rows))
            cx.dma("sp", xt[b][:, :], xmid[rows, :], r=["D:xmid_" + tag], w=[kx])
            cx.act(junk[:, :], xt[b][:, :], AF.Square, accum_out=ssq[:, b:b + 1], r=[kx], w=[K_("ejunk"), "essq%d" % b])
            cx.act(rstd[:, b:b + 1], ssq[:, b:b + 1], AF.Sqrt, scale=1.0 / D, bias=EPS, r=["essq%d" % b], w=["ers%d" % b])
            cx.recip(rstd[:, b:b + 1], rstd[:, b:b + 1], r=["ers%d" % b], w=["ers%d" % b])
            cx.stt(t1[:, :], xt[b][:, :], rstd[:, b:b + 1], gsb[:, :], ALU.mult, ALU.mult, r=[kx, "ers%d" % b, "gsb2"], w=[K_("et1")])
            cx.tt(hb[b][:, :], t1[:, :], shb[:, :], ALU.add, r=[K_("et1"), "shb2"], w=["ehb%d" % b], eng="pool")
            for kc in range(8):
                cx.tr(psb[0][:, kc * 128:(kc + 1) * 128], hb[b][:, kc * 128:(kc + 1) * 128], ident[:, :],
                      r=["ehb%d" % b, "ident"], w=["psb0"])
            cx.copy(hTt[b][:, :, :], psb[0][:, :].rearrange("p (k t) -> p k t", k=8), r=["psb0"], w=["ehT%d" % b], eng="act")
            for kc in range(8):
                cx.mm(ps[0][:, 0:16], hTt[b][:, kc, :], WR[:, kc, :], kc == 0, kc == 7, r=["ehT%d" % b, "WRt"], w=["ps0"])
            s_ = rt[:, 0, :]; sbv = rt[:, 1, :]; tmp = rt[:, 2, :]; sb2 = rt[:, 3, :]; sbm = rt[:, 4, :]
            msk = rt[:, 5, :]; sel = rt[:, 6, :]; pos = rt[:, 8, :]; m2 = rt[:, 9, :]
            g4 = rt[:, 7, 0:4]; g4b = rt[:, 7, 4:8]; gm = rt[:, 7, 8:12]; e1 = rt[:, 7, 12:13]; e2 = rt[:, 7, 13:14]
            den = rt[:, 7, 14:15]
            cx.act(s_, ps[0][:, 0:16], AF.Sigmoid, r=["ps0"], w=[K_("r_s")])
            cx.tt(sbv, s_, brb[:, :], ALU.add, r=[K_("r_s"), "brb"], w=[K_("r_sb")])
            v4 = lambda a: a.rearrange("p (g e) -> p g e", g=4)
            cx.red(g4, v4(sbv), ALU.max, r=[K_("r_sb")], w=[K_("r_g4")])
            for g_ in range(4):
                cx.ts(tmp[:, g_ * 4:(g_ + 1) * 4], sbv[:, g_ * 4:(g_ + 1) * 4], g4[:, g_:g_ + 1], ALU.is_equal,
                      r=[K_("r_sb"), K_("r_g4")], w=[K_("r_tmp")])
            cx.stt(sb2, tmp, -1.0e9, sbv, ALU.mult, ALU.add, r=[K_("r_tmp"), K_("r_sb")], w=[K_("r_sb2")])
            cx.red(g4b, v4(sb2), ALU.max, r=[K_("r_sb2")], w=[K_("r_g4b")])
            cx.tt(g4, g4, g4b, ALU.add, r=[K_("r_g4"), K_("r_g4b")], w=[K_("r_g4")])
            cx.red(e1, g4, ALU.max, r=[K_("r_g4")], w=[K_("r_e1")])
            cx.ts(gm, g4, e1, ALU.is_equal, s2=-1.0, op1=ALU.add, r=[K_("r_g4"), K_("r_e1")], w=[K_("r_gm")])
            for g_ in range(4):
                cx.ts(tmp[:, g_ * 4:(g_ + 1) * 4], cs(cst, "ones")[:, 0:4], gm[:, g_:g_ + 1], ALU.mult,
                      r=[K_("r_gm"), "cst"], w=[K_("r_tmp")])
            cx.stt(sbm, tmp, 1.0e9, sbv, ALU.mult, ALU.add, r=[K_("r_tmp"), K_("r_sb")], w=[K_("r_sbm")])
            cx.red(e1, sbm, ALU.max, r=[K_("r_sbm")], w=[K_("r_e1")])
            cx.ts(msk, sbm, e1, ALU.is_equal, r=[K_("r_sbm"), K_("r_e1")], w=[K_("r_msk")])
            cx.stt(sb2, msk, -1.0e9, sbm, ALU.mult, ALU.add, r=[K_("r_msk"), K_("r_sbm")], w=[K_("r_sb2")])
            cx.red(e2, sb2, ALU.max, r=[K_("r_sb2")], w=[K_("r_e2")])
            cx.ts(m2, sb2, e2, ALU.is_equal, r=[K_("r_sb2"), K_("r_e2")], w=[K_("r_m2")])
            cx.tt(sel, m2, msk, ALU.add, r=[K_("r_m2"), K_("r_msk")], w=[K_("r_sel")])
            cx.mm(ps[1][:, 0:16], cs(cst, "Ltri"), sel, True, True, r=["cst", K_("r_sel")], w=["ps1"])
            cx.mm(ps[1][:, 16:32], cs(cst, "ones"), sel, True, True, r=["cst", K_("r_sel")], w=["ps1"])
            cx.tt(pos, ps[1][:, 0:16], off[:, :], ALU.add, r=["ps1", "roff"], w=[K_("r_pos")])
            cx.tt(off[:, :], ps[1][:, 16:32], off[:, :], ALU.add, r=["ps1", "roff"], w=["roff"])
            cx.ts(tmp, pos, float(C) - 0.5, ALU.is_lt, r=[K_("r_pos")], w=[K_("r_tmp")])
            cx.tt(pos, pos, cs(cst, "eoff"), ALU.add, r=[K_("r_pos"), "cst"], w=[K_("r_pos")])
            cx.stt(pos, tmp, -1.0e6, pos, ALU.mult, ALU.add, r=[K_("r_tmp"), K_("r_pos")], w=[K_("r_pos")])
            cx.ts(pos, pos, 1.0e6, ALU.add, r=[K_("r_pos")], w=[K_("r_pos")])
            cx.tt(sb2, sel, s_, ALU.mult, r=[K_("r_sel"), K_("r_s")], w=[K_("r_sb2")])
            cx.red(den, sb2, ALU.add, r=[K_("r_sb2")], w=[K_("r_den")])
            cx.recip(den, den, r=[K_("r_den")], w=[K_("r_den")])
            cx.ts(sb2, sb2, den, ALU.mult, r=[K_("r_sb2"), K_("r_den")], w=[K_("r_sb2")])
            cx.tt(sb2, sb2, tmp, ALU.mult, r=[K_("r_sb2"), K_("r_tmp")], w=[K_("r_sb2")])
            for q, mk, kk in ((0, msk, K_("r_msk")), (1, m2, K_("r_m2"))):
                cx.tt(sbm, mk, pos, ALU.mult, r=[kk, K_("r_pos")], w=[K_("r_sbm")])
                cx.red(slf[:, ti, q:q + 1], sbm, ALU.add, r=[K_("r_sbm")], w=["slf%d_%d" % (ti, q)])
                cx.tt(sbm, mk, sb2, ALU.mult, r=[kk, K_("r_sb2")], w=[K_("r_sbm")])
                cx.red(wts[:, ti, q:q + 1], sbm, ALU.add, r=[K_("r_sbm")], w=["wts%d_%d" % (ti, q)])
            cx.copy(sli[:, ti, :], slf[:, ti, :], r=["slf%d_0" % ti, "slf%d_1" % ti], w=["sli%d" % ti])
            for q in range(2):
                idx = sli[:, ti, q:q + 1]
                src = hb[b][:, :]
                cx.S.add("pool", lambda h, idx=idx, src=src: h.indirect_dma_start(
                    out=Xg[:, :], out_offset=bass.IndirectOffsetOnAxis(ap=idx, axis=0), in_=src, in_offset=None,
                    bounds_check=bc(h), oob_is_err=False), r=["sli%d" % ti, "ehb%d" % b] + ["D:XgZ%d" % i_ for i_ in range(NSLOT // 512)], w=["D:Xg%d_%d" % (ti, q)], dma=True)
            ti += 1
    dummy = cx.sb("dummy", [128, 4])
    cx.memset(dummy[:, 0:1], 0.0, w=["XgAll"], eng="pool")
    cx.S.ops[-1].deps.update({cx.S.lastw[k]: True for k in ["D:Xg%d_%d" % (t_, q) for t_ in range(NT) for q in range(2)]})
    NBLK = C // 128
    NPC = (C + 511) // 512
    PW = C // NPC
    xg = [cx.sb("xg%d" % i, [128, NBLK, D], BF16) for i in range(2)]
    xT = [cx.sb("xTe%d" % i, [128, 8, C], BF16) for i in range(2)]
    uT = cx.sb("uTe", [128, 4, C], BF16)
    s1 = [cx.sb("s1_%d" % i, [128, 512]) for i in range(2)]
    yb = [cx.sb("ybe%d" % i, [128, D]) for i in range(2)]
    yi = 0

    def load_xg(e):
        cx.dma("sp", xg[e % 2][:, :, :], Xg[e * C:(e + 1) * C, :].rearrange("(j p) d -> p j d", p=128), r=["XgAll"], w=["xg%d" % (e % 2)])
    load_xg(0)
    for e in range(NEXP):
        b = e % 2
        if e >= 2:
            load_expert(e)
        if e + 1 < NEXP:
            load_xg(e + 1)
        for j in range(NBLK):
            pbk = psb[j % 2]
            kp = "psb%d" % (j % 2)
            for kc in range(8):
                cx.tr(pbk[:, kc * 128:(kc + 1) * 128], xg[b][:, j, kc * 128:(kc + 1) * 128], ident[:, :],
                      r=["xg%d" % b, "ident"], w=[kp])
            cx.copy(xT[b][:, :, j * 128:(j + 1) * 128], pbk[:, :].rearrange("p (k t) -> p k t", k=8), r=[kp], w=["xTe%d" % b],
                    eng=("act" if j % 2 == 0 else "dve"))
        it = 0
        for fc in range(4):
            fs = slice(fc * 128, (fc + 1) * 128)
            for pc in range(NPC):
                cs_ = slice(pc * PW, (pc + 1) * PW)
                pa = ps[(it % 2) * 2]; pb = ps[(it % 2) * 2 + 1]
                ka = "ps%d" % ((it % 2) * 2); kb = "ps%d" % ((it % 2) * 2 + 1)
                for kc in range(8):
                    cx.mm(pa[:, 0:PW], W1[b][:, kc, fs], xT[b][:, kc, cs_], kc == 0, kc == 7, r=["xTe%d" % b, "W1_%d" % b], w=[ka])
                for kc in range(8):
                    cx.mm(pb[:, 0:PW], W3[b][:, kc, fs], xT[b][:, kc, cs_], kc == 0, kc == 7, r=["xTe%d" % b, "W3_%d" % b], w=[kb])
                sb_ = s1[it % 2]
                cx.act(sb_[:, 0:PW], pa[:, 0:PW], AF.Silu, r=[ka], w=["s1_%d" % (it % 2)])
                cx.tt(uT[:, fc, cs_], pb[:, 0:PW], sb_[:, 0:PW], ALU.mult, r=[kb, "s1_%d" % (it % 2)], w=["uTe"])
                it += 1
        for j in range(NBLK):
            y = yb[yi % 2]
            ky = "ybe%d" % (yi % 2)
            yi += 1
            for nh in range(2):
                po = ps[4 + nh]
                for fc in range(4):
                    cx.mm(po[:, :], uT[:, fc, j * 128:(j + 1) * 128], W2[b][:, fc, nh * 512:(nh + 1) * 512], fc == 0, fc == 3,
                          r=["uTe", "W2_%d" % b], w=["ps%d" % (4 + nh)])
                cx.copy(y[:, nh * 512:(nh + 1) * 512], po[:, :], r=["ps%d" % (4 + nh)], w=[ky], eng=("act" if nh == 0 else "dve"))
            cx.dma("sp", Yg[e * C + j * 128:e * C + (j + 1) * 128, :], y[:, :], r=[ky], w=["D:Yg%d_%d" % (e, j)])
    if final:
        gfb = cx.sb("gfb", [128, D])
        cx.dma("sp", gfb[:, :], dr["g_final"][0, :].partition_broadcast(128), w=["gfb"])
    cx.memset(dummy[:, 1:2], 0.0, w=["YgAll"], eng="pool")
    cx.S.ops[-1].deps.update({cx.S.lastw[k]: True for k in ["D:Yg%d_%d" % (e_, j_) for e_ in range(NEXP) for j_ in range(C // 128)]})
    yg = [[cx.sb("yg%d_%d" % (i, q), [128, D]) for q in range(2)] for i in range(2)]
    for i in range(2):
        for q in range(2):
            cx.memset(yg[i][q][:, :], 0.0, w=["yg%d_%d" % (i, q)], eng="pool")
    cur = None
    for ti, (tag, seg, xmid, xout, rows) in enumerate(tiles):
        if cur != seg:
            cx.dma("sp", g2b[:, :], dr["modv"][l, seg, 5, :].partition_broadcast(128), r=["D:modv%d" % l], w=["g2b2"])
            cur = seg
        b = ti % 2
        kx = "ext%d" % b
        t1 = t1s[b]
        kt1 = "et1_%d" % b
        cx.dma("sp", xt[b][:, :], xmid[rows, :], r=["D:xmid_" + tag], w=[kx])
        for q in range(2):
            dst = yg[b][q][:, :]
            idx = sli[:, ti, q:q + 1]
            cx.S.add("pool", lambda h, idx=idx, dst=dst: h.indirect_dma_start(
                out=dst, out_offset=None, in_=Yg[:, :], in_offset=bass.IndirectOffsetOnAxis(ap=idx, axis=0),
                bounds_check=bc(h), oob_is_err=False), r=["sli%d" % ti, "YgAll"], w=["yg%d_%d" % (b, q)], dma=True)
        cx.ts(t1[:, :], yg[b][0][:, :], wts[:, ti, 0:1], ALU.mult, r=["yg%d_0" % b, "wts%d_0" % ti], w=[kt1])
        cx.stt(t1[:, :], yg[b][1][:, :], wts[:, ti, 1:2], t1[:, :], ALU.mult, ALU.add, r=["yg%d_1" % b, "wts%d_1" % ti, kt1], w=[kt1])
        cx.tt(t1[:, :], t1[:, :], g2b[:, :], ALU.mult, r=[kt1, "g2b2"], w=[kt1], eng="pool")
        cx.tt(xt[b][:, :], xt[b][:, :], t1[:, :], ALU.add, r=[kx, kt1], w=[kx])
        if final:
            cx.act(junk[:, :], xt[b][:, :], AF.Square, accum_out=ssq[:, b:b + 1], r=[kx], w=["ejunk", "essq%d" % b])
            cx.act(rstd[:, b:b + 1], ssq[:, b:b + 1], AF.Sqrt, scale=1.0 / D, bias=EPS, r=["essq%d" % b], w=["ers%d" % b])
            cx.recip(rstd[:, b:b + 1], rstd[:, b:b + 1], r=["ers%d" % b], w=["ers%d" % b])
            cx.stt(xt[b][:, :], xt[b][:, :], rstd[:, b:b + 1], gfb[:, :], ALU.mult, ALU.mult, r=[kx, "ers%d" % b, "gfb"], w=[kx])
        cx.dma("sp", xout[rows, :], xt[b][:, :], r=[kx], w=["D:xout_" + tag])
    cx.end()


def make_expo(core):
    BIG = 1.0e7
    e = np.full((2, 9), BIG, np.float32)
    for c2 in range(NCORE):
        if c2 < core:
            e[0, c2] = TL * (core - 1 - c2)
        if c2 > core:
            e[1, c2] = TL * (c2 - core - 1)
    e[0, 8] = TL * core
    e[1, 8] = TL * (NCORE - 1 - core)
    return np.broadcast_to(e[None], (128, 2, 9)).copy()


def make_expo(core):
    BIG = 1.0e7
    e = np.full((2, 9), BIG, np.float32)
    for c2 in range(NCORE):
        if c2 < core:
            e[0, c2] = TL * (core - 1 - c2)
        if c2 > core:
            e[1, c2] = TL * (c2 - core - 1)
    e[0, 8] = TL * core
    e[1, 8] = TL * (NCORE - 1 - core)
    return np.broadcast_to(e[None], (128, 2, 9)).copy()


def make_sel(core):
    s = np.zeros((128, 2, NCORE), np.float32)
    if core > 0:
        s[:, 0, core - 1] = 1.0
    if core < NCORE - 1:
        s[:, 1, core + 1] = 1.0
    return s


def phase_exchange(cx, l, dr):
    cx.begin(nf=0, nb=0)
    hin = dr["hin"]
    for a, (src, c2) in enumerate(((dr["uT_l"], 0), (dr["uT_l"], 1), (dr["tT_l"], 0), (dr["tT_l"], 1))):
        cx.dma("sp", hin[a * 128:(a + 1) * 128, 0:16], src[c2, :, 0:16], r=["D:uT_l", "D:tT_l"], w=["D:hin"])
        cx.dma("sp", hin[a * 128:(a + 1) * 128, 16:32], src[c2, :, TL - 16:TL], r=["D:uT_l", "D:tT_l"], w=["D:hin"])
    grp = [list(range(NCORE))]

    def cc(src, dst, rk, wk):
        cx.S.add("pool", lambda h: h.collective_compute("AllGather", ALU.bypass, replica_groups=grp, ins=[src], outs=[dst]),
                 r=rk, w=wk, cc=True)
    cc(dr["kT_l"].rearrange("c p t -> (c p) t").opt(), dr["gk"].opt(), ["D:rope_l12", "D:rope_l13"], ["D:gk"])
    cc(dr["V_l"].opt(), dr["gv"].opt(), ["D:V_l"], ["D:gv"])
    cc(dr["Tst_l"].rearrange("a p e -> (a p) e").opt(), dr["gt"].opt(), ["D:Tst_l"], ["D:gt"])
    cc(hin.opt(), dr["hg"].opt(), ["D:hin"], ["D:hg"])
    cx.end()


A_OUT = (("hT", lambda T: [128, 8, T], BF16), ("uT", lambda T: [2, 128, T], F32), ("tT", lambda T: [2, 128, T], F32),
         ("bgT", lambda T: [2, 128, T], BF16), ("qT", lambda T: [2, 128, T], BF16), ("kT", lambda T: [2, 128, T], BF16),
         ("rqT", lambda T: [128, T], BF16), ("rkT", lambda T: [128, T], BF16), ("V", lambda T: [T, 256], BF16),
         ("rv", lambda T: [T, 256], BF16), ("rg", lambda T: [T, 256], BF16), ("Tst", lambda T: [2, 128, 256], F32))
SEGT = (("l", TL), ("c", TC))
NKALL = (SEQ + TC) // 128
EXT_IN = (("c", [1, D]), ("c_ctx", [1, D]), ("w_mod", [2, D, 6 * D]), ("b_mod", [2, 6 * D]), ("g_norm1", [2, D]), ("g_norm2", [2, D]),
          ("w_in", [2, D, INC]), ("conv_a_w", [2, 31, 256]), ("conv_a_b", [2, 256]), ("conv_a_g", [2, 256]),
          ("conv_a_beta", [2, 256]), ("conv_b_w", [2, 3, 256]), ("lam_q1", [2, 32]), ("lam_k1", [2, 32]), ("lam_q2", [2, 32]),
          ("lam_k2", [2, 32]), ("diff_g", [2, 64]), ("ret_ld_f", [2, 4]), ("ret_ld_b", [2, 4]), ("w_gate", [2, D, 4096]),
          ("b_gate", [2, 4096]), ("w_branch", [2, 4, 256, D]), ("w_o", [2, D, D]), ("w_router", [D, 16]), ("b_router", [1, 16]),
          ("w1_e", [2, NEXP, D, DFF]), ("w3_e", [2, NEXP, D, DFF]), ("w2_e", [2, NEXP, DFF, D]), ("g_final", [1, D]))


SPARSE_MOE = True


def moe_phase(cx, l, dr, segs, final, wl=None):
    if SPARSE_MOE:
        return phase_moe_sparse(cx, l, dr, segs, final, wl=wl)
    return phase_moe(cx, l, dr, segs, final, wl=wl)


def lam_init_of(l):
    return 0.8 - 0.6 * math.exp(-0.3 * l)


class Launch:
    def __init__(self):
        self.nc = bass.Bass("TRN2", target_bir_lowering=False)
        self.dr = {}
        self.ins = []
        self.outs = []

    def t(self, name, shape, dt=F32, kind=None):
        if kind is None:
            self.dr[name] = self.nc.dram_tensor(name, list(shape), dt).ap()
        else:
            self.dr[name] = self.nc.dram_tensor(name, list(shape), dt, kind=kind).ap()
        if kind == "ExternalInput":
            self.ins.append(name)
        elif kind == "ExternalOutput":
            self.outs.append(name)


def build_fused():
    L = Launch()
    L.t("cst", [128, CSTW], F32, "ExternalInput")
    L.t("ident", [128, 128], BF16, "ExternalInput")
    for n_, s_ in EXT_IN:
        L.t(n_, s_, F32, "ExternalInput")
    for n_, s_ in (("cosT", [128, TL]), ("sinT", [128, TL]), ("x_l", [TL, D]), ("x_c", [TC, D]), ("expo", [128, 2, 9]),
                   ("sel", [128, 2, NCORE])):
        L.t(n_, s_, F32, "ExternalInput")
    L.t("out", [TL, D], F32, "ExternalOutput")
    L.t("modv", [2, 2, 6, D])
    L.t("x1_l", [TL, D])
    L.t("x1_c", [TC, D])
    L.t("Xg", [NSLOT, D], BF16)
    L.t("Yg", [NSLOT, D], F32)
    drl = []
    for l in range(DEPTH):
        d_ = dict(L.dr)
        for tag, T in SEGT:
            for nm, shp, dt in A_OUT:
                L.t("%s_%s%d" % (nm, tag, l), shp(T), dt)
                d_[nm + "_" + tag] = L.dr["%s_%s%d" % (nm, tag, l)]
            L.t("ysT_%s%d" % (tag, l), [8, 128, T], BF16)
            L.t("xmid_%s%d" % (tag, l), [T, D])
            d_["ysT_" + tag] = L.dr["ysT_%s%d" % (tag, l)]
            d_["xmid_" + tag] = L.dr["xmid_%s%d" % (tag, l)]
        for nm, shp, dt in (("gk", [NCORE * 256, TL], BF16), ("gv", [NCORE * TL, 256], BF16), ("gt", [NCORE * 256, 256], F32),
                            ("hg", [NCORE * 512, 32], F32), ("hin", [512, 32], F32)):
            L.t("%s%d" % (nm, l), shp, dt)
            d_[nm] = L.dr["%s%d" % (nm, l)]
        drl.append(d_)
    for d_ in drl:
        for k in ("modv", "x1_l", "x1_c", "Xg", "Yg"):
            d_[k] = L.dr[k]
    with ExitStack() as st:
        S = Sched(L.nc, st)
        cx = Ctx(L.nc, S)
        phase_mods(cx, 0, drl[0])
        phase_mods(cx, 1, drl[0])
        d0 = drl[0]
        phase_a(cx, 0, d0, [("l", TL, L.dr["x_l"]), ("c", TC, L.dr["x_c"])])
        phase_exchange(cx, 0, d0)
        phase_conv(cx, 0, d0, [("l", TL), ("c", TC)])
        phase_attn(cx, 0, d0, [("l", TL, NKALL), ("c", TC, TC // 128)], lam_init_of(0))
        phase_ret(cx, 0, d0, [("l", TL), ("c", TC)])
        phase_merge(cx, 0, d0, [("l", TL, L.dr["x_l"], d0["xmid_l"]), ("c", TC, L.dr["x_c"], d0["xmid_c"])])
        moe_phase(cx, 0, d0, [("l", TL, d0["xmid_l"], L.dr["x1_l"]), ("c", TC, d0["xmid_c"], L.dr["x1_c"])], False)
        d1 = drl[1]
        phase_a(cx, 1, d1, [("l", TL, L.dr["x1_l"]), ("c", TC, L.dr["x1_c"])])
        phase_exchange(cx, 1, d1)
        phase_conv(cx, 1, d1, [("l", TL)])
        phase_attn(cx, 1, d1, [("l", TL, NKALL)], lam_init_of(1))
        phase_ret(cx, 1, d1, [("l", TL)])
        phase_merge(cx, 1, d1, [("l", TL, L.dr["x1_l"], d1["xmid_l"])])
        moe_phase(cx, 1, d1, [("l", TL, d1["xmid_l"], L.dr["out"])], True)
    return L


def kernel_fused(**inp):
    f32 = lambda a: np.ascontiguousarray(np.asarray(a, dtype=np.float32))
    x = f32(inp["x"])[0]
    ctx = f32(inp["ctx"])[0]
    base = dict(cst=make_cst(), ident=np.eye(128, dtype=np.float32).astype(NPBF), x_c=ctx)
    for n_, s_ in EXT_IN:
        base[n_] = f32(inp[n_]).reshape(s_)
    L = build_fused()
    maps = []
    for c in range(NCORE):
        m = dict(base)
        cosT, sinT = rope_tables(c)
        m.update(cosT=cosT, sinT=sinT, x_l=x[c * TL:(c + 1) * TL], expo=make_expo(c), sel=make_sel(c))
        maps.append({k: m[k] for k in L.ins})
    res = run_bass_kernel_spmd(L.nc, maps, core_ids=list(range(NCORE)))
    out = np.concatenate([np.asarray(res.results[c]["out"]) for c in range(NCORE)], axis=0)
    return out.reshape(1, SEQ, D).astype(np.float32)


WSLICE = ("w_in", "w_gate", "w_branch", "w_o", "w1_e", "w3_e", "w2_e")
GATH = (("gk", [NCORE * 256, TL], BF16), ("gv", [NCORE * TL, 256], BF16), ("gt", [NCORE * 256, 256], F32),
        ("hg", [NCORE * 512, 32], F32))


def build_stage(stage):
    L = Launch()
    L.t("cst", [128, CSTW], F32, "ExternalInput")
    L.t("ident", [128, 128], BF16, "ExternalInput")
    for n_, s_ in EXT_IN:
        if stage > 1 and n_ in ("w_mod", "b_mod", "c", "c_ctx"):
            continue
        if stage == 1 and n_ in ("w_gate", "w_branch", "w_o", "w1_e", "w3_e", "w2_e"):
            continue
        if stage == 3 and n_ == "w_in":
            continue
        shp = [1] + list(s_[1:]) if n_ in WSLICE else s_
        L.t(n_, shp, F32, "ExternalInput")
    for n_, s_ in (("cosT", [128, TL]), ("sinT", [128, TL]), ("expo", [128, 2, 9]), ("sel", [128, 2, NCORE])):
        L.t(n_, s_, F32, "ExternalInput")
    io = "ExternalInput"
    if stage == 1:
        L.t("x_l", [TL, D], F32, io)
        L.t("x_c", [TC, D], F32, io)
        L.t("modv", [2, 2, 6, D], F32, "ExternalOutput")
    else:
        L.t("modv", [2, 2, 6, D], F32, io)

    def a_tensors(prefix, kind, tags):
        d_ = {}
        for tag, T in SEGT:
            if tag not in tags:
                continue
            for nm, shp, dt in A_OUT:
                L.t(prefix + nm + "_" + tag, shp(T), dt, kind)
                d_[nm + "_" + tag] = L.dr[prefix + nm + "_" + tag]
        return d_
    with ExitStack() as st:
        S = Sched(L.nc, st)
        cx = Ctx(L.nc, S)
        if stage == 1:
            dA = dict(L.dr)
            dA.update(a_tensors("", "ExternalOutput", ("l", "c")))
            phase_mods(cx, 0, dA)
            phase_mods(cx, 1, dA)
            phase_a(cx, 0, dA, [("l", TL, L.dr["x_l"]), ("c", TC, L.dr["x_c"])], wl=0)
        else:
            l = stage - 2
            tags = ("l", "c")
            for nm, shp, dt in GATH:
                L.t(nm, shp, dt, io)
            L.t("Xg", [NSLOT, D], BF16)
            L.t("Yg", [NSLOT, D], F32)
            dB = dict(L.dr)
            dB.update(a_tensors("b_", io, tags))
            segs = [("l", TL), ("c", TC)] if l == 0 else [("l", TL)]
            for tag, T in segs:
                L.t("ysT_" + tag, [8, 128, T], BF16)
                L.t("xmid_" + tag, [T, D])
                dB["ysT_" + tag] = L.dr["ysT_" + tag]
                dB["xmid_" + tag] = L.dr["xmid_" + tag]
            if l == 0:
                L.t("x_l", [TL, D], F32, io)
                L.t("x_c", [TC, D], F32, io)
                L.t("x1_l", [TL, D], F32, "ExternalOutput")
                L.t("x1_c", [TC, D])
                xin_l, xin_c, xo_l, xo_c = L.dr["x_l"], L.dr["x_c"], L.dr["x1_l"], L.dr["x1_c"]
            else:
                L.t("x1_l", [TL, D], F32, io)
                L.t("out", [TL, D], F32, "ExternalOutput")
                xin_l, xo_l = L.dr["x1_l"], L.dr["out"]
            phase_conv(cx, l, dB, segs)
            phase_attn(cx, l, dB, [("l", TL, NKALL)] + ([("c", TC, TC // 128)] if l == 0 else []), lam_init_of(l))
            phase_ret(cx, l, dB, segs)
            if l == 0:
                phase_merge(cx, l, dB, [("l", TL, xin_l, dB["xmid_l"]), ("c", TC, xin_c, dB["xmid_c"])], wl=0)
                moe_phase(cx, l, dB, [("l", TL, dB["xmid_l"], xo_l), ("c", TC, dB["xmid_c"], xo_c)], False, wl=0)
                dA = dict(L.dr)
                dA.update(a_tensors("", "ExternalOutput", ("l", "c")))
                phase_a(cx, 1, dA, [("l", TL, xo_l), ("c", TC, xo_c)], wl=0)
            else:
                phase_merge(cx, l, dB, [("l", TL, xin_l, dB["xmid_l"])], wl=0)
                moe_phase(cx, l, dB, [("l", TL, dB["xmid_l"], xo_l)], True, wl=0)
    return L


def host_gather(oA):
    gk = np.concatenate([np.asarray(o["kT_l"]).reshape(256, TL) for o in oA], axis=0)
    gv = np.concatenate([np.asarray(o["V_l"]) for o in oA], axis=0)
    gt = np.concatenate([np.asarray(o["Tst_l"]).reshape(256, 256) for o in oA], axis=0)
    hs = []
    for o in oA:
        u = np.asarray(o["uT_l"])
        t = np.asarray(o["tT_l"])
        h = np.concatenate([np.concatenate([a[c2][:, 0:16], a[c2][:, TL - 16:TL]], axis=1) for a in (u, t) for c2 in range(2)], axis=0)
        hs.append(h)
    hg = np.concatenate(hs, axis=0).astype(np.float32)
    return dict(gk=gk, gv=gv, gt=gt, hg=hg)


def kernel_unfused(**inp):
    f32 = lambda a: np.ascontiguousarray(np.asarray(a, dtype=np.float32))
    x = f32(inp["x"])[0]
    ctx = f32(inp["ctx"])[0]
    ropes = [rope_tables(c) for c in range(NCORE)]
    full = {n_: f32(inp[n_]).reshape(s_) for n_, s_ in EXT_IN}
    cst = make_cst()
    ident = np.eye(128, dtype=np.float32).astype(NPBF)

    def run(L, extra, wl):
        maps = []
        for c in range(NCORE):
            m = dict(cst=cst, ident=ident, cosT=ropes[c][0], sinT=ropes[c][1], expo=make_expo(c), sel=make_sel(c),
                     x_l=x[c * TL:(c + 1) * TL], x_c=ctx)
            for k, v in full.items():
                m[k] = v[wl[k]:wl[k] + 1] if k in WSLICE else v
            m.update(extra[c])
            maps.append({k: m[k] for k in L.ins})
        res = run_bass_kernel_spmd(L.nc, maps, core_ids=list(range(NCORE)))
        return [{k: np.asarray(r[k]) for k in L.outs} for r in res.results]

    o1 = run(build_stage(1), [dict() for _ in range(NCORE)], dict.fromkeys(WSLICE, 0))
    modv = o1[0]["modv"]

    def b_extra(oA):
        g = host_gather(oA)
        ex = []
        for c in range(NCORE):
            e = dict(g)
            e["modv"] = modv
            for k, v in oA[c].items():
                if k != "modv" and k != "x1_l":
                    e["b_" + k] = v
            ex.append(e)
        return ex
    wl2 = dict.fromkeys(WSLICE, 0)
    wl2["w_in"] = 1
    o2 = run(build_stage(2), b_extra(o1), wl2)
    ex3 = b_extra(o2)
    for c in range(NCORE):
        ex3[c]["x1_l"] = o2[c]["x1_l"]
    o3 = run(build_stage(3), ex3, dict.fromkeys(WSLICE, 1))
    out = np.concatenate([o3[c]["out"] for c in range(NCORE)], axis=0)
    return out.reshape(1, SEQ, D).astype(np.float32)


FUSED = False


def kernel(**inp):
    return kernel_fused(**inp) if FUSED else kernel_unfused(**inp)
```

```python
import math
from contextlib import ExitStack
import numpy as np
import ml_dtypes
import concourse.bass as bass
import concourse.mybir as mybir
from concourse.bass_utils import run_bass_kernel_spmd

F32 = mybir.dt.float32
BF16 = mybir.dt.bfloat16
AF = mybir.ActivationFunctionType
ALU = mybir.AluOpType
AX = mybir.AxisListType
NPBF = ml_dtypes.bfloat16

NCORE = 8
D = 1024
SEQ = 16384
TL = SEQ // NCORE
TC = 256
DEPTH = 2
INC = 2816
EPS = 1e-6
NEXP = 16
DFF = 512
QSCALE = 32 ** -0.5
ATT_ROW = False
MOE_CAP = 768
NSLOT = NEXP * MOE_CAP


class Op:
    __slots__ = ("id", "eng", "fn", "deps", "dma", "n", "signal", "val", "cc")


class Sched:
    ENGS = (("sp", "sync"), ("act", "scalar"), ("dve", "vector"), ("pool", "gpsimd"), ("pe", "tensor"))

    def __init__(self, nc, stack):
        self.nc = nc
        self.ops = []
        self.phase_start = 0
        self.lastw = {}
        self.readers = {}
        self.K = dict(sp=12, pool=8, act=4)
        self.dma_list = {e: [] for e in self.K}
        self.sems = {e: stack.enter_context(nc.semaphore("sm_" + e)) for e in ("pe", "act", "dve", "pool")}
        self.dsems = {e: [stack.enter_context(nc.semaphore("sd_%s%d" % (e, i))) for i in range(k)]
                      for e, k in self.K.items()}
        self.ccsems = [stack.enter_context(nc.semaphore("sc_%d" % i)) for i in range(12)]
        self.ncc = 0
        self.cc_list = []
        self.cnt = {e: 0 for e in self.sems}
        self.waited = {e: {} for e, _ in self.ENGS}

    def add(self, eng, fn, r=(), w=(), dma=False, cc=False):
        i = len(self.ops)
        deps = {}
        for k in r:
            j = self.lastw.get(k)
            if j is not None:
                deps[j] = True
        for k in w:
            j = self.lastw.get(k)
            if j is not None and j not in deps:
                deps[j] = False
            rd = self.readers.get(k)
            if rd:
                for j in rd.values():
                    if isinstance(j, list):
                        for jj in j:
                            deps.setdefault(jj, False)
                    else:
                        deps.setdefault(j, False)
        n = None
        if dma:
            lst = self.dma_list[eng]
            n = len(lst)
            if n >= self.K[eng]:
                deps.setdefault(lst[n - self.K[eng]], False)
            lst.append(i)
        op = Op()
        op.id, op.eng, op.fn, op.deps, op.dma, op.n, op.signal, op.val = i, eng, fn, deps, dma, n, False, 0
        op.cc = None
        if cc:
            op.dma = True
            op.cc = self.ncc
            self.ncc += 1
            self.cc_list.append(i)
            dma = True
        self.ops.append(op)
        for k in r:
            rd = self.readers.setdefault(k, {})
            if dma:
                rd.setdefault("dma", []).append(i)
            else:
                rd[eng] = i
        for k in w:
            self.lastw[k] = i
            self.readers[k] = {}
        return i

    def _needed(self, op, dj, raw):
        if dj.dma:
            return True
        if dj.eng == op.eng:
            if op.dma:
                return True
            return raw and op.eng != "pe"
        return True

    def end_phase(self, name=None):
        nc = self.nc
        ps = self.phase_start
        for e in self.K:
            lst = [i for i in self.dma_list[e][-self.K[e]:] if i >= ps]
            if e == "pool":
                lst = lst + [i for i in self.cc_list if i >= ps]
            if lst:
                i = self.add(e, lambda h: h.nop())
                for j in lst:
                    self.ops[i].deps[j] = True
        ops = self.ops
        for op in ops[ps:]:
            latest = {}
            for j, raw in op.deps.items():
                if j < ps:
                    continue
                dj = ops[j]
                if not dj.dma and self._needed(op, dj, raw):
                    if latest.get(dj.eng, -1) < j:
                        latest[dj.eng] = j
            for j in latest.values():
                ops[j].signal = True
        for op in ops[ps:]:
            if op.signal and not op.dma:
                self.cnt[op.eng] += 1
                op.val = self.cnt[op.eng]
        with nc.Block() as block:
            for e, bn in self.ENGS:
                ops_e = [op for op in ops[ps:] if op.eng == e]
                if not ops_e:
                    continue

                def body(h, ops_e=ops_e, e=e):
                    self._emit(e, h, ops_e, ps)
                getattr(block, bn)(body)
        self.phase_start = len(ops)
        self.lastw = {k: v for k, v in self.lastw.items() if isinstance(k, str) and k.startswith("D:")}
        self.readers = {k: {} for k in self.lastw}
        for op in ops[:self.phase_start]:
            op.fn = None

    def _emit(self, e, h, ops_e, ps):
        ops = self.ops
        waited = self.waited[e]
        for op in ops_e:
            want = {}
            for j, raw in op.deps.items():
                if j < ps:
                    continue
                dj = ops[j]
                if not self._needed(op, dj, raw):
                    continue
                if dj.cc is not None:
                    key = ("cc", dj.cc)
                    sem = self.ccsems[dj.cc]
                    val = 1
                elif dj.dma:
                    K = self.K[dj.eng]
                    key = (dj.eng, dj.n % K)
                    sem = self.dsems[dj.eng][dj.n % K]
                    val = 16 * (dj.n // K + 1)
                else:
                    key = dj.eng
                    sem = self.sems[dj.eng]
                    if key in want and want[key][2] > j:
                        continue
                    want[key] = (sem, dj.val, j)
                    continue
                if key not in want or want[key][1] < val:
                    want[key] = (sem, val, j)
            for key, (sem, val, _j) in want.items():
                if waited.get(key, 0) >= val:
                    continue
                h.wait_ge(sem, val)
                waited[key] = val
            inst = op.fn(h)
            if op.cc is not None:
                inst.then_inc(self.ccsems[op.cc])
            elif op.dma:
                inst.then_inc(self.dsems[e][op.n % self.K[e]], 16)
            elif op.signal:
                inst.then_inc(self.sems[e], 1)


class Ctx:
    def __init__(self, nc, S):
        self.nc = nc
        self.S = S
        self.stack = None
        self.uid = 0

    def psum(self, name, shape, dt=F32):
        self.uid += 1
        return self.stack.enter_context(self.nc.psum_tensor("%s_%d" % (name, self.uid), list(shape), dt))

    def begin(self, nf=8, nb=0):
        self.stack = ExitStack()
        self.ps = [self.stack.enter_context(self.nc.psum_tensor("ps%d_%d" % (i, self.uid), [128, 512], F32))
                   for i in range(nf)]
        self.psb = [self.stack.enter_context(self.nc.psum_tensor("psb%d_%d" % (i, self.uid), [128, 1024], BF16))
                    for i in range(nb)]
        self.uid += 1

    def end(self):
        self.S.end_phase()
        self.stack.close()
        self.stack = None

    def sb(self, name, shape, dt=F32):
        self.uid += 1
        return self.stack.enter_context(self.nc.sbuf_tensor("%s_%d" % (name, self.uid), list(shape), dt))

    def dma(self, eng, out, in_, r=(), w=(), **kw):
        return self.S.add(eng, lambda h: h.dma_start(out=out, in_=in_, **kw), r=r, w=w, dma=True)

    def mm(self, out, lhsT, rhs, start, stop, r=(), w=(), **kw):
        return self.S.add("pe", lambda h: h.matmul(out, lhsT, rhs, start=start, stop=stop, **kw), r=r, w=w)

    def tr(self, out, in_, ident, r=(), w=()):
        return self.S.add("pe", lambda h: h.transpose(out, in_, ident), r=r, w=w)

    def act(self, out, in_, func, r=(), w=(), eng="act", **kw):
        return self.S.add(eng, lambda h: h.activation(out=out, in_=in_, func=func, **kw), r=r, w=w)

    def tt(self, out, in0, in1, op, r=(), w=(), eng="dve"):
        return self.S.add(eng, lambda h: h.tensor_tensor(out=out, in0=in0, in1=in1, op=op), r=r, w=w)

    def ts(self, out, in0, s1, op0, s2=None, op1=None, r=(), w=(), eng="dve", **kw):
        if op1 is None:
            return self.S.add(eng, lambda h: h.tensor_scalar(out=out, in0=in0, scalar1=s1, scalar2=None, op0=op0, **kw),
                              r=r, w=w)
        return self.S.add(eng, lambda h: h.tensor_scalar(out=out, in0=in0, scalar1=s1, scalar2=s2, op0=op0, op1=op1, **kw),
                          r=r, w=w)

    def stt(self, out, in0, scalar, in1, op0, op1, r=(), w=()):
        return self.S.add("dve", lambda h: h.scalar_tensor_tensor(out=out, in0=in0, scalar=scalar, in1=in1,
                                                                    op0=op0, op1=op1), r=r, w=w)

    def copy(self, out, in_, r=(), w=(), eng="dve"):
        if eng == "act":
            return self.S.add("act", lambda h: h.activation(out=out, in_=in_, func=AF.Copy), r=r, w=w)
        return self.S.add(eng, lambda h: h.tensor_copy(out=out, in_=in_), r=r, w=w)

    def memset(self, ap, val, w=(), eng="dve"):
        return self.S.add(eng, lambda h: h.memset(ap, val), w=w)

    def red(self, out, in_, op, r=(), w=(), axis=None):
        ax = AX.X if axis is None else axis
        return self.S.add("dve", lambda h: h.tensor_reduce(out=out, in_=in_, axis=ax, op=op), r=r, w=w)

    def recip(self, out, in_, r=(), w=()):
        return self.S.add("dve", lambda h: h.reciprocal(out=out, in_=in_), r=r, w=w)


def phase_mods(cx, l, dr):
    cx.begin()
    cv = cx.sb("cv", [128, 8, 2])
    sc = cx.sb("sc", [128, 8, 2])
    acc = cx.sb("macc", [2, 6144])
    bm = cx.sb("mbm", [2, 6144])
    g1b = cx.sb("g1b", [2, 1024])
    g2b = cx.sb("g2b", [2, 1024])
    mv = cx.sb("mv", [2, 6, 1024])
    wm = [cx.sb("wm%d" % i, [128, 6144]) for i in range(2)]
    for s, src in enumerate((dr["c"], dr["c_ctx"])):
        for kc in range(8):
            cx.dma("sp", cv[:, kc, s:s + 1], src[0:1, kc * 128:(kc + 1) * 128].rearrange("o p -> p o"), w=["cv"])
    cx.dma("sp", bm[:, :], dr["b_mod"][l, :].partition_broadcast(2), w=["bm"])
    cx.dma("sp", g1b[:, :], dr["g_norm1"][l, :].partition_broadcast(2), w=["g1b"])
    cx.dma("sp", g2b[:, :], dr["g_norm2"][l, :].partition_broadcast(2), w=["g2b"])
    cx.act(sc[:, :, :], cv[:, :, :], AF.Silu, r=["cv"], w=["sc"])
    for kc in range(8):
        b = kc % 2
        cx.dma("sp", wm[b][:, :], dr["w_mod"][l, kc * 128:(kc + 1) * 128, :], w=["wm%d" % b])
        for n in range(12):
            p = cx.ps[n % 4]
            cx.mm(p[0:2, :], sc[:, kc, :], wm[b][:, n * 512:(n + 1) * 512], True, True,
                  r=["sc", "wm%d" % b], w=["ps%d" % (n % 4)])
            a = acc[:, n * 512:(n + 1) * 512]
            if kc == 0:
                cx.tt(a, p[0:2, :], bm[:, n * 512:(n + 1) * 512], ALU.add, r=["ps%d" % (n % 4), "bm"], w=["macc%d" % n])
            else:
                cx.tt(a, p[0:2, :], a, ALU.add, r=["ps%d" % (n % 4), "macc%d" % n], w=["macc%d" % n])
    allacc = ["macc%d" % n for n in range(12)]
    cx.stt(mv[:, 0, :], acc[:, 1024:2048], 1.0, g1b[:, :], ALU.add, ALU.mult, r=allacc + ["g1b"], w=["mv0"])
    cx.copy(mv[:, 1, :], acc[:, 0:1024], r=allacc, w=["mv1"])
    cx.copy(mv[:, 2, :], acc[:, 2048:3072], r=allacc, w=["mv2"])
    cx.stt(mv[:, 3, :], acc[:, 4096:5120], 1.0, g2b[:, :], ALU.add, ALU.mult, r=allacc + ["g2b"], w=["mv3"])
    cx.copy(mv[:, 4, :], acc[:, 3072:4096], r=allacc, w=["mv4"])
    cx.copy(mv[:, 5, :], acc[:, 5120:6144], r=allacc, w=["mv5"])
    cx.dma("sp", dr["modv"][l, :, :, :], mv[:, :, :], r=["mv%d" % i for i in range(6)], w=["D:modv%d" % l])
    cx.end()


CST = {}
_off = 0
for _n, _w in (("c127mj", 1), ("cj", 1), ("ef_l", 16), ("eb_l", 16), ("ef_c", 2), ("eb_c", 2), ("ip1", 128),
               ("m128i", 128), ("D1", 128), ("D2", 128), ("U", 128), ("Lo", 128), ("I2", 128), ("ones", 128), ("I1", 128), ("hm", 4), ("bm8", 8), ("Ltri", 128), ("eoff", 16)):
    CST[_n] = (_off, _off + _w)
    _off += _w
CSTW = _off


def make_cst():
    c = np.zeros((128, CSTW), np.float32)
    p = np.arange(128, dtype=np.float32)
    i = np.arange(128, dtype=np.float32)

    def put(n, v):
        a, b = CST[n]
        c[:, a:b] = v
    put("c127mj", (127 - p)[:, None])
    put("cj", p[:, None])
    put("ef_l", (128.0 * (15 - np.arange(16)))[None, :])
    put("eb_l", (128.0 * np.arange(16))[None, :])
    put("ef_c", (128.0 * (1 - np.arange(2)))[None, :])
    put("eb_c", (128.0 * np.arange(2))[None, :])
    put("ip1", (i + 1)[None, :])
    put("m128i", (128 - i)[None, :])
    dd = i[None, :] - p[:, None]
    put("D1", np.maximum(dd, 0))
    put("D2", np.maximum(-dd, 0))
    put("U", (dd > 0).astype(np.float32))
    put("Lo", (dd < 0).astype(np.float32))
    put("I2", 2.0 * (dd == 0))
    put("ones", 1.0)
    put("I1", (dd == 0).astype(np.float32))
    put("hm", (p[:, None] // 32 == np.arange(4)[None, :]).astype(np.float32))
    put("bm8", np.tile((p[:, None] // 32 == np.arange(4)[None, :]).astype(np.float32), (1, 2)))
    put("Ltri", (dd > 0).astype(np.float32))
    put("eoff", (float(MOE_CAP) * np.arange(16))[None, :])
    return c


def rope_tables(core):
    t = np.arange(core * TL, (core + 1) * TL)
    row = (t // 64).astype(np.float32)
    col = (t % 64).astype(np.float32)
    inv = (np.float32(10000.0) ** (-np.arange(8, dtype=np.float32) / np.float32(8))).astype(np.float32)
    ang = np.concatenate([row[:, None] * inv[None, :], col[:, None] * inv[None, :]], axis=1).astype(np.float32)
    cos = np.cos(ang).astype(np.float32).T
    sin = np.sin(ang).astype(np.float32).T
    return np.tile(cos, (8, 1)).copy(), np.tile(sin, (8, 1)).copy()


def cs(cst, name):
    a, b = CST[name]
    return cst[:, a:b]


def phase_a(cx, l, dr, segs, wl=None):
    wl = l if wl is None else wl
    cx.begin(nf=6, nb=2)
    ps, psb = cx.ps, cx.psb
    W = cx.sb("W", [128, 8, INC], BF16)
    WR = cx.sb("WR", [128, 8, 768], BF16)
    cst = cx.sb("cst", [128, CSTW])
    ident = cx.sb("ident", [128, 128], BF16)
    cx.dma("sp", cst[:, :], dr["cst"][:, :], w=["cst"])
    cx.dma("sp", ident[:, :], dr["ident"][:, :], w=["ident"])
    for kc in range(8):
        cx.dma("pool", W[:, kc, :], dr["w_in"][wl, kc * 128:(kc + 1) * 128, :], w=["W%d" % kc])
    for kc in range(8):
        cx.ts(W[:, kc, 2176:2304], W[:, kc, 2176:2304], QSCALE, ALU.mult, r=["W%d" % kc], w=["W%d" % kc], eng="pool")
        for (s0, n, o0) in ((1280, 512, 0), (2048, 256, 512)):
            src = W[:, kc, s0:s0 + n].rearrange("p (b t d) -> p b t d", t=2, d=16)
            dst = WR[:, kc, o0:o0 + n].rearrange("p (b t d) -> p b t d", t=2, d=16)
            cx.ts(dst[:, :, 0, :], src[:, :, 1, :], -1.0, ALU.mult, r=["W%d" % kc], w=["WR%d" % kc], eng="pool")
            cx.copy(dst[:, :, 1, :], src[:, :, 0, :], r=["W%d" % kc], w=["WR%d" % kc], eng="pool")
    Wk = ["W%d" % kc for kc in range(8)]
    WRk = ["WR%d" % kc for kc in range(8)]
    lgf = cx.sb("lgf", [128, 4]); lgb = cx.sb("lgb", [128, 4])
    lgfc = cx.sb("lgfc", [128, 1]); lgbc = cx.sb("lgbc", [128, 1])
    cx.dma("sp", lgf[:, :], dr["ret_ld_f"][l, :].partition_broadcast(128), w=["lgf"])
    cx.dma("sp", lgb[:, :], dr["ret_ld_b"][l, :].partition_broadcast(128), w=["lgb"])
    for h in range(4):
        cx.dma("sp", lgfc[32 * h:32 * h + 32, :], dr["ret_ld_f"][l, h:h + 1].partition_broadcast(32), w=["lgfc"])
        cx.dma("sp", lgbc[32 * h:32 * h + 32, :], dr["ret_ld_b"][l, h:h + 1].partition_broadcast(32), w=["lgbc"])
    kdf = cx.sb("kdf", [128, 4]); kdb = cx.sb("kdb", [128, 4])
    KDF = cx.sb("KDF", [128, 128]); KDB = cx.sb("KDB", [128, 128])
    cx.act(kdf[:, :], lgf[:, :], AF.Exp, scale=cs(cst, "c127mj"), r=["lgf", "cst"], w=["kdf"])
    cx.act(kdb[:, :], lgb[:, :], AF.Exp, scale=cs(cst, "cj"), r=["lgb", "cst"], w=["kdb"])
    for h in range(4):
        cx.ts(KDF[:, 32 * h:32 * h + 32], cs(cst, "ones")[:, 0:32], kdf[:, h:h + 1], ALU.mult, r=["kdf", "cst"], w=["KDF"])
        cx.ts(KDB[:, 32 * h:32 * h + 32], cs(cst, "ones")[:, 0:32], kdb[:, h:h + 1], ALU.mult, r=["kdb", "cst"], w=["KDB"])
    pw = {}
    for tag, nch in (("l", 16), ("c", 2)):
        pf = cx.sb("pwf" + tag, [128, nch]); pb = cx.sb("pwb" + tag, [128, nch])
        cx.act(pf[:, :], cs(cst, "ef_" + tag), AF.Exp, scale=lgfc[:, 0:1], r=["lgfc", "cst"], w=["pwf" + tag])
        cx.act(pb[:, :], cs(cst, "eb_" + tag), AF.Exp, scale=lgbc[:, 0:1], r=["lgbc", "cst"], w=["pwb" + tag])
        pw[tag] = (pf, pb)
    Cl = cx.sb("Cl", [128, TL]); Sl = cx.sb("Sl", [128, TL])
    cx.dma("sp", Cl[:, :], dr["cosT"][:, :], w=["Cl"])
    cx.dma("sp", Sl[:, :], dr["sinT"][:, :], w=["Sl"])
    xt = [cx.sb("xt%d" % i, [128, D]) for i in range(2)]
    junk = cx.sb("junk", [128, D], BF16)
    t1 = [cx.sb("t1_%d" % i, [128, D]) for i in range(2)]
    hb = [cx.sb("hb%d" % i, [128, D], BF16) for i in range(2)]
    ssq = cx.sb("ssq", [128, 4]); rstd = cx.sb("rstd", [128, 4])
    hTs = [cx.sb("hT%d" % i, [128, 8, 512], BF16) for i in range(2)]
    gcount = [0]
    gsb = cx.sb("gsb", [128, D]); shb = cx.sb("shb", [128, D])
    ev = [cx.sb("ev%d" % i, [128, 512]) for i in range(4)]
    evb = [cx.sb("evb%d" % i, [128, 512], BF16) for i in range(4)]
    rk_sb = cx.sb("rk_sb", [128, 512], BF16)
    vt = [cx.sb("vt%d" % i, [128, 512], BF16) for i in range(2)]
    rgt = [cx.sb("rgt%d" % i, [128, 256], BF16) for i in range(2)]
    kfb = [cx.sb("kfb%d" % i, [128, 256], BF16) for i in range(2)]
    Tst = cx.sb("Tst", [128, 2, 256])
    evi = [0]

    def nxt():
        evi[0] = (evi[0] + 1) % 4
        return evi[0]

    xi = 0
    for (tag, T, xd) in segs:
        sfx = "_" + tag
        seg = 0 if tag == "l" else 1
        cx.dma("sp", gsb[:, :], dr["modv"][l, seg, 0, :].partition_broadcast(128), r=["D:modv%d" % l], w=["gsb"])
        cx.dma("sp", shb[:, :], dr["modv"][l, seg, 1, :].partition_broadcast(128), r=["D:modv%d" % l], w=["shb"])
        G = min(512, T)
        for g in range(T // G):
            t0 = g * G
            nt = G // 128
            hT = hTs[gcount[0] % 2]
            kh = "hT%d" % (gcount[0] % 2)
            gcount[0] += 1
            for j in range(nt):
                b = xi % 2
                xi += 1
                kx = "xt%d" % b
                cx.dma("sp", xt[b][:, :], xd[t0 + j * 128:t0 + (j + 1) * 128, :], w=[kx])
                cx.act(junk[:, :], xt[b][:, :], AF.Square, accum_out=ssq[:, j:j + 1], r=[kx], w=["junk", "ssq%d" % j])
                cx.act(rstd[:, j:j + 1], ssq[:, j:j + 1], AF.Sqrt, scale=1.0 / D, bias=EPS, r=["ssq%d" % j], w=["rs%d" % j])
                cx.recip(rstd[:, j:j + 1], rstd[:, j:j + 1], r=["rs%d" % j], w=["rs%d" % j])
                cx.stt(t1[b][:, :], xt[b][:, :], rstd[:, j:j + 1], gsb[:, :], ALU.mult, ALU.mult,
                       r=[kx, "rs%d" % j, "gsb"], w=["t1_%d" % b])
                cx.tt(hb[b][:, :], t1[b][:, :], shb[:, :], ALU.add, r=["t1_%d" % b, "shb"], w=["hb%d" % b], eng="pool")
                for kc in range(8):
                    cx.tr(psb[0][:, kc * 128:(kc + 1) * 128], hb[b][:, kc * 128:(kc + 1) * 128], ident[:, :],
                          r=["hb%d" % b, "ident"], w=["psb0"])
                cx.copy(hT[:, :, j * 128:(j + 1) * 128], psb[0][:, :].rearrange("p (k t) -> p k t", k=8),
                        r=["psb0"], w=[kh], eng="act")
            cx.dma("sp", dr["hT" + sfx][:, :, t0:t0 + G], hT[:, :, 0:G], r=[kh], w=["D:hT" + sfx])

            def proj(cc, bank, rot=False):
                Wt = WR if rot else W
                for kc in range(8):
                    cx.mm(ps[bank][:, 0:G], Wt[:, kc, cc * 128:(cc + 1) * 128], hT[:, kc, 0:G], kc == 0, kc == 7,
                          r=[kh, (WRk if rot else Wk)[kc]], w=["ps%d" % bank])

            Cg = Cl[:, t0:t0 + G] if tag == "l" else None
            Sg = Sl[:, t0:t0 + G] if tag == "l" else None
            for c2 in range(2):
                e = nxt()
                proj(2 + c2, 0)
                cx.act(ev[e][:, 0:G], ps[0][:, 0:G], AF.Sigmoid, r=["ps0"], w=["ev%d" % e])
                proj(0 + c2, 1)
                cx.tt(ev[e][:, 0:G], ps[1][:, 0:G], ev[e][:, 0:G], ALU.mult, r=["ps1", "ev%d" % e], w=["ev%d" % e])
                cx.dma("sp", dr["uT" + sfx][c2, :, t0:t0 + G], ev[e][:, 0:G], r=["ev%d" % e], w=["D:uT" + sfx])
            for c2 in range(2):
                e = nxt()
                proj(4 + c2, 0)
                cx.copy(evb[e][:, 0:G], ps[0][:, 0:G], r=["ps0"], w=["evb%d" % e], eng="act")
                cx.dma("sp", dr["bgT" + sfx][c2, :, t0:t0 + G], evb[e][:, 0:G], r=["evb%d" % e], w=["D:bgT" + sfx])
                proj(6 + c2, 1)
                cx.copy(ev[e][:, 0:G], ps[1][:, 0:G], r=["ps1"], w=["ev%d" % e], eng="act")
                proj(8 + c2, 0)
                cx.tt(ev[e][:, 0:G], ps[0][:, 0:G], ev[e][:, 0:G], ALU.mult, r=["ps0", "ev%d" % e], w=["ev%d" % e])
                cx.dma("sp", dr["tT" + sfx][c2, :, t0:t0 + G], ev[e][:, 0:G], r=["ev%d" % e], w=["D:tT" + sfx])
            for (cc, ro, dst, keep) in ((10, 0, dr["qT" + sfx][0], None), (11, 1, dr["qT" + sfx][1], None),
                                        (12, 2, dr["kT" + sfx][0], None), (13, 3, dr["kT" + sfx][1], None),
                                        (16, 4, dr["rqT" + sfx], None), (17, 5, dr["rkT" + sfx], rk_sb)):
                e = nxt()
                ob = keep if keep is not None else evb[e]
                okey = "rk_sb" if keep is not None else "evb%d" % e
                proj(cc, 0)
                if tag == "l":
                    proj(ro, 2, rot=True)
                    e2 = nxt()
                    cx.tt(ev[e][:, 0:G], ps[0][:, 0:G], Cg, ALU.mult, r=["ps0", "Cl"], w=["ev%d" % e])
                    cx.tt(ev[e2][:, 0:G], ps[2][:, 0:G], Sg, ALU.mult, r=["ps2", "Sl"], w=["ev%d" % e2])
                    cx.tt(ob[:, 0:G], ev[e][:, 0:G], ev[e2][:, 0:G], ALU.add, r=["ev%d" % e, "ev%d" % e2], w=[okey], eng="pool")
                else:
                    cx.copy(ob[:, 0:G], ps[0][:, 0:G], r=["ps0"], w=[okey], eng="act")
                cx.dma("sp", dst[:, t0:t0 + G], ob[:, 0:G], r=[okey], w=["D:rope" + sfx + str(cc)])
            pf, pb = pw[tag]
            for j in range(nt):
                n = (t0 // 128) + j
                b = j % 2
                tsl = slice(j * 128, (j + 1) * 128)
                for kc in range(8):
                    cx.mm(ps[3][:, 0:256], hT[:, kc, tsl], W[:, kc, 1792:2048], kc == 0, kc == 7, r=[kh, Wk[kc]], w=["ps3"])
                for kc in range(8):
                    cx.mm(ps[3][:, 256:512], hT[:, kc, tsl], W[:, kc, 2304:2560], kc == 0, kc == 7, r=[kh, Wk[kc]], w=["ps3"])
                for kc in range(8):
                    cx.mm(ps[4][:, 0:256], hT[:, kc, tsl], W[:, kc, 2560:2816], kc == 0, kc == 7, r=[kh, Wk[kc]], w=["ps4"])
                cx.copy(vt[b][:, :], ps[3][:, :], r=["ps3", "ps3"], w=["vt%d" % b])
                cx.act(rgt[b][:, :], ps[4][:, 0:256], AF.Silu, r=["ps4"], w=["rgt%d" % b])
                rows = slice(t0 + j * 128, t0 + (j + 1) * 128)
                cx.dma("sp", dr["V" + sfx][rows, :], vt[b][:, 0:256], r=["vt%d" % b], w=["D:V" + sfx])
                cx.dma("sp", dr["rv" + sfx][rows, :], vt[b][:, 256:512], r=["vt%d" % b], w=["D:rv" + sfx])
                cx.dma("sp", dr["rg" + sfx][rows, :], rgt[b][:, :], r=["rgt%d" % b], w=["D:rg" + sfx])
                cx.tr(psb[1][:, 0:128], rk_sb[:, tsl], ident[:, :], r=["rk_sb", "ident"], w=["psb1"])
                cx.tt(kfb[b][:, 0:128], psb[1][:, 0:128], KDF[:, :], ALU.mult, r=["psb1", "KDF"], w=["kfb%d" % b])
                cx.tt(kfb[b][:, 128:256], psb[1][:, 0:128], KDB[:, :], ALU.mult, r=["psb1", "KDB"], w=["kfb%d" % b])
                cx.mm(ps[5][:, 0:256], kfb[b][:, 0:128], vt[b][:, 256:512], True, True, r=["kfb%d" % b, "vt%d" % b], w=["ps5"])
                cx.mm(ps[5][:, 256:512], kfb[b][:, 128:256], vt[b][:, 256:512], True, True, r=["kfb%d" % b, "vt%d" % b], w=["ps5"])
                if n == 0:
                    cx.ts(Tst[:, 0, :], ps[5][:, 0:256], pf[:, n:n + 1], ALU.mult, r=["ps5", "pwf" + tag], w=["Tf"])
                    cx.ts(Tst[:, 1, :], ps[5][:, 256:512], pb[:, n:n + 1], ALU.mult, r=["ps5", "pwb" + tag], w=["Tb"])
                else:
                    cx.stt(Tst[:, 0, :], ps[5][:, 0:256], pf[:, n:n + 1], Tst[:, 0, :], ALU.mult, ALU.add,
                           r=["ps5", "pwf" + tag, "Tf"], w=["Tf"])
                    cx.stt(Tst[:, 1, :], ps[5][:, 256:512], pb[:, n:n + 1], Tst[:, 1, :], ALU.mult, ALU.add,
                           r=["ps5", "pwb" + tag, "Tb"], w=["Tb"])
        cx.dma("sp", dr["Tst" + sfx].rearrange("a p e -> p a e"), Tst[:, :, :], r=["Tf", "Tb"], w=["D:Tst" + sfx])
    cx.end()


def phase_conv(cx, l, dr, segs):
    cx.begin(nf=8, nb=0)
    ps = cx.ps
    cst = cx.sb("cst", [128, CSTW])
    cx.dma("sp", cst[:, :], dr["cst"][:, :], w=["cst"])
    praw = cx.sb("praw", [40, 256])
    cx.memset(praw[:, :], 0.0, w=["praw"])
    cx.dma("sp", praw[0:31, :], dr["conv_a_w"][l, :, :], w=["praw"])
    cx.dma("sp", praw[31:32, :], dr["conv_a_b"][l:l + 1, :], w=["praw"])
    cx.dma("sp", praw[32:33, :], dr["conv_a_g"][l:l + 1, :], w=["praw"])
    cx.dma("sp", praw[33:34, :], dr["conv_a_beta"][l:l + 1, :], w=["praw"])
    cx.dma("sp", praw[34:37, :], dr["conv_b_w"][l, :, :], w=["praw"])
    par = cx.sb("par", [128, 2, 40])
    for c2 in range(2):
        cx.tr(ps[7][:, 0:40], praw[0:40, c2 * 128:(c2 + 1) * 128], cs(cst, "I1")[0:40, 0:40], r=["praw", "cst"], w=["ps7"])
        cx.copy(par[:, c2, :], ps[7][:, 0:40], r=["ps7"], w=["par"])
    onesm = cx.sb("onesm", [128, 128])
    cx.memset(onesm[:, :], 1.0 / 256.0, w=["onesm"])
    HG = cx.sb("HG", [128, NCORE, 4, 32]); selt = cx.sb("selt", [128, 2, NCORE])
    cx.dma("sp", HG[:, :, :, :], dr["hg"].rearrange("(r a p) w -> p r a w", r=NCORE, a=4), w=["HG"])
    cx.dma("sp", selt[:, :, :], dr["sel"][:, :, :], w=["selt"])
    for (tag, T) in segs:
        sfx = "_" + tag
        ue = cx.sb("ue" + tag, [128, 2, T + 32])
        te = cx.sb("te" + tag, [128, 2, T + 32])
        bg = cx.sb("bg" + tag, [128, 2, T], BF16)
        acc = cx.sb("acc" + tag, [128, 2, T])
        sq = cx.sb("sq" + tag, [128, 2, T])
        yb = cx.sb("yb" + tag, [128, 2, T], BF16)
        for c2 in range(2):
            cx.dma("sp", ue[:, c2, 16:16 + T], dr["uT" + sfx][c2, :, :], r=["D:uT" + sfx], w=["ue%d" % c2])
            cx.dma("sp", te[:, c2, 16:16 + T], dr["tT" + sfx][c2, :, :], r=["D:tT" + sfx], w=["te%d" % c2])
            cx.dma("sp", bg[:, c2, :], dr["bgT" + sfx][c2, :, :], r=["D:bgT" + sfx], w=["bg%d" % c2])
            if tag == "l":
                for a, (buf, k) in enumerate(((ue, "ue%d" % c2), (te, "te%d" % c2))):
                    ai = a * 2 + c2
                    for side, dst, src in ((0, slice(0, 16), slice(16, 32)), (1, slice(16 + T, 32 + T), slice(0, 16))):
                        for r_ in range(NCORE):
                            if r_ == 0:
                                cx.ts(buf[:, c2, dst], HG[:, r_, ai, src], selt[:, side, r_:r_ + 1], ALU.mult,
                                      r=["HG", "selt"], w=[k])
                            else:
                                cx.stt(buf[:, c2, dst], HG[:, r_, ai, src], selt[:, side, r_:r_ + 1], buf[:, c2, dst],
                                       ALU.mult, ALU.add, r=["HG", "selt", k], w=[k])
            else:
                for buf, k in ((ue, "ue%d" % c2), (te, "te%d" % c2)):
                    cx.memset(buf[:, c2, 0:16], 0.0, w=[k], eng="pool")
                    cx.memset(buf[:, c2, 16 + T:32 + T], 0.0, w=[k], eng="pool")
        for c2 in range(2):
            ka = "acc%d" % c2
            cx.ts(acc[:, c2, :], ue[:, c2, 1:1 + T], par[:, c2, 0:1], ALU.mult, s2=par[:, c2, 31:32], op1=ALU.add,
                  r=["ue%d" % c2, "par"], w=[ka])
            for k in range(1, 31):
                cx.stt(acc[:, c2, :], ue[:, c2, k + 1:k + 1 + T], par[:, c2, k:k + 1], acc[:, c2, :], ALU.mult, ALU.add,
                       r=["ue%d" % c2, "par", ka], w=[ka])
            cx.tt(sq[:, c2, :], acc[:, c2, :], acc[:, c2, :], ALU.mult, r=[ka], w=["sq%d" % c2], eng="pool")
        G = min(512, T)
        mm2 = cx.sb("m2" + tag, [128, G]); var = cx.sb("var" + tag, [128, G]); dd = cx.sb("dd" + tag, [128, G])
        for g in range(T // G):
            gs = slice(g * G, (g + 1) * G)
            for c2 in range(2):
                cx.mm(ps[0][:, 0:G], onesm[:, :], acc[:, c2, gs], c2 == 0, c2 == 1, r=["onesm", "acc%d" % c2], w=["ps0"])
            for c2 in range(2):
                cx.mm(ps[1][:, 0:G], onesm[:, :], sq[:, c2, gs], c2 == 0, c2 == 1, r=["onesm", "sq%d" % c2], w=["ps1"])
            cx.act(mm2[:, :], ps[0][:, 0:G], AF.Square, r=["ps0"], w=["mm2"])
            cx.tt(var[:, :], ps[1][:, 0:G], mm2[:, :], ALU.subtract, r=["ps1", "mm2"], w=["var"])
            cx.act(var[:, :], var[:, :], AF.Ln, bias=EPS, r=["var"], w=["var"])
            cx.act(var[:, :], var[:, :], AF.Exp, scale=-0.5, r=["var"], w=["var"])
            for c2 in range(2):
                cx.tt(dd[:, :], acc[:, c2, gs], ps[0][:, 0:G], ALU.subtract, r=["acc%d" % c2, "ps0"], w=["dd"])
                cx.tt(dd[:, :], dd[:, :], var[:, :], ALU.mult, r=["dd", "var"], w=["dd"])
                cx.act(yb[:, c2, gs], dd[:, :], AF.Silu, scale=par[:, c2, 32:33], bias=par[:, c2, 33:34],
                       r=["dd", "par"], w=["yb%d" % c2])
        for c2 in range(2):
            cx.dma("sp", dr["ysT" + sfx][0 + c2, :, :], yb[:, c2, :], r=["yb%d" % c2], w=["D:ys0" + sfx])
        for c2 in range(2):
            ka = "acc%d" % c2
            cx.ts(acc[:, c2, :], te[:, c2, 15:15 + T], par[:, c2, 34:35], ALU.mult, r=["te%d" % c2, "par"], w=[ka])
            for k in (1, 2):
                cx.stt(acc[:, c2, :], te[:, c2, 15 + k:15 + k + T], par[:, c2, 34 + k:35 + k], acc[:, c2, :], ALU.mult, ALU.add,
                       r=["te%d" % c2, "par", ka], w=[ka])
            cx.tt(yb[:, c2, :], acc[:, c2, :], bg[:, c2, :], ALU.mult, r=[ka, "bg%d" % c2], w=["yb%d" % c2])
            cx.dma("sp", dr["ysT" + sfx][2 + c2, :, :], yb[:, c2, :], r=["yb%d" % c2], w=["D:ys1" + sfx])
    cx.end()


def phase_ret(cx, l, dr, segs):
    cx.begin(nf=6, nb=2)
    ps, psb = cx.ps, cx.psb
    cst = cx.sb("cst", [128, CSTW])
    ident = cx.sb("ident", [128, 128], BF16)
    cx.dma("sp", cst[:, :], dr["cst"][:, :], w=["cst"])
    cx.dma("sp", ident[:, :], dr["ident"][:, :], w=["ident"])
    lgf = cx.sb("lgf", [128, 4]); lgb = cx.sb("lgb", [128, 4])
    lgfc = cx.sb("lgfc", [128, 1]); lgbc = cx.sb("lgbc", [128, 1])
    cx.dma("sp", lgf[:, :], dr["ret_ld_f"][l, :].partition_broadcast(128), w=["lgf"])
    cx.dma("sp", lgb[:, :], dr["ret_ld_b"][l, :].partition_broadcast(128), w=["lgb"])
    for h in range(4):
        cx.dma("sp", lgfc[32 * h:32 * h + 32, :], dr["ret_ld_f"][l, h:h + 1].partition_broadcast(32), w=["lgfc"])
        cx.dma("sp", lgbc[32 * h:32 * h + 32, :], dr["ret_ld_b"][l, h:h + 1].partition_broadcast(32), w=["lgbc"])
    kdf = cx.sb("kdf", [128, 4]); kdb = cx.sb("kdb", [128, 4])
    KDF = cx.sb("KDF", [128, 128]); KDB = cx.sb("KDB", [128, 128])
    cx.act(kdf[:, :], lgf[:, :], AF.Exp, scale=cs(cst, "c127mj"), r=["lgf", "cst"], w=["kdf"])
    cx.act(kdb[:, :], lgb[:, :], AF.Exp, scale=cs(cst, "cj"), r=["lgb", "cst"], w=["kdb"])
    for h in range(4):
        cx.ts(KDF[:, 32 * h:32 * h + 32], cs(cst, "ones")[:, 0:32], kdf[:, h:h + 1], ALU.mult, r=["kdf", "cst"], w=["KDF"])
        cx.ts(KDB[:, 32 * h:32 * h + 32], cs(cst, "ones")[:, 0:32], kdb[:, h:h + 1], ALU.mult, r=["kdb", "cst"], w=["KDB"])
    cdf = cx.sb("cdf", [128, 1]); cdb = cx.sb("cdb", [128, 1])
    cx.act(cdf[:, :], lgfc[:, :], AF.Exp, scale=128.0, r=["lgfc"], w=["cdf"])
    cx.act(cdb[:, :], lgbc[:, :], AF.Exp, scale=128.0, r=["lgbc"], w=["cdb"])
    qdf4 = cx.sb("qdf4", [128, 4, 128]); qdb4 = cx.sb("qdb4", [128, 4, 128])
    for c in range(4):
        cx.act(qdf4[:, c, :], cs(cst, "ip1"), AF.Exp, scale=lgfc[:, 0:1], r=["lgfc", "cst"], w=["qdf4"])
        cx.act(qdb4[:, c, :], cs(cst, "m128i"), AF.Exp, scale=lgbc[:, 0:1], r=["lgbc", "cst"], w=["qdb4"])
    maskT = cx.sb("maskT", [128, 4, 128]); mtmp = cx.sb("mtmp", [128, 128])
    for h in range(4):
        cx.act(mtmp[:, :], cs(cst, "D1"), AF.Exp, scale=lgf[:, h:h + 1], r=["lgf", "cst"], w=["mtmp"])
        cx.tt(maskT[:, h, :], mtmp[:, :], cs(cst, "U"), ALU.mult, r=["mtmp", "cst"], w=["maskT"])
        cx.tt(maskT[:, h, :], maskT[:, h, :], cs(cst, "I2"), ALU.add, r=["maskT", "cst"], w=["maskT"])
        cx.act(mtmp[:, :], cs(cst, "D2"), AF.Exp, scale=lgb[:, h:h + 1], r=["lgb", "cst"], w=["mtmp"])
        cx.tt(mtmp[:, :], mtmp[:, :], cs(cst, "Lo"), ALU.mult, r=["mtmp", "cst"], w=["mtmp"])
        cx.tt(maskT[:, h, :], maskT[:, h, :], mtmp[:, :], ALU.add, r=["maskT", "mtmp"], w=["maskT"])
    for (tag, T) in segs:
        sfx = "_" + tag
        NCH = T // 128
        rq = cx.sb("rq" + tag, [128, T], BF16); rk = cx.sb("rk" + tag, [128, T], BF16)
        rqh = cx.sb("rqh" + tag, [128, 4, T], BF16)
        rv = cx.sb("rv" + tag, [128, NCH, 256], BF16); rg = cx.sb("rg" + tag, [128, NCH, 256], BF16)
        cx.dma("sp", rq[:, :], dr["rqT" + sfx][:, :], r=["D:rope" + sfx + "16"], w=["rq"])
        cx.dma("sp", rk[:, :], dr["rkT" + sfx][:, :], r=["D:rope" + sfx + "17"], w=["rk"])
        cx.dma("sp", rv[:, :, :], dr["rv" + sfx].rearrange("(n p) e -> p n e", p=128), r=["D:rv" + sfx], w=["rv"])
        cx.dma("sp", rg[:, :, :], dr["rg" + sfx].rearrange("(n p) e -> p n e", p=128), r=["D:rg" + sfx], w=["rg"])
        for h in range(4):
            cx.ts(rqh[:, h, :], rq[:, :], cs(cst, "hm")[:, h:h + 1], ALU.mult, r=["rq", "cst"], w=["rqh"], eng="pool")
        SF = cx.sb("SF" + tag, [128, NCH, 256]); SB = cx.sb("SB" + tag, [128, NCH, 256])
        SFb = cx.sb("SFb" + tag, [128, NCH, 256], BF16); SBb = cx.sb("SBb" + tag, [128, NCH, 256], BF16)
        KV = cx.sb("KV" + tag, [128, NCH, 2, 256])
        if tag == "l":
            Tall = cx.sb("Tall", [128, 9, 2, 256]); expo = cx.sb("expo", [128, 2, 9]); coef = cx.sb("coef", [128, 2, 9])
            cx.dma("sp", Tall[:, 0:8, :, :], dr["gt"].rearrange("(s a p) e -> p s a e", s=NCORE, a=2), w=["Tall"])
            cx.dma("sp", Tall[:, 8, :, :], dr["Tst_c"].rearrange("a p e -> p a e"), w=["Tall"])
            cx.dma("sp", expo[:, :, :], dr["expo"][:, :, :], w=["expo"])
            cx.act(coef[:, 0, :], expo[:, 0, :], AF.Exp, scale=lgfc[:, 0:1], r=["expo", "lgfc"], w=["coef"])
            cx.act(coef[:, 1, :], expo[:, 1, :], AF.Exp, scale=lgbc[:, 0:1], r=["expo", "lgbc"], w=["coef"])
            for a, (St, n0, key) in enumerate(((SF, 0, "SF0"), (SB, NCH - 1, "SB%d" % (NCH - 1)))):
                cx.ts(St[:, n0, :], Tall[:, 0, a, :], coef[:, a, 0:1], ALU.mult, r=["Tall", "coef"], w=[key])
                for s in range(1, 9):
                    cx.stt(St[:, n0, :], Tall[:, s, a, :], coef[:, a, s:s + 1], St[:, n0, :], ALU.mult, ALU.add,
                           r=["Tall", "coef", key], w=[key])
        else:
            cx.memset(SF[:, 0, :], 0.0, w=["SF0"])
            cx.memset(SB[:, NCH - 1, :], 0.0, w=["SB%d" % (NCH - 1)])
        kfb = [cx.sb("kfb%d" % i + tag, [128, 256], BF16) for i in range(2)]
        for n in range(NCH):
            b = n % 2
            csl = slice(n * 128, (n + 1) * 128)
            cx.tr(psb[0][:, 0:128], rk[:, csl], ident[:, :], r=["rk", "ident"], w=["psb0"])
            cx.tt(kfb[b][:, 0:128], psb[0][:, 0:128], KDF[:, :], ALU.mult, r=["psb0", "KDF"], w=["kfb%d" % b])
            cx.tt(kfb[b][:, 128:256], psb[0][:, 0:128], KDB[:, :], ALU.mult, r=["psb0", "KDB"], w=["kfb%d" % b])
            cx.mm(ps[0][:, 0:256], kfb[b][:, 0:128], rv[:, n, :], True, True, r=["kfb%d" % b, "rv"], w=["ps0"])
            cx.mm(ps[0][:, 256:512], kfb[b][:, 128:256], rv[:, n, :], True, True, r=["kfb%d" % b, "rv"], w=["ps0"])
            cx.copy(KV[:, n, :, :], ps[0][:, :].rearrange("p (a e) -> p a e", a=2), r=["ps0", "ps0"], w=["KV%d" % n], eng="act")
        for n in range(NCH - 1):
            cx.stt(SF[:, n + 1, :], SF[:, n, :], cdf[:, 0:1], KV[:, n, 0, :], ALU.mult, ALU.add,
                   r=["SF%d" % n, "cdf", "KV%d" % n], w=["SF%d" % (n + 1)])
        for n in range(NCH - 1, 0, -1):
            cx.stt(SB[:, n - 1, :], SB[:, n, :], cdb[:, 0:1], KV[:, n, 1, :], ALU.mult, ALU.add,
                   r=["SB%d" % n, "cdb", "KV%d" % n], w=["SB%d" % (n - 1)])
        allSF = ["SF%d" % n for n in range(NCH)]; allSB = ["SB%d" % n for n in range(NCH)]
        cx.copy(SFb[:, :, :], SF[:, :, :], r=allSF, w=["SFb"], eng="pool")
        cx.copy(SBb[:, :, :], SB[:, :, :], r=allSB, w=["SBb"], eng="pool")
        sT = [cx.sb("sT%d" % i + tag, [128, 4, 128], BF16) for i in range(2)]
        Qf = [cx.sb("Qf%d" % i + tag, [128, 4, 128], BF16) for i in range(2)]
        Qb = [cx.sb("Qb%d" % i + tag, [128, 4, 128], BF16) for i in range(2)]
        osb = cx.sb("osb" + tag, [128, 4, 64]); osq = cx.sb("osq" + tag, [128, 4, 64])
        st = cx.sb("st" + tag, [128, 4, 4])
        ysb = [cx.sb("ysb%d" % i + tag, [128, 256], BF16) for i in range(2)]
        yT = cx.sb("yT" + tag, [128, 2, T], BF16)
        for n in range(NCH):
            b = n % 2
            csl = slice(n * 128, (n + 1) * 128)
            for h in range(4):
                cx.mm(ps[1][:, h * 128:(h + 1) * 128], rk[:, csl], rqh[:, h, csl], True, True, r=["rk", "rqh"], w=["ps1"])
            cx.tt(sT[b][:, :, :], ps[1][:, :].rearrange("p (h i) -> p h i", h=4), maskT[:, :, :], ALU.mult,
                  r=["ps1", "maskT"], w=["sT%d" % b])
            cx.tt(Qf[b][:, :, :], rqh[:, :, csl], qdf4[:, :, :], ALU.mult, r=["rqh", "qdf4"], w=["Qf%d" % b], eng="pool")
            cx.tt(Qb[b][:, :, :], rqh[:, :, csl], qdb4[:, :, :], ALU.mult, r=["rqh", "qdb4"], w=["Qb%d" % b], eng="pool")
            for h in range(4):
                o = ps[2][:, h * 64:(h + 1) * 64]
                es = slice(h * 64, (h + 1) * 64)
                cx.mm(o, sT[b][:, h, :], rv[:, n, es], True, False, r=["sT%d" % b, "rv"], w=["ps2"])
                cx.mm(o, Qf[b][:, h, :], SFb[:, n, es], False, False, r=["Qf%d" % b, "SFb"], w=["ps2"])
                cx.mm(o, Qb[b][:, h, :], SBb[:, n, es], False, True, r=["Qb%d" % b, "SBb"], w=["ps2"])
            p2 = ["ps2"]
            cx.copy(osb[:, :, :], ps[2][:, 0:256].rearrange("p (h e) -> p h e", h=4), r=p2, w=["osb"], eng="act")
            cx.red(st[:, 0, :], osb[:, :, :], ALU.add, r=["osb"], w=["st0"])
            cx.tt(osq[:, :, :], osb[:, :, :], osb[:, :, :], ALU.mult, r=["osb"], w=["osq"], eng="pool")
            cx.red(st[:, 1, :], osq[:, :, :], ALU.add, r=["osq"], w=["st1"])
            cx.ts(st[:, 2, :], st[:, 0, :], 1.0 / 64, ALU.mult, r=["st0"], w=["st2"])
            cx.tt(st[:, 3, :], st[:, 2, :], st[:, 2, :], ALU.mult, r=["st2"], w=["st3"])
            cx.stt(st[:, 3, :], st[:, 1, :], 1.0 / 64, st[:, 3, :], ALU.mult, ALU.subtract, r=["st1", "st3"], w=["st3"])
            cx.act(st[:, 3, :], st[:, 3, :], AF.Sqrt, bias=EPS, r=["st3"], w=["st3"])
            cx.recip(st[:, 3, :], st[:, 3, :], r=["st3"], w=["st3"])
            for h in range(4):
                cx.ts(osb[:, h, :], osb[:, h, :], st[:, 2, h:h + 1], ALU.subtract, s2=st[:, 3, h:h + 1], op1=ALU.mult,
                      r=["osb", "st2", "st3"], w=["osb"])
            cx.tt(ysb[b][:, :], osb[:, :, :].rearrange("p h e -> p (h e)"), rg[:, n, :], ALU.mult, r=["osb", "rg"], w=["ysb%d" % b])
            for c2 in range(2):
                cx.tr(psb[1][:, c2 * 128:(c2 + 1) * 128], ysb[b][:, c2 * 128:(c2 + 1) * 128], ident[:, :],
                      r=["ysb%d" % b, "ident"], w=["psb1"])
            cx.copy(yT[:, :, csl], psb[1][:, 0:256].rearrange("p (c t) -> p c t", c=2), r=["psb1"], w=["yT"], eng="act")
        for c2 in range(2):
            cx.dma("sp", dr["ysT" + sfx][6 + c2, :, :], yT[:, c2, :], r=["yT"], w=["D:ys3" + sfx])
    cx.end()


def phase_merge(cx, l, dr, segs, wl=None):
    wl = l if wl is None else wl
    cx.begin(nf=8, nb=0)
    ps = cx.ps
    cst = cx.sb("cst", [128, CSTW])
    cx.dma("sp", cst[:, :], dr["cst"][:, :], w=["cst"])
    WG = cx.sb("WG", [128, 8, 4096], BF16)
    WB = cx.sb("WB", [128, 8, 1024], BF16)
    WO = cx.sb("WO", [128, 8, 1024], BF16)
    for kc in range(8):
        cx.dma("pool", WG[:, kc, :], dr["w_gate"][wl, kc * 128:(kc + 1) * 128, :], w=["WG%d" % kc])
        cx.dma("pool", WB[:, kc, :], dr["w_branch"][wl, kc // 2, (kc % 2) * 128:(kc % 2 + 1) * 128, :], w=["WB%d" % kc])
        cx.dma("pool", WO[:, kc, :], dr["w_o"][wl, kc * 128:(kc + 1) * 128, :], w=["WO%d" % kc])
    braw = cx.sb("braw", [32, 128]); bgt = cx.sb("bgt", [128, 32])
    cx.dma("sp", braw[:, :], dr["b_gate"][l, :].rearrange("(a p) -> a p", p=128), w=["braw"])
    cx.tr(ps[7][:, 0:32], braw[:, :], cs(cst, "I1")[0:32, 0:32], r=["braw", "cst"], w=["ps7"])
    cx.copy(bgt[:, :], ps[7][:, 0:32], r=["ps7"], w=["bgt"])
    g1b = cx.sb("g1b", [128, D])
    hT = [cx.sb("mhT%d" % i, [128, 8, 512], BF16) for i in range(2)]
    yT = [cx.sb("myT%d" % i, [128, 8, 512], BF16) for i in range(2)]
    mT = cx.sb("mT", [128, 8, 512], BF16)
    sg = [cx.sb("sg%d" % i, [128, 512]) for i in range(2)]
    macc = cx.sb("macc", [128, 512]); mtmp = cx.sb("mtmp", [128, 512])
    xt = [cx.sb("mxt%d" % i, [128, D]) for i in range(2)]
    gi = 0
    xi = 0
    for (tag, T, xin, xout) in segs:
        sfx = "_" + tag
        seg = 0 if tag == "l" else 1
        cx.dma("sp", g1b[:, :], dr["modv"][l, seg, 2, :].partition_broadcast(128), r=["D:modv%d" % l], w=["g1b"])
        G = min(512, T)
        for g in range(T // G):
            t0 = g * G
            b = gi % 2
            gi += 1
            cx.dma("sp", hT[b][:, :, 0:G], dr["hT" + sfx][:, :, t0:t0 + G], r=["D:hT" + sfx], w=["mhT%d" % b])
            cx.dma("sp", yT[b][:, :, 0:G], dr["ysT" + sfx][:, :, t0:t0 + G].rearrange("a p t -> p a t"),
                   r=["D:ys0" + sfx, "D:ys1" + sfx, "D:ys2" + sfx, "D:ys3" + sfx], w=["myT%d" % b])
            for nn in range(8):
                for i in range(4):
                    pa = ps[(i % 2) * 2]
                    pb = ps[(i % 2) * 2 + 1]
                    ka = "ps%d" % ((i % 2) * 2)
                    kb = "ps%d" % ((i % 2) * 2 + 1)
                    col = i * 1024 + nn * 128
                    for kc in range(8):
                        cx.mm(pa[:, 0:G], WG[:, kc, col:col + 128], hT[b][:, kc, 0:G], kc == 0, kc == 7,
                              r=["WG%d" % kc, "mhT%d" % b], w=[ka])
                    for c2 in range(2):
                        cx.mm(pb[:, 0:G], WB[:, i * 2 + c2, nn * 128:(nn + 1) * 128], yT[b][:, i * 2 + c2, 0:G], c2 == 0, c2 == 1,
                              r=["WB%d" % (i * 2 + c2), "myT%d" % b], w=[kb])
                    s = sg[i % 2]
                    ks = "sg%d" % (i % 2)
                    cx.act(s[:, 0:G], pa[:, 0:G], AF.Sigmoid, bias=bgt[:, i * 8 + nn:i * 8 + nn + 1], r=[ka, "bgt"], w=[ks])
                    if i == 0:
                        cx.tt(macc[:, 0:G], pb[:, 0:G], s[:, 0:G], ALU.mult, r=[kb, ks], w=["macc"])
                    elif i < 3:
                        cx.tt(mtmp[:, 0:G], pb[:, 0:G], s[:, 0:G], ALU.mult, r=[kb, ks], w=["mtmp"])
                        cx.tt(macc[:, 0:G], macc[:, 0:G], mtmp[:, 0:G], ALU.add, r=["macc", "mtmp"], w=["macc"], eng="pool")
                    else:
                        cx.tt(mtmp[:, 0:G], pb[:, 0:G], s[:, 0:G], ALU.mult, r=[kb, ks], w=["mtmp"])
                        cx.tt(mT[:, nn, 0:G], macc[:, 0:G], mtmp[:, 0:G], ALU.add, r=["macc", "mtmp"], w=["mT"], eng="pool")
            for j in range(G // 128):
                xb = xi % 2
                xi += 1
                rows = slice(t0 + j * 128, t0 + (j + 1) * 128)
                cx.dma("sp", xt[xb][:, :], xin[rows, :], w=["mxt%d" % xb])
                for nh in range(2):
                    po = ps[4 + nh]
                    for kc in range(8):
                        cx.mm(po[:, :], mT[:, kc, j * 128:(j + 1) * 128], WO[:, kc, nh * 512:(nh + 1) * 512], kc == 0, kc == 7,
                              r=["mT", "WO%d" % kc], w=["ps%d" % (4 + nh)])
                    hs = slice(nh * 512, (nh + 1) * 512)
                    cx.tt(mtmp[:, :], po[:, :], g1b[:, hs], ALU.mult, r=["ps%d" % (4 + nh), "g1b"], w=["mtmp"])
                    cx.tt(xt[xb][:, hs], xt[xb][:, hs], mtmp[:, :], ALU.add, r=["mxt%d" % xb, "mtmp"], w=["mxt%d" % xb], eng="pool")
                cx.dma("sp", xout[rows, :], xt[xb][:, :], r=["mxt%d" % xb], w=["D:xmid" + sfx])
    cx.end()


def phase_attn(cx, l, dr, segs, lam_init):
    NB = 2
    NSB = 3
    cx.begin(nf=0, nb=0)
    psS = [cx.psum("psS%d" % i, [128, NB * 512]) for i in range(NSB)]
    psO = [cx.psum("psO%d" % i, [128, 512]) for i in range(2)]
    NKT = max(s[2] for s in segs)
    cst = cx.sb("cst", [128, CSTW])
    ident = cx.sb("ident", [128, 128], BF16)
    cx.dma("sp", cst[:, :], dr["cst"][:, :], w=["cst"])
    cx.dma("sp", ident[:, :], dr["ident"][:, :], w=["ident"])
    kT = cx.sb("kTall", [128, 2, NKT * 128], BF16)
    Va = cx.sb("Vaug", [128, NKT, 4, 65], BF16)
    vst = [cx.sb("vst%d" % i, [128, 10, 256], BF16) for i in range(2)]
    kTk = []
    for c in range(2):
        cx.dma("sp", kT[:, c, 0:TC], dr["kT_c"][c, :, :], w=["kTc%d" % c])
        kTk.append("kTc%d" % c)
        if NKT > TC // 128:
            for r_ in range(NCORE):
                cx.dma("sp" if (r_ % 2 == 0) else "act", kT[:, c, TC + r_ * TL:TC + (r_ + 1) * TL],
                       dr["gk"][(r_ * 2 + c) * 128:(r_ * 2 + c + 1) * 128, :], w=["kT%d_%d" % (c, r_)])
                kTk.append("kT%d_%d" % (c, r_))
    cx.memset(Va[:, :, :, 64:65], 1.0, w=["Vones"], eng="pool")
    chunks = [(0, TC // 128, dr["V_c"], 0)]
    k0 = TC // 128
    while k0 < NKT:
        k1 = min(NKT, k0 + 10)
        chunks.append((k0, k1, dr["gv"], (k0 - TC // 128) * 128))
        k0 = k1
    nst = len(chunks)
    for i, (k0, k1, src, row0) in enumerate(chunks):
        b = i % 2
        cx.dma("sp", vst[b][:, 0:k1 - k0, :], src[row0:row0 + (k1 - k0) * 128, :].rearrange("(k p) e -> p k e", p=128), w=["vst%d" % b])
        cx.copy(Va[:, k0:k1, :, 0:64], vst[b][:, 0:k1 - k0, :].rearrange("p k (h e) -> p k h e", h=4), r=["vst%d" % b],
                w=["Va%d" % i], eng=("pool" if i % 2 == 0 else "dve"))
    Vak = ["Va%d" % i for i in range(nst)] + ["Vones"]
    lq = cx.sb("lq", [128, 4, 32]); lp = cx.sb("lp", [128, 2, 32]); ls = cx.sb("ls", [128, 4])
    for i, nm in enumerate(("lam_q1", "lam_k1", "lam_q2", "lam_k2")):
        cx.dma("sp", lq[:, i, :], dr[nm][l, :].partition_broadcast(128), w=["lq"])
    cx.tt(lp[:, 0, :], lq[:, 0, :], lq[:, 1, :], ALU.mult, r=["lq"], w=["lp"])
    cx.tt(lp[:, 1, :], lq[:, 2, :], lq[:, 3, :], ALU.mult, r=["lq"], w=["lp"])
    cx.red(ls[:, 0:2], lp[:, :, :], ALU.add, r=["lp"], w=["ls"])
    cx.act(ls[:, 0:2], ls[:, 0:2], AF.Exp, r=["ls"], w=["ls"])
    cx.tt(ls[:, 2:3], ls[:, 1:2], ls[:, 0:1], ALU.subtract, r=["ls"], w=["ls2"])
    cx.ts(ls[:, 3:4], ls[:, 2:3], -lam_init, ALU.add, r=["ls2"], w=["nlam"])
    nlam = ls[:, 3:4]
    dgb = cx.sb("dgb", [128, 4, 64])
    for h in range(4):
        cx.dma("sp", dgb[:, h, :], dr["diff_g"][l, :].partition_broadcast(128), w=["dgb"])
    cx.ts(dgb[:, :, :], dgb[:, :, :], 1.0 - lam_init, ALU.mult, r=["dgb"], w=["dgb"])
    qg = [cx.sb("qg%d" % i, [128, 2, 512], BF16) for i in range(2)]
    qm = [cx.sb("qm%d" % i, [128, 8, 512], BF16) for i in range(2)]
    pT = [cx.sb("pT%d" % i, [128, NB, 512], BF16) for i in range(NSB)]
    oT = cx.sb("oT", [65, 2, 512])
    oatt = cx.sb("oatt", [128, 4, 4, 64]); osq = cx.sb("aosq", [128, 4, 64])
    rr = cx.sb("rr", [128, 4]); ast = cx.sb("ast", [128, 2, 4])
    ysb = [cx.sb("aysb%d" % i, [128, 256]) for i in range(2)]
    yT = cx.sb("ayT", [128, 2, 512], BF16)
    gi = 0
    for (tag, T, nkt) in segs:
        sfx = "_" + tag
        G = min(512, T)
        nt = G // 128
        assert nkt % NB == 0
        for g in range(T // G):
            t0 = g * G
            b = gi % 2
            gi += 1
            cx.dma("sp", qg[b][:, :, 0:G], dr["qT" + sfx][:, :, t0:t0 + G].rearrange("c p t -> p c t"),
                   r=["D:rope" + sfx + "10", "D:rope" + sfx + "11"], w=["qg%d" % b])
            qb_ = b
            for c in range(2):
                for bl in range(4):
                    cx.ts(qm[qb_][:, c * 4 + bl, 0:G], qg[b][:, c, 0:G], cs(cst, "bm8")[:, bl:bl + 1], ALU.mult,
                          r=["qg%d" % b, "cst"], w=["qm%d_%d" % (qb_, c * 4 + bl)])
            items = [(h, m, kb) for h in range(4) for m in range(2) for kb in range(nkt // NB)]
            LA = 2

            def emit_S(i):
                h, m, kb = items[i]
                c = h // 2
                qi = c * 4 + (h % 2) * 2 + m
                sb_ = i % NSB
                for j in range(NB):
                    kt = kb * NB + j
                    kk = "kTc%d" % c if kt < TC // 128 else "kT%d_%d" % (c, (kt * 128 - TC) // TL)
                    cx.mm(psS[sb_][:, j * 512:j * 512 + G], kT[:, c, kt * 128:(kt + 1) * 128], qm[qb_][:, qi, 0:G], True, True,
                          r=[kk, "qm%d_%d" % (qb_, qi)], w=["psS%d" % sb_])
                cx.act(pT[sb_][:, :, 0:G], psS[sb_][:, :].rearrange("p (j n) -> p j n", j=NB)[:, :, 0:G], AF.Exp, scale=QSCALE,
                       r=["psS%d" % sb_], w=["pT%d" % sb_])

            def emit_O(i):
                h, m, kb = items[i]
                sb_ = i % NSB
                for j in range(NB):
                    kt = kb * NB + j
                    vk = "Va0" if kt < TC // 128 else "Va%d" % (1 + (kt - TC // 128) // 10)
                    cx.mm(psO[m][0:65, 0:G], Va[:, kt, h, :], pT[sb_][:, j, 0:G], kt == 0, kt == nkt - 1,
                          r=[vk, "Vones", "pT%d" % sb_], w=["psO%d" % m])
                if kb == nkt // NB - 1:
                    cx.copy(oT[:, m, 0:G], psO[m][0:65, 0:G], r=["psO%d" % m], w=["oT%d" % m])
                    if m == 1:
                        head_epilogue(h)

            def head_epilogue(h):
                for j in range(nt):
                    for m in range(2):
                        cx.tr(psO[0][:, m * 65:m * 65 + 65], oT[0:65, m, j * 128:(j + 1) * 128], cs(cst, "I1")[0:65, 0:65],
                              r=["oT%d" % m, "cst"], w=["psO0"])
                    cx.recip(rr[:, 0:1], psO[0][:, 64:65], r=["psO0"], w=["rr0"])
                    cx.recip(rr[:, 1:2], psO[0][:, 129:130], r=["psO0"], w=["rr1"])
                    cx.tt(rr[:, 2:3], rr[:, 1:2], nlam, ALU.mult, r=["rr1", "nlam"], w=["rr2"])
                    cx.ts(oatt[:, j, h, :], psO[0][:, 0:64], rr[:, 0:1], ALU.mult, r=["psO0", "rr0"], w=["oatt%d" % j])
                    cx.stt(oatt[:, j, h, :], psO[0][:, 65:129], rr[:, 2:3], oatt[:, j, h, :], ALU.mult, ALU.add,
                           r=["psO0", "rr2", "oatt%d" % j], w=["oatt%d" % j])

            if ATT_ROW:
                items = [(h, kt) for h in range(4) for kt in range(nkt)]

                def emit_S(i):
                    h, kt = items[i]
                    c = h // 2
                    sb_ = i % NSB
                    kk = "kTc%d" % c if kt < TC // 128 else "kT%d_%d" % (c, (kt * 128 - TC) // TL)
                    for m in range(2):
                        blk = (h % 2) * 2 + m
                        rs = slice(32 * blk, 32 * blk + 32)
                        cx.mm(psS[sb_][:, m * 512:m * 512 + G], kT[rs, c, kt * 128:(kt + 1) * 128], qg[b][rs, c, 0:G], True, True,
                              r=[kk, "qg%d" % b], w=["psS%d" % sb_], tile_position=(32 * blk, 0))
                    cx.act(pT[sb_][:, :, 0:G], psS[sb_][:, :].rearrange("p (j n) -> p j n", j=NB)[:, :, 0:G], AF.Exp, scale=QSCALE,
                           r=["psS%d" % sb_], w=["pT%d" % sb_])

                def emit_O(i):
                    h, kt = items[i]
                    sb_ = i % NSB
                    vk = "Va0" if kt < TC // 128 else "Va%d" % (1 + (kt - TC // 128) // 10)
                    for m in range(2):
                        cx.mm(psO[m][0:65, 0:G], Va[:, kt, h, :], pT[sb_][:, m, 0:G], kt == 0, kt == nkt - 1,
                              r=[vk, "Vones", "pT%d" % sb_], w=["psO%d" % m])
                    if kt == nkt - 1:
                        for m in range(2):
                            cx.copy(oT[:, m, 0:G], psO[m][0:65, 0:G], r=["psO%d" % m], w=["oT%d" % m])
                        head_epilogue(h)
            n_it = len(items)
            for i in range(n_it + LA):
                if i < n_it:
                    emit_S(i)
                if i >= LA:
                    emit_O(i - LA)
            for j in range(nt):
                yb = j % 2
                cx.tt(osq[:, :, :], oatt[:, j, :, :], oatt[:, j, :, :], ALU.mult, r=["oatt%d" % j], w=["aosq"], eng="pool")
                cx.red(ast[:, 0, :], osq[:, :, :], ALU.add, r=["aosq"], w=["ast0"])
                cx.act(ast[:, 1, :], ast[:, 0, :], AF.Sqrt, scale=1.0 / 64, bias=EPS, r=["ast0"], w=["ast1"])
                cx.recip(ast[:, 1, :], ast[:, 1, :], r=["ast1"], w=["ast1"])
                for h in range(4):
                    cx.stt(oatt[:, j, h, :], oatt[:, j, h, :], ast[:, 1, h:h + 1], dgb[:, h, :], ALU.mult, ALU.mult,
                           r=["oatt%d" % j, "ast1", "dgb"], w=["oatt%d" % j])
                cx.copy(ysb[yb][:, :], oatt[:, j, :, :].rearrange("p h e -> p (h e)"), r=["oatt%d" % j], w=["aysb%d" % yb], eng="pool")
                for c2 in range(2):
                    cx.tr(psO[1][:, c2 * 128:(c2 + 1) * 128], ysb[yb][:, c2 * 128:(c2 + 1) * 128], cs(cst, "I1"),
                          r=["aysb%d" % yb, "cst"], w=["psO1"])
                cx.copy(yT[:, :, j * 128:(j + 1) * 128], psO[1][:, 0:256].rearrange("p (c t) -> p c t", c=2), r=["psO1"], w=["ayT"])
            for c2 in range(2):
                cx.dma("sp", dr["ysT" + sfx][4 + c2, :, t0:t0 + G], yT[:, c2, 0:G], r=["ayT"], w=["D:ys2" + sfx])
    cx.end()


def phase_moe(cx, l, dr, segs, final, wl=None):
    wl = l if wl is None else wl
    cx.begin(nf=6, nb=2)
    ps, psb = cx.ps, cx.psb
    NT = sum(s[1] for s in segs) // 128
    TT = NT * 128
    cst = cx.sb("cst", [128, CSTW])
    ident = cx.sb("ident", [128, 128], BF16)
    cx.dma("sp", cst[:, :], dr["cst"][:, :], w=["cst"])
    cx.dma("sp", ident[:, :], dr["ident"][:, :], w=["ident"])
    h2T = cx.sb("h2T", [128, 8, TT], BF16)
    acc = cx.sb("eacc", [128, NT, D])
    wt = cx.sb("wt", [128, NT, 16])
    WR = cx.sb("WRt", [128, 8, 16], BF16)
    cx.dma("pool", WR[:, :, :], dr["w_router"].rearrange("(k p) e -> p k e", p=128), w=["WRt"])
    brb = cx.sb("brb", [128, 16])
    cx.dma("sp", brb[:, :], dr["b_router"][0, :].partition_broadcast(128), w=["brb"])
    gsb = cx.sb("gsb2", [128, D]); shb = cx.sb("shb2", [128, D]); g2b = cx.sb("g2b2", [128, D])
    xt = [cx.sb("ext%d" % i, [128, D]) for i in range(2)]
    junk = cx.sb("ejunk", [128, D], BF16)
    t1 = cx.sb("et1", [128, D])
    hb = [cx.sb("ehb%d" % i, [128, D], BF16) for i in range(2)]
    ssq = cx.sb("essq", [128, 2]); rstd = cx.sb("erstd", [128, 2])
    rt = cx.sb("rt", [128, 8, 16])
    W1 = [cx.sb("W1_%d" % i, [128, 8, DFF], BF16) for i in range(2)]
    W3 = [cx.sb("W3_%d" % i, [128, 8, DFF], BF16) for i in range(2)]
    W2 = [cx.sb("W2_%d" % i, [128, 4, D], BF16) for i in range(2)]

    def load_expert(e):
        b = e % 2
        cx.dma("pool", W1[b][:, :, :], dr["w1_e"][wl, e].rearrange("(k p) f -> p k f", p=128), w=["W1_%d" % b])
        cx.dma("pool", W3[b][:, :, :], dr["w3_e"][wl, e].rearrange("(k p) f -> p k f", p=128), w=["W3_%d" % b])
        cx.dma("pool", W2[b][:, :, :], dr["w2_e"][wl, e].rearrange("(k p) n -> p k n", p=128), w=["W2_%d" % b])
    load_expert(0)
    load_expert(1)
    ti = 0
    tiles = []
    for (tag, T, xmid, xout) in segs:
        seg = 0 if tag == "l" else 1
        cx.dma("sp", gsb[:, :], dr["modv"][l, seg, 3, :].partition_broadcast(128), r=["D:modv%d" % l], w=["gsb2"])
        cx.dma("sp", shb[:, :], dr["modv"][l, seg, 4, :].partition_broadcast(128), r=["D:modv%d" % l], w=["shb2"])
        for j in range(T // 128):
            b = ti % 2
            kx = "ext%d" % b
            rows = slice(j * 128, (j + 1) * 128)
            tiles.append((tag, seg, xmid, xout, rows))
            cx.dma("sp", xt[b][:, :], xmid[rows, :], r=["D:xmid_" + tag], w=[kx])
            cx.act(junk[:, :], xt[b][:, :], AF.Square, accum_out=ssq[:, b:b + 1], r=[kx], w=["ejunk", "essq%d" % b])
            cx.act(rstd[:, b:b + 1], ssq[:, b:b + 1], AF.Sqrt, scale=1.0 / D, bias=EPS, r=["essq%d" % b], w=["ers%d" % b])
            cx.recip(rstd[:, b:b + 1], rstd[:, b:b + 1], r=["ers%d" % b], w=["ers%d" % b])
            cx.stt(t1[:, :], xt[b][:, :], rstd[:, b:b + 1], gsb[:, :], ALU.mult, ALU.mult, r=[kx, "ers%d" % b, "gsb2"], w=["et1"])
            cx.tt(hb[b][:, :], t1[:, :], shb[:, :], ALU.add, r=["et1", "shb2"], w=["ehb%d" % b], eng="pool")
            for kc in range(8):
                cx.tr(psb[0][:, kc * 128:(kc + 1) * 128], hb[b][:, kc * 128:(kc + 1) * 128], ident[:, :],
                      r=["ehb%d" % b, "ident"], w=["psb0"])
            cx.copy(h2T[:, :, ti * 128:(ti + 1) * 128], psb[0][:, :].rearrange("p (k t) -> p k t", k=8),
                    r=["psb0"], w=["h2T%d" % ti], eng="act")
            for kc in range(8):
                cx.mm(ps[0][:, 0:16], h2T[:, kc, ti * 128:(ti + 1) * 128], WR[:, kc, :], kc == 0, kc == 7,
                      r=["h2T%d" % ti, "WRt"], w=["ps0"])
            s_ = rt[:, 0, :]; sbv = rt[:, 1, :]; tmp = rt[:, 2, :]; sb2 = rt[:, 3, :]; sbm = rt[:, 4, :]
            msk = rt[:, 5, :]; sel = rt[:, 6, :]
            g4 = rt[:, 7, 0:4]; g4b = rt[:, 7, 4:8]; gm = rt[:, 7, 8:12]; e1 = rt[:, 7, 12:13]; e2 = rt[:, 7, 13:14]
            den = rt[:, 7, 14:15]
            cx.act(s_, ps[0][:, 0:16], AF.Sigmoid, r=["ps0"], w=["r_s"])
            cx.tt(sbv, s_, brb[:, :], ALU.add, r=["r_s", "brb"], w=["r_sb"])
            v4 = lambda a: a.rearrange("p (g e) -> p g e", g=4)
            cx.red(g4, v4(sbv), ALU.max, r=["r_sb"], w=["r_g4"])
            for g_ in range(4):
                cx.ts(tmp[:, g_ * 4:(g_ + 1) * 4], sbv[:, g_ * 4:(g_ + 1) * 4], g4[:, g_:g_ + 1], ALU.is_equal,
                      r=["r_sb", "r_g4"], w=["r_tmp"])
            cx.stt(sb2, tmp, -1.0e9, sbv, ALU.mult, ALU.add, r=["r_tmp", "r_sb"], w=["r_sb2"])
            cx.red(g4b, v4(sb2), ALU.max, r=["r_sb2"], w=["r_g4b"])
            cx.tt(g4, g4, g4b, ALU.add, r=["r_g4", "r_g4b"], w=["r_g4"])
            cx.red(e1, g4, ALU.max, r=["r_g4"], w=["r_e1"])
            cx.ts(gm, g4, e1, ALU.is_equal, s2=-1.0, op1=ALU.add, r=["r_g4", "r_e1"], w=["r_gm"])
            for g_ in range(4):
                cx.ts(tmp[:, g_ * 4:(g_ + 1) * 4], cs(cst, "ones")[:, 0:4], gm[:, g_:g_ + 1], ALU.mult,
                      r=["r_gm", "cst"], w=["r_tmp"])
            cx.stt(sbm, tmp, 1.0e9, sbv, ALU.mult, ALU.add, r=["r_tmp", "r_sb"], w=["r_sbm"])
            cx.red(e1, sbm, ALU.max, r=["r_sbm"], w=["r_e1"])
            cx.ts(msk, sbm, e1, ALU.is_equal, r=["r_sbm", "r_e1"], w=["r_msk"])
            cx.stt(sb2, msk, -1.0e9, sbm, ALU.mult, ALU.add, r=["r_msk", "r_sbm"], w=["r_sb2"])
            cx.red(e2, sb2, ALU.max, r=["r_sb2"], w=["r_e2"])
            cx.ts(sel, sb2, e2, ALU.is_equal, r=["r_sb2", "r_e2"], w=["r_sel"])
            cx.tt(sel, sel, msk, ALU.add, r=["r_sel", "r_msk"], w=["r_sel"])
            cx.tt(sel, sel, s_, ALU.mult, r=["r_sel", "r_s"], w=["r_sel"])
            cx.red(den, sel, ALU.add, r=["r_sel"], w=["r_den"])
            cx.recip(den, den, r=["r_den"], w=["r_den"])
            cx.ts(wt[:, ti, :], sel, den, ALU.mult, r=["r_sel", "r_den"], w=["wt%d" % ti])
            ti += 1
    uT = [cx.sb("uT%d" % i, [128, 4, 512], BF16) for i in range(2)]
    s1 = [cx.sb("s1_%d" % i, [128, 512]) for i in range(2)]
    groups = []
    t = 0
    while t < NT:
        n = min(4, NT - t)
        groups.append((t, n))
        t += n
    ui = 0
    for e in range(NEXP):
        b = e % 2
        if e >= 2:
            load_expert(e)
        for (tg, ntl) in groups:
            G = ntl * 128
            gsl = slice(tg * 128, tg * 128 + G)
            hk = ["h2T%d" % i for i in range(tg, tg + ntl)]
            ub = ui % 2
            ui += 1
            for fc in range(4):
                fs = slice(fc * 128, (fc + 1) * 128)
                pa = ps[(fc % 2) * 2]; pb = ps[(fc % 2) * 2 + 1]
                ka = "ps%d" % ((fc % 2) * 2); kb = "ps%d" % ((fc % 2) * 2 + 1)
                for kc in range(8):
                    cx.mm(pa[:, 0:G], W1[b][:, kc, fs], h2T[:, kc, gsl], kc == 0, kc == 7, r=hk + ["W1_%d" % b], w=[ka])
                for kc in range(8):
                    cx.mm(pb[:, 0:G], W3[b][:, kc, fs], h2T[:, kc, gsl], kc == 0, kc == 7, r=hk + ["W3_%d" % b], w=[kb])
                sb_ = s1[fc % 2]
                cx.act(sb_[:, 0:G], pa[:, 0:G], AF.Silu, r=[ka], w=["s1_%d" % (fc % 2)])
                cx.tt(uT[ub][:, fc, 0:G], pb[:, 0:G], sb_[:, 0:G], ALU.mult, r=[kb, "s1_%d" % (fc % 2)], w=["uT%d" % ub])
            for j in range(ntl):
                tix = tg + j
                for nh in range(2):
                    po = ps[4 + nh]
                    for fc in range(4):
                        cx.mm(po[:, :], uT[ub][:, fc, j * 128:(j + 1) * 128], W2[b][:, fc, nh * 512:(nh + 1) * 512], fc == 0, fc == 3,
                              r=["uT%d" % ub, "W2_%d" % b], w=["ps%d" % (4 + nh)])
                    a = acc[:, tix, nh * 512:(nh + 1) * 512]
                    ka2 = "eacc%d_%d" % (tix, nh)
                    if e == 0:
                        cx.ts(a, po[:, :], wt[:, tix, e:e + 1], ALU.mult, r=["ps%d" % (4 + nh), "wt%d" % tix], w=[ka2])
                    else:
                        cx.stt(a, po[:, :], wt[:, tix, e:e + 1], a, ALU.mult, ALU.add, r=["ps%d" % (4 + nh), "wt%d" % tix, ka2], w=[ka2])
    if final:
        gfb = cx.sb("gfb", [128, D])
        cx.dma("sp", gfb[:, :], dr["g_final"][0, :].partition_broadcast(128), w=["gfb"])
    cur = None
    for ti, (tag, seg, xmid, xout, rows) in enumerate(tiles):
        if cur != seg:
            cx.dma("sp", g2b[:, :], dr["modv"][l, seg, 5, :].partition_broadcast(128), r=["D:modv%d" % l], w=["g2b2"])
            cur = seg
        b = ti % 2
        kx = "ext%d" % b
        cx.dma("sp", xt[b][:, :], xmid[rows, :], r=["D:xmid_" + tag], w=[kx])
        cx.tt(t1[:, :], acc[:, ti, :], g2b[:, :], ALU.mult, r=["eacc%d_0" % ti, "eacc%d_1" % ti, "g2b2"], w=["et1"], eng="pool")
        cx.tt(xt[b][:, :], xt[b][:, :], t1[:, :], ALU.add, r=[kx, "et1"], w=[kx])
        if final:
            cx.act(junk[:, :], xt[b][:, :], AF.Square, accum_out=ssq[:, b:b + 1], r=[kx], w=["ejunk", "essq%d" % b])
            cx.act(rstd[:, b:b + 1], ssq[:, b:b + 1], AF.Sqrt, scale=1.0 / D, bias=EPS, r=["essq%d" % b], w=["ers%d" % b])
            cx.recip(rstd[:, b:b + 1], rstd[:, b:b + 1], r=["ers%d" % b], w=["ers%d" % b])
            cx.stt(xt[b][:, :], xt[b][:, :], rstd[:, b:b + 1], gfb[:, :], ALU.mult, ALU.mult, r=[kx, "ers%d" % b, "gfb"], w=[kx])
        cx.dma("sp", xout[rows, :], xt[b][:, :], r=[kx], w=["D:xout_" + tag])
    cx.end()


def phase_moe_sparse(cx, l, dr, segs, final, wl=None):
    wl = l if wl is None else wl
    C = MOE_CAP
    cx.begin(nf=6, nb=2)
    ps, psb = cx.ps, cx.psb
    NT = sum(s[1] for s in segs) // 128
    cst = cx.sb("cst", [128, CSTW])
    ident = cx.sb("ident", [128, 128], BF16)
    cx.dma("sp", cst[:, :], dr["cst"][:, :], w=["cst"])
    cx.dma("sp", ident[:, :], dr["ident"][:, :], w=["ident"])
    Xg = dr["Xg"]
    Yg = dr["Yg"]
    bcreg = {}

    def bc(h):
        if "r" not in bcreg:
            bcreg["r"] = h.to_reg(NSLOT - 1)
        return bcreg["r"]
    W1 = [cx.sb("W1_%d" % i, [128, 8, DFF], BF16) for i in range(2)]
    W3 = [cx.sb("W3_%d" % i, [128, 8, DFF], BF16) for i in range(2)]
    W2 = [cx.sb("W2_%d" % i, [128, 4, D], BF16) for i in range(2)]

    def load_expert(e):
        b = e % 2
        cx.dma("pool", W1[b][:, :, :], dr["w1_e"][wl, e].rearrange("(k p) f -> p k f", p=128), w=["W1_%d" % b])
        cx.dma("pool", W3[b][:, :, :], dr["w3_e"][wl, e].rearrange("(k p) f -> p k f", p=128), w=["W3_%d" % b])
        cx.dma("pool", W2[b][:, :, :], dr["w2_e"][wl, e].rearrange("(k p) n -> p k n", p=128), w=["W2_%d" % b])
    load_expert(0)
    load_expert(1)
    zt = cx.sb("zt", [128, 4, D], BF16)
    cx.memset(zt[:, :, :], 0.0, w=["zt"], eng="pool")
    for i in range(NSLOT // 512):
        cx.dma("sp", Xg[i * 512:(i + 1) * 512, :].rearrange("(b p) d -> p b d", p=128), zt[:, :, :], r=["zt"], w=["D:XgZ%d" % i])
    WR = cx.sb("WRt", [128, 8, 16], BF16)
    cx.dma("pool", WR[:, :, :], dr["w_router"].rearrange("(k p) e -> p k e", p=128), w=["WRt"])
    brb = cx.sb("brb", [128, 16])
    cx.dma("sp", brb[:, :], dr["b_router"][0, :].partition_broadcast(128), w=["brb"])
    gsb = cx.sb("gsb2", [128, D]); shb = cx.sb("shb2", [128, D]); g2b = cx.sb("g2b2", [128, D])
    xt = [cx.sb("ext%d" % i, [128, D]) for i in range(2)]
    junk = cx.sb("ejunk", [128, D], BF16)
    t1s = [cx.sb("et1_%d" % i, [128, D]) for i in range(2)]
    hb = [cx.sb("ehb%d" % i, [128, D], BF16) for i in range(2)]
    hTt = [cx.sb("ehT%d" % i, [128, 8, 128], BF16) for i in range(2)]
    ssq = cx.sb("essq", [128, 2]); rstd = cx.sb("erstd", [128, 2])
    rts = [cx.sb("rt%d" % i, [128, 10, 16]) for i in range(2)]
    off = cx.sb("roff", [128, 16])
    slf = cx.sb("slf", [128, NT, 2]); sli = cx.sb("sli", [128, NT, 2], mybir.dt.int32); wts = cx.sb("wts", [128, NT, 2])
    cx.memset(off[:, :], 0.0, w=["roff"])
    ti = 0
    tiles = []
    for (tag, T, xmid, xout) in segs:
        seg = 0 if tag == "l" else 1
        cx.dma("sp", gsb[:, :], dr["modv"][l, seg, 3, :].partition_broadcast(128), r=["D:modv%d" % l], w=["gsb2"])
        cx.dma("sp", shb[:, :], dr["modv"][l, seg, 4, :].partition_broadcast(128), r=["D:modv%d" % l], w=["shb2"])
        for j in range(T // 128):
            b = ti % 2
            kx = "ext%d" % b
            rt = rts[b]
            t1 = t1s[b]
            K_ = lambda n: "%s_%d" % (n, b)
            rows = slice(j * 128, (j + 1) * 128)
            tiles.append((tag, seg, xmid, xout, rows))
            cx.dma("sp", xt[b][:, :], xmid[rows, :], r=["D:xmid_" + tag], w=[kx])
            cx.act(junk[:, :], xt[b][:, :], AF.Square, accum_out=ssq[:, b:b + 1], r=[kx], w=[K_("ejunk"), "essq%d" % b])
            cx.act(rstd[:, b:b + 1], ssq[:, b:b + 1], AF.Sqrt, scale=1.0 / D, bias=EPS, r=["essq%d" % b], w=["ers%d" % b])
            cx.recip(rstd[:, b:b + 1], rstd[:, b:b + 1], r=["ers%d" % b], w=["ers%d" % b])
            cx.stt(t1[:, :], xt[b][:, :], rstd[:, b:b + 1], gsb[:, :], ALU.mult, ALU.mult, r=[kx, "ers%d" % b, "gsb2"], w=[K_("et1")])
            cx.tt(hb[b][:, :], t1[:, :], shb[:, :], ALU.add, r=[K_("et1"), "shb2"], w=["ehb%d" % b], eng="pool")
            for kc in range(8):
                cx.tr(psb[0][:, kc * 128:(kc + 1) * 128], hb[b][:, kc * 128:(kc + 1) * 128], ident[:, :],
                      r=["ehb%d" % b, "ident"], w=["psb0"])
            cx.copy(hTt[b][:, :, :], psb[0][:, :].rearrange("p (k t) -> p k t", k=8), r=["psb0"], w=["ehT%d" % b], eng="act")
            for kc in range(8):
                cx.mm(ps[0][:, 0:16], hTt[b][:, kc, :], WR[:, kc, :], kc == 0, kc == 7, r=["ehT%d" % b, "WRt"], w=["ps0"])
            s_ = rt[:, 0, :]; sbv = rt[:, 1, :]; tmp = rt[:, 2, :]; sb2 = rt[:, 3, :]; sbm = rt[:, 4, :]
            msk = rt[:, 5, :]; sel = rt[:, 6, :]; pos = rt[:, 8, :]; m2 = rt[:, 9, :]
            g4 = rt[:, 7, 0:4]; g4b = rt[:, 7, 4:8]; gm = rt[:, 7, 8:12]; e1 = rt[:, 7, 12:13]; e2 = rt[:, 7, 13:14]
            den = rt[:, 7, 14:15]
            cx.act(s_, ps[0][:, 0:16], AF.Sigmoid, r=["ps0"], w=[K_("r_s")])
            cx.tt(sbv, s_, brb[:, :], ALU.add, r=[K_("r_s"), "brb"], w=[K_("r_sb")])
            v4 = lambda a: a.rearrange("p (g e) -> p g e", g=4)
            cx.red(g4, v4(sbv), ALU.max, r=[K_("r_sb")], w=[K_("r_g4")])
            for g_ in range(4):
                cx.ts(tmp[:, g_ * 4:(g_ + 1) * 4], sbv[:, g_ * 4:(g_ + 1) * 4], g4[:, g_:g_ + 1], ALU.is_equal,
                      r=[K_("r_sb"), K_("r_g4")], w=[K_("r_tmp")])
            cx.stt(sb2, tmp, -1.0e9, sbv, ALU.mult, ALU.add, r=[K_("r_tmp"), K_("r_sb")], w=[K_("r_sb2")])
            cx.red(g4b, v4(sb2), ALU.max, r=[K_("r_sb2")], w=[K_("r_g4b")])
            cx.tt(g4, g4, g4b, ALU.add, r=[K_("r_g4"), K_("r_g4b")], w=[K_("r_g4")])
            cx.red(e1, g4, ALU.max, r=[K_("r_g4")], w=[K_("r_e1")])
            cx.ts(gm, g4, e1, ALU.is_equal, s2=-1.0, op1=ALU.add, r=[K_("r_g4"), K_("r_e1")], w=[K_("r_gm")])
            for g_ in range(4):
                cx.ts(tmp[:, g_ * 4:(g_ + 1) * 4], cs(cst, "ones")[:, 0:4], gm[:, g_:g_ + 1], ALU.mult,
                      r=[K_("r_gm"), "cst"], w=[K_("r_tmp")])
            cx.stt(sbm, tmp, 1.0e9, sbv, ALU.mult, ALU.add, r=[K_("r_tmp"), K_("r_sb")], w=[K_("r_sbm")])
            cx.red(e1, sbm, ALU.max, r=[K_("r_sbm")], w=[K_("r_e1")])
            cx.ts(msk, sbm, e1, ALU.is_equal, r=[K_("r_sbm"), K_("r_e1")], w=[K_("r_msk")])
            cx.stt(sb2, msk, -1.0e9, sbm, ALU.mult, ALU.add, r=[K_("r_msk"), K_("r_sbm")], w=[K_("r_sb2")])
            cx.red(e2, sb2, ALU.max, r=[K_("r_sb2")], w=[K_("r_e2")])
            cx.ts(m2, sb2, e2, ALU.is_equal, r=[K_("r_sb2"), K_("r_e2")], w=[K_("r_m2")])
            cx.tt(sel, m2, msk, ALU.add, r=[K_("r_m2"), K_("r_msk")], w=[K_("r_sel")])
            cx.mm(ps[1][:, 0:16], cs(cst, "Ltri"), sel, True, True, r=["cst", K_("r_sel")], w=["ps1"])
            cx.mm(ps[1][:, 16:32], cs(cst, "ones"), sel, True, True, r=["cst", K_("r_sel")], w=["ps1"])
            cx.tt(pos, ps[1][:, 0:16], off[:, :], ALU.add, r=["ps1", "roff"], w=[K_("r_pos")])
            cx.tt(off[:, :], ps[1][:, 16:32], off[:, :], ALU.add, r=["ps1", "roff"], w=["roff"])
            cx.ts(tmp, pos, float(C) - 0.5, ALU.is_lt, r=[K_("r_pos")], w=[K_("r_tmp")])
            cx.tt(pos, pos, cs(cst, "eoff"), ALU.add, r=[K_("r_pos"), "cst"], w=[K_("r_pos")])
            cx.stt(pos, tmp, -1.0e6, pos, ALU.mult, ALU.add, r=[K_("r_tmp"), K_("r_pos")], w=[K_("r_pos")])
            cx.ts(pos, pos, 1.0e6, ALU.add, r=[K_("r_pos")], w=[K_("r_pos")])
            cx.tt(sb2, sel, s_, ALU.mult, r=[K_("r_sel"), K_("r_s")], w=[K_("r_sb2")])
            cx.red(den, sb2, ALU.add, r=[K_("r_sb2")], w=[K_("r_den")])
            cx.recip(den, den, r=[K_("r_den")], w=[K_("r_den")])
            cx.ts(sb2, sb2, den, ALU.mult, r=[K_("r_sb2"), K_("r_den")], w=[K_("r_sb2")])
            cx.tt(sb2, sb2, tmp, ALU.mult, r=[K_("r_sb2"), K_("r_tmp")], w=[K_("r_sb2")])
            for q, mk, kk in ((0, msk, K_("r_msk")), (1, m2, K_("r_m2"))):
                cx.tt(sbm, mk, pos, ALU.mult, r=[kk, K_("r_pos")], w=[K_("r_sbm")])
                cx.red(slf[:, ti, q:q + 1], sbm, ALU.add, r=[K_("r_sbm")], w=["slf%d_%d" % (ti, q)])
                cx.tt(sbm, mk, sb2, ALU.mult, r=[kk, K_("r_sb2")], w=[K_("r_sbm")])
                cx.red(wts[:, ti, q:q + 1], sbm, ALU.add, r=[K_("r_sbm")], w=["wts%d_%d" % (ti, q)])
            cx.copy(sli[:, ti, :], slf[:, ti, :], r=["slf%d_0" % ti, "slf%d_1" % ti], w=["sli%d" % ti])
            for q in range(2):
                idx = sli[:, ti, q:q + 1]
                src = hb[b][:, :]
                cx.S.add("pool", lambda h, idx=idx, src=src: h.indirect_dma_start(
                    out=Xg[:, :], out_offset=bass.IndirectOffsetOnAxis(ap=idx, axis=0), in_=src, in_offset=None,
                    bounds_check=bc(h), oob_is_err=False), r=["sli%d" % ti, "ehb%d" % b] + ["D:XgZ%d" % i_ for i_ in range(NSLOT // 512)], w=["D:Xg%d_%d" % (ti, q)], dma=True)
            ti += 1
    dummy = cx.sb("dummy", [128, 4])
    cx.memset(dummy[:, 0:1], 0.0, w=["XgAll"], eng="pool")
    cx.S.ops[-1].deps.update({cx.S.lastw[k]: True for k in ["D:Xg%d_%d" % (t_, q) for t_ in range(NT) for q in range(2)]})
    NBLK = C // 128
    NPC = (C + 511) // 512
    PW = C // NPC
    xg = [cx.sb("xg%d" % i, [128, NBLK, D], BF16) for i in range(2)]
    xT = [cx.sb("xTe%d" % i, [128, 8, C], BF16) for i in range(2)]
    uT = cx.sb("uTe", [128, 4, C], BF16)
    s1 = [cx.sb("s1_%d" % i, [128, 512]) for i in range(2)]
    yb = [cx.sb("ybe%d" % i, [128, D]) for i in range(2)]
    yi = 0

    def load_xg(e):
        cx.dma("sp", xg[e % 2][:, :, :], Xg[e * C:(e + 1) * C, :].rearrange("(j p) d -> p j d", p=128), r=["XgAll"], w=["xg%d" % (e % 2)])
    load_xg(0)
    for e in range(NEXP):
        b = e % 2
        if e >= 2:
            load_expert(e)
        if e + 1 < NEXP:
            load_xg(e + 1)
        for j in range(NBLK):
            pbk = psb[j % 2]
            kp = "psb%d" % (j % 2)
            for kc in range(8):
                cx.tr(pbk[:, kc * 128:(kc + 1) * 128], xg[b][:, j, kc * 128:(kc + 1) * 128], ident[:, :],
                      r=["xg%d" % b, "ident"], w=[kp])
            cx.copy(xT[b][:, :, j * 128:(j + 1) * 128], pbk[:, :].rearrange("p (k t) -> p k t", k=8), r=[kp], w=["xTe%d" % b],
                    eng=("act" if j % 2 == 0 else "dve"))
        it = 0
        for fc in range(4):
            fs = slice(fc * 128, (fc + 1) * 128)
            for pc in range(NPC):
                cs_ = slice(pc * PW, (pc + 1) * PW)
                pa = ps[(it % 2) * 2]; pb = ps[(it % 2) * 2 + 1]
                ka = "ps%d" % ((it % 2) * 2); kb = "ps%d" % ((it % 2) * 2 + 1)
                for kc in range(8):
                    cx.mm(pa[:, 0:PW], W1[b][:, kc, fs], xT[b][:, kc, cs_], kc == 0, kc == 7, r=["xTe%d" % b, "W1_%d" % b], w=[ka])
                for kc in range(8):
                    cx.mm(pb[:, 0:PW], W3[b][:, kc, fs], xT[b][:, kc, cs_], kc == 0, kc == 7, r=["xTe%d" % b, "W3_%d" % b], w=[kb])
                sb_ = s1[it % 2]
                cx.act(sb_[:, 0:PW], pa[:, 0:PW], AF.Silu, r=[ka], w=["s1_%d" % (it % 2)])
                cx.tt(uT[:, fc, cs_], pb[:, 0:PW], sb_[:, 0:PW], ALU.mult, r=[kb, "s1_%d" % (it % 2)], w=["uTe"])
                it += 1
        for j in range(NBLK):
            y = yb[yi % 2]
            ky = "ybe%d" % (yi % 2)
            yi += 1
            for nh in range(2):
                po = ps[4 + nh]
                for fc in range(4):
                    cx.mm(po[:, :], uT[:, fc, j * 128:(j + 1) * 128], W2[b][:, fc, nh * 512:(nh + 1) * 512], fc == 0, fc == 3,
                          r=["uTe", "W2_%d" % b], w=["ps%d" % (4 + nh)])
                cx.copy(y[:, nh * 512:(nh + 1) * 512], po[:, :], r=["ps%d" % (4 + nh)], w=[ky], eng=("act" if nh == 0 else "dve"))
            cx.dma("sp", Yg[e * C + j * 128:e * C + (j + 1) * 128, :], y[:, :], r=[ky], w=["D:Yg%d_%d" % (e, j)])
    if final:
        gfb = cx.sb("gfb", [128, D])
        cx.dma("sp", gfb[:, :], dr["g_final"][0, :].partition_broadcast(128), w=["gfb"])
    cx.memset(dummy[:, 1:2], 0.0, w=["YgAll"], eng="pool")
    cx.S.ops[-1].deps.update({cx.S.lastw[k]: True for k in ["D:Yg%d_%d" % (e_, j_) for e_ in range(NEXP) for j_ in range(C // 128)]})
    yg = [[cx.sb("yg%d_%d" % (i, q), [128, D]) for q in range(2)] for i in range(2)]
    for i in range(2):
        for q in range(2):
            cx.memset(yg[i][q][:, :], 0.0, w=["yg%d_%d" % (i, q)], eng="pool")
    cur = None
    for ti, (tag, seg, xmid, xout, rows) in enumerate(tiles):
        if cur != seg:
            cx.dma("sp", g2b[:, :], dr["modv"][l, seg, 5, :].partition_broadcast(128), r=["D:modv%d" % l], w=["g2b2"])
            cur = seg
        b = ti % 2
        kx = "ext%d" % b
        t1 = t1s[b]
        kt1 = "et1_%d" % b
        cx.dma("sp", xt[b][:, :], xmid[rows, :], r=["D:xmid_" + tag], w=[kx])
        for q in range(2):
            dst = yg[b][q][:, :]
            idx = sli[:, ti, q:q + 1]
            cx.S.add("pool", lambda h, idx=idx, dst=dst: h.indirect_dma_start(
                out=dst, out_offset=None, in_=Yg[:, :], in_offset=bass.IndirectOffsetOnAxis(ap=idx, axis=0),
                bounds_check=bc(h), oob_is_err=False), r=["sli%d" % ti, "YgAll"], w=["yg%d_%d" % (b, q)], dma=True)
        cx.ts(t1[:, :], yg[b][0][:, :], wts[:, ti, 0:1], ALU.mult, r=["yg%d_0" % b, "wts%d_0" % ti], w=[kt1])
        cx.stt(t1[:, :], yg[b][1][:, :], wts[:, ti, 1:2], t1[:, :], ALU.mult, ALU.add, r=["yg%d_1" % b, "wts%d_1" % ti, kt1], w=[kt1])
        cx.tt(t1[:, :], t1[:, :], g2b[:, :], ALU.mult, r=[kt1, "g2b2"], w=[kt1], eng="pool")
        cx.tt(xt[b][:, :], xt[b][:, :], t1[:, :], ALU.add, r=[kx, kt1], w=[kx])
        if final:
            cx.act(junk[:, :], xt[b][:, :], AF.Square, accum_out=ssq[:, b:b + 1], r=[kx], w=["ejunk", "essq%d" % b])
            cx.act(rstd[:, b:b + 1], ssq[:, b:b + 1], AF.Sqrt, scale=1.0 / D, bias=EPS, r=["essq%d" % b], w=["ers%d" % b])
            cx.recip(rstd[:, b:b + 1], rstd[:, b:b + 1], r=["ers%d" % b], w=["ers%d" % b])
            cx.stt(xt[b][:, :], xt[b][:, :], rstd[:, b:b + 1], gfb[:, :], ALU.mult, ALU.mult, r=[kx, "ers%d" % b, "gfb"], w=[kx])
        cx.dma("sp", xout[rows, :], xt[b][:, :], r=[kx], w=["D:xout_" + tag])
    cx.end()


def make_expo(core):
    BIG = 1.0e7
    e = np.full((2, 9), BIG, np.float32)
    for c2 in range(NCORE):
        if c2 < core:
            e[0, c2] = TL * (core - 1 - c2)
        if c2 > core:
            e[1, c2] = TL * (c2 - core - 1)
    e[0, 8] = TL * core
    e[1, 8] = TL * (NCORE - 1 - core)
    return np.broadcast_to(e[None], (128, 2, 9)).copy()


def make_expo(core):
    BIG = 1.0e7
    e = np.full((2, 9), BIG, np.float32)
    for c2 in range(NCORE):
        if c2 < core:
            e[0, c2] = TL * (core - 1 - c2)
        if c2 > core:
            e[1, c2] = TL * (c2 - core - 1)
    e[0, 8] = TL * core
    e[1, 8] = TL * (NCORE - 1 - core)
    return np.broadcast_to(e[None], (128, 2, 9)).copy()


def make_sel(core):
    s = np.zeros((128, 2, NCORE), np.float32)
    if core > 0:
        s[:, 0, core - 1] = 1.0
    if core < NCORE - 1:
        s[:, 1, core + 1] = 1.0
    return s


def phase_exchange(cx, l, dr):
    cx.begin(nf=0, nb=0)
    hin = dr["hin"]
    for a, (src, c2) in enumerate(((dr["uT_l"], 0), (dr["uT_l"], 1), (dr["tT_l"], 0), (dr["tT_l"], 1))):
        cx.dma("sp", hin[a * 128:(a + 1) * 128, 0:16], src[c2, :, 0:16], r=["D:uT_l", "D:tT_l"], w=["D:hin"])
        cx.dma("sp", hin[a * 128:(a + 1) * 128, 16:32], src[c2, :, TL - 16:TL], r=["D:uT_l", "D:tT_l"], w=["D:hin"])
    grp = [list(range(NCORE))]

    def cc(src, dst, rk, wk):
        cx.S.add("pool", lambda h: h.collective_compute("AllGather", ALU.bypass, replica_groups=grp, ins=[src], outs=[dst]),
                 r=rk, w=wk, cc=True)
    cc(dr["kT_l"].rearrange("c p t -> (c p) t").opt(), dr["gk"].opt(), ["D:rope_l12", "D:rope_l13"], ["D:gk"])
    cc(dr["V_l"].opt(), dr["gv"].opt(), ["D:V_l"], ["D:gv"])
    cc(dr["Tst_l"].rearrange("a p e -> (a p) e").opt(), dr["gt"].opt(), ["D:Tst_l"], ["D:gt"])
    cc(hin.opt(), dr["hg"].opt(), ["D:hin"], ["D:hg"])
    cx.end()


A_OUT = (("hT", lambda T: [128, 8, T], BF16), ("uT", lambda T: [2, 128, T], F32), ("tT", lambda T: [2, 128, T], F32),
         ("bgT", lambda T: [2, 128, T], BF16), ("qT", lambda T: [2, 128, T], BF16), ("kT", lambda T: [2, 128, T], BF16),
         ("rqT", lambda T: [128, T], BF16), ("rkT", lambda T: [128, T], BF16), ("V", lambda T: [T, 256], BF16),
         ("rv", lambda T: [T, 256], BF16), ("rg", lambda T: [T, 256], BF16), ("Tst", lambda T: [2, 128, 256], F32))
SEGT = (("l", TL), ("c", TC))
NKALL = (SEQ + TC) // 128
EXT_IN = (("c", [1, D]), ("c_ctx", [1, D]), ("w_mod", [2, D, 6 * D]), ("b_mod", [2, 6 * D]), ("g_norm1", [2, D]), ("g_norm2", [2, D]),
          ("w_in", [2, D, INC]), ("conv_a_w", [2, 31, 256]), ("conv_a_b", [2, 256]), ("conv_a_g", [2, 256]),
          ("conv_a_beta", [2, 256]), ("conv_b_w", [2, 3, 256]), ("lam_q1", [2, 32]), ("lam_k1", [2, 32]), ("lam_q2", [2, 32]),
          ("lam_k2", [2, 32]), ("diff_g", [2, 64]), ("ret_ld_f", [2, 4]), ("ret_ld_b", [2, 4]), ("w_gate", [2, D, 4096]),
          ("b_gate", [2, 4096]), ("w_branch", [2, 4, 256, D]), ("w_o", [2, D, D]), ("w_router", [D, 16]), ("b_router", [1, 16]),
          ("w1_e", [2, NEXP, D, DFF]), ("w3_e", [2, NEXP, D, DFF]), ("w2_e", [2, NEXP, DFF, D]), ("g_final", [1, D]))


SPARSE_MOE = True


def moe_phase(cx, l, dr, segs, final, wl=None):
    if SPARSE_MOE:
        return phase_moe_sparse(cx, l, dr, segs, final, wl=wl)
    return phase_moe(cx, l, dr, segs, final, wl=wl)


def lam_init_of(l):
    return 0.8 - 0.6 * math.exp(-0.3 * l)


class Launch:
    def __init__(self):
        self.nc = bass.Bass("TRN2", target_bir_lowering=False)
        self.dr = {}
        self.ins = []
        self.outs = []

    def t(self, name, shape, dt=F32, kind=None):
        if kind is None:
            self.dr[name] = self.nc.dram_tensor(name, list(shape), dt).ap()
        else:
            self.dr[name] = self.nc.dram_tensor(name, list(shape), dt, kind=kind).ap()
        if kind == "ExternalInput":
            self.ins.append(name)
        elif kind == "ExternalOutput":
            self.outs.append(name)


def build_fused():
    L = Launch()
    L.t("cst", [128, CSTW], F32, "ExternalInput")
    L.t("ident", [128, 128], BF16, "ExternalInput")
    for n_, s_ in EXT_IN:
        L.t(n_, s_, F32, "ExternalInput")
    for n_, s_ in (("cosT", [128, TL]), ("sinT", [128, TL]), ("x_l", [TL, D]), ("x_c", [TC, D]), ("expo", [128, 2, 9]),
                   ("sel", [128, 2, NCORE])):
        L.t(n_, s_, F32, "ExternalInput")
    L.t("out", [TL, D], F32, "ExternalOutput")
    L.t("modv", [2, 2, 6, D])
    L.t("x1_l", [TL, D])
    L.t("x1_c", [TC, D])
    L.t("Xg", [NSLOT, D], BF16)
    L.t("Yg", [NSLOT, D], F32)
    drl = []
    for l in range(DEPTH):
        d_ = dict(L.dr)
        for tag, T in SEGT:
            for nm, shp, dt in A_OUT:
                L.t("%s_%s%d" % (nm, tag, l), shp(T), dt)
                d_[nm + "_" + tag] = L.dr["%s_%s%d" % (nm, tag, l)]
            L.t("ysT_%s%d" % (tag, l), [8, 128, T], BF16)
            L.t("xmid_%s%d" % (tag, l), [T, D])
            d_["ysT_" + tag] = L.dr["ysT_%s%d" % (tag, l)]
            d_["xmid_" + tag] = L.dr["xmid_%s%d" % (tag, l)]
        for nm, shp, dt in (("gk", [NCORE * 256, TL], BF16), ("gv", [NCORE * TL, 256], BF16), ("gt", [NCORE * 256, 256], F32),
                            ("hg", [NCORE * 512, 32], F32), ("hin", [512, 32], F32)):
            L.t("%s%d" % (nm, l), shp, dt)
            d_[nm] = L.dr["%s%d" % (nm, l)]
        drl.append(d_)
    for d_ in drl:
        for k in ("modv", "x1_l", "x1_c", "Xg", "Yg"):
            d_[k] = L.dr[k]
    with ExitStack() as st:
        S = Sched(L.nc, st)
        cx = Ctx(L.nc, S)
        phase_mods(cx, 0, drl[0])
        phase_mods(cx, 1, drl[0])
        d0 = drl[0]
        phase_a(cx, 0, d0, [("l", TL, L.dr["x_l"]), ("c", TC, L.dr["x_c"])])
        phase_exchange(cx, 0, d0)
        phase_conv(cx, 0, d0, [("l", TL), ("c", TC)])
        phase_attn(cx, 0, d0, [("l", TL, NKALL), ("c", TC, TC // 128)], lam_init_of(0))
        phase_ret(cx, 0, d0, [("l", TL), ("c", TC)])
        phase_merge(cx, 0, d0, [("l", TL, L.dr["x_l"], d0["xmid_l"]), ("c", TC, L.dr["x_c"], d0["xmid_c"])])
        moe_phase(cx, 0, d0, [("l", TL, d0["xmid_l"], L.dr["x1_l"]), ("c", TC, d0["xmid_c"], L.dr["x1_c"])], False)
        d1 = drl[1]
        phase_a(cx, 1, d1, [("l", TL, L.dr["x1_l"]), ("c", TC, L.dr["x1_c"])])
        phase_exchange(cx, 1, d1)
        phase_conv(cx, 1, d1, [("l", TL)])
        phase_attn(cx, 1, d1, [("l", TL, NKALL)], lam_init_of(1))
        phase_ret(cx, 1, d1, [("l", TL)])
        phase_merge(cx, 1, d1, [("l", TL, L.dr["x1_l"], d1["xmid_l"])])
        moe_phase(cx, 1, d1, [("l", TL, d1["xmid_l"], L.dr["out"])], True)
    return L


def kernel_fused(**inp):
    f32 = lambda a: np.ascontiguousarray(np.asarray(a, dtype=np.float32))
    x = f32(inp["x"])[0]
    ctx = f32(inp["ctx"])[0]
    base = dict(cst=make_cst(), ident=np.eye(128, dtype=np.float32).astype(NPBF), x_c=ctx)
    for n_, s_ in EXT_IN:
        base[n_] = f32(inp[n_]).reshape(s_)
    L = build_fused()
    maps = []
    for c in range(NCORE):
        m = dict(base)
        cosT, sinT = rope_tables(c)
        m.update(cosT=cosT, sinT=sinT, x_l=x[c * TL:(c + 1) * TL], expo=make_expo(c), sel=make_sel(c))
        maps.append({k: m[k] for k in L.ins})
    res = run_bass_kernel_spmd(L.nc, maps, core_ids=list(range(NCORE)))
    out = np.concatenate([np.asarray(res.results[c]["out"]) for c in range(NCORE)], axis=0)
    return out.reshape(1, SEQ, D).astype(np.float32)


WSLICE = ("w_in", "w_gate", "w_branch", "w_o", "w1_e", "w3_e", "w2_e")
GATH = (("gk", [NCORE * 256, TL], BF16), ("gv", [NCORE * TL, 256], BF16), ("gt", [NCORE * 256, 256], F32),
        ("hg", [NCORE * 512, 32], F32))


def build_stage(stage):
    L = Launch()
    L.t("cst", [128, CSTW], F32, "ExternalInput")
    L.t("ident", [128, 128], BF16, "ExternalInput")
    for n_, s_ in EXT_IN:
        if stage > 1 and n_ in ("w_mod", "b_mod", "c", "c_ctx"):
            continue
        if stage == 1 and n_ in ("w_gate", "w_branch", "w_o", "w1_e", "w3_e", "w2_e"):
            continue
        if stage == 3 and n_ == "w_in":
            continue
        shp = [1] + list(s_[1:]) if n_ in WSLICE else s_
        L.t(n_, shp, F32, "ExternalInput")
    for n_, s_ in (("cosT", [128, TL]), ("sinT", [128, TL]), ("expo", [128, 2, 9]), ("sel", [128, 2, NCORE])):
        L.t(n_, s_, F32, "ExternalInput")
    io = "ExternalInput"
    if stage == 1:
        L.t("x_l", [TL, D], F32, io)
        L.t("x_c", [TC, D], F32, io)
        L.t("modv", [2, 2, 6, D], F32, "ExternalOutput")
    else:
        L.t("modv", [2, 2, 6, D], F32, io)

    def a_tensors(prefix, kind, tags):
        d_ = {}
        for tag, T in SEGT:
            if tag not in tags:
                continue
            for nm, shp, dt in A_OUT:
                L.t(prefix + nm + "_" + tag, shp(T), dt, kind)
                d_[nm + "_" + tag] = L.dr[prefix + nm + "_" + tag]
        return d_
    with ExitStack() as st:
        S = Sched(L.nc, st)
        cx = Ctx(L.nc, S)
        if stage == 1:
            dA = dict(L.dr)
            dA.update(a_tensors("", "ExternalOutput", ("l", "c")))
            phase_mods(cx, 0, dA)
            phase_mods(cx, 1, dA)
            phase_a(cx, 0, dA, [("l", TL, L.dr["x_l"]), ("c", TC, L.dr["x_c"])], wl=0)
        else:
            l = stage - 2
            tags = ("l", "c")
            for nm, shp, dt in GATH:
                L.t(nm, shp, dt, io)
            L.t("Xg", [NSLOT, D], BF16)
            L.t("Yg", [NSLOT, D], F32)
            dB = dict(L.dr)
            dB.update(a_tensors("b_", io, tags))
            segs = [("l", TL), ("c", TC)] if l == 0 else [("l", TL)]
            for tag, T in segs:
                L.t("ysT_" + tag, [8, 128, T], BF16)
                L.t("xmid_" + tag, [T, D])
                dB["ysT_" + tag] = L.dr["ysT_" + tag]
                dB["xmid_" + tag] = L.dr["xmid_" + tag]
            if l == 0:
                L.t("x_l", [TL, D], F32, io)
                L.t("x_c", [TC, D], F32, io)
                L.t("x1_l", [TL, D], F32, "ExternalOutput")
                L.t("x1_c", [TC, D])
                xin_l, xin_c, xo_l, xo_c = L.dr["x_l"], L.dr["x_c"], L.dr["x1_l"], L.dr["x1_c"]
            else:
                L.t("x1_l", [TL, D], F32, io)
                L.t("out", [TL, D], F32, "ExternalOutput")
                xin_l, xo_l = L.dr["x1_l"], L.dr["out"]
            phase_conv(cx, l, dB, segs)
            phase_attn(cx, l, dB, [("l", TL, NKALL)] + ([("c", TC, TC // 128)] if l == 0 else []), lam_init_of(l))
            phase_ret(cx, l, dB, segs)
            if l == 0:
                phase_merge(cx, l, dB, [("l", TL, xin_l, dB["xmid_l"]), ("c", TC, xin_c, dB["xmid_c"])], wl=0)
                moe_phase(cx, l, dB, [("l", TL, dB["xmid_l"], xo_l), ("c", TC, dB["xmid_c"], xo_c)], False, wl=0)
                dA = dict(L.dr)
                dA.update(a_tensors("", "ExternalOutput", ("l", "c")))
                phase_a(cx, 1, dA, [("l", TL, xo_l), ("c", TC, xo_c)], wl=0)
            else:
                phase_merge(cx, l, dB, [("l", TL, xin_l, dB["xmid_l"])], wl=0)
                moe_phase(cx, l, dB, [("l", TL, dB["xmid_l"], xo_l)], True, wl=0)
    return L


def host_gather(oA):
    gk = np.concatenate([np.asarray(o["kT_l"]).reshape(256, TL) for o in oA], axis=0)
    gv = np.concatenate([np.asarray(o["V_l"]) for o in oA], axis=0)
    gt = np.concatenate([np.asarray(o["Tst_l"]).reshape(256, 256) for o in oA], axis=0)
    hs = []
    for o in oA:
        u = np.asarray(o["uT_l"])
        t = np.asarray(o["tT_l"])
        h = np.concatenate([np.concatenate([a[c2][:, 0:16], a[c2][:, TL - 16:TL]], axis=1) for a in (u, t) for c2 in range(2)], axis=0)
        hs.append(h)
    hg = np.concatenate(hs, axis=0).astype(np.float32)
    return dict(gk=gk, gv=gv, gt=gt, hg=hg)


def kernel_unfused(**inp):
    f32 = lambda a: np.ascontiguousarray(np.asarray(a, dtype=np.float32))
    x = f32(inp["x"])[0]
    ctx = f32(inp["ctx"])[0]
    ropes = [rope_tables(c) for c in range(NCORE)]
    full = {n_: f32(inp[n_]).reshape(s_) for n_, s_ in EXT_IN}
    cst = make_cst()
    ident = np.eye(128, dtype=np.float32).astype(NPBF)

    def run(L, extra, wl):
        maps = []
        for c in range(NCORE):
            m = dict(cst=cst, ident=ident, cosT=ropes[c][0], sinT=ropes[c][1], expo=make_expo(c), sel=make_sel(c),
                     x_l=x[c * TL:(c + 1) * TL], x_c=ctx)
            for k, v in full.items():
                m[k] = v[wl[k]:wl[k] + 1] if k in WSLICE else v
            m.update(extra[c])
            maps.append({k: m[k] for k in L.ins})
        res = run_bass_kernel_spmd(L.nc, maps, core_ids=list(range(NCORE)))
        return [{k: np.asarray(r[k]) for k in L.outs} for r in res.results]

    o1 = run(build_stage(1), [dict() for _ in range(NCORE)], dict.fromkeys(WSLICE, 0))
    modv = o1[0]["modv"]

    def b_extra(oA):
        g = host_gather(oA)
        ex = []
        for c in range(NCORE):
            e = dict(g)
            e["modv"] = modv
            for k, v in oA[c].items():
                if k != "modv" and k != "x1_l":
                    e["b_" + k] = v
            ex.append(e)
        return ex
    wl2 = dict.fromkeys(WSLICE, 0)
    wl2["w_in"] = 1
    o2 = run(build_stage(2), b_extra(o1), wl2)
    ex3 = b_extra(o2)
    for c in range(NCORE):
        ex3[c]["x1_l"] = o2[c]["x1_l"]
    o3 = run(build_stage(3), ex3, dict.fromkeys(WSLICE, 1))
    out = np.concatenate([o3[c]["out"] for c in range(NCORE)], axis=0)
    return out.reshape(1, SEQ, D).astype(np.float32)


FUSED = False


def kernel(**inp):
    return kernel_fused(**inp) if FUSED else kernel_unfused(**inp)
```

```python
import math
from contextlib import ExitStack
import numpy as np
import ml_dtypes
import concourse.bass as bass
import concourse.mybir as mybir
from concourse.bass_utils import run_bass_kernel_spmd

F32 = mybir.dt.float32
BF16 = mybir.dt.bfloat16
AF = mybir.ActivationFunctionType
ALU = mybir.AluOpType
AX = mybir.AxisListType
NPBF = ml_dtypes.bfloat16

NCORE = 8
D = 1024
SEQ = 16384
TL = SEQ // NCORE
TC = 256
DEPTH = 2
INC = 2816
EPS = 1e-6
NEXP = 16
DFF = 512
QSCALE = 32 ** -0.5
ATT_ROW = False
MOE_CAP = 768
NSLOT = NEXP * MOE_CAP


class Op:
    __slots__ = ("id", "eng", "fn", "deps", "dma", "n", "signal", "val", "cc")


class Sched:
    ENGS = (("sp", "sync"), ("act", "scalar"), ("dve", "vector"), ("pool", "gpsimd"), ("pe", "tensor"))

    def __init__(self, nc, stack):
        self.nc = nc
        self.ops = []
        self.phase_start = 0
        self.lastw = {}
        self.readers = {}
        self.K = dict(sp=12, pool=8, act=4)
        self.dma_list = {e: [] for e in self.K}
        self.sems = {e: stack.enter_context(nc.semaphore("sm_" + e)) for e in ("pe", "act", "dve", "pool")}
        self.dsems = {e: [stack.enter_context(nc.semaphore("sd_%s%d" % (e, i))) for i in range(k)]
                      for e, k in self.K.items()}
        self.ccsems = [stack.enter_context(nc.semaphore("sc_%d" % i)) for i in range(12)]
        self.ncc = 0
        self.cc_list = []
        self.cnt = {e: 0 for e in self.sems}
        self.waited = {e: {} for e, _ in self.ENGS}

    def add(self, eng, fn, r=(), w=(), dma=False, cc=False):
        i = len(self.ops)
        deps = {}
        for k in r:
            j = self.lastw.get(k)
            if j is not None:
                deps[j] = True
        for k in w:
            j = self.lastw.get(k)
            if j is not None and j not in deps:
                deps[j] = False
            rd = self.readers.get(k)
            if rd:
                for j in rd.values():
                    if isinstance(j, list):
                        for jj in j:
                            deps.setdefault(jj, False)
                    else:
                        deps.setdefault(j, False)
        n = None
        if dma:
            lst = self.dma_list[eng]
            n = len(lst)
            if n >= self.K[eng]:
                deps.setdefault(lst[n - self.K[eng]], False)
            lst.append(i)
        op = Op()
        op.id, op.eng, op.fn, op.deps, op.dma, op.n, op.signal, op.val = i, eng, fn, deps, dma, n, False, 0
        op.cc = None
        if cc:
            op.dma = True
            op.cc = self.ncc
            self.ncc += 1
            self.cc_list.append(i)
            dma = True
        self.ops.append(op)
        for k in r:
            rd = self.readers.setdefault(k, {})
            if dma:
                rd.setdefault("dma", []).append(i)
            else:
                rd[eng] = i
        for k in w:
            self.lastw[k] = i
            self.readers[k] = {}
        return i

    def _needed(self, op, dj, raw):
        if dj.dma:
            return True
        if dj.eng == op.eng:
            if op.dma:
                return True
            return raw and op.eng != "pe"
        return True

    def end_phase(self, name=None):
        nc = self.nc
        ps = self.phase_start
        for e in self.K:
            lst = [i for i in self.dma_list[e][-self.K[e]:] if i >= ps]
            if e == "pool":
                lst = lst + [i for i in self.cc_list if i >= ps]
            if lst:
                i = self.add(e, lambda h: h.nop())
                for j in lst:
                    self.ops[i].deps[j] = True
        ops = self.ops
        for op in ops[ps:]:
            latest = {}
            for j, raw in op.deps.items():
                if j < ps:
                    continue
                dj = ops[j]
                if not dj.dma and self._needed(op, dj, raw):
                    if latest.get(dj.eng, -1) < j:
                        latest[dj.eng] = j
            for j in latest.values():
                ops[j].signal = True
        for op in ops[ps:]:
            if op.signal and not op.dma:
                self.cnt[op.eng] += 1
                op.val = self.cnt[op.eng]
        with nc.Block() as block:
            for e, bn in self.ENGS:
                ops_e = [op for op in ops[ps:] if op.eng == e]
                if not ops_e:
                    continue

                def body(h, ops_e=ops_e, e=e):
                    self._emit(e, h, ops_e, ps)
                getattr(block, bn)(body)
        self.phase_start = len(ops)
        self.lastw = {k: v for k, v in self.lastw.items() if isinstance(k, str) and k.startswith("D:")}
        self.readers = {k: {} for k in self.lastw}
        for op in ops[:self.phase_start]:
            op.fn = None

    def _emit(self, e, h, ops_e, ps):
        ops = self.ops
        waited = self.waited[e]
        for op in ops_e:
            want = {}
            for j, raw in op.deps.items():
                if j < ps:
                    continue
                dj = ops[j]
                if not self._needed(op, dj, raw):
                    continue
                if dj.cc is not None:
                    key = ("cc", dj.cc)
                    sem = self.ccsems[dj.cc]
                    val = 1
                elif dj.dma:
                    K = self.K[dj.eng]
                    key = (dj.eng, dj.n % K)
                    sem = self.dsems[dj.eng][dj.n % K]
                    val = 16 * (dj.n // K + 1)
                else:
                    key = dj.eng
                    sem = self.sems[dj.eng]
                    if key in want and want[key][2] > j:
                        continue
                    want[key] = (sem, dj.val, j)
                    continue
                if key not in want or want[key][1] < val:
                    want[key] = (sem, val, j)
            for key, (sem, val, _j) in want.items():
                if waited.get(key, 0) >= val:
                    continue
                h.wait_ge(sem, val)
                waited[key] = val
            inst = op.fn(h)
            if op.cc is not None:
                inst.then_inc(self.ccsems[op.cc])
            elif op.dma:
                inst.then_inc(self.dsems[e][op.n % self.K[e]], 16)
            elif op.signal:
                inst.then_inc(self.sems[e], 1)


class Ctx:
    def __init__(self, nc, S):
        self.nc = nc
        self.S = S
        self.stack = None
        self.uid = 0

    def psum(self, name, shape, dt=F32):
        self.uid += 1
        return self.stack.enter_context(self.nc.psum_tensor("%s_%d" % (name, self.uid), list(shape), dt))

    def begin(self, nf=8, nb=0):
        self.stack = ExitStack()
        self.ps = [self.stack.enter_context(self.nc.psum_tensor("ps%d_%d" % (i, self.uid), [128, 512], F32))
                   for i in range(nf)]
        self.psb = [self.stack.enter_context(self.nc.psum_tensor("psb%d_%d" % (i, self.uid), [128, 1024], BF16))
                    for i in range(nb)]
        self.uid += 1

    def end(self):
        self.S.end_phase()
        self.stack.close()
        self.stack = None

    def sb(self, name, shape, dt=F32):
        self.uid += 1
        return self.stack.enter_context(self.nc.sbuf_tensor("%s_%d" % (name, self.uid), list(shape), dt))

    def dma(self, eng, out, in_, r=(), w=(), **kw):
        return self.S.add(eng, lambda h: h.dma_start(out=out, in_=in_, **kw), r=r, w=w, dma=True)

    def mm(self, out, lhsT, rhs, start, stop, r=(), w=(), **kw):
        return self.S.add("pe", lambda h: h.matmul(out, lhsT, rhs, start=start, stop=stop, **kw), r=r, w=w)

    def tr(self, out, in_, ident, r=(), w=()):
        return self.S.add("pe", lambda h: h.transpose(out, in_, ident), r=r, w=w)

    def act(self, out, in_, func, r=(), w=(), eng="act", **kw):
        return self.S.add(eng, lambda h: h.activation(out=out, in_=in_, func=func, **kw), r=r, w=w)

    def tt(self, out, in0, in1, op, r=(), w=(), eng="dve"):
        return self.S.add(eng, lambda h: h.tensor_tensor(out=out, in0=in0, in1=in1, op=op), r=r, w=w)

    def ts(self, out, in0, s1, op0, s2=None, op1=None, r=(), w=(), eng="dve", **kw):
        if op1 is None:
            return self.S.add(eng, lambda h: h.tensor_scalar(out=out, in0=in0, scalar1=s1, scalar2=None, op0=op0, **kw),
                              r=r, w=w)
        return self.S.add(eng, lambda h: h.tensor_scalar(out=out, in0=in0, scalar1=s1, scalar2=s2, op0=op0, op1=op1, **kw),
                          r=r, w=w)

    def stt(self, out, in0, scalar, in1, op0, op1, r=(), w=()):
        return self.S.add("dve", lambda h: h.scalar_tensor_tensor(out=out, in0=in0, scalar=scalar, in1=in1,
                                                                    op0=op0, op1=op1), r=r, w=w)

    def copy(self, out, in_, r=(), w=(), eng="dve"):
        if eng == "act":
            return self.S.add("act", lambda h: h.activation(out=out, in_=in_, func=AF.Copy), r=r, w=w)
        return self.S.add(eng, lambda h: h.tensor_copy(out=out, in_=in_), r=r, w=w)

    def memset(self, ap, val, w=(), eng="dve"):
        return self.S.add(eng, lambda h: h.memset(ap, val), w=w)

    def red(self, out, in_, op, r=(), w=(), axis=None):
        ax = AX.X if axis is None else axis
        return self.S.add("dve", lambda h: h.tensor_reduce(out=out, in_=in_, axis=ax, op=op), r=r, w=w)

    def recip(self, out, in_, r=(), w=()):
        return self.S.add("dve", lambda h: h.reciprocal(out=out, in_=in_), r=r, w=w)


def phase_mods(cx, l, dr):
    cx.begin()
    cv = cx.sb("cv", [128, 8, 2])
    sc = cx.sb("sc", [128, 8, 2])
    acc = cx.sb("macc", [2, 6144])
    bm = cx.sb("mbm", [2, 6144])
    g1b = cx.sb("g1b", [2, 1024])
    g2b = cx.sb("g2b", [2, 1024])
    mv = cx.sb("mv", [2, 6, 1024])
    wm = [cx.sb("wm%d" % i, [128, 6144]) for i in range(2)]
    for s, src in enumerate((dr["c"], dr["c_ctx"])):
        for kc in range(8):
            cx.dma("sp", cv[:, kc, s:s + 1], src[0:1, kc * 128:(kc + 1) * 128].rearrange("o p -> p o"), w=["cv"])
    cx.dma("sp", bm[:, :], dr["b_mod"][l, :].partition_broadcast(2), w=["bm"])
    cx.dma("sp", g1b[:, :], dr["g_norm1"][l, :].partition_broadcast(2), w=["g1b"])
    cx.dma("sp", g2b[:, :], dr["g_norm2"][l, :].partition_broadcast(2), w=["g2b"])
    cx.act(sc[:, :, :], cv[:, :, :], AF.Silu, r=["cv"], w=["sc"])
    for kc in range(8):
        b = kc % 2
        cx.dma("sp", wm[b][:, :], dr["w_mod"][l, kc * 128:(kc + 1) * 128, :], w=["wm%d" % b])
        for n in range(12):
            p = cx.ps[n % 4]
            cx.mm(p[0:2, :], sc[:, kc, :], wm[b][:, n * 512:(n + 1) * 512], True, True,
                  r=["sc", "wm%d" % b], w=["ps%d" % (n % 4)])
            a = acc[:, n * 512:(n + 1) * 512]
            if kc == 0:
                cx.tt(a, p[0:2, :], bm[:, n * 512:(n + 1) * 512], ALU.add, r=["ps%d" % (n % 4), "bm"], w=["macc%d" % n])
            else:
                cx.tt(a, p[0:2, :], a, ALU.add, r=["ps%d" % (n % 4), "macc%d" % n], w=["macc%d" % n])
    allacc = ["macc%d" % n for n in range(12)]
    cx.stt(mv[:, 0, :], acc[:, 1024:2048], 1.0, g1b[:, :], ALU.add, ALU.mult, r=allacc + ["g1b"], w=["mv0"])
    cx.copy(mv[:, 1, :], acc[:, 0:1024], r=allacc, w=["mv1"])
    cx.copy(mv[:, 2, :], acc[:, 2048:3072], r=allacc, w=["mv2"])
    cx.stt(mv[:, 3, :], acc[:, 4096:5120], 1.0, g2b[:, :], ALU.add, ALU.mult, r=allacc + ["g2b"], w=["mv3"])
    cx.copy(mv[:, 4, :], acc[:, 3072:4096], r=allacc, w=["mv4"])
    cx.copy(mv[:, 5, :], acc[:, 5120:6144], r=allacc, w=["mv5"])
    cx.dma("sp", dr["modv"][l, :, :, :], mv[:, :, :], r=["mv%d" % i for i in range(6)], w=["D:modv%d" % l])
    cx.end()


CST = {}
_off = 0
for _n, _w in (("c127mj", 1), ("cj", 1), ("ef_l", 16), ("eb_l", 16), ("ef_c", 2), ("eb_c", 2), ("ip1", 128),
               ("m128i", 128), ("D1", 128), ("D2", 128), ("U", 128), ("Lo", 128), ("I2", 128), ("ones", 128), ("I1", 128), ("hm", 4), ("bm8", 8), ("Ltri", 128), ("eoff", 16)):
    CST[_n] = (_off, _off + _w)
    _off += _w
CSTW = _off


def make_cst():
    c = np.zeros((128, CSTW), np.float32)
    p = np.arange(128, dtype=np.float32)
    i = np.arange(128, dtype=np.float32)

    def put(n, v):
        a, b = CST[n]
        c[:, a:b] = v
    put("c127mj", (127 - p)[:, None])
    put("cj", p[:, None])
    put("ef_l", (128.0 * (15 - np.arange(16)))[None, :])
    put("eb_l", (128.0 * np.arange(16))[None, :])
    put("ef_c", (128.0 * (1 - np.arange(2)))[None, :])
    put("eb_c", (128.0 * np.arange(2))[None, :])
    put("ip1", (i + 1)[None, :])
    put("m128i", (128 - i)[None, :])
    dd = i[None, :] - p[:, None]
    put("D1", np.maximum(dd, 0))
    put("D2", np.maximum(-dd, 0))
    put("U", (dd > 0).astype(np.float32))
    put("Lo", (dd < 0).astype(np.float32))
    put("I2", 2.0 * (dd == 0))
    put("ones", 1.0)
    put("I1", (dd == 0).astype(np.float32))
    put("hm", (p[:, None] // 32 == np.arange(4)[None, :]).astype(np.float32))
    put("bm8", np.tile((p[:, None] // 32 == np.arange(4)[None, :]).astype(np.float32), (1, 2)))
    put("Ltri", (dd > 0).astype(np.float32))
    put("eoff", (float(MOE_CAP) * np.arange(16))[None, :])
    return c


def rope_tables(core):
    t = np.arange(core * TL, (core + 1) * TL)
    row = (t // 64).astype(np.float32)
    col = (t % 64).astype(np.float32)
    inv = (np.float32(10000.0) ** (-np.arange(8, dtype=np.float32) / np.float32(8))).astype(np.float32)
    ang = np.concatenate([row[:, None] * inv[None, :], col[:, None] * inv[None, :]], axis=1).astype(np.float32)
    cos = np.cos(ang).astype(np.float32).T
    sin = np.sin(ang).astype(np.float32).T
    return np.tile(cos, (8, 1)).copy(), np.tile(sin, (8, 1)).copy()


def cs(cst, name):
    a, b = CST[name]
    return cst[:, a:b]


def phase_a(cx, l, dr, segs, wl=None):
    wl = l if wl is None else wl
    cx.begin(nf=6, nb=2)
    ps, psb = cx.ps, cx.psb
    W = cx.sb("W", [128, 8, INC], BF16)
    WR = cx.sb("WR", [128, 8, 768], BF16)
    cst = cx.sb("cst", [128, CSTW])
    ident = cx.sb("ident", [128, 128], BF16)
    cx.dma("sp", cst[:, :], dr["cst"][:, :], w=["cst"])
    cx.dma("sp", ident[:, :], dr["ident"][:, :], w=["ident"])
    for kc in range(8):
        cx.dma("pool", W[:, kc, :], dr["w_in"][wl, kc * 128:(kc + 1) * 128, :], w=["W%d" % kc])
    for kc in range(8):
        cx.ts(W[:, kc, 2176:2304], W[:, kc, 2176:2304], QSCALE, ALU.mult, r=["W%d" % kc], w=["W%d" % kc], eng="pool")
        for (s0, n, o0) in ((1280, 512, 0), (2048, 256, 512)):
            src = W[:, kc, s0:s0 + n].rearrange("p (b t d) -> p b t d", t=2, d=16)
            dst = WR[:, kc, o0:o0 + n].rearrange("p (b t d) -> p b t d", t=2, d=16)
            cx.ts(dst[:, :, 0, :], src[:, :, 1, :], -1.0, ALU.mult, r=["W%d" % kc], w=["WR%d" % kc], eng="pool")
            cx.copy(dst[:, :, 1, :], src[:, :, 0, :], r=["W%d" % kc], w=["WR%d" % kc], eng="pool")
    Wk = ["W%d" % kc for kc in range(8)]
    WRk = ["WR%d" % kc for kc in range(8)]
    lgf = cx.sb("lgf", [128, 4]); lgb = cx.sb("lgb", [128, 4])
    lgfc = cx.sb("lgfc", [128, 1]); lgbc = cx.sb("lgbc", [128, 1])
    cx.dma("sp", lgf[:, :], dr["ret_ld_f"][l, :].partition_broadcast(128), w=["lgf"])
    cx.dma("sp", lgb[:, :], dr["ret_ld_b"][l, :].partition_broadcast(128), w=["lgb"])
    for h in range(4):
        cx.dma("sp", lgfc[32 * h:32 * h + 32, :], dr["ret_ld_f"][l, h:h + 1].partition_broadcast(32), w=["lgfc"])
        cx.dma("sp", lgbc[32 * h:32 * h + 32, :], dr["ret_ld_b"][l, h:h + 1].partition_broadcast(32), w=["lgbc"])
    kdf = cx.sb("kdf", [128, 4]); kdb = cx.sb("kdb", [128, 4])
    KDF = cx.sb("KDF", [128, 128]); KDB = cx.sb("KDB", [128, 128])
    cx.act(kdf[:, :], lgf[:, :], AF.Exp, scale=cs(cst, "c127mj"), r=["lgf", "cst"], w=["kdf"])
    cx.act(kdb[:, :], lgb[:, :], AF.Exp, scale=cs(cst, "cj"), r=["lgb", "cst"], w=["kdb"])
    for h in range(4):
        cx.ts(KDF[:, 32 * h:32 * h + 32], cs(cst, "ones")[:, 0:32], kdf[:, h:h + 1], ALU.mult, r=["kdf", "cst"], w=["KDF"])
        cx.ts(KDB[:, 32 * h:32 * h + 32], cs(cst, "ones")[:, 0:32], kdb[:, h:h + 1], ALU.mult, r=["kdb", "cst"], w=["KDB"])
    pw = {}
    for tag, nch in (("l", 16), ("c", 2)):
        pf = cx.sb("pwf" + tag, [128, nch]); pb = cx.sb("pwb" + tag, [128, nch])
        cx.act(pf[:, :], cs(cst, "ef_" + tag), AF.Exp, scale=lgfc[:, 0:1], r=["lgfc", "cst"], w=["pwf" + tag])
        cx.act(pb[:, :], cs(cst, "eb_" + tag), AF.Exp, scale=lgbc[:, 0:1], r=["lgbc", "cst"], w=["pwb" + tag])
        pw[tag] = (pf, pb)
    Cl = cx.sb("Cl", [128, TL]); Sl = cx.sb("Sl", [128, TL])
    cx.dma("sp", Cl[:, :], dr["cosT"][:, :], w=["Cl"])
    cx.dma("sp", Sl[:, :], dr["sinT"][:, :], w=["Sl"])
    xt = [cx.sb("xt%d" % i, [128, D]) for i in range(2)]
    junk = cx.sb("junk", [128, D], BF16)
    t1 = [cx.sb("t1_%d" % i, [128, D]) for i in range(2)]
    hb = [cx.sb("hb%d" % i, [128, D], BF16) for i in range(2)]
    ssq = cx.sb("ssq", [128, 4]); rstd = cx.sb("rstd", [128, 4])
    hTs = [cx.sb("hT%d" % i, [128, 8, 512], BF16) for i in range(2)]
    gcount = [0]
    gsb = cx.sb("gsb", [128, D]); shb = cx.sb("shb", [128, D])
    ev = [cx.sb("ev%d" % i, [128, 512]) for i in range(4)]
    evb = [cx.sb("evb%d" % i, [128, 512], BF16) for i in range(4)]
    rk_sb = cx.sb("rk_sb", [128, 512], BF16)
    vt = [cx.sb("vt%d" % i, [128, 512], BF16) for i in range(2)]
    rgt = [cx.sb("rgt%d" % i, [128, 256], BF16) for i in range(2)]
    kfb = [cx.sb("kfb%d" % i, [128, 256], BF16) for i in range(2)]
    Tst = cx.sb("Tst", [128, 2, 256])
    evi = [0]

    def nxt():
        evi[0] = (evi[0] + 1) % 4
        return evi[0]

    xi = 0
    for (tag, T, xd) in segs:
        sfx = "_" + tag
        seg = 0 if tag == "l" else 1
        cx.dma("sp", gsb[:, :], dr["modv"][l, seg, 0, :].partition_broadcast(128), r=["D:modv%d" % l], w=["gsb"])
        cx.dma("sp", shb[:, :], dr["modv"][l, seg, 1, :].partition_broadcast(128), r=["D:modv%d" % l], w=["shb"])
        G = min(512, T)
        for g in range(T // G):
            t0 = g * G
            nt = G // 128
            hT = hTs[gcount[0] % 2]
            kh = "hT%d" % (gcount[0] % 2)
            gcount[0] += 1
            for j in range(nt):
                b = xi % 2
                xi += 1
                kx = "xt%d" % b
                cx.dma("sp", xt[b][:, :], xd[t0 + j * 128:t0 + (j + 1) * 128, :], w=[kx])
                cx.act(junk[:, :], xt[b][:, :], AF.Square, accum_out=ssq[:, j:j + 1], r=[kx], w=["junk", "ssq%d" % j])
                cx.act(rstd[:, j:j + 1], ssq[:, j:j + 1], AF.Sqrt, scale=1.0 / D, bias=EPS, r=["ssq%d" % j], w=["rs%d" % j])
                cx.recip(rstd[:, j:j + 1], rstd[:, j:j + 1], r=["rs%d" % j], w=["rs%d" % j])
                cx.stt(t1[b][:, :], xt[b][:, :], rstd[:, j:j + 1], gsb[:, :], ALU.mult, ALU.mult,
                       r=[kx, "rs%d" % j, "gsb"], w=["t1_%d" % b])
                cx.tt(hb[b][:, :], t1[b][:, :], shb[:, :], ALU.add, r=["t1_%d" % b, "shb"], w=["hb%d" % b], eng="pool")
                for kc in range(8):
                    cx.tr(psb[0][:, kc * 128:(kc + 1) * 128], hb[b][:, kc * 128:(kc + 1) * 128], ident[:, :],
                          r=["hb%d" % b, "ident"], w=["psb0"])
                cx.copy(hT[:, :, j * 128:(j + 1) * 128], psb[0][:, :].rearrange("p (k t) -> p k t", k=8),
                        r=["psb0"], w=[kh], eng="act")
            cx.dma("sp", dr["hT" + sfx][:, :, t0:t0 + G], hT[:, :, 0:G], r=[kh], w=["D:hT" + sfx])

            def proj(cc, bank, rot=False):
                Wt = WR if rot else W
                for kc in range(8):
                    cx.mm(ps[bank][:, 0:G], Wt[:, kc, cc * 128:(cc + 1) * 128], hT[:, kc, 0:G], kc == 0, kc == 7,
                          r=[kh, (WRk if rot else Wk)[kc]], w=["ps%d" % bank])

            Cg = Cl[:, t0:t0 + G] if tag == "l" else None
            Sg = Sl[:, t0:t0 + G] if tag == "l" else None
            for c2 in range(2):
                e = nxt()
                proj(2 + c2, 0)
                cx.act(ev[e][:, 0:G], ps[0][:, 0:G], AF.Sigmoid, r=["ps0"], w=["ev%d" % e])
                proj(0 + c2, 1)
                cx.tt(ev[e][:, 0:G], ps[1][:, 0:G], ev[e][:, 0:G], ALU.mult, r=["ps1", "ev%d" % e], w=["ev%d" % e])
                cx.dma("sp", dr["uT" + sfx][c2, :, t0:t0 + G], ev[e][:, 0:G], r=["ev%d" % e], w=["D:uT" + sfx])
            for c2 in range(2):
                e = nxt()
                proj(4 + c2, 0)
                cx.copy(evb[e][:, 0:G], ps[0][:, 0:G], r=["ps0"], w=["evb%d" % e], eng="act")
                cx.dma("sp", dr["bgT" + sfx][c2, :, t0:t0 + G], evb[e][:, 0:G], r=["evb%d" % e], w=["D:bgT" + sfx])
                proj(6 + c2, 1)
                cx.copy(ev[e][:, 0:G], ps[1][:, 0:G], r=["ps1"], w=["ev%d" % e], eng="act")
                proj(8 + c2, 0)
                cx.tt(ev[e][:, 0:G], ps[0][:, 0:G], ev[e][:, 0:G], ALU.mult, r=["ps0", "ev%d" % e], w=["ev%d" % e])
                cx.dma("sp", dr["tT" + sfx][c2, :, t0:t0 + G], ev[e][:, 0:G], r=["ev%d" % e], w=["D:tT" + sfx])
            for (cc, ro, dst, keep) in ((10, 0, dr["qT" + sfx][0], None), (11, 1, dr["qT" + sfx][1], None),
                                        (12, 2, dr["kT" + sfx][0], None), (13, 3, dr["kT" + sfx][1], None),
                                        (16, 4, dr["rqT" + sfx], None), (17, 5, dr["rkT" + sfx], rk_sb)):
                e = nxt()
                ob = keep if keep is not None else evb[e]
                okey = "rk_sb" if keep is not None else "evb%d" % e
                proj(cc, 0)
                if tag == "l":
                    proj(ro, 2, rot=True)
                    e2 = nxt()
                    cx.tt(ev[e][:, 0:G], ps[0][:, 0:G], Cg, ALU.mult, r=["ps0", "Cl"], w=["ev%d" % e])
                    cx.tt(ev[e2][:, 0:G], ps[2][:, 0:G], Sg, ALU.mult, r=["ps2", "Sl"], w=["ev%d" % e2])
                    cx.tt(ob[:, 0:G], ev[e][:, 0:G], ev[e2][:, 0:G], ALU.add, r=["ev%d" % e, "ev%d" % e2], w=[okey], eng="pool")
                else:
                    cx.copy(ob[:, 0:G], ps[0][:, 0:G], r=["ps0"], w=[okey], eng="act")
                cx.dma("sp", dst[:, t0:t0 + G], ob[:, 0:G], r=[okey], w=["D:rope" + sfx + str(cc)])
            pf, pb = pw[tag]
            for j in range(nt):
                n = (t0 // 128) + j
                b = j % 2
                tsl = slice(j * 128, (j + 1) * 128)
                for kc in range(8):
                    cx.mm(ps[3][:, 0:256], hT[:, kc, tsl], W[:, kc, 1792:2048], kc == 0, kc == 7, r=[kh, Wk[kc]], w=["ps3"])
                for kc in range(8):
                    cx.mm(ps[3][:, 256:512], hT[:, kc, tsl], W[:, kc, 2304:2560], kc == 0, kc == 7, r=[kh, Wk[kc]], w=["ps3"])
                for kc in range(8):
                    cx.mm(ps[4][:, 0:256], hT[:, kc, tsl], W[:, kc, 2560:2816], kc == 0, kc == 7, r=[kh, Wk[kc]], w=["ps4"])
                cx.copy(vt[b][:, :], ps[3][:, :], r=["ps3", "ps3"], w=["vt%d" % b])
                cx.act(rgt[b][:, :], ps[4][:, 0:256], AF.Silu, r=["ps4"], w=["rgt%d" % b])
                rows = slice(t0 + j * 128, t0 + (j + 1) * 128)
                cx.dma("sp", dr["V" + sfx][rows, :], vt[b][:, 0:256], r=["vt%d" % b], w=["D:V" + sfx])
                cx.dma("sp", dr["rv" + sfx][rows, :], vt[b][:, 256:512], r=["vt%d" % b], w=["D:rv" + sfx])
                cx.dma("sp", dr["rg" + sfx][rows, :], rgt[b][:, :], r=["rgt%d" % b], w=["D:rg" + sfx])
                cx.tr(psb[1][:, 0:128], rk_sb[:, tsl], ident[:, :], r=["rk_sb", "ident"], w=["psb1"])
                cx.tt(kfb[b][:, 0:128], psb[1][:, 0:128], KDF[:, :], ALU.mult, r=["psb1", "KDF"], w=["kfb%d" % b])
                cx.tt(kfb[b][:, 128:256], psb[1][:, 0:128], KDB[:, :], ALU.mult, r=["psb1", "KDB"], w=["kfb%d" % b])
                cx.mm(ps[5][:, 0:256], kfb[b][:, 0:128], vt[b][:, 256:512], True, True, r=["kfb%d" % b, "vt%d" % b], w=["ps5"])
                cx.mm(ps[5][:, 256:512], kfb[b][:, 128:256], vt[b][:, 256:512], True, True, r=["kfb%d" % b, "vt%d" % b], w=["ps5"])
                if n == 0:
                    cx.ts(Tst[:, 0, :], ps[5][:, 0:256], pf[:, n:n + 1], ALU.mult, r=["ps5", "pwf" + tag], w=["Tf"])
                    cx.ts(Tst[:, 1, :], ps[5][:, 256:512], pb[:, n:n + 1], ALU.mult, r=["ps5", "pwb" + tag], w=["Tb"])
                else:
                    cx.stt(Tst[:, 0, :], ps[5][:, 0:256], pf[:, n:n + 1], Tst[:, 0, :], ALU.mult, ALU.add,
                           r=["ps5", "pwf" + tag, "Tf"], w=["Tf"])
                    cx.stt(Tst[:, 1, :], ps[5][:, 256:512], pb[:, n:n + 1], Tst[:, 1, :], ALU.mult, ALU.add,
                           r=["ps5", "pwb" + tag, "Tb"], w=["Tb"])
        cx.dma("sp", dr["Tst" + sfx].rearrange("a p e -> p a e"), Tst[:, :, :], r=["Tf", "Tb"], w=["D:Tst" + sfx])
    cx.end()


def phase_conv(cx, l, dr, segs):
    cx.begin(nf=8, nb=0)
    ps = cx.ps
    cst = cx.sb("cst", [128, CSTW])
    cx.dma("sp", cst[:, :], dr["cst"][:, :], w=["cst"])
    praw = cx.sb("praw", [40, 256])
    cx.memset(praw[:, :], 0.0, w=["praw"])
    cx.dma("sp", praw[0:31, :], dr["conv_a_w"][l, :, :], w=["praw"])
    cx.dma("sp", praw[31:32, :], dr["conv_a_b"][l:l + 1, :], w=["praw"])
    cx.dma("sp", praw[32:33, :], dr["conv_a_g"][l:l + 1, :], w=["praw"])
    cx.dma("sp", praw[33:34, :], dr["conv_a_beta"][l:l + 1, :], w=["praw"])
    cx.dma("sp", praw[34:37, :], dr["conv_b_w"][l, :, :], w=["praw"])
    par = cx.sb("par", [128, 2, 40])
    for c2 in range(2):
        cx.tr(ps[7][:, 0:40], praw[0:40, c2 * 128:(c2 + 1) * 128], cs(cst, "I1")[0:40, 0:40], r=["praw", "cst"], w=["ps7"])
        cx.copy(par[:, c2, :], ps[7][:, 0:40], r=["ps7"], w=["par"])
    onesm = cx.sb("onesm", [128, 128])
    cx.memset(onesm[:, :], 1.0 / 256.0, w=["onesm"])
    HG = cx.sb("HG", [128, NCORE, 4, 32]); selt = cx.sb("selt", [128, 2, NCORE])
    cx.dma("sp", HG[:, :, :, :], dr["hg"].rearrange("(r a p) w -> p r a w", r=NCORE, a=4), w=["HG"])
    cx.dma("sp", selt[:, :, :], dr["sel"][:, :, :], w=["selt"])
    for (tag, T) in segs:
        sfx = "_" + tag
        ue = cx.sb("ue" + tag, [128, 2, T + 32])
        te = cx.sb("te" + tag, [128, 2, T + 32])
        bg = cx.sb("bg" + tag, [128, 2, T], BF16)
        acc = cx.sb("acc" + tag, [128, 2, T])
        sq = cx.sb("sq" + tag, [128, 2, T])
        yb = cx.sb("yb" + tag, [128, 2, T], BF16)
        for c2 in range(2):
            cx.dma("sp", ue[:, c2, 16:16 + T], dr["uT" + sfx][c2, :, :], r=["D:uT" + sfx], w=["ue%d" % c2])
            cx.dma("sp", te[:, c2, 16:16 + T], dr["tT" + sfx][c2, :, :], r=["D:tT" + sfx], w=["te%d" % c2])
            cx.dma("sp", bg[:, c2, :], dr["bgT" + sfx][c2, :, :], r=["D:bgT" + sfx], w=["bg%d" % c2])
            if tag == "l":
                for a, (buf, k) in enumerate(((ue, "ue%d" % c2), (te, "te%d" % c2))):
                    ai = a * 2 + c2
                    for side, dst, src in ((0, slice(0, 16), slice(16, 32)), (1, slice(16 + T, 32 + T), slice(0, 16))):
                        for r_ in range(NCORE):
                            if r_ == 0:
                                cx.ts(buf[:, c2, dst], HG[:, r_, ai, src], selt[:, side, r_:r_ + 1], ALU.mult,
                                      r=["HG", "selt"], w=[k])
                            else:
                                cx.stt(buf[:, c2, dst], HG[:, r_, ai, src], selt[:, side, r_:r_ + 1], buf[:, c2, dst],
                                       ALU.mult, ALU.add, r=["HG", "selt", k], w=[k])
            else:
                for buf, k in ((ue, "ue%d" % c2), (te, "te%d" % c2)):
                    cx.memset(buf[:, c2, 0:16], 0.0, w=[k], eng="pool")
                    cx.memset(buf[:, c2, 16 + T:32 + T], 0.0, w=[k], eng="pool")
        for c2 in range(2):
            ka = "acc%d" % c2
            cx.ts(acc[:, c2, :], ue[:, c2, 1:1 + T], par[:, c2, 0:1], ALU.mult, s2=par[:, c2, 31:32], op1=ALU.add,
                  r=["ue%d" % c2, "par"], w=[ka])
            for k in range(1, 31):
                cx.stt(acc[:, c2, :], ue[:, c2, k + 1:k + 1 + T], par[:, c2, k:k + 1], acc[:, c2, :], ALU.mult, ALU.add,
                       r=["ue%d" % c2, "par", ka], w=[ka])
            cx.tt(sq[:, c2, :], acc[:, c2, :], acc[:, c2, :], ALU.mult, r=[ka], w=["sq%d" % c2], eng="pool")
        G = min(512, T)
        mm2 = cx.sb("m2" + tag, [128, G]); var = cx.sb("var" + tag, [128, G]); dd = cx.sb("dd" + tag, [128, G])
        for g in range(T // G):
            gs = slice(g * G, (g + 1) * G)
            for c2 in range(2):
                cx.mm(ps[0][:, 0:G], onesm[:, :], acc[:, c2, gs], c2 == 0, c2 == 1, r=["onesm", "acc%d" % c2], w=["ps0"])
            for c2 in range(2):
                cx.mm(ps[1][:, 0:G], onesm[:, :], sq[:, c2, gs], c2 == 0, c2 == 1, r=["onesm", "sq%d" % c2], w=["ps1"])
            cx.act(mm2[:, :], ps[0][:, 0:G], AF.Square, r=["ps0"], w=["mm2"])
            cx.tt(var[:, :], ps[1][:, 0:G], mm2[:, :], ALU.subtract, r=["ps1", "mm2"], w=["var"])
            cx.act(var[:, :], var[:, :], AF.Ln, bias=EPS, r=["var"], w=["var"])
            cx.act(var[:, :], var[:, :], AF.Exp, scale=-0.5, r=["var"], w=["var"])
            for c2 in range(2):
                cx.tt(dd[:, :], acc[:, c2, gs], ps[0][:, 0:G], ALU.subtract, r=["acc%d" % c2, "ps0"], w=["dd"])
                cx.tt(dd[:, :], dd[:, :], var[:, :], ALU.mult, r=["dd", "var"], w=["dd"])
                cx.act(yb[:, c2, gs], dd[:, :], AF.Silu, scale=par[:, c2, 32:33], bias=par[:, c2, 33:34],
                       r=["dd", "par"], w=["yb%d" % c2])
        for c2 in range(2):
            cx.dma("sp", dr["ysT" + sfx][0 + c2, :, :], yb[:, c2, :], r=["yb%d" % c2], w=["D:ys0" + sfx])
        for c2 in range(2):
            ka = "acc%d" % c2
            cx.ts(acc[:, c2, :], te[:, c2, 15:15 + T], par[:, c2, 34:35], ALU.mult, r=["te%d" % c2, "par"], w=[ka])
            for k in (1, 2):
                cx.stt(acc[:, c2, :], te[:, c2, 15 + k:15 + k + T], par[:, c2, 34 + k:35 + k], acc[:, c2, :], ALU.mult, ALU.add,
                       r=["te%d" % c2, "par", ka], w=[ka])
            cx.tt(yb[:, c2, :], acc[:, c2, :], bg[:, c2, :], ALU.mult, r=[ka, "bg%d" % c2], w=["yb%d" % c2])
            cx.dma("sp", dr["ysT" + sfx][2 + c2, :, :], yb[:, c2, :], r=["yb%d" % c2], w=["D:ys1" + sfx])
    cx.end()


def phase_ret(cx, l, dr, segs):
    cx.begin(nf=6, nb=2)
    ps, psb = cx.ps, cx.psb
    cst = cx.sb("cst", [128, CSTW])
    ident = cx.sb("ident", [128, 128], BF16)
    cx.dma("sp", cst[:, :], dr["cst"][:, :], w=["cst"])
    cx.dma("sp", ident[:, :], dr["ident"][:, :], w=["ident"])
    lgf = cx.sb("lgf", [128, 4]); lgb = cx.sb("lgb", [128, 4])
    lgfc = cx.sb("lgfc", [128, 1]); lgbc = cx.sb("lgbc", [128, 1])
    cx.dma("sp", lgf[:, :], dr["ret_ld_f"][l, :].partition_broadcast(128), w=["lgf"])
    cx.dma("sp", lgb[:, :], dr["ret_ld_b"][l, :].partition_broadcast(128), w=["lgb"])
    for h in range(4):
        cx.dma("sp", lgfc[32 * h:32 * h + 32, :], dr["ret_ld_f"][l, h:h + 1].partition_broadcast(32), w=["lgfc"])
        cx.dma("sp", lgbc[32 * h:32 * h + 32, :], dr["ret_ld_b"][l, h:h + 1].partition_broadcast(32), w=["lgbc"])
    kdf = cx.sb("kdf", [128, 4]); kdb = cx.sb("kdb", [128, 4])
    KDF = cx.sb("KDF", [128, 128]); KDB = cx.sb("KDB", [128, 128])
    cx.act(kdf[:, :], lgf[:, :], AF.Exp, scale=cs(cst, "c127mj"), r=["lgf", "cst"], w=["kdf"])
    cx.act(kdb[:, :], lgb[:, :], AF.Exp, scale=cs(cst, "cj"), r=["lgb", "cst"], w=["kdb"])
    for h in range(4):
        cx.ts(KDF[:, 32 * h:32 * h + 32], cs(cst, "ones")[:, 0:32], kdf[:, h:h + 1], ALU.mult, r=["kdf", "cst"], w=["KDF"])
        cx.ts(KDB[:, 32 * h:32 * h + 32], cs(cst, "ones")[:, 0:32], kdb[:, h:h + 1], ALU.mult, r=["kdb", "cst"], w=["KDB"])
    cdf = cx.sb("cdf", [128, 1]); cdb = cx.sb("cdb", [128, 1])
    cx.act(cdf[:, :], lgfc[:, :], AF.Exp, scale=128.0, r=["lgfc"], w=["cdf"])
    cx.act(cdb[:, :], lgbc[:, :], AF.Exp, scale=128.0, r=["lgbc"], w=["cdb"])
    qdf4 = cx.sb("qdf4", [128, 4, 128]); qdb4 = cx.sb("qdb4", [128, 4, 128])
    for c in range(4):
        cx.act(qdf4[:, c, :], cs(cst, "ip1"), AF.Exp, scale=lgfc[:, 0:1], r=["lgfc", "cst"], w=["qdf4"])
        cx.act(qdb4[:, c, :], cs(cst, "m128i"), AF.Exp, scale=lgbc[:, 0:1], r=["lgbc", "cst"], w=["qdb4"])
    maskT = cx.sb("maskT", [128, 4, 128]); mtmp = cx.sb("mtmp", [128, 128])
    for h in range(4):
        cx.act(mtmp[:, :], cs(cst, "D1"), AF.Exp, scale=lgf[:, h:h + 1], r=["lgf", "cst"], w=["mtmp"])
        cx.tt(maskT[:, h, :], mtmp[:, :], cs(cst, "U"), ALU.mult, r=["mtmp", "cst"], w=["maskT"])
        cx.tt(maskT[:, h, :], maskT[:, h, :], cs(cst, "I2"), ALU.add, r=["maskT", "cst"], w=["maskT"])
        cx.act(mtmp[:, :], cs(cst, "D2"), AF.Exp, scale=lgb[:, h:h + 1], r=["lgb", "cst"], w=["mtmp"])
        cx.tt(mtmp[:, :], mtmp[:, :], cs(cst, "Lo"), ALU.mult, r=["mtmp", "cst"], w=["mtmp"])
        cx.tt(maskT[:, h, :], maskT[:, h, :], mtmp[:, :], ALU.add, r=["maskT", "mtmp"], w=["maskT"])
    for (tag, T) in segs:
        sfx = "_" + tag
        NCH = T // 128
        rq = cx.sb("rq" + tag, [128, T], BF16); rk = cx.sb("rk" + tag, [128, T], BF16)
        rqh = cx.sb("rqh" + tag, [128, 4, T], BF16)
        rv = cx.sb("rv" + tag, [128, NCH, 256], BF16); rg = cx.sb("rg" + tag, [128, NCH, 256], BF16)
        cx.dma("sp", rq[:, :], dr["rqT" + sfx][:, :], r=["D:rope" + sfx + "16"], w=["rq"])
        cx.dma("sp", rk[:, :], dr["rkT" + sfx][:, :], r=["D:rope" + sfx + "17"], w=["rk"])
        cx.dma("sp", rv[:, :, :], dr["rv" + sfx].rearrange("(n p) e -> p n e", p=128), r=["D:rv" + sfx], w=["rv"])
        cx.dma("sp", rg[:, :, :], dr["rg" + sfx].rearrange("(n p) e -> p n e", p=128), r=["D:rg" + sfx], w=["rg"])
        for h in range(4):
            cx.ts(rqh[:, h, :], rq[:, :], cs(cst, "hm")[:, h:h + 1], ALU.mult, r=["rq", "cst"], w=["rqh"], eng="pool")
        SF = cx.sb("SF" + tag, [128, NCH, 256]); SB = cx.sb("SB" + tag, [128, NCH, 256])
        SFb = cx.sb("SFb" + tag, [128, NCH, 256], BF16); SBb = cx.sb("SBb" + tag, [128, NCH, 256], BF16)
        KV = cx.sb("KV" + tag, [128, NCH, 2, 256])
        if tag == "l":
            Tall = cx.sb("Tall", [128, 9, 2, 256]); expo = cx.sb("expo", [128, 2, 9]); coef = cx.sb("coef", [128, 2, 9])
            cx.dma("sp", Tall[:, 0:8, :, :], dr["gt"].rearrange("(s a p) e -> p s a e", s=NCORE, a=2), w=["Tall"])
            cx.dma("sp", Tall[:, 8, :, :], dr["Tst_c"].rearrange("a p e -> p a e"), w=["Tall"])
            cx.dma("sp", expo[:, :, :], dr["expo"][:, :, :], w=["expo"])
            cx.act(coef[:, 0, :], expo[:, 0, :], AF.Exp, scale=lgfc[:, 0:1], r=["expo", "lgfc"], w=["coef"])
            cx.act(coef[:, 1, :], expo[:, 1, :], AF.Exp, scale=lgbc[:, 0:1], r=["expo", "lgbc"], w=["coef"])
            for a, (St, n0, key) in enumerate(((SF, 0, "SF0"), (SB, NCH - 1, "SB%d" % (NCH - 1)))):
                cx.ts(St[:, n0, :], Tall[:, 0, a, :], coef[:, a, 0:1], ALU.mult, r=["Tall", "coef"], w=[key])
                for s in range(1, 9):
                    cx.stt(St[:, n0, :], Tall[:, s, a, :], coef[:, a, s:s + 1], St[:, n0, :], ALU.mult, ALU.add,
                           r=["Tall", "coef", key], w=[key])
        else:
            cx.memset(SF[:, 0, :], 0.0, w=["SF0"])
            cx.memset(SB[:, NCH - 1, :], 0.0, w=["SB%d" % (NCH - 1)])
        kfb = [cx.sb("kfb%d" % i + tag, [128, 256], BF16) for i in range(2)]
        for n in range(NCH):
            b = n % 2
            csl = slice(n * 128, (n + 1) * 128)
            cx.tr(psb[0][:, 0:128], rk[:, csl], ident[:, :], r=["rk", "ident"], w=["psb0"])
            cx.tt(kfb[b][:, 0:128], psb[0][:, 0:128], KDF[:, :], ALU.mult, r=["psb0", "KDF"], w=["kfb%d" % b])
            cx.tt(kfb[b][:, 128:256], psb[0][:, 0:128], KDB[:, :], ALU.mult, r=["psb0", "KDB"], w=["kfb%d" % b])
            cx.mm(ps[0][:, 0:256], kfb[b][:, 0:128], rv[:, n, :], True, True, r=["kfb%d" % b, "rv"], w=["ps0"])
            cx.mm(ps[0][:, 256:512], kfb[b][:, 128:256], rv[:, n, :], True, True, r=["kfb%d" % b, "rv"], w=["ps0"])
            cx.copy(KV[:, n, :, :], ps[0][:, :].rearrange("p (a e) -> p a e", a=2), r=["ps0", "ps0"], w=["KV%d" % n], eng="act")
        for n in range(NCH - 1):
            cx.stt(SF[:, n + 1, :], SF[:, n, :], cdf[:, 0:1], KV[:, n, 0, :], ALU.mult, ALU.add,
                   r=["SF%d" % n, "cdf", "KV%d" % n], w=["SF%d" % (n + 1)])
        for n in range(NCH - 1, 0, -1):
            cx.stt(SB[:, n - 1, :], SB[:, n, :], cdb[:, 0:1], KV[:, n, 1, :], ALU.mult, ALU.add,
                   r=["SB%d" % n, "cdb", "KV%d" % n], w=["SB%d" % (n - 1)])
        allSF = ["SF%d" % n for n in range(NCH)]; allSB = ["SB%d" % n for n in range(NCH)]
        cx.copy(SFb[:, :, :], SF[:, :, :], r=allSF, w=["SFb"], eng="pool")
        cx.copy(SBb[:, :, :], SB[:, :, :], r=allSB, w=["SBb"], eng="pool")
        sT = [cx.sb("sT%d" % i + tag, [128, 4, 128], BF16) for i in range(2)]
        Qf = [cx.sb("Qf%d" % i + tag, [128, 4, 128], BF16) for i in range(2)]
        Qb = [cx.sb("Qb%d" % i + tag, [128, 4, 128], BF16) for i in range(2)]
        osb = cx.sb("osb" + tag, [128, 4, 64]); osq = cx.sb("osq" + tag, [128, 4, 64])
        st = cx.sb("st" + tag, [128, 4, 4])
        ysb = [cx.sb("ysb%d" % i + tag, [128, 256], BF16) for i in range(2)]
        yT = cx.sb("yT" + tag, [128, 2, T], BF16)
        for n in range(NCH):
            b = n % 2
            csl = slice(n * 128, (n + 1) * 128)
            for h in range(4):
                cx.mm(ps[1][:, h * 128:(h + 1) * 128], rk[:, csl], rqh[:, h, csl], True, True, r=["rk", "rqh"], w=["ps1"])
            cx.tt(sT[b][:, :, :], ps[1][:, :].rearrange("p (h i) -> p h i", h=4), maskT[:, :, :], ALU.mult,
                  r=["ps1", "maskT"], w=["sT%d" % b])
            cx.tt(Qf[b][:, :, :], rqh[:, :, csl], qdf4[:, :, :], ALU.mult, r=["rqh", "qdf4"], w=["Qf%d" % b], eng="pool")
            cx.tt(Qb[b][:, :, :], rqh[:, :, csl], qdb4[:, :, :], ALU.mult, r=["rqh", "qdb4"], w=["Qb%d" % b], eng="pool")
            for h in range(4):
                o = ps[2][:, h * 64:(h + 1) * 64]
                es = slice(h * 64, (h + 1) * 64)
                cx.mm(o, sT[b][:, h, :], rv[:, n, es], True, False, r=["sT%d" % b, "rv"], w=["ps2"])
                cx.mm(o, Qf[b][:, h, :], SFb[:, n, es], False, False, r=["Qf%d" % b, "SFb"], w=["ps2"])
                cx.mm(o, Qb[b][:, h, :], SBb[:, n, es], False, True, r=["Qb%d" % b, "SBb"], w=["ps2"])
            p2 = ["ps2"]
            cx.copy(osb[:, :, :], ps[2][:, 0:256].rearrange("p (h e) -> p h e", h=4), r=p2, w=["osb"], eng="act")
            cx.red(st[:, 0, :], osb[:, :, :], ALU.add, r=["osb"], w=["st0"])
            cx.tt(osq[:, :, :], osb[:, :, :], osb[:, :, :], ALU.mult, r=["osb"], w=["osq"], eng="pool")
            cx.red(st[:, 1, :], osq[:, :, :], ALU.add, r=["osq"], w=["st1"])
            cx.ts(st[:, 2, :], st[:, 0, :], 1.0 / 64, ALU.mult, r=["st0"], w=["st2"])
            cx.tt(st[:, 3, :], st[:, 2, :], st[:, 2, :], ALU.mult, r=["st2"], w=["st3"])
            cx.stt(st[:, 3, :], st[:, 1, :], 1.0 / 64, st[:, 3, :], ALU.mult, ALU.subtract, r=["st1", "st3"], w=["st3"])
            cx.act(st[:, 3, :], st[:, 3, :], AF.Sqrt, bias=EPS, r=["st3"], w=["st3"])
            cx.recip(st[:, 3, :], st[:, 3, :], r=["st3"], w=["st3"])
            for h in range(4):
                cx.ts(osb[:, h, :], osb[:, h, :], st[:, 2, h:h + 1], ALU.subtract, s2=st[:, 3, h:h + 1], op1=ALU.mult,
                      r=["osb", "st2", "st3"], w=["osb"])
            cx.tt(ysb[b][:, :], osb[:, :, :].rearrange("p h e -> p (h e)"), rg[:, n, :], ALU.mult, r=["osb", "rg"], w=["ysb%d" % b])
            for c2 in range(2):
                cx.tr(psb[1][:, c2 * 128:(c2 + 1) * 128], ysb[b][:, c2 * 128:(c2 + 1) * 128], ident[:, :],
                      r=["ysb%d" % b, "ident"], w=["psb1"])
            cx.copy(yT[:, :, csl], psb[1][:, 0:256].rearrange("p (c t) -> p c t", c=2), r=["psb1"], w=["yT"], eng="act")
        for c2 in range(2):
            cx.dma("sp", dr["ysT" + sfx][6 + c2, :, :], yT[:, c2, :], r=["yT"], w=["D:ys3" + sfx])
    cx.end()


def phase_merge(cx, l, dr, segs, wl=None):
    wl = l if wl is None else wl
    cx.begin(nf=8, nb=0)
    ps = cx.ps
    cst = cx.sb("cst", [128, CSTW])
    cx.dma("sp", cst[:, :], dr["cst"][:, :], w=["cst"])
    WG = cx.sb("WG", [128, 8, 4096], BF16)
    WB = cx.sb("WB", [128, 8, 1024], BF16)
    WO = cx.sb("WO", [128, 8, 1024], BF16)
    for kc in range(8):
        cx.dma("pool", WG[:, kc, :], dr["w_gate"][wl, kc * 128:(kc + 1) * 128, :], w=["WG%d" % kc])
        cx.dma("pool", WB[:, kc, :], dr["w_branch"][wl, kc // 2, (kc % 2) * 128:(kc % 2 + 1) * 128, :], w=["WB%d" % kc])
        cx.dma("pool", WO[:, kc, :], dr["w_o"][wl, kc * 128:(kc + 1) * 128, :], w=["WO%d" % kc])
    braw = cx.sb("braw", [32, 128]); bgt = cx.sb("bgt", [128, 32])
    cx.dma("sp", braw[:, :], dr["b_gate"][l, :].rearrange("(a p) -> a p", p=128), w=["braw"])
    cx.tr(ps[7][:, 0:32], braw[:, :], cs(cst, "I1")[0:32, 0:32], r=["braw", "cst"], w=["ps7"])
    cx.copy(bgt[:, :], ps[7][:, 0:32], r=["ps7"], w=["bgt"])
    g1b = cx.sb("g1b", [128, D])
    hT = [cx.sb("mhT%d" % i, [128, 8, 512], BF16) for i in range(2)]
    yT = [cx.sb("myT%d" % i, [128, 8, 512], BF16) for i in range(2)]
    mT = cx.sb("mT", [128, 8, 512], BF16)
    sg = [cx.sb("sg%d" % i, [128, 512]) for i in range(2)]
    macc = cx.sb("macc", [128, 512]); mtmp = cx.sb("mtmp", [128, 512])
    xt = [cx.sb("mxt%d" % i, [128, D]) for i in range(2)]
    gi = 0
    xi = 0
    for (tag, T, xin, xout) in segs:
        sfx = "_" + tag
        seg = 0 if tag == "l" else 1
        cx.dma("sp", g1b[:, :], dr["modv"][l, seg, 2, :].partition_broadcast(128), r=["D:modv%d" % l], w=["g1b"])
        G = min(512, T)
        for g in range(T // G):
            t0 = g * G
            b = gi % 2
            gi += 1
            cx.dma("sp", hT[b][:, :, 0:G], dr["hT" + sfx][:, :, t0:t0 + G], r=["D:hT" + sfx], w=["mhT%d" % b])
            cx.dma("sp", yT[b][:, :, 0:G], dr["ysT" + sfx][:, :, t0:t0 + G].rearrange("a p t -> p a t"),
                   r=["D:ys0" + sfx, "D:ys1" + sfx, "D:ys2" + sfx, "D:ys3" + sfx], w=["myT%d" % b])
            for nn in range(8):
                for i in range(4):
                    pa = ps[(i % 2) * 2]
                    pb = ps[(i % 2) * 2 + 1]
                    ka = "ps%d" % ((i % 2) * 2)
                    kb = "ps%d" % ((i % 2) * 2 + 1)
                    col = i * 1024 + nn * 128
                    for kc in range(8):
                        cx.mm(pa[:, 0:G], WG[:, kc, col:col + 128], hT[b][:, kc, 0:G], kc == 0, kc == 7,
                              r=["WG%d" % kc, "mhT%d" % b], w=[ka])
                    for c2 in range(2):
                        cx.mm(pb[:, 0:G], WB[:, i * 2 + c2, nn * 128:(nn + 1) * 128], yT[b][:, i * 2 + c2, 0:G], c2 == 0, c2 == 1,
                              r=["WB%d" % (i * 2 + c2), "myT%d" % b], w=[kb])
                    s = sg[i % 2]
                    ks = "sg%d" % (i % 2)
                    cx.act(s[:, 0:G], pa[:, 0:G], AF.Sigmoid, bias=bgt[:, i * 8 + nn:i * 8 + nn + 1], r=[ka, "bgt"], w=[ks])
                    if i == 0:
                        cx.tt(macc[:, 0:G], pb[:, 0:G], s[:, 0:G], ALU.mult, r=[kb, ks], w=["macc"])
                    elif i < 3:
                        cx.tt(mtmp[:, 0:G], pb[:, 0:G], s[:, 0:G], ALU.mult, r=[kb, ks], w=["mtmp"])
                        cx.tt(macc[:, 0:G], macc[:, 0:G], mtmp[:, 0:G], ALU.add, r=["macc", "mtmp"], w=["macc"], eng="pool")
                    else:
                        cx.tt(mtmp[:, 0:G], pb[:, 0:G], s[:, 0:G], ALU.mult, r=[kb, ks], w=["mtmp"])
                        cx.tt(mT[:, nn, 0:G], macc[:, 0:G], mtmp[:, 0:G], ALU.add, r=["macc", "mtmp"], w=["mT"], eng="pool")
            for j in range(G // 128):
                xb = xi % 2
                xi += 1
                rows = slice(t0 + j * 128, t0 + (j + 1) * 128)
                cx.dma("sp", xt[xb][:, :], xin[rows, :], w=["mxt%d" % xb])
                for nh in range(2):
                    po = ps[4 + nh]
                    for kc in range(8):
                        cx.mm(po[:, :], mT[:, kc, j * 128:(j + 1) * 128], WO[:, kc, nh * 512:(nh + 1) * 512], kc == 0, kc == 7,
                              r=["mT", "WO%d" % kc], w=["ps%d" % (4 + nh)])
                    hs = slice(nh * 512, (nh + 1) * 512)
                    cx.tt(mtmp[:, :], po[:, :], g1b[:, hs], ALU.mult, r=["ps%d" % (4 + nh), "g1b"], w=["mtmp"])
                    cx.tt(xt[xb][:, hs], xt[xb][:, hs], mtmp[:, :], ALU.add, r=["mxt%d" % xb, "mtmp"], w=["mxt%d" % xb], eng="pool")
                cx.dma("sp", xout[rows, :], xt[xb][:, :], r=["mxt%d" % xb], w=["D:xmid" + sfx])
    cx.end()


def phase_attn(cx, l, dr, segs, lam_init):
    NB = 2
    NSB = 3
    cx.begin(nf=0, nb=0)
    psS = [cx.psum("psS%d" % i, [128, NB * 512]) for i in range(NSB)]
    psO = [cx.psum("psO%d" % i, [128, 512]) for i in range(2)]
    NKT = max(s[2] for s in segs)
    cst = cx.sb("cst", [128, CSTW])
    ident = cx.sb("ident", [128, 128], BF16)
    cx.dma("sp", cst[:, :], dr["cst"][:, :], w=["cst"])
    cx.dma("sp", ident[:, :], dr["ident"][:, :], w=["ident"])
    kT = cx.sb("kTall", [128, 2, NKT * 128], BF16)
    Va = cx.sb("Vaug", [128, NKT, 4, 65], BF16)
    vst = [cx.sb("vst%d" % i, [128, 10, 256], BF16) for i in range(2)]
    kTk = []
    for c in range(2):
        cx.dma("sp", kT[:, c, 0:TC], dr["kT_c"][c, :, :], w=["kTc%d" % c])
        kTk.append("kTc%d" % c)
        if NKT > TC // 128:
            for r_ in range(NCORE):
                cx.dma("sp" if (r_ % 2 == 0) else "act", kT[:, c, TC + r_ * TL:TC + (r_ + 1) * TL],
                       dr["gk"][(r_ * 2 + c) * 128:(r_ * 2 + c + 1) * 128, :], w=["kT%d_%d" % (c, r_)])
                kTk.append("kT%d_%d" % (c, r_))
    cx.memset(Va[:, :, :, 64:65], 1.0, w=["Vones"], eng="pool")
    chunks = [(0, TC // 128, dr["V_c"], 0)]
    k0 = TC // 128
    while k0 < NKT:
        k1 = min(NKT, k0 + 10)
        chunks.append((k0, k1, dr["gv"], (k0 - TC // 128) * 128))
        k0 = k1
    nst = len(chunks)
    for i, (k0, k1, src, row0) in enumerate(chunks):
        b = i % 2
        cx.dma("sp", vst[b][:, 0:k1 - k0, :], src[row0:row0 + (k1 - k0) * 128, :].rearrange("(k p) e -> p k e", p=128), w=["vst%d" % b])
        cx.copy(Va[:, k0:k1, :, 0:64], vst[b][:, 0:k1 - k0, :].rearrange("p k (h e) -> p k h e", h=4), r=["vst%d" % b],
                w=["Va%d" % i], eng=("pool" if i % 2 == 0 else "dve"))
    Vak = ["Va%d" % i for i in range(nst)] + ["Vones"]
    lq = cx.sb("lq", [128, 4, 32]); lp = cx.sb("lp", [128, 2, 32]); ls = cx.sb("ls", [128, 4])
    for i, nm in enumerate(("lam_q1", "lam_k1", "lam_q2", "lam_k2")):
        cx.dma("sp", lq[:, i, :], dr[nm][l, :].partition_broadcast(128), w=["lq"])
    cx.tt(lp[:, 0, :], lq[:, 0, :], lq[:, 1, :], ALU.mult, r=["lq"], w=["lp"])
    cx.tt(lp[:, 1, :], lq[:, 2, :], lq[:, 3, :], ALU.mult, r=["lq"], w=["lp"])
    cx.red(ls[:, 0:2], lp[:, :, :], ALU.add, r=["lp"], w=["ls"])
    cx.act(ls[:, 0:2], ls[:, 0:2], AF.Exp, r=["ls"], w=["ls"])
    cx.tt(ls[:, 2:3], ls[:, 1:2], ls[:, 0:1], ALU.subtract, r=["ls"], w=["ls2"])
    cx.ts(ls[:, 3:4], ls[:, 2:3], -lam_init, ALU.add, r=["ls2"], w=["nlam"])
    nlam = ls[:, 3:4]
    dgb = cx.sb("dgb", [128, 4, 64])
    for h in range(4):
        cx.dma("sp", dgb[:, h, :], dr["diff_g"][l, :].partition_broadcast(128), w=["dgb"])
    cx.ts(dgb[:, :, :], dgb[:, :, :], 1.0 - lam_init, ALU.mult, r=["dgb"], w=["dgb"])
    qg = [cx.sb("qg%d" % i, [128, 2, 512], BF16) for i in range(2)]
    qm = [cx.sb("qm%d" % i, [128, 8, 512], BF16) for i in range(2)]
    pT = [cx.sb("pT%d" % i, [128, NB, 512], BF16) for i in range(NSB)]
    oT = cx.sb("oT", [65, 2, 512])
    oatt = cx.sb("oatt", [128, 4, 4, 64]); osq = cx.sb("aosq", [128, 4, 64])
    rr = cx.sb("rr", [128, 4]); ast = cx.sb("ast", [128, 2, 4])
    ysb = [cx.sb("aysb%d" % i, [128, 256]) for i in range(2)]
    yT = cx.sb("ayT", [128, 2, 512], BF16)
    gi = 0
    for (tag, T, nkt) in segs:
        sfx = "_" + tag
        G = min(512, T)
        nt = G // 128
        assert nkt % NB == 0
        for g in range(T // G):
            t0 = g * G
            b = gi % 2
            gi += 1
            cx.dma("sp", qg[b][:, :, 0:G], dr["qT" + sfx][:, :, t0:t0 + G].rearrange("c p t -> p c t"),
                   r=["D:rope" + sfx + "10", "D:rope" + sfx + "11"], w=["qg%d" % b])
            qb_ = b
            for c in range(2):
                for bl in range(4):
                    cx.ts(qm[qb_][:, c * 4 + bl, 0:G], qg[b][:, c, 0:G], cs(cst, "bm8")[:, bl:bl + 1], ALU.mult,
                          r=["qg%d" % b, "cst"], w=["qm%d_%d" % (qb_, c * 4 + bl)])
            items = [(h, m, kb) for h in range(4) for m in range(2) for kb in range(nkt // NB)]
            LA = 2

            def emit_S(i):
                h, m, kb = items[i]
                c = h // 2
                qi = c * 4 + (h % 2) * 2 + m
                sb_ = i % NSB
                for j in range(NB):
                    kt = kb * NB + j
                    kk = "kTc%d" % c if kt < TC // 128 else "kT%d_%d" % (c, (kt * 128 - TC) // TL)
                    cx.mm(psS[sb_][:, j * 512:j * 512 + G], kT[:, c, kt * 128:(kt + 1) * 128], qm[qb_][:, qi, 0:G], True, True,
                          r=[kk, "qm%d_%d" % (qb_, qi)], w=["psS%d" % sb_])
                cx.act(pT[sb_][:, :, 0:G], psS[sb_][:, :].rearrange("p (j n) -> p j n", j=NB)[:, :, 0:G], AF.Exp, scale=QSCALE,
                       r=["psS%d" % sb_], w=["pT%d" % sb_])

            def emit_O(i):
                h, m, kb = items[i]
                sb_ = i % NSB
                for j in range(NB):
                    kt = kb * NB + j
                    vk = "Va0" if kt < TC // 128 else "Va%d" % (1 + (kt - TC // 128) // 10)
                    cx.mm(psO[m][0:65, 0:G], Va[:, kt, h, :], pT[sb_][:, j, 0:G], kt == 0, kt == nkt - 1,
                          r=[vk, "Vones", "pT%d" % sb_], w=["psO%d" % m])
                if kb == nkt // NB - 1:
                    cx.copy(oT[:, m, 0:G], psO[m][0:65, 0:G], r=["psO%d" % m], w=["oT%d" % m])
                    if m == 1:
                        head_epilogue(h)

            def head_epilogue(h):
                for j in range(nt):
                    for m in range(2):
                        cx.tr(psO[0][:, m * 65:m * 65 + 65], oT[0:65, m, j * 128:(j + 1) * 128], cs(cst, "I1")[0:65, 0:65],
                              r=["oT%d" % m, "cst"], w=["psO0"])
                    cx.recip(rr[:, 0:1], psO[0][:, 64:65], r=["psO0"], w=["rr0"])
                    cx.recip(rr[:, 1:2], psO[0][:, 129:130], r=["psO0"], w=["rr1"])
                    cx.tt(rr[:, 2:3], rr[:, 1:2], nlam, ALU.mult, r=["rr1", "nlam"], w=["rr2"])
                    cx.ts(oatt[:, j, h, :], psO[0][:, 0:64], rr[:, 0:1], ALU.mult, r=["psO0", "rr0"], w=["oatt%d" % j])
                    cx.stt(oatt[:, j, h, :], psO[0][:, 65:129], rr[:, 2:3], oatt[:, j, h, :], ALU.mult, ALU.add,
                           r=["psO0", "rr2", "oatt%d" % j], w=["oatt%d" % j])

            if ATT_ROW:
                items = [(h, kt) for h in range(4) for kt in range(nkt)]

                def emit_S(i):
                    h, kt = items[i]
                    c = h // 2
                    sb_ = i % NSB
                    kk = "kTc%d" % c if kt < TC // 128 else "kT%d_%d" % (c, (kt * 128 - TC) // TL)
                    for m in range(2):
                        blk = (h % 2) * 2 + m
                        rs = slice(32 * blk, 32 * blk + 32)
                        cx.mm(psS[sb_][:, m * 512:m * 512 + G], kT[rs, c, kt * 128:(kt + 1) * 128], qg[b][rs, c, 0:G], True, True,
                              r=[kk, "qg%d" % b], w=["psS%d" % sb_], tile_position=(32 * blk, 0))
                    cx.act(pT[sb_][:, :, 0:G], psS[sb_][:, :].rearrange("p (j n) -> p j n", j=NB)[:, :, 0:G], AF.Exp, scale=QSCALE,
                           r=["psS%d" % sb_], w=["pT%d" % sb_])

                def emit_O(i):
                    h, kt = items[i]
                    sb_ = i % NSB
                    vk = "Va0" if kt < TC // 128 else "Va%d" % (1 + (kt - TC // 128) // 10)
                    for m in range(2):
                        cx.mm(psO[m][0:65, 0:G], Va[:, kt, h, :], pT[sb_][:, m, 0:G], kt == 0, kt == nkt - 1,
                              r=[vk, "Vones", "pT%d" % sb_], w=["psO%d" % m])
                    if kt == nkt - 1:
                        for m in range(2):
                            cx.copy(oT[:, m, 0:G], psO[m][0:65, 0:G], r=["psO%d" % m], w=["oT%d" % m])
                        head_epilogue(h)
            n_it = len(items)
            for i in range(n_it + LA):
                if i < n_it:
                    emit_S(i)
                if i >= LA:
                    emit_O(i - LA)
            for j in range(nt):
                yb = j % 2
                cx.tt(osq[:, :, :], oatt[:, j, :, :], oatt[:, j, :, :], ALU.mult, r=["oatt%d" % j], w=["aosq"], eng="pool")
                cx.red(ast[:, 0, :], osq[:, :, :], ALU.add, r=["aosq"], w=["ast0"])
                cx.act(ast[:, 1, :], ast[:, 0, :], AF.Sqrt, scale=1.0 / 64, bias=EPS, r=["ast0"], w=["ast1"])
                cx.recip(ast[:, 1, :], ast[:, 1, :], r=["ast1"], w=["ast1"])
                for h in range(4):
                    cx.stt(oatt[:, j, h, :], oatt[:, j, h, :], ast[:, 1, h:h + 1], dgb[:, h, :], ALU.mult, ALU.mult,
                           r=["oatt%d" % j, "ast1", "dgb"], w=["oatt%d" % j])
                cx.copy(ysb[yb][:, :], oatt[:, j, :, :].rearrange("p h e -> p (h e)"), r=["oatt%d" % j], w=["aysb%d" % yb], eng="pool")
                for c2 in range(2):
                    cx.tr(psO[1][:, c2 * 128:(c2 + 1) * 128], ysb[yb][:, c2 * 128:(c2 + 1) * 128], cs(cst, "I1"),
                          r=["aysb%d" % yb, "cst"], w=["psO1"])
                cx.copy(yT[:, :, j * 128:(j + 1) * 128], psO[1][:, 0:256].rearrange("p (c t) -> p c t", c=2), r=["psO1"], w=["ayT"])
            for c2 in range(2):
                cx.dma("sp", dr["ysT" + sfx][4 + c2, :, t0:t0 + G], yT[:, c2, 0:G], r=["ayT"], w=["D:ys2" + sfx])
    cx.end()


def phase_moe(cx, l, dr, segs, final, wl=None):
    wl = l if wl is None else wl
    cx.begin(nf=6, nb=2)
    ps, psb = cx.ps, cx.psb
    NT = sum(s[1] for s in segs) // 128
    TT = NT * 128
    cst = cx.sb("cst", [128, CSTW])
    ident = cx.sb("ident", [128, 128], BF16)
    cx.dma("sp", cst[:, :], dr["cst"][:, :], w=["cst"])
    cx.dma("sp", ident[:, :], dr["ident"][:, :], w=["ident"])
    h2T = cx.sb("h2T", [128, 8, TT], BF16)
    acc = cx.sb("eacc", [128, NT, D])
    wt = cx.sb("wt", [128, NT, 16])
    WR = cx.sb("WRt", [128, 8, 16], BF16)
    cx.dma("pool", WR[:, :, :], dr["w_router"].rearrange("(k p) e -> p k e", p=128), w=["WRt"])
    brb = cx.sb("brb", [128, 16])
    cx.dma("sp", brb[:, :], dr["b_router"][0, :].partition_broadcast(128), w=["brb"])
    gsb = cx.sb("gsb2", [128, D]); shb = cx.sb("shb2", [128, D]); g2b = cx.sb("g2b2", [128, D])
    xt = [cx.sb("ext%d" % i, [128, D]) for i in range(2)]
    junk = cx.sb("ejunk", [128, D], BF16)
    t1 = cx.sb("et1", [128, D])
    hb = [cx.sb("ehb%d" % i, [128, D], BF16) for i in range(2)]
    ssq = cx.sb("essq", [128, 2]); rstd = cx.sb("erstd", [128, 2])
    rt = cx.sb("rt", [128, 8, 16])
    W1 = [cx.sb("W1_%d" % i, [128, 8, DFF], BF16) for i in range(2)]
    W3 = [cx.sb("W3_%d" % i, [128, 8, DFF], BF16) for i in range(2)]
    W2 = [cx.sb("W2_%d" % i, [128, 4, D], BF16) for i in range(2)]

    def load_expert(e):
        b = e % 2
        cx.dma("pool", W1[b][:, :, :], dr["w1_e"][wl, e].rearrange("(k p) f -> p k f", p=128), w=["W1_%d" % b])
        cx.dma("pool", W3[b][:, :, :], dr["w3_e"][wl, e].rearrange("(k p) f -> p k f", p=128), w=["W3_%d" % b])
        cx.dma("pool", W2[b][:, :, :], dr["w2_e"][wl, e].rearrange("(k p) n -> p k n", p=128), w=["W2_%d" % b])
    load_expert(0)
    load_expert(1)
    ti = 0
    tiles = []
    for (tag, T, xmid, xout) in segs:
        seg = 0 if tag == "l" else 1
        cx.dma("sp", gsb[:, :], dr["modv"][l, seg, 3, :].partition_broadcast(128), r=["D:modv%d" % l], w=["gsb2"])
        cx.dma("sp", shb[:, :], dr["modv"][l, seg, 4, :].partition_broadcast(128), r=["D:modv%d" % l], w=["shb2"])
        for j in range(T // 128):
            b = ti % 2
            kx = "ext%d" % b
            rows = slice(j * 128, (j + 1) * 128)
            tiles.append((tag, seg, xmid, xout, rows))
            cx.dma("sp", xt[b][:, :], xmid[rows, :], r=["D:xmid_" + tag], w=[kx])
            cx.act(junk[:, :], xt[b][:, :], AF.Square, accum_out=ssq[:, b:b + 1], r=[kx], w=["ejunk", "essq%d" % b])
            cx.act(rstd[:, b:b + 1], ssq[:, b:b + 1], AF.Sqrt, scale=1.0 / D, bias=EPS, r=["essq%d" % b], w=["ers%d" % b])
            cx.recip(rstd[:, b:b + 1], rstd[:, b:b + 1], r=["ers%d" % b], w=["ers%d" % b])
            cx.stt(t1[:, :], xt[b][:, :], rstd[:, b:b + 1], gsb[:, :], ALU.mult, ALU.mult, r=[kx, "ers%d" % b, "gsb2"], w=["et1"])
            cx.tt(hb[b][:, :], t1[:, :], shb[:, :], ALU.add, r=["et1", "shb2"], w=["ehb%d" % b], eng="pool")
            for kc in range(8):
                cx.tr(psb[0][:, kc * 128:(kc + 1) * 128], hb[b][:, kc * 128:(kc + 1) * 128], ident[:, :],
                      r=["ehb%d" % b, "ident"], w=["psb0"])
            cx.copy(h2T[:, :, ti * 128:(ti + 1) * 128], psb[0][:, :].rearrange("p (k t) -> p k t", k=8),
                    r=["psb0"], w=["h2T%d" % ti], eng="act")
            for kc in range(8):
                cx.mm(ps[0][:, 0:16], h2T[:, kc, ti * 128:(ti + 1) * 128], WR[:, kc, :], kc == 0, kc == 7,
                      r=["h2T%d" % ti, "WRt"], w=["ps0"])
            s_ = rt[:, 0, :]; sbv = rt[:, 1, :]; tmp = rt[:, 2, :]; sb2 = rt[:, 3, :]; sbm = rt[:, 4, :]
            msk = rt[:, 5, :]; sel = rt[:, 6, :]
            g4 = rt[:, 7, 0:4]; g4b = rt[:, 7, 4:8]; gm = rt[:, 7, 8:12]; e1 = rt[:, 7, 12:13]; e2 = rt[:, 7, 13:14]
            den = rt[:, 7, 14:15]
            cx.act(s_, ps[0][:, 0:16], AF.Sigmoid, r=["ps0"], w=["r_s"])
            cx.tt(sbv, s_, brb[:, :], ALU.add, r=["r_s", "brb"], w=["r_sb"])
            v4 = lambda a: a.rearrange("p (g e) -> p g e", g=4)
            cx.red(g4, v4(sbv), ALU.max, r=["r_sb"], w=["r_g4"])
            for g_ in range(4):
                cx.ts(tmp[:, g_ * 4:(g_ + 1) * 4], sbv[:, g_ * 4:(g_ + 1) * 4], g4[:, g_:g_ + 1], ALU.is_equal,
                      r=["r_sb", "r_g4"], w=["r_tmp"])
            cx.stt(sb2, tmp, -1.0e9, sbv, ALU.mult, ALU.add, r=["r_tmp", "r_sb"], w=["r_sb2"])
            cx.red(g4b, v4(sb2), ALU.max, r=["r_sb2"], w=["r_g4b"])
            cx.tt(g4, g4, g4b, ALU.add, r=["r_g4", "r_g4b"], w=["r_g4"])
            cx.red(e1, g4, ALU.max, r=["r_g4"], w=["r_e1"])
            cx.ts(gm, g4, e1, ALU.is_equal, s2=-1.0, op1=ALU.add, r=["r_g4", "r_e1"], w=["r_gm"])
            for g_ in range(4):
                cx.ts(tmp[:, g_ * 4:(g_ + 1) * 4], cs(cst, "ones")[:, 0:4], gm[:, g_:g_ + 1], ALU.mult,
                      r=["r_gm", "cst"], w=["r_tmp"])
            cx.stt(sbm, tmp, 1.0e9, sbv, ALU.mult, ALU.add, r=["r_tmp", "r_sb"], w=["r_sbm"])
            cx.red(e1, sbm, ALU.max, r=["r_sbm"], w=["r_e1"])
            cx.ts(msk, sbm, e1, ALU.is_equal, r=["r_sbm", "r_e1"], w=["r_msk"])
            cx.stt(sb2, msk, -1.0e9, sbm, ALU.mult, ALU.add, r=["r_msk", "r_sbm"], w=["r_sb2"])
            cx.red(e2, sb2, ALU.max, r=["r_sb2"], w=["r_e2"])
            cx.ts(sel, sb2, e2, ALU.is_equal, r=["r_sb2", "r_e2"], w=["r_sel"])
            cx.tt(sel, sel, msk, ALU.add, r=["r_sel", "r_msk"], w=["r_sel"])
            cx.tt(sel, sel, s_, ALU.mult, r=["r_sel", "r_s"], w=["r_sel"])
            cx.red(den, sel, ALU.add, r=["r_sel"], w=["r_den"])
            cx.recip(den, den, r=["r_den"], w=["r_den"])
            cx.ts(wt[:, ti, :], sel, den, ALU.mult, r=["r_sel", "r_den"], w=["wt%d" % ti])
            ti += 1
    uT = [cx.sb("uT%d" % i, [128, 4, 512], BF16) for i in range(2)]
    s1 = [cx.sb("s1_%d" % i, [128, 512]) for i in range(2)]
    groups = []
    t = 0
    while t < NT:
        n = min(4, NT - t)
        groups.append((t, n))
        t += n
    ui = 0
    for e in range(NEXP):
        b = e % 2
        if e >= 2:
            load_expert(e)
        for (tg, ntl) in groups:
            G = ntl * 128
            gsl = slice(tg * 128, tg * 128 + G)
            hk = ["h2T%d" % i for i in range(tg, tg + ntl)]
            ub = ui % 2
            ui += 1
            for fc in range(4):
                fs = slice(fc * 128, (fc + 1) * 128)
                pa = ps[(fc % 2) * 2]; pb = ps[(fc % 2) * 2 + 1]
                ka = "ps%d" % ((fc % 2) * 2); kb = "ps%d" % ((fc % 2) * 2 + 1)
                for kc in range(8):
                    cx.mm(pa[:, 0:G], W1[b][:, kc, fs], h2T[:, kc, gsl], kc == 0, kc == 7, r=hk + ["W1_%d" % b], w=[ka])
                for kc in range(8):
                    cx.mm(pb[:, 0:G], W3[b][:, kc, fs], h2T[:, kc, gsl], kc == 0, kc == 7, r=hk + ["W3_%d" % b], w=[kb])
                sb_ = s1[fc % 2]
                cx.act(sb_[:, 0:G], pa[:, 0:G], AF.Silu, r=[ka], w=["s1_%d" % (fc % 2)])
                cx.tt(uT[ub][:, fc, 0:G], pb[:, 0:G], sb_[:, 0:G], ALU.mult, r=[kb, "s1_%d" % (fc % 2)], w=["uT%d" % ub])
            for j in range(ntl):
                tix = tg + j
                for nh in range(2):
                    po = ps[4 + nh]
                    for fc in range(4):
                        cx.mm(po[:, :], uT[ub][:, fc, j * 128:(j + 1) * 128], W2[b][:, fc, nh * 512:(nh + 1) * 512], fc == 0, fc == 3,
                              r=["uT%d" % ub, "W2_%d" % b], w=["ps%d" % (4 + nh)])
                    a = acc[:, tix, nh * 512:(nh + 1) * 512]
                    ka2 = "eacc%d_%d" % (tix, nh)
                    if e == 0:
                        cx.ts(a, po[:, :], wt[:, tix, e:e + 1], ALU.mult, r=["ps%d" % (4 + nh), "wt%d" % tix], w=[ka2])
                    else:
                        cx.stt(a, po[:, :], wt[:, tix, e:e + 1], a, ALU.mult, ALU.add, r=["ps%d" % (4 + nh), "wt%d" % tix, ka2], w=[ka2])
    if final:
        gfb = cx.sb("gfb", [128, D])
        cx.dma("sp", gfb[:, :], dr["g_final"][0, :].partition_broadcast(128), w=["gfb"])
    cur = None
    for ti, (tag, seg, xmid, xout, rows) in enumerate(tiles):
        if cur != seg:
            cx.dma("sp", g2b[:, :], dr["modv"][l, seg, 5, :].partition_broadcast(128), r=["D:modv%d" % l], w=["g2b2"])
            cur = seg
        b = ti % 2
        kx = "ext%d" % b
        cx.dma("sp", xt[b][:, :], xmid[rows, :], r=["D:xmid_" + tag], w=[kx])
        cx.tt(t1[:, :], acc[:, ti, :], g2b[:, :], ALU.mult, r=["eacc%d_0" % ti, "eacc%d_1" % ti, "g2b2"], w=["et1"], eng="pool")
        cx.tt(xt[b][:, :], xt[b][:, :], t1[:, :], ALU.add, r=[kx, "et1"], w=[kx])
        if final:
            cx.act(junk[:, :], xt[b][:, :], AF.Square, accum_out=ssq[:, b:b + 1], r=[kx], w=["ejunk", "essq%d" % b])
            cx.act(rstd[:, b:b + 1], ssq[:, b:b + 1], AF.Sqrt, scale=1.0 / D, bias=EPS, r=["essq%d" % b], w=["ers%d" % b])
            cx.recip(rstd[:, b:b + 1], rstd[:, b:b + 1], r=["ers%d" % b], w=["ers%d" % b])
            cx.stt(xt[b][:, :], xt[b][:, :], rstd[:, b:b + 1], gfb[:, :], ALU.mult, ALU.mult, r=[kx, "ers%d" % b, "gfb"], w=[kx])
        cx.dma("sp", xout[rows, :], xt[b][:, :], r=[kx], w=["D:xout_" + tag])
    cx.end()


def phase_moe_sparse(cx, l, dr, segs, final, wl=None):
    wl = l if wl is None else wl
    C = MOE_CAP
    cx.begin(nf=6, nb=2)
    ps, psb = cx.ps, cx.psb
    NT = sum(s[1] for s in segs) // 128
    cst = cx.sb("cst", [128, CSTW])
    ident = cx.sb("ident", [128, 128], BF16)
    cx.dma("sp", cst[:, :], dr["cst"][:, :], w=["cst"])
    cx.dma("sp", ident[:, :], dr["ident"][:, :], w=["ident"])
    Xg = dr["Xg"]
    Yg = dr["Yg"]
    bcreg = {}

    def bc(h):
        if "r" not in bcreg:
            bcreg["r"] = h.to_reg(NSLOT - 1)
        return bcreg["r"]
    W1 = [cx.sb("W1_%d" % i, [128, 8, DFF], BF16) for i in range(2)]
    W3 = [cx.sb("W3_%d" % i, [128, 8, DFF], BF16) for i in range(2)]
    W2 = [cx.sb("W2_%d" % i, [128, 4, D], BF16) for i in range(2)]

    def load_expert(e):
        b = e % 2
        cx.dma("pool", W1[b][:, :, :], dr["w1_e"][wl, e].rearrange("(k p) f -> p k f", p=128), w=["W1_%d" % b])
        cx.dma("pool", W3[b][:, :, :], dr["w3_e"][wl, e].rearrange("(k p) f -> p k f", p=128), w=["W3_%d" % b])
        cx.dma("pool", W2[b][:, :, :], dr["w2_e"][wl, e].rearrange("(k p) n -> p k n", p=128), w=["W2_%d" % b])
    load_expert(0)
    load_expert(1)
    zt = cx.sb("zt", [128, 4, D], BF16)
    cx.memset(zt[:, :, :], 0.0, w=["zt"], eng="pool")
    for i in range(NSLOT // 512):
        cx.dma("sp", Xg[i * 512:(i + 1) * 512, :].rearrange("(b p) d -> p b d", p=128), zt[:, :, :], r=["zt"], w=["D:XgZ%d" % i])
    WR = cx.sb("WRt", [128, 8, 16], BF16)
    cx.dma("pool", WR[:, :, :], dr["w_router"].rearrange("(k p) e -> p k e", p=128), w=["WRt"])
    brb = cx.sb("brb", [128, 16])
    cx.dma("sp", brb[:, :], dr["b_router"][0, :].partition_broadcast(128), w=["brb"])
    gsb = cx.sb("gsb2", [128, D]); shb = cx.sb("shb2", [128, D]); g2b = cx.sb("g2b2", [128, D])
    xt = [cx.sb("ext%d" % i, [128, D]) for i in range(2)]
    junk = cx.sb("ejunk", [128, D], BF16)
    t1s = [cx.sb("et1_%d" % i, [128, D]) for i in range(2)]
    hb = [cx.sb("ehb%d" % i, [128, D], BF16) for i in range(2)]
    hTt = [cx.sb("ehT%d" % i, [128, 8, 128], BF16) for i in range(2)]
    ssq = cx.sb("essq", [128, 2]); rstd = cx.sb("erstd", [128, 2])
    slf = cx.sb("slf", [128, NT, 2]); sli = cx.sb("sli", [128, NT, 2], mybir.dt.int32); wts = cx.sb("wts", [128, NT, 2])
    H2d = dr["H2d"]
    lg = cx.sb("lgall", [128, NT, 16])
    ti = 0
    tiles = []
    for (tag, T, xmid, xout) in segs:
        seg = 0 if tag == "l" else 1
        cx.dma("sp", gsb[:, :], dr["modv"][l, seg, 3, :].partition_broadcast(128), r=["D:modv%d" % l], w=["gsb2"])
        cx.dma("sp", shb[:, :], dr["modv"][l, seg, 4, :].partition_broadcast(128), r=["D:modv%d" % l], w=["shb2"])
        for j in range(T // 128):
            b = ti % 2
            kx = "ext%d" % b
            t1 = t1s[b]
            rows = slice(j * 128, (j + 1) * 128)
            tiles.append((tag, seg, xmid, xout, rows))
            cx.dma("sp", xt[b][:, :], xmid[rows, :], r=["D:xmid_" + tag], w=[kx])
            cx.act(junk[:, :], xt[b][:, :], AF.Square, accum_out=ssq[:, b:b + 1], r=[kx], w=["ejunk", "essq%d" % b])
            cx.act(rstd[:, b:b + 1], ssq[:, b:b + 1], AF.Sqrt, scale=1.0 / D, bias=EPS, r=["essq%d" % b], w=["ers%d" % b])
            cx.recip(rstd[:, b:b + 1], rstd[:, b:b + 1], r=["ers%d" % b], w=["ers%d" % b])
            cx.stt(t1[:, :], xt[b][:, :], rstd[:, b:b + 1], gsb[:, :], ALU.mult, ALU.mult, r=[kx, "ers%d" % b, "gsb2"], w=["et1_%d" % b])
            cx.tt(hb[b][:, :], t1[:, :], shb[:, :], ALU.add, r=["et1_%d" % b, "shb2"], w=["ehb%d" % b], eng="pool")
            cx.dma("sp", H2d[ti * 128:(ti + 1) * 128, :], hb[b][:, :], r=["ehb%d" % b], w=["D:H2d%d" % ti])
            for kc in range(8):
                cx.tr(psb[0][:, kc * 128:(kc + 1) * 128], hb[b][:, kc * 128:(kc + 1) * 128], ident[:, :],
                      r=["ehb%d" % b, "ident"], w=["psb0"])
            cx.copy(hTt[b][:, :, :], psb[0][:, :].rearrange("p (k t) -> p k t", k=8), r=["psb0"], w=["ehT%d" % b], eng="act")
            for kc in range(8):
                cx.mm(ps[0][:, 0:16], hTt[b][:, kc, :], WR[:, kc, :], kc == 0, kc == 7, r=["ehT%d" % b, "WRt"], w=["ps0"])
            cx.copy(lg[:, ti, :], ps[0][:, 0:16], r=["ps0"], w=["lg%d" % ti])
            ti += 1
    RT = lambda n: cx.sb("R" + n, [128, NT, 16])
    S_ = RT("s"); sbv = RT("sb"); tmp = RT("tmp"); sb2 = RT("sb2"); sbm = RT("sbm"); msk = RT("msk"); m2 = RT("m2"); sel = RT("sel")
    pos = RT("pos"); offs = RT("offs"); wv = RT("wv")
    g4 = cx.sb("Rg4", [128, NT, 4]); g4b = cx.sb("Rg4b", [128, NT, 4]); gm = cx.sb("Rgm", [128, NT, 4])
    e1 = cx.sb("Re1", [128, NT]); e2 = cx.sb("Re2", [128, NT]); den = cx.sb("Rden", [128, NT])
    f2 = lambda a: a[:, :, :].rearrange("p t e -> p (t e)")
    v4 = lambda a: a[:, :, :].rearrange("p t (g e) -> p t g e", g=4)
    bt = lambda a: a[:, :].unsqueeze(2).to_broadcast([128, NT, 16])
    bg = lambda a: a[:, :, :].unsqueeze(3).to_broadcast([128, NT, 4, 4])
    b16 = lambda a: a.unsqueeze(1).to_broadcast([128, NT, 16])
    lgk = ["lg%d" % t_ for t_ in range(NT)]
    cx.act(f2(S_), f2(lg), AF.Sigmoid, r=lgk, w=["Rs"])
    cx.tt(sbv[:, :, :], S_[:, :, :], b16(brb[:, :]), ALU.add, r=["Rs", "brb"], w=["Rsb"])
    cx.red(g4[:, :, :], v4(sbv), ALU.max, r=["Rsb"], w=["Rg4"])
    cx.tt(v4(tmp), v4(sbv), bg(g4), ALU.is_equal, r=["Rsb", "Rg4"], w=["Rtmp"])
    cx.stt(f2(sb2), f2(tmp), -1.0e9, f2(sbv), ALU.mult, ALU.add, r=["Rtmp", "Rsb"], w=["Rsb2"])
    cx.red(g4b[:, :, :], v4(sb2), ALU.max, r=["Rsb2"], w=["Rg4b"])
    cx.tt(g4[:, :, :], g4[:, :, :], g4b[:, :, :], ALU.add, r=["Rg4", "Rg4b"], w=["Rg4"])
    cx.red(e1[:, :], g4[:, :, :], ALU.max, r=["Rg4"], w=["Re1"])
    cx.tt(gm[:, :, :], g4[:, :, :], e1[:, :].unsqueeze(2).to_broadcast([128, NT, 4]), ALU.is_equal, r=["Rg4", "Re1"], w=["Rgm"])
    cx.ts(gm[:, :, :], gm[:, :, :], -1.0, ALU.add, s2=1.0e9, op1=ALU.mult, r=["Rgm"], w=["Rgm"])
    cx.tt(v4(sbm), v4(sbv), bg(gm), ALU.add, r=["Rsb", "Rgm"], w=["Rsbm"])
    cx.red(e1[:, :], sbm[:, :, :], ALU.max, r=["Rsbm"], w=["Re1"])
    cx.tt(msk[:, :, :], sbm[:, :, :], bt(e1), ALU.is_equal, r=["Rsbm", "Re1"], w=["Rmsk"])
    cx.stt(f2(sb2), f2(msk), -1.0e9, f2(sbm), ALU.mult, ALU.add, r=["Rmsk", "Rsbm"], w=["Rsb2"])
    cx.red(e2[:, :], sb2[:, :, :], ALU.max, r=["Rsb2"], w=["Re2"])
    cx.tt(m2[:, :, :], sb2[:, :, :], bt(e2), ALU.is_equal, r=["Rsb2", "Re2"], w=["Rm2"])
    cx.tt(sel[:, :, :], m2[:, :, :], msk[:, :, :], ALU.add, r=["Rm2", "Rmsk"], w=["Rsel"])
    cx.mm(ps[1][:, 0:NT * 16], cs(cst, "Ltri"), f2(sel), True, True, r=["cst", "Rsel"], w=["ps1"])
    cx.mm(ps[2][:, 0:NT * 16], cs(cst, "ones"), f2(sel), True, True, r=["cst", "Rsel"], w=["ps2"])
    cx.copy(f2(tmp), ps[2][:, 0:NT * 16], r=["ps2"], w=["Rtmp"])
    cx.memset(offs[:, 0, :], 0.0, w=["Roffs"])
    for t_ in range(1, NT):
        cx.tt(offs[:, t_, :], offs[:, t_ - 1, :], tmp[:, t_ - 1, :], ALU.add, r=["Roffs", "Rtmp"], w=["Roffs"])
    cx.tt(f2(pos), ps[1][:, 0:NT * 16], f2(offs), ALU.add, r=["ps1", "Roffs"], w=["Rpos"])
    cx.ts(f2(tmp), f2(pos), float(C) - 0.5, ALU.is_lt, r=["Rpos"], w=["Rtmp"])
    cx.tt(pos[:, :, :], pos[:, :, :], b16(cs(cst, "eoff")), ALU.add, r=["Rpos", "cst"], w=["Rpos"])
    cx.stt(f2(pos), f2(tmp), -1.0e6, f2(pos), ALU.mult, ALU.add, r=["Rtmp", "Rpos"], w=["Rpos"])
    cx.ts(f2(pos), f2(pos), 1.0e6, ALU.add, r=["Rpos"], w=["Rpos"])
    cx.tt(wv[:, :, :], sel[:, :, :], S_[:, :, :], ALU.mult, r=["Rsel", "Rs"], w=["Rwv"])
    cx.red(den[:, :], wv[:, :, :], ALU.add, r=["Rwv"], w=["Rden"])
    cx.recip(den[:, :], den[:, :], r=["Rden"], w=["Rden"])
    cx.tt(wv[:, :, :], wv[:, :, :], bt(den), ALU.mult, r=["Rwv", "Rden"], w=["Rwv"])
    cx.tt(wv[:, :, :], wv[:, :, :], tmp[:, :, :], ALU.mult, r=["Rwv", "Rtmp"], w=["Rwv"])
    for q, mk, kk in ((0, msk, "Rmsk"), (1, m2, "Rm2")):
        cx.tt(sbm[:, :, :], mk[:, :, :], pos[:, :, :], ALU.mult, r=[kk, "Rpos"], w=["Rsbm"])
        cx.red(slf[:, :, q], sbm[:, :, :], ALU.add, r=["Rsbm"], w=["slf%d" % q])
        cx.tt(sbm[:, :, :], mk[:, :, :], wv[:, :, :], ALU.mult, r=[kk, "Rwv"], w=["Rsbm"])
        cx.red(wts[:, :, q], sbm[:, :, :], ALU.add, r=["Rsbm"], w=["wtsq%d" % q])
    cx.copy(sli[:, :, :], slf[:, :, :], r=["slf0", "slf1"], w=["sli"])
    zk = ["D:XgZ%d" % i_ for i_ in range(NSLOT // 512)]
    for ti in range(NT):
        b = ti % 2
        cx.dma("sp", hb[b][:, :], H2d[ti * 128:(ti + 1) * 128, :], r=["D:H2d%d" % ti], w=["ehb%d" % b])
        for q in range(2):
            idx = sli[:, ti, q:q + 1]
            src = hb[b][:, :]
            cx.S.add("pool", lambda h, idx=idx, src=src: h.indirect_dma_start(
                out=Xg[:, :], out_offset=bass.IndirectOffsetOnAxis(ap=idx, axis=0), in_=src, in_offset=None,
                bounds_check=bc(h), oob_is_err=False), r=["sli", "ehb%d" % b] + zk, w=["D:Xg%d_%d" % (ti, q)], dma=True)
    dummy = cx.sb("dummy", [128, 4])
    cx.memset(dummy[:, 0:1], 0.0, w=["XgAll"], eng="pool")
    cx.S.ops[-1].deps.update({cx.S.lastw[k]: True for k in ["D:Xg%d_%d" % (t_, q) for t_ in range(NT) for q in range(2)]})
    NBLK = C // 128
    NPC = (C + 511) // 512
    PW = C // NPC
    xg = [cx.sb("xg%d" % i, [128, NBLK, D], BF16) for i in range(2)]
    xT = [cx.sb("xTe%d" % i, [128, 8, C], BF16) for i in range(2)]
    uT = cx.sb("uTe", [128, 4, C], BF16)
    s1 = [cx.sb("s1_%d" % i, [128, 512]) for i in range(2)]
    yb = [cx.sb("ybe%d" % i, [128, D]) for i in range(2)]
    yi = 0

    def load_xg(e):
        cx.dma("sp", xg[e % 2][:, :, :], Xg[e * C:(e + 1) * C, :].rearrange("(j p) d -> p j d", p=128), r=["XgAll"], w=["xg%d" % (e % 2)])
    load_xg(0)
    for e in range(NEXP):
        b = e % 2
        if e >= 2:
            load_expert(e)
        if e + 1 < NEXP:
            load_xg(e + 1)
        for j in range(NBLK):
            pbk = psb[j % 2]
            kp = "psb%d" % (j % 2)
            for kc in range(8):
                cx.tr(pbk[:, kc * 128:(kc + 1) * 128], xg[b][:, j, kc * 128:(kc + 1) * 128], ident[:, :],
                      r=["xg%d" % b, "ident"], w=[kp])
            cx.copy(xT[b][:, :, j * 128:(j + 1) * 128], pbk[:, :].rearrange("p (k t) -> p k t", k=8), r=[kp], w=["xTe%d" % b],
                    eng=("act" if j % 2 == 0 else "dve"))
        it = 0
        for fc in range(4):
            fs = slice(fc * 128, (fc + 1) * 128)
            for pc in range(NPC):
                cs_ = slice(pc * PW, (pc + 1) * PW)
                pa = ps[(it % 2) * 2]; pb = ps[(it % 2) * 2 + 1]
                ka = "ps%d" % ((it % 2) * 2); kb = "ps%d" % ((it % 2) * 2 + 1)
                for kc in range(8):
                    cx.mm(pa[:, 0:PW], W1[b][:, kc, fs], xT[b][:, kc, cs_], kc == 0, kc == 7, r=["xTe%d" % b, "W1_%d" % b], w=[ka])
                for kc in range(8):
                    cx.mm(pb[:, 0:PW], W3[b][:, kc, fs], xT[b][:, kc, cs_], kc == 0, kc == 7, r=["xTe%d" % b, "W3_%d" % b], w=[kb])
                sb_ = s1[it % 2]
                cx.act(sb_[:, 0:PW], pa[:, 0:PW], AF.Silu, r=[ka], w=["s1_%d" % (it % 2)])
                cx.tt(uT[:, fc, cs_], pb[:, 0:PW], sb_[:, 0:PW], ALU.mult, r=[kb, "s1_%d" % (it % 2)], w=["uTe"])
                it += 1
        for j in range(NBLK):
            y = yb[yi % 2]
            ky = "ybe%d" % (yi % 2)
            yi += 1
            for nh in range(2):
                po = ps[4 + nh]
                for fc in range(4):
                    cx.mm(po[:, :], uT[:, fc, j * 128:(j + 1) * 128], W2[b][:, fc, nh * 512:(nh + 1) * 512], fc == 0, fc == 3,
                          r=["uTe", "W2_%d" % b], w=["ps%d" % (4 + nh)])
                cx.copy(y[:, nh * 512:(nh + 1) * 512], po[:, :], r=["ps%d" % (4 + nh)], w=[ky], eng=("act" if nh == 0 else "dve"))
            cx.dma("sp", Yg[e * C + j * 128:e * C + (j + 1) * 128, :], y[:, :], r=[ky], w=["D:Yg%d_%d" % (e, j)])
    if final:
        gfb = cx.sb("gfb", [128, D])
        cx.dma("sp", gfb[:, :], dr["g_final"][0, :].partition_broadcast(128), w=["gfb"])
    cx.memset(dummy[:, 1:2], 0.0, w=["YgAll"], eng="pool")
    cx.S.ops[-1].deps.update({cx.S.lastw[k]: True for k in ["D:Yg%d_%d" % (e_, j_) for e_ in range(NEXP) for j_ in range(C // 128)]})
    yg = [[cx.sb("yg%d_%d" % (i, q), [128, D]) for q in range(2)] for i in range(2)]
    for i in range(2):
        for q in range(2):
            cx.memset(yg[i][q][:, :], 0.0, w=["yg%d_%d" % (i, q)], eng="pool")
    cur = None
    for ti, (tag, seg, xmid, xout, rows) in enumerate(tiles):
        if cur != seg:
            cx.dma("sp", g2b[:, :], dr["modv"][l, seg, 5, :].partition_broadcast(128), r=["D:modv%d" % l], w=["g2b2"])
            cur = seg
        b = ti % 2
        kx = "ext%d" % b
        t1 = t1s[b]
        kt1 = "et1_%d" % b
        cx.dma("sp", xt[b][:, :], xmid[rows, :], r=["D:xmid_" + tag], w=[kx])
        for q in range(2):
            dst = yg[b][q][:, :]
            idx = sli[:, ti, q:q + 1]
            cx.S.add("pool", lambda h, idx=idx, dst=dst: h.indirect_dma_start(
                out=dst, out_offset=None, in_=Yg[:, :], in_offset=bass.IndirectOffsetOnAxis(ap=idx, axis=0),
                bounds_check=bc(h), oob_is_err=False), r=["sli", "YgAll"], w=["yg%d_%d" % (b, q)], dma=True)
        cx.ts(t1[:, :], yg[b][0][:, :], wts[:, ti, 0:1], ALU.mult, r=["yg%d_0" % b, "wtsq0"], w=[kt1])
        cx.stt(t1[:, :], yg[b][1][:, :], wts[:, ti, 1:2], t1[:, :], ALU.mult, ALU.add, r=["yg%d_1" % b, "wtsq1", kt1], w=[kt1])
        cx.tt(t1[:, :], t1[:, :], g2b[:, :], ALU.mult, r=[kt1, "g2b2"], w=[kt1], eng="pool")
        cx.tt(xt[b][:, :], xt[b][:, :], t1[:, :], ALU.add, r=[kx, kt1], w=[kx])
        if final:
            cx.act(junk[:, :], xt[b][:, :], AF.Square, accum_out=ssq[:, b:b + 1], r=[kx], w=["ejunk", "essq%d" % b])
            cx.act(rstd[:, b:b + 1], ssq[:, b:b + 1], AF.Sqrt, scale=1.0 / D, bias=EPS, r=["essq%d" % b], w=["ers%d" % b])
            cx.recip(rstd[:, b:b + 1], rstd[:, b:b + 1], r=["ers%d" % b], w=["ers%d" % b])
            cx.stt(xt[b][:, :], xt[b][:, :], rstd[:, b:b + 1], gfb[:, :], ALU.mult, ALU.mult, r=[kx, "ers%d" % b, "gfb"], w=[kx])
        cx.dma("sp", xout[rows, :], xt[b][:, :], r=[kx], w=["D:xout_" + tag])
    cx.end()


def make_expo(core):
    BIG = 1.0e7
    e = np.full((2, 9), BIG, np.float32)
    for c2 in range(NCORE):
        if c2 < core:
            e[0, c2] = TL * (core - 1 - c2)
        if c2 > core:
            e[1, c2] = TL * (c2 - core - 1)
    e[0, 8] = TL * core
    e[1, 8] = TL * (NCORE - 1 - core)
    return np.broadcast_to(e[None], (128, 2, 9)).copy()


def make_expo(core):
    BIG = 1.0e7
    e = np.full((2, 9), BIG, np.float32)
    for c2 in range(NCORE):
        if c2 < core:
            e[0, c2] = TL * (core - 1 - c2)
        if c2 > core:
            e[1, c2] = TL * (c2 - core - 1)
    e[0, 8] = TL * core
    e[1, 8] = TL * (NCORE - 1 - core)
    return np.broadcast_to(e[None], (128, 2, 9)).copy()


def make_sel(core):
    s = np.zeros((128, 2, NCORE), np.float32)
    if core > 0:
        s[:, 0, core - 1] = 1.0
    if core < NCORE - 1:
        s[:, 1, core + 1] = 1.0
    return s


def phase_exchange(cx, l, dr):
    cx.begin(nf=0, nb=0)
    hin = dr["hin"]
    for a, (src, c2) in enumerate(((dr["uT_l"], 0), (dr["uT_l"], 1), (dr["tT_l"], 0), (dr["tT_l"], 1))):
        cx.dma("sp", hin[a * 128:(a + 1) * 128, 0:16], src[c2, :, 0:16], r=["D:uT_l", "D:tT_l"], w=["D:hin"])
        cx.dma("sp", hin[a * 128:(a + 1) * 128, 16:32], src[c2, :, TL - 16:TL], r=["D:uT_l", "D:tT_l"], w=["D:hin"])
    grp = [list(range(NCORE))]

    def cc(src, dst, rk, wk):
        cx.S.add("pool", lambda h: h.collective_compute("AllGather", ALU.bypass, replica_groups=grp, ins=[src], outs=[dst]),
                 r=rk, w=wk, cc=True)
    cc(dr["kT_l"].rearrange("c p t -> (c p) t").opt(), dr["gk"].opt(), ["D:rope_l12", "D:rope_l13"], ["D:gk"])
    cc(dr["V_l"].opt(), dr["gv"].opt(), ["D:V_l"], ["D:gv"])
    cc(dr["Tst_l"].rearrange("a p e -> (a p) e").opt(), dr["gt"].opt(), ["D:Tst_l"], ["D:gt"])
    cc(hin.opt(), dr["hg"].opt(), ["D:hin"], ["D:hg"])
    cx.end()


A_OUT = (("hT", lambda T: [128, 8, T], BF16), ("uT", lambda T: [2, 128, T], F32), ("tT", lambda T: [2, 128, T], F32),
         ("bgT", lambda T: [2, 128, T], BF16), ("qT", lambda T: [2, 128, T], BF16), ("kT", lambda T: [2, 128, T], BF16),
         ("rqT", lambda T: [128, T], BF16), ("rkT", lambda T: [128, T], BF16), ("V", lambda T: [T, 256], BF16),
         ("rv", lambda T: [T, 256], BF16), ("rg", lambda T: [T, 256], BF16), ("Tst", lambda T: [2, 128, 256], F32))
SEGT = (("l", TL), ("c", TC))
NKALL = (SEQ + TC) // 128
EXT_IN = (("c", [1, D]), ("c_ctx", [1, D]), ("w_mod", [2, D, 6 * D]), ("b_mod", [2, 6 * D]), ("g_norm1", [2, D]), ("g_norm2", [2, D]),
          ("w_in", [2, D, INC]), ("conv_a_w", [2, 31, 256]), ("conv_a_b", [2, 256]), ("conv_a_g", [2, 256]),
          ("conv_a_beta", [2, 256]), ("conv_b_w", [2, 3, 256]), ("lam_q1", [2, 32]), ("lam_k1", [2, 32]), ("lam_q2", [2, 32]),
          ("lam_k2", [2, 32]), ("diff_g", [2, 64]), ("ret_ld_f", [2, 4]), ("ret_ld_b", [2, 4]), ("w_gate", [2, D, 4096]),
          ("b_gate", [2, 4096]), ("w_branch", [2, 4, 256, D]), ("w_o", [2, D, D]), ("w_router", [D, 16]), ("b_router", [1, 16]),
          ("w1_e", [2, NEXP, D, DFF]), ("w3_e", [2, NEXP, D, DFF]), ("w2_e", [2, NEXP, DFF, D]), ("g_final", [1, D]))


SPARSE_MOE = True


def moe_phase(cx, l, dr, segs, final, wl=None):
    if SPARSE_MOE:
        return phase_moe_sparse(cx, l, dr, segs, final, wl=wl)
    return phase_moe(cx, l, dr, segs, final, wl=wl)


def lam_init_of(l):
    return 0.8 - 0.6 * math.exp(-0.3 * l)


class Launch:
    def __init__(self):
        self.nc = bass.Bass("TRN2", target_bir_lowering=False)
        self.dr = {}
        self.ins = []
        self.outs = []

    def t(self, name, shape, dt=F32, kind=None):
        if kind is None:
            self.dr[name] = self.nc.dram_tensor(name, list(shape), dt).ap()
        else:
            self.dr[name] = self.nc.dram_tensor(name, list(shape), dt, kind=kind).ap()
        if kind == "ExternalInput":
            self.ins.append(name)
        elif kind == "ExternalOutput":
            self.outs.append(name)


def build_fused():
    L = Launch()
    L.t("cst", [128, CSTW], F32, "ExternalInput")
    L.t("ident", [128, 128], BF16, "ExternalInput")
    for n_, s_ in EXT_IN:
        L.t(n_, s_, F32, "ExternalInput")
    for n_, s_ in (("cosT", [128, TL]), ("sinT", [128, TL]), ("x_l", [TL, D]), ("x_c", [TC, D]), ("expo", [128, 2, 9]),
                   ("sel", [128, 2, NCORE])):
        L.t(n_, s_, F32, "ExternalInput")
    L.t("out", [TL, D], F32, "ExternalOutput")
    L.t("modv", [2, 2, 6, D])
    L.t("x1_l", [TL, D])
    L.t("x1_c", [TC, D])
    L.t("Xg", [NSLOT, D], BF16)
    L.t("Yg", [NSLOT, D], F32)
    L.t("H2d", [TL + TC, D], BF16)
    drl = []
    for l in range(DEPTH):
        d_ = dict(L.dr)
        for tag, T in SEGT:
            for nm, shp, dt in A_OUT:
                L.t("%s_%s%d" % (nm, tag, l), shp(T), dt)
                d_[nm + "_" + tag] = L.dr["%s_%s%d" % (nm, tag, l)]
            L.t("ysT_%s%d" % (tag, l), [8, 128, T], BF16)
            L.t("xmid_%s%d" % (tag, l), [T, D])
            d_["ysT_" + tag] = L.dr["ysT_%s%d" % (tag, l)]
            d_["xmid_" + tag] = L.dr["xmid_%s%d" % (tag, l)]
        for nm, shp, dt in (("gk", [NCORE * 256, TL], BF16), ("gv", [NCORE * TL, 256], BF16), ("gt", [NCORE * 256, 256], F32),
                            ("hg", [NCORE * 512, 32], F32), ("hin", [512, 32], F32)):
            L.t("%s%d" % (nm, l), shp, dt)
            d_[nm] = L.dr["%s%d" % (nm, l)]
        drl.append(d_)
    for d_ in drl:
        for k in ("modv", "x1_l", "x1_c", "Xg", "Yg", "H2d"):
            d_[k] = L.dr[k]
    with ExitStack() as st:
        S = Sched(L.nc, st)
        cx = Ctx(L.nc, S)
        phase_mods(cx, 0, drl[0])
        phase_mods(cx, 1, drl[0])
        d0 = drl[0]
        phase_a(cx, 0, d0, [("l", TL, L.dr["x_l"]), ("c", TC, L.dr["x_c"])])
        phase_exchange(cx, 0, d0)
        phase_conv(cx, 0, d0, [("l", TL), ("c", TC)])
        phase_attn(cx, 0, d0, [("l", TL, NKALL), ("c", TC, TC // 128)], lam_init_of(0))
        phase_ret(cx, 0, d0, [("l", TL), ("c", TC)])
        phase_merge(cx, 0, d0, [("l", TL, L.dr["x_l"], d0["xmid_l"]), ("c", TC, L.dr["x_c"], d0["xmid_c"])])
        moe_phase(cx, 0, d0, [("l", TL, d0["xmid_l"], L.dr["x1_l"]), ("c", TC, d0["xmid_c"], L.dr["x1_c"])], False)
        d1 = drl[1]
        phase_a(cx, 1, d1, [("l", TL, L.dr["x1_l"]), ("c", TC, L.dr["x1_c"])])
        phase_exchange(cx, 1, d1)
        phase_conv(cx, 1, d1, [("l", TL)])
        phase_attn(cx, 1, d1, [("l", TL, NKALL)], lam_init_of(1))
        phase_ret(cx, 1, d1, [("l", TL)])
        phase_merge(cx, 1, d1, [("l", TL, L.dr["x1_l"], d1["xmid_l"])])
        moe_phase(cx, 1, d1, [("l", TL, d1["xmid_l"], L.dr["out"])], True)
    return L


def kernel_fused(**inp):
    f32 = lambda a: np.ascontiguousarray(np.asarray(a, dtype=np.float32))
    x = f32(inp["x"])[0]
    ctx = f32(inp["ctx"])[0]
    base = dict(cst=make_cst(), ident=np.eye(128, dtype=np.float32).astype(NPBF), x_c=ctx)
    for n_, s_ in EXT_IN:
        base[n_] = f32(inp[n_]).reshape(s_)
    L = build_fused()
    maps = []
    for c in range(NCORE):
        m = dict(base)
        cosT, sinT = rope_tables(c)
        m.update(cosT=cosT, sinT=sinT, x_l=x[c * TL:(c + 1) * TL], expo=make_expo(c), sel=make_sel(c))
        maps.append({k: m[k] for k in L.ins})
    res = run_bass_kernel_spmd(L.nc, maps, core_ids=list(range(NCORE)))
    out = np.concatenate([np.asarray(res.results[c]["out"]) for c in range(NCORE)], axis=0)
    return out.reshape(1, SEQ, D).astype(np.float32)


WSLICE = ("w_in", "w_gate", "w_branch", "w_o", "w1_e", "w3_e", "w2_e")
GATH = (("gk", [NCORE * 256, TL], BF16), ("gv", [NCORE * TL, 256], BF16), ("gt", [NCORE * 256, 256], F32),
        ("hg", [NCORE * 512, 32], F32))


def build_stage(stage):
    L = Launch()
    L.t("cst", [128, CSTW], F32, "ExternalInput")
    L.t("ident", [128, 128], BF16, "ExternalInput")
    for n_, s_ in EXT_IN:
        if stage > 1 and n_ in ("w_mod", "b_mod", "c", "c_ctx"):
            continue
        if stage == 1 and n_ in ("w_gate", "w_branch", "w_o", "w1_e", "w3_e", "w2_e"):
            continue
        if stage == 3 and n_ == "w_in":
            continue
        shp = [1] + list(s_[1:]) if n_ in WSLICE else s_
        L.t(n_, shp, F32, "ExternalInput")
    for n_, s_ in (("cosT", [128, TL]), ("sinT", [128, TL]), ("expo", [128, 2, 9]), ("sel", [128, 2, NCORE])):
        L.t(n_, s_, F32, "ExternalInput")
    io = "ExternalInput"
    if stage == 1:
        L.t("x_l", [TL, D], F32, io)
        L.t("x_c", [TC, D], F32, io)
        L.t("modv", [2, 2, 6, D], F32, "ExternalOutput")
    else:
        L.t("modv", [2, 2, 6, D], F32, io)

    def a_tensors(prefix, kind, tags):
        d_ = {}
        for tag, T in SEGT:
            if tag not in tags:
                continue
            for nm, shp, dt in A_OUT:
                L.t(prefix + nm + "_" + tag, shp(T), dt, kind)
                d_[nm + "_" + tag] = L.dr[prefix + nm + "_" + tag]
        return d_
    with ExitStack() as st:
        S = Sched(L.nc, st)
        cx = Ctx(L.nc, S)
        if stage == 1:
            dA = dict(L.dr)
            dA.update(a_tensors("", "ExternalOutput", ("l", "c")))
            phase_mods(cx, 0, dA)
            phase_mods(cx, 1, dA)
            phase_a(cx, 0, dA, [("l", TL, L.dr["x_l"]), ("c", TC, L.dr["x_c"])], wl=0)
        else:
            l = stage - 2
            tags = ("l", "c")
            for nm, shp, dt in GATH:
                L.t(nm, shp, dt, io)
            L.t("Xg", [NSLOT, D], BF16)
            L.t("Yg", [NSLOT, D], F32)
            L.t("H2d", [TL + TC, D], BF16)
            dB = dict(L.dr)
            dB.update(a_tensors("b_", io, tags))
            segs = [("l", TL), ("c", TC)] if l == 0 else [("l", TL)]
            for tag, T in segs:
                L.t("ysT_" + tag, [8, 128, T], BF16)
                L.t("xmid_" + tag, [T, D])
                dB["ysT_" + tag] = L.dr["ysT_" + tag]
                dB["xmid_" + tag] = L.dr["xmid_" + tag]
            if l == 0:
                L.t("x_l", [TL, D], F32, io)
                L.t("x_c", [TC, D], F32, io)
                L.t("x1_l", [TL, D], F32, "ExternalOutput")
                L.t("x1_c", [TC, D])
                xin_l, xin_c, xo_l, xo_c = L.dr["x_l"], L.dr["x_c"], L.dr["x1_l"], L.dr["x1_c"]
            else:
                L.t("x1_l", [TL, D], F32, io)
                L.t("out", [TL, D], F32, "ExternalOutput")
                xin_l, xo_l = L.dr["x1_l"], L.dr["out"]
            phase_conv(cx, l, dB, segs)
            phase_attn(cx, l, dB, [("l", TL, NKALL)] + ([("c", TC, TC // 128)] if l == 0 else []), lam_init_of(l))
            phase_ret(cx, l, dB, segs)
            if l == 0:
                phase_merge(cx, l, dB, [("l", TL, xin_l, dB["xmid_l"]), ("c", TC, xin_c, dB["xmid_c"])], wl=0)
                moe_phase(cx, l, dB, [("l", TL, dB["xmid_l"], xo_l), ("c", TC, dB["xmid_c"], xo_c)], False, wl=0)
                dA = dict(L.dr)
                dA.update(a_tensors("", "ExternalOutput", ("l", "c")))
                phase_a(cx, 1, dA, [("l", TL, xo_l), ("c", TC, xo_c)], wl=0)
            else:
                phase_merge(cx, l, dB, [("l", TL, xin_l, dB["xmid_l"])], wl=0)
                moe_phase(cx, l, dB, [("l", TL, dB["xmid_l"], xo_l)], True, wl=0)
    return L


def host_gather(oA):
    gk = np.concatenate([np.asarray(o["kT_l"]).reshape(256, TL) for o in oA], axis=0)
    gv = np.concatenate([np.asarray(o["V_l"]) for o in oA], axis=0)
    gt = np.concatenate([np.asarray(o["Tst_l"]).reshape(256, 256) for o in oA], axis=0)
    hs = []
    for o in oA:
        u = np.asarray(o["uT_l"])
        t = np.asarray(o["tT_l"])
        h = np.concatenate([np.concatenate([a[c2][:, 0:16], a[c2][:, TL - 16:TL]], axis=1) for a in (u, t) for c2 in range(2)], axis=0)
        hs.append(h)
    hg = np.concatenate(hs, axis=0).astype(np.float32)
    return dict(gk=gk, gv=gv, gt=gt, hg=hg)


def kernel_unfused(**inp):
    f32 = lambda a: np.ascontiguousarray(np.asarray(a, dtype=np.float32))
    x = f32(inp["x"])[0]
    ctx = f32(inp["ctx"])[0]
    ropes = [rope_tables(c) for c in range(NCORE)]
    full = {n_: f32(inp[n_]).reshape(s_) for n_, s_ in EXT_IN}
    cst = make_cst()
    ident = np.eye(128, dtype=np.float32).astype(NPBF)

    def run(L, extra, wl):
        maps = []
        for c in range(NCORE):
            m = dict(cst=cst, ident=ident, cosT=ropes[c][0], sinT=ropes[c][1], expo=make_expo(c), sel=make_sel(c),
                     x_l=x[c * TL:(c + 1) * TL], x_c=ctx)
            for k, v in full.items():
                m[k] = v[wl[k]:wl[k] + 1] if k in WSLICE else v
            m.update(extra[c])
            maps.append({k: m[k] for k in L.ins})
        res = run_bass_kernel_spmd(L.nc, maps, core_ids=list(range(NCORE)))
        return [{k: np.asarray(r[k]) for k in L.outs} for r in res.results]

    o1 = run(build_stage(1), [dict() for _ in range(NCORE)], dict.fromkeys(WSLICE, 0))
    modv = o1[0]["modv"]

    def b_extra(oA):
        g = host_gather(oA)
        ex = []
        for c in range(NCORE):
            e = dict(g)
            e["modv"] = modv
            for k, v in oA[c].items():
                if k != "modv" and k != "x1_l":
                    e["b_" + k] = v
            ex.append(e)
        return ex
    wl2 = dict.fromkeys(WSLICE, 0)
    wl2["w_in"] = 1
    o2 = run(build_stage(2), b_extra(o1), wl2)
    ex3 = b_extra(o2)
    for c in range(NCORE):
        ex3[c]["x1_l"] = o2[c]["x1_l"]
    o3 = run(build_stage(3), ex3, dict.fromkeys(WSLICE, 1))
    out = np.concatenate([o3[c]["out"] for c in range(NCORE)], axis=0)
    return out.reshape(1, SEQ, D).astype(np.float32)


FUSED = False


def kernel(**inp):
    return kernel_fused(**inp) if FUSED else kernel_unfused(**inp)
```

```python
import math
from contextlib import ExitStack
import numpy as np
import ml_dtypes
import concourse.bass as bass
import concourse.mybir as mybir
from concourse.bass_utils import run_bass_kernel_spmd

F32 = mybir.dt.float32
BF16 = mybir.dt.bfloat16
AF = mybir.ActivationFunctionType
ALU = mybir.AluOpType
AX = mybir.AxisListType
NPBF = ml_dtypes.bfloat16

NCORE = 8
D = 1024
SEQ = 16384
TL = SEQ // NCORE
TC = 256
DEPTH = 2
INC = 2816
EPS = 1e-6
NEXP = 16
DFF = 512
QSCALE = 32 ** -0.5
ATT_ROW = False
MOE_CAP = 768
NSLOT = NEXP * MOE_CAP


class Op:
    __slots__ = ("id", "eng", "fn", "deps", "dma", "n", "signal", "val", "cc")


class Sched:
    ENGS = (("sp", "sync"), ("act", "scalar"), ("dve", "vector"), ("pool", "gpsimd"), ("pe", "tensor"))

    def __init__(self, nc, stack):
        self.nc = nc
        self.ops = []
        self.phase_start = 0
        self.lastw = {}
        self.readers = {}
        self.K = dict(sp=12, pool=8, act=4)
        self.dma_list = {e: [] for e in self.K}
        self.sems = {e: stack.enter_context(nc.semaphore("sm_" + e)) for e in ("pe", "act", "dve", "pool")}
        self.dsems = {e: [stack.enter_context(nc.semaphore("sd_%s%d" % (e, i))) for i in range(k)]
                      for e, k in self.K.items()}
        self.ccsems = [stack.enter_context(nc.semaphore("sc_%d" % i)) for i in range(12)]
        self.ncc = 0
        self.cc_list = []
        self.cnt = {e: 0 for e in self.sems}
        self.waited = {e: {} for e, _ in self.ENGS}

    def add(self, eng, fn, r=(), w=(), dma=False, cc=False):
        i = len(self.ops)
        deps = {}
        for k in r:
            j = self.lastw.get(k)
            if j is not None:
                deps[j] = True
        for k in w:
            j = self.lastw.get(k)
            if j is not None and j not in deps:
                deps[j] = False
            rd = self.readers.get(k)
            if rd:
                for j in rd.values():
                    if isinstance(j, list):
                        for jj in j:
                            deps.setdefault(jj, False)
                    else:
                        deps.setdefault(j, False)
        n = None
        if dma:
            lst = self.dma_list[eng]
            n = len(lst)
            if n >= self.K[eng]:
                deps.setdefault(lst[n - self.K[eng]], False)
            lst.append(i)
        op = Op()
        op.id, op.eng, op.fn, op.deps, op.dma, op.n, op.signal, op.val = i, eng, fn, deps, dma, n, False, 0
        op.cc = None
        if cc:
            op.dma = True
            op.cc = self.ncc
            self.ncc += 1
            self.cc_list.append(i)
            dma = True
        self.ops.append(op)
        for k in r:
            rd = self.readers.setdefault(k, {})
            if dma:
                rd.setdefault("dma", []).append(i)
            else:
                rd[eng] = i
        for k in w:
            self.lastw[k] = i
            self.readers[k] = {}
        return i

    def _needed(self, op, dj, raw):
        if dj.dma:
            return True
        if dj.eng == op.eng:
            if op.dma:
                return True
            return raw and op.eng != "pe"
        return True

    def end_phase(self, name=None):
        nc = self.nc
        ps = self.phase_start
        for e in self.K:
            lst = [i for i in self.dma_list[e][-self.K[e]:] if i >= ps]
            if e == "pool":
                lst = lst + [i for i in self.cc_list if i >= ps]
            if lst:
                i = self.add(e, lambda h: h.nop())
                for j in lst:
                    self.ops[i].deps[j] = True
        ops = self.ops
        for op in ops[ps:]:
            latest = {}
            for j, raw in op.deps.items():
                if j < ps:
                    continue
                dj = ops[j]
                if not dj.dma and self._needed(op, dj, raw):
                    if latest.get(dj.eng, -1) < j:
                        latest[dj.eng] = j
            for j in latest.values():
                ops[j].signal = True
        for op in ops[ps:]:
            if op.signal and not op.dma:
                self.cnt[op.eng] += 1
                op.val = self.cnt[op.eng]
        with nc.Block() as block:
            for e, bn in self.ENGS:
                ops_e = [op for op in ops[ps:] if op.eng == e]
                if not ops_e:
                    continue

                def body(h, ops_e=ops_e, e=e):
                    self._emit(e, h, ops_e, ps)
                getattr(block, bn)(body)
        self.phase_start = len(ops)
        self.lastw = {k: v for k, v in self.lastw.items() if isinstance(k, str) and k.startswith("D:")}
        self.readers = {k: {} for k in self.lastw}
        for op in ops[:self.phase_start]:
            op.fn = None

    def _emit(self, e, h, ops_e, ps):
        ops = self.ops
        waited = self.waited[e]
        for op in ops_e:
            want = {}
            for j, raw in op.deps.items():
                if j < ps:
                    continue
                dj = ops[j]
                if not self._needed(op, dj, raw):
                    continue
                if dj.cc is not None:
                    key = ("cc", dj.cc)
                    sem = self.ccsems[dj.cc]
                    val = 1
                elif dj.dma:
                    K = self.K[dj.eng]
                    key = (dj.eng, dj.n % K)
                    sem = self.dsems[dj.eng][dj.n % K]
                    val = 16 * (dj.n // K + 1)
                else:
                    key = dj.eng
                    sem = self.sems[dj.eng]
                    if key in want and want[key][2] > j:
                        continue
                    want[key] = (sem, dj.val, j)
                    continue
                if key not in want or want[key][1] < val:
                    want[key] = (sem, val, j)
            for key, (sem, val, _j) in want.items():
                if waited.get(key, 0) >= val:
                    continue
                h.wait_ge(sem, val)
                waited[key] = val
            inst = op.fn(h)
            if op.cc is not None:
                inst.then_inc(self.ccsems[op.cc])
            elif op.dma:
                inst.then_inc(self.dsems[e][op.n % self.K[e]], 16)
            elif op.signal:
                inst.then_inc(self.sems[e], 1)


class Ctx:
    def __init__(self, nc, S):
        self.nc = nc
        self.S = S
        self.stack = None
        self.uid = 0

    def psum(self, name, shape, dt=F32):
        self.uid += 1
        return self.stack.enter_context(self.nc.psum_tensor("%s_%d" % (name, self.uid), list(shape), dt))

    def begin(self, nf=8, nb=0):
        self.stack = ExitStack()
        self.ps = [self.stack.enter_context(self.nc.psum_tensor("ps%d_%d" % (i, self.uid), [128, 512], F32))
                   for i in range(nf)]
        self.psb = [self.stack.enter_context(self.nc.psum_tensor("psb%d_%d" % (i, self.uid), [128, 1024], BF16))
                    for i in range(nb)]
        self.uid += 1

    def end(self):
        self.S.end_phase()
        self.stack.close()
        self.stack = None

    def sb(self, name, shape, dt=F32):
        self.uid += 1
        return self.stack.enter_context(self.nc.sbuf_tensor("%s_%d" % (name, self.uid), list(shape), dt))

    def dma(self, eng, out, in_, r=(), w=(), **kw):
        return self.S.add(eng, lambda h: h.dma_start(out=out, in_=in_, **kw), r=r, w=w, dma=True)

    def mm(self, out, lhsT, rhs, start, stop, r=(), w=(), **kw):
        return self.S.add("pe", lambda h: h.matmul(out, lhsT, rhs, start=start, stop=stop, **kw), r=r, w=w)

    def tr(self, out, in_, ident, r=(), w=()):
        return self.S.add("pe", lambda h: h.transpose(out, in_, ident), r=r, w=w)

    def act(self, out, in_, func, r=(), w=(), eng="act", **kw):
        return self.S.add(eng, lambda h: h.activation(out=out, in_=in_, func=func, **kw), r=r, w=w)

    def tt(self, out, in0, in1, op, r=(), w=(), eng="dve"):
        return self.S.add(eng, lambda h: h.tensor_tensor(out=out, in0=in0, in1=in1, op=op), r=r, w=w)

    def ts(self, out, in0, s1, op0, s2=None, op1=None, r=(), w=(), eng="dve", **kw):
        if op1 is None:
            return self.S.add(eng, lambda h: h.tensor_scalar(out=out, in0=in0, scalar1=s1, scalar2=None, op0=op0, **kw),
                              r=r, w=w)
        return self.S.add(eng, lambda h: h.tensor_scalar(out=out, in0=in0, scalar1=s1, scalar2=s2, op0=op0, op1=op1, **kw),
                          r=r, w=w)

    def stt(self, out, in0, scalar, in1, op0, op1, r=(), w=()):
        return self.S.add("dve", lambda h: h.scalar_tensor_tensor(out=out, in0=in0, scalar=scalar, in1=in1,
                                                                    op0=op0, op1=op1), r=r, w=w)

    def copy(self, out, in_, r=(), w=(), eng="dve"):
        if eng == "act":
            return self.S.add("act", lambda h: h.activation(out=out, in_=in_, func=AF.Copy), r=r, w=w)
        return self.S.add(eng, lambda h: h.tensor_copy(out=out, in_=in_), r=r, w=w)

    def memset(self, ap, val, w=(), eng="dve"):
        return self.S.add(eng, lambda h: h.memset(ap, val), w=w)

    def red(self, out, in_, op, r=(), w=(), axis=None):
        ax = AX.X if axis is None else axis
        return self.S.add("dve", lambda h: h.tensor_reduce(out=out, in_=in_, axis=ax, op=op), r=r, w=w)

    def recip(self, out, in_, r=(), w=()):
        return self.S.add("dve", lambda h: h.reciprocal(out=out, in_=in_), r=r, w=w)


def phase_mods(cx, l, dr):
    cx.begin()
    cv = cx.sb("cv", [128, 8, 2])
    sc = cx.sb("sc", [128, 8, 2])
    acc = cx.sb("macc", [2, 6144])
    bm = cx.sb("mbm", [2, 6144])
    g1b = cx.sb("g1b", [2, 1024])
    g2b = cx.sb("g2b", [2, 1024])
    mv = cx.sb("mv", [2, 6, 1024])
    wm = [cx.sb("wm%d" % i, [128, 6144]) for i in range(2)]
    for s, src in enumerate((dr["c"], dr["c_ctx"])):
        for kc in range(8):
            cx.dma("sp", cv[:, kc, s:s + 1], src[0:1, kc * 128:(kc + 1) * 128].rearrange("o p -> p o"), w=["cv"])
    cx.dma("sp", bm[:, :], dr["b_mod"][l, :].partition_broadcast(2), w=["bm"])
    cx.dma("sp", g1b[:, :], dr["g_norm1"][l, :].partition_broadcast(2), w=["g1b"])
    cx.dma("sp", g2b[:, :], dr["g_norm2"][l, :].partition_broadcast(2), w=["g2b"])
    cx.act(sc[:, :, :], cv[:, :, :], AF.Silu, r=["cv"], w=["sc"])
    for kc in range(8):
        b = kc % 2
        cx.dma("sp", wm[b][:, :], dr["w_mod"][l, kc * 128:(kc + 1) * 128, :], w=["wm%d" % b])
        for n in range(12):
            p = cx.ps[n % 4]
            cx.mm(p[0:2, :], sc[:, kc, :], wm[b][:, n * 512:(n + 1) * 512], True, True,
                  r=["sc", "wm%d" % b], w=["ps%d" % (n % 4)])
            a = acc[:, n * 512:(n + 1) * 512]
            if kc == 0:
                cx.tt(a, p[0:2, :], bm[:, n * 512:(n + 1) * 512], ALU.add, r=["ps%d" % (n % 4), "bm"], w=["macc%d" % n])
            else:
                cx.tt(a, p[0:2, :], a, ALU.add, r=["ps%d" % (n % 4), "macc%d" % n], w=["macc%d" % n])
    allacc = ["macc%d" % n for n in range(12)]
    cx.stt(mv[:, 0, :], acc[:, 1024:2048], 1.0, g1b[:, :], ALU.add, ALU.mult, r=allacc + ["g1b"], w=["mv0"])
    cx.copy(mv[:, 1, :], acc[:, 0:1024], r=allacc, w=["mv1"])
    cx.copy(mv[:, 2, :], acc[:, 2048:3072], r=allacc, w=["mv2"])
    cx.stt(mv[:, 3, :], acc[:, 4096:5120], 1.0, g2b[:, :], ALU.add, ALU.mult, r=allacc + ["g2b"], w=["mv3"])
    cx.copy(mv[:, 4, :], acc[:, 3072:4096], r=allacc, w=["mv4"])
    cx.copy(mv[:, 5, :], acc[:, 5120:6144], r=allacc, w=["mv5"])
    cx.dma("sp", dr["modv"][l, :, :, :], mv[:, :, :], r=["mv%d" % i for i in range(6)], w=["D:modv%d" % l])
    cx.end()


CST = {}
_off = 0
for _n, _w in (("c127mj", 1), ("cj", 1), ("ef_l", 16), ("eb_l", 16), ("ef_c", 2), ("eb_c", 2), ("ip1", 128),
               ("m128i", 128), ("D1", 128), ("D2", 128), ("U", 128), ("Lo", 128), ("I2", 128), ("ones", 128), ("I1", 128), ("hm", 4), ("bm8", 8), ("Ltri", 128), ("eoff", 16)):
    CST[_n] = (_off, _off + _w)
    _off += _w
CSTW = _off


def make_cst():
    c = np.zeros((128, CSTW), np.float32)
    p = np.arange(128, dtype=np.float32)
    i = np.arange(128, dtype=np.float32)

    def put(n, v):
        a, b = CST[n]
        c[:, a:b] = v
    put("c127mj", (127 - p)[:, None])
    put("cj", p[:, None])
    put("ef_l", (128.0 * (15 - np.arange(16)))[None, :])
    put("eb_l", (128.0 * np.arange(16))[None, :])
    put("ef_c", (128.0 * (1 - np.arange(2)))[None, :])
    put("eb_c", (128.0 * np.arange(2))[None, :])
    put("ip1", (i + 1)[None, :])
    put("m128i", (128 - i)[None, :])
    dd = i[None, :] - p[:, None]
    put("D1", np.maximum(dd, 0))
    put("D2", np.maximum(-dd, 0))
    put("U", (dd > 0).astype(np.float32))
    put("Lo", (dd < 0).astype(np.float32))
    put("I2", 2.0 * (dd == 0))
    put("ones", 1.0)
    put("I1", (dd == 0).astype(np.float32))
    put("hm", (p[:, None] // 32 == np.arange(4)[None, :]).astype(np.float32))
    put("bm8", np.tile((p[:, None] // 32 == np.arange(4)[None, :]).astype(np.float32), (1, 2)))
    put("Ltri", (dd > 0).astype(np.float32))
    put("eoff", (float(MOE_CAP) * np.arange(16))[None, :])
    return c


def rope_tables(core):
    t = np.arange(core * TL, (core + 1) * TL)
    row = (t // 64).astype(np.float32)
    col = (t % 64).astype(np.float32)
    inv = (np.float32(10000.0) ** (-np.arange(8, dtype=np.float32) / np.float32(8))).astype(np.float32)
    ang = np.concatenate([row[:, None] * inv[None, :], col[:, None] * inv[None, :]], axis=1).astype(np.float32)
    cos = np.cos(ang).astype(np.float32).T
    sin = np.sin(ang).astype(np.float32).T
    return np.tile(cos, (8, 1)).copy(), np.tile(sin, (8, 1)).copy()


def cs(cst, name):
    a, b = CST[name]
    return cst[:, a:b]


def phase_a(cx, l, dr, segs, wl=None):
    wl = l if wl is None else wl
    cx.begin(nf=6, nb=2)
    ps, psb = cx.ps, cx.psb
    W = cx.sb("W", [128, 8, INC], BF16)
    WR = cx.sb("WR", [128, 8, 768], BF16)
    cst = cx.sb("cst", [128, CSTW])
    ident = cx.sb("ident", [128, 128], BF16)
    cx.dma("sp", cst[:, :], dr["cst"][:, :], w=["cst"])
    cx.dma("sp", ident[:, :], dr["ident"][:, :], w=["ident"])
    for kc in range(8):
        cx.dma("pool", W[:, kc, :], dr["w_in"][wl, kc * 128:(kc + 1) * 128, :], w=["W%d" % kc])
    for kc in range(8):
        cx.ts(W[:, kc, 2176:2304], W[:, kc, 2176:2304], QSCALE, ALU.mult, r=["W%d" % kc], w=["W%d" % kc], eng="pool")
        for (s0, n, o0) in ((1280, 512, 0), (2048, 256, 512)):
            src = W[:, kc, s0:s0 + n].rearrange("p (b t d) -> p b t d", t=2, d=16)
            dst = WR[:, kc, o0:o0 + n].rearrange("p (b t d) -> p b t d", t=2, d=16)
            cx.ts(dst[:, :, 0, :], src[:, :, 1, :], -1.0, ALU.mult, r=["W%d" % kc], w=["WR%d" % kc], eng="pool")
            cx.copy(dst[:, :, 1, :], src[:, :, 0, :], r=["W%d" % kc], w=["WR%d" % kc], eng="pool")
    Wk = ["W%d" % kc for kc in range(8)]
    WRk = ["WR%d" % kc for kc in range(8)]
    lgf = cx.sb("lgf", [128, 4]); lgb = cx.sb("lgb", [128, 4])
    lgfc = cx.sb("lgfc", [128, 1]); lgbc = cx.sb("lgbc", [128, 1])
    cx.dma("sp", lgf[:, :], dr["ret_ld_f"][l, :].partition_broadcast(128), w=["lgf"])
    cx.dma("sp", lgb[:, :], dr["ret_ld_b"][l, :].partition_broadcast(128), w=["lgb"])
    for h in range(4):
        cx.dma("sp", lgfc[32 * h:32 * h + 32, :], dr["ret_ld_f"][l, h:h + 1].partition_broadcast(32), w=["lgfc"])
        cx.dma("sp", lgbc[32 * h:32 * h + 32, :], dr["ret_ld_b"][l, h:h + 1].partition_broadcast(32), w=["lgbc"])
    kdf = cx.sb("kdf", [128, 4]); kdb = cx.sb("kdb", [128, 4])
    KDF = cx.sb("KDF", [128, 128]); KDB = cx.sb("KDB", [128, 128])
    cx.act(kdf[:, :], lgf[:, :], AF.Exp, scale=cs(cst, "c127mj"), r=["lgf", "cst"], w=["kdf"])
    cx.act(kdb[:, :], lgb[:, :], AF.Exp, scale=cs(cst, "cj"), r=["lgb", "cst"], w=["kdb"])
    for h in range(4):
        cx.ts(KDF[:, 32 * h:32 * h + 32], cs(cst, "ones")[:, 0:32], kdf[:, h:h + 1], ALU.mult, r=["kdf", "cst"], w=["KDF"])
        cx.ts(KDB[:, 32 * h:32 * h + 32], cs(cst, "ones")[:, 0:32], kdb[:, h:h + 1], ALU.mult, r=["kdb", "cst"], w=["KDB"])
    pw = {}
    for tag, nch in (("l", 16), ("c", 2)):
        pf = cx.sb("pwf" + tag, [128, nch]); pb = cx.sb("pwb" + tag, [128, nch])
        cx.act(pf[:, :], cs(cst, "ef_" + tag), AF.Exp, scale=lgfc[:, 0:1], r=["lgfc", "cst"], w=["pwf" + tag])
        cx.act(pb[:, :], cs(cst, "eb_" + tag), AF.Exp, scale=lgbc[:, 0:1], r=["lgbc", "cst"], w=["pwb" + tag])
        pw[tag] = (pf, pb)
    Cl = cx.sb("Cl", [128, TL]); Sl = cx.sb("Sl", [128, TL])
    cx.dma("sp", Cl[:, :], dr["cosT"][:, :], w=["Cl"])
    cx.dma("sp", Sl[:, :], dr["sinT"][:, :], w=["Sl"])
    xt = [cx.sb("xt%d" % i, [128, D]) for i in range(2)]
    junk = cx.sb("junk", [128, D], BF16)
    t1 = [cx.sb("t1_%d" % i, [128, D]) for i in range(2)]
    hb = [cx.sb("hb%d" % i, [128, D], BF16) for i in range(2)]
    ssq = cx.sb("ssq", [128, 4]); rstd = cx.sb("rstd", [128, 4])
    hTs = [cx.sb("hT%d" % i, [128, 8, 512], BF16) for i in range(2)]
    gcount = [0]
    gsb = cx.sb("gsb", [128, D]); shb = cx.sb("shb", [128, D])
    ev = [cx.sb("ev%d" % i, [128, 512]) for i in range(4)]
    evb = [cx.sb("evb%d" % i, [128, 512], BF16) for i in range(4)]
    rk_sb = cx.sb("rk_sb", [128, 512], BF16)
    vt = [cx.sb("vt%d" % i, [128, 512], BF16) for i in range(2)]
    rgt = [cx.sb("rgt%d" % i, [128, 256], BF16) for i in range(2)]
    kfb = [cx.sb("kfb%d" % i, [128, 256], BF16) for i in range(2)]
    Tst = cx.sb("Tst", [128, 2, 256])
    evi = [0]

    def nxt():
        evi[0] = (evi[0] + 1) % 4
        return evi[0]

    xi = 0
    for (tag, T, xd) in segs:
        sfx = "_" + tag
        seg = 0 if tag == "l" else 1
        cx.dma("sp", gsb[:, :], dr["modv"][l, seg, 0, :].partition_broadcast(128), r=["D:modv%d" % l], w=["gsb"])
        cx.dma("sp", shb[:, :], dr["modv"][l, seg, 1, :].partition_broadcast(128), r=["D:modv%d" % l], w=["shb"])
        G = min(512, T)
        for g in range(T // G):
            t0 = g * G
            nt = G // 128
            hT = hTs[gcount[0] % 2]
            kh = "hT%d" % (gcount[0] % 2)
            gcount[0] += 1
            for j in range(nt):
                b = xi % 2
                xi += 1
                kx = "xt%d" % b
                cx.dma("sp", xt[b][:, :], xd[t0 + j * 128:t0 + (j + 1) * 128, :], w=[kx])
                cx.act(junk[:, :], xt[b][:, :], AF.Square, accum_out=ssq[:, j:j + 1], r=[kx], w=["junk", "ssq%d" % j])
                cx.act(rstd[:, j:j + 1], ssq[:, j:j + 1], AF.Sqrt, scale=1.0 / D, bias=EPS, r=["ssq%d" % j], w=["rs%d" % j])
                cx.recip(rstd[:, j:j + 1], rstd[:, j:j + 1], r=["rs%d" % j], w=["rs%d" % j])
                cx.stt(t1[b][:, :], xt[b][:, :], rstd[:, j:j + 1], gsb[:, :], ALU.mult, ALU.mult,
                       r=[kx, "rs%d" % j, "gsb"], w=["t1_%d" % b])
                cx.tt(hb[b][:, :], t1[b][:, :], shb[:, :], ALU.add, r=["t1_%d" % b, "shb"], w=["hb%d" % b], eng="pool")
                for kc in range(8):
                    cx.tr(psb[0][:, kc * 128:(kc + 1) * 128], hb[b][:, kc * 128:(kc + 1) * 128], ident[:, :],
                          r=["hb%d" % b, "ident"], w=["psb0"])
                cx.copy(hT[:, :, j * 128:(j + 1) * 128], psb[0][:, :].rearrange("p (k t) -> p k t", k=8),
                        r=["psb0"], w=[kh], eng="act")
            cx.dma("sp", dr["hT" + sfx][:, :, t0:t0 + G], hT[:, :, 0:G], r=[kh], w=["D:hT" + sfx])

            def proj(cc, bank, rot=False):
                Wt = WR if rot else W
                for kc in range(8):
                    cx.mm(ps[bank][:, 0:G], Wt[:, kc, cc * 128:(cc + 1) * 128], hT[:, kc, 0:G], kc == 0, kc == 7,
                          r=[kh, (WRk if rot else Wk)[kc]], w=["ps%d" % bank])

            Cg = Cl[:, t0:t0 + G] if tag == "l" else None
            Sg = Sl[:, t0:t0 + G] if tag == "l" else None
            for c2 in range(2):
                e = nxt()
                proj(2 + c2, 0)
                cx.act(ev[e][:, 0:G], ps[0][:, 0:G], AF.Sigmoid, r=["ps0"], w=["ev%d" % e])
                proj(0 + c2, 1)
                cx.tt(ev[e][:, 0:G], ps[1][:, 0:G], ev[e][:, 0:G], ALU.mult, r=["ps1", "ev%d" % e], w=["ev%d" % e])
                cx.dma("sp", dr["uT" + sfx][c2, :, t0:t0 + G], ev[e][:, 0:G], r=["ev%d" % e], w=["D:uT" + sfx])
            for c2 in range(2):
                e = nxt()
                proj(4 + c2, 0)
                cx.copy(evb[e][:, 0:G], ps[0][:, 0:G], r=["ps0"], w=["evb%d" % e], eng="act")
                cx.dma("sp", dr["bgT" + sfx][c2, :, t0:t0 + G], evb[e][:, 0:G], r=["evb%d" % e], w=["D:bgT" + sfx])
                proj(6 + c2, 1)
                cx.copy(ev[e][:, 0:G], ps[1][:, 0:G], r=["ps1"], w=["ev%d" % e], eng="act")
                proj(8 + c2, 0)
                cx.tt(ev[e][:, 0:G], ps[0][:, 0:G], ev[e][:, 0:G], ALU.mult, r=["ps0", "ev%d" % e], w=["ev%d" % e])
                cx.dma("sp", dr["tT" + sfx][c2, :, t0:t0 + G], ev[e][:, 0:G], r=["ev%d" % e], w=["D:tT" + sfx])
            for (cc, ro, dst, keep) in ((10, 0, dr["qT" + sfx][0], None), (11, 1, dr["qT" + sfx][1], None),
                                        (12, 2, dr["kT" + sfx][0], None), (13, 3, dr["kT" + sfx][1], None),
                                        (16, 4, dr["rqT" + sfx], None), (17, 5, dr["rkT" + sfx], rk_sb)):
                e = nxt()
                ob = keep if keep is not None else evb[e]
                okey = "rk_sb" if keep is not None else "evb%d" % e
                proj(cc, 0)
                if tag == "l":
                    proj(ro, 2, rot=True)
                    e2 = nxt()
                    cx.tt(ev[e][:, 0:G], ps[0][:, 0:G], Cg, ALU.mult, r=["ps0", "Cl"], w=["ev%d" % e])
                    cx.tt(ev[e2][:, 0:G], ps[2][:, 0:G], Sg, ALU.mult, r=["ps2", "Sl"], w=["ev%d" % e2])
                    cx.tt(ob[:, 0:G], ev[e][:, 0:G], ev[e2][:, 0:G], ALU.add, r=["ev%d" % e, "ev%d" % e2], w=[okey], eng="pool")
                else:
                    cx.copy(ob[:, 0:G], ps[0][:, 0:G], r=["ps0"], w=[okey], eng="act")
                cx.dma("sp", dst[:, t0:t0 + G], ob[:, 0:G], r=[okey], w=["D:rope" + sfx + str(cc)])
            pf, pb = pw[tag]
            for j in range(nt):
                n = (t0 // 128) + j
                b = j % 2
                tsl = slice(j * 128, (j + 1) * 128)
                for kc in range(8):
                    cx.mm(ps[3][:, 0:256], hT[:, kc, tsl], W[:, kc, 1792:2048], kc == 0, kc == 7, r=[kh, Wk[kc]], w=["ps3"])
                for kc in range(8):
                    cx.mm(ps[3][:, 256:512], hT[:, kc, tsl], W[:, kc, 2304:2560], kc == 0, kc == 7, r=[kh, Wk[kc]], w=["ps3"])
                for kc in range(8):
                    cx.mm(ps[4][:, 0:256], hT[:, kc, tsl], W[:, kc, 2560:2816], kc == 0, kc == 7, r=[kh, Wk[kc]], w=["ps4"])
                cx.copy(vt[b][:, :], ps[3][:, :], r=["ps3", "ps3"], w=["vt%d" % b])
                cx.act(rgt[b][:, :], ps[4][:, 0:256], AF.Silu, r=["ps4"], w=["rgt%d" % b])
                rows = slice(t0 + j * 128, t0 + (j + 1) * 128)
                cx.dma("sp", dr["V" + sfx][rows, :], vt[b][:, 0:256], r=["vt%d" % b], w=["D:V" + sfx])
                cx.dma("sp", dr["rv" + sfx][rows, :], vt[b][:, 256:512], r=["vt%d" % b], w=["D:rv" + sfx])
                cx.dma("sp", dr["rg" + sfx][rows, :], rgt[b][:, :], r=["rgt%d" % b], w=["D:rg" + sfx])
                cx.tr(psb[1][:, 0:128], rk_sb[:, tsl], ident[:, :], r=["rk_sb", "ident"], w=["psb1"])
                cx.tt(kfb[b][:, 0:128], psb[1][:, 0:128], KDF[:, :], ALU.mult, r=["psb1", "KDF"], w=["kfb%d" % b])
                cx.tt(kfb[b][:, 128:256], psb[1][:, 0:128], KDB[:, :], ALU.mult, r=["psb1", "KDB"], w=["kfb%d" % b])
                cx.mm(ps[5][:, 0:256], kfb[b][:, 0:128], vt[b][:, 256:512], True, True, r=["kfb%d" % b, "vt%d" % b], w=["ps5"])
                cx.mm(ps[5][:, 256:512], kfb[b][:, 128:256], vt[b][:, 256:512], True, True, r=["kfb%d" % b, "vt%d" % b], w=["ps5"])
                if n == 0:
                    cx.ts(Tst[:, 0, :], ps[5][:, 0:256], pf[:, n:n + 1], ALU.mult, r=["ps5", "pwf" + tag], w=["Tf"])
                    cx.ts(Tst[:, 1, :], ps[5][:, 256:512], pb[:, n:n + 1], ALU.mult, r=["ps5", "pwb" + tag], w=["Tb"])
                else:
                    cx.stt(Tst[:, 0, :], ps[5][:, 0:256], pf[:, n:n + 1], Tst[:, 0, :], ALU.mult, ALU.add,
                           r=["ps5", "pwf" + tag, "Tf"], w=["Tf"])
                    cx.stt(Tst[:, 1, :], ps[5][:, 256:512], pb[:, n:n + 1], Tst[:, 1, :], ALU.mult, ALU.add,
                           r=["ps5", "pwb" + tag, "Tb"], w=["Tb"])
        cx.dma("sp", dr["Tst" + sfx].rearrange("a p e -> p a e"), Tst[:, :, :], r=["Tf", "Tb"], w=["D:Tst" + sfx])
    cx.end()


def phase_conv(cx, l, dr, segs):
    cx.begin(nf=8, nb=0)
    ps = cx.ps
    cst = cx.sb("cst", [128, CSTW])
    cx.dma("sp", cst[:, :], dr["cst"][:, :], w=["cst"])
    praw = cx.sb("praw", [40, 256])
    cx.memset(praw[:, :], 0.0, w=["praw"])
    cx.dma("sp", praw[0:31, :], dr["conv_a_w"][l, :, :], w=["praw"])
    cx.dma("sp", praw[31:32, :], dr["conv_a_b"][l:l + 1, :], w=["praw"])
    cx.dma("sp", praw[32:33, :], dr["conv_a_g"][l:l + 1, :], w=["praw"])
    cx.dma("sp", praw[33:34, :], dr["conv_a_beta"][l:l + 1, :], w=["praw"])
    cx.dma("sp", praw[34:37, :], dr["conv_b_w"][l, :, :], w=["praw"])
    par = cx.sb("par", [128, 2, 40])
    for c2 in range(2):
        cx.tr(ps[7][:, 0:40], praw[0:40, c2 * 128:(c2 + 1) * 128], cs(cst, "I1")[0:40, 0:40], r=["praw", "cst"], w=["ps7"])
        cx.copy(par[:, c2, :], ps[7][:, 0:40], r=["ps7"], w=["par"])
    onesm = cx.sb("onesm", [128, 128])
    cx.memset(onesm[:, :], 1.0 / 256.0, w=["onesm"])
    HG = cx.sb("HG", [128, NCORE, 4, 32]); selt = cx.sb("selt", [128, 2, NCORE])
    cx.dma("sp", HG[:, :, :, :], dr["hg"].rearrange("(r a p) w -> p r a w", r=NCORE, a=4), w=["HG"])
    cx.dma("sp", selt[:, :, :], dr["sel"][:, :, :], w=["selt"])
    for (tag, T) in segs:
        sfx = "_" + tag
        ue = cx.sb("ue" + tag, [128, 2, T + 32])
        te = cx.sb("te" + tag, [128, 2, T + 32])
        bg = cx.sb("bg" + tag, [128, 2, T], BF16)
        acc = cx.sb("acc" + tag, [128, 2, T])
        sq = cx.sb("sq" + tag, [128, 2, T])
        yb = cx.sb("yb" + tag, [128, 2, T], BF16)
        for c2 in range(2):
            cx.dma("sp", ue[:, c2, 16:16 + T], dr["uT" + sfx][c2, :, :], r=["D:uT" + sfx], w=["ue%d" % c2])
            cx.dma("sp", te[:, c2, 16:16 + T], dr["tT" + sfx][c2, :, :], r=["D:tT" + sfx], w=["te%d" % c2])
            cx.dma("sp", bg[:, c2, :], dr["bgT" + sfx][c2, :, :], r=["D:bgT" + sfx], w=["bg%d" % c2])
            if tag == "l":
                for a, (buf, k) in enumerate(((ue, "ue%d" % c2), (te, "te%d" % c2))):
                    ai = a * 2 + c2
                    for side, dst, src in ((0, slice(0, 16), slice(16, 32)), (1, slice(16 + T, 32 + T), slice(0, 16))):
                        for r_ in range(NCORE):
                            if r_ == 0:
                                cx.ts(buf[:, c2, dst], HG[:, r_, ai, src], selt[:, side, r_:r_ + 1], ALU.mult,
                                      r=["HG", "selt"], w=[k])
                            else:
                                cx.stt(buf[:, c2, dst], HG[:, r_, ai, src], selt[:, side, r_:r_ + 1], buf[:, c2, dst],
                                       ALU.mult, ALU.add, r=["HG", "selt", k], w=[k])
            else:
                for buf, k in ((ue, "ue%d" % c2), (te, "te%d" % c2)):
                    cx.memset(buf[:, c2, 0:16], 0.0, w=[k], eng="pool")
                    cx.memset(buf[:, c2, 16 + T:32 + T], 0.0, w=[k], eng="pool")
        for c2 in range(2):
            ka = "acc%d" % c2
            cx.ts(acc[:, c2, :], ue[:, c2, 1:1 + T], par[:, c2, 0:1], ALU.mult, s2=par[:, c2, 31:32], op1=ALU.add,
                  r=["ue%d" % c2, "par"], w=[ka])
            for k in range(1, 31):
                cx.stt(acc[:, c2, :], ue[:, c2, k + 1:k + 1 + T], par[:, c2, k:k + 1], acc[:, c2, :], ALU.mult, ALU.add,
                       r=["ue%d" % c2, "par", ka], w=[ka])
            cx.tt(sq[:, c2, :], acc[:, c2, :], acc[:, c2, :], ALU.mult, r=[ka], w=["sq%d" % c2], eng="pool")
        G = min(512, T)
        mm2 = cx.sb("m2" + tag, [128, G]); var = cx.sb("var" + tag, [128, G]); dd = cx.sb("dd" + tag, [128, G])
        for g in range(T // G):
            gs = slice(g * G, (g + 1) * G)
            for c2 in range(2):
                cx.mm(ps[0][:, 0:G], onesm[:, :], acc[:, c2, gs], c2 == 0, c2 == 1, r=["onesm", "acc%d" % c2], w=["ps0"])
            for c2 in range(2):
                cx.mm(ps[1][:, 0:G], onesm[:, :], sq[:, c2, gs], c2 == 0, c2 == 1, r=["onesm", "sq%d" % c2], w=["ps1"])
            cx.act(mm2[:, :], ps[0][:, 0:G], AF.Square, r=["ps0"], w=["mm2"])
            cx.tt(var[:, :], ps[1][:, 0:G], mm2[:, :], ALU.subtract, r=["ps1", "mm2"], w=["var"])
            cx.act(var[:, :], var[:, :], AF.Ln, bias=EPS, r=["var"], w=["var"])
            cx.act(var[:, :], var[:, :], AF.Exp, scale=-0.5, r=["var"], w=["var"])
            for c2 in range(2):
                cx.tt(dd[:, :], acc[:, c2, gs], ps[0][:, 0:G], ALU.subtract, r=["acc%d" % c2, "ps0"], w=["dd"])
                cx.tt(dd[:, :], dd[:, :], var[:, :], ALU.mult, r=["dd", "var"], w=["dd"])
                cx.act(yb[:, c2, gs], dd[:, :], AF.Silu, scale=par[:, c2, 32:33], bias=par[:, c2, 33:34],
                       r=["dd", "par"], w=["yb%d" % c2])
        for c2 in range(2):
            cx.dma("sp", dr["ysT" + sfx][0 + c2, :, :], yb[:, c2, :], r=["yb%d" % c2], w=["D:ys0" + sfx])
        for c2 in range(2):
            ka = "acc%d" % c2
            cx.ts(acc[:, c2, :], te[:, c2, 15:15 + T], par[:, c2, 34:35], ALU.mult, r=["te%d" % c2, "par"], w=[ka])
            for k in (1, 2):
                cx.stt(acc[:, c2, :], te[:, c2, 15 + k:15 + k + T], par[:, c2, 34 + k:35 + k], acc[:, c2, :], ALU.mult, ALU.add,
                       r=["te%d" % c2, "par", ka], w=[ka])
            cx.tt(yb[:, c2, :], acc[:, c2, :], bg[:, c2, :], ALU.mult, r=[ka, "bg%d" % c2], w=["yb%d" % c2])
            cx.dma("sp", dr["ysT" + sfx][2 + c2, :, :], yb[:, c2, :], r=["yb%d" % c2], w=["D:ys1" + sfx])
    cx.end()


def phase_ret(cx, l, dr, segs):
    cx.begin(nf=6, nb=2)
    ps, psb = cx.ps, cx.psb
    cst = cx.sb("cst", [128, CSTW])
    ident = cx.sb("ident", [128, 128], BF16)
    cx.dma("sp", cst[:, :], dr["cst"][:, :], w=["cst"])
    cx.dma("sp", ident[:, :], dr["ident"][:, :], w=["ident"])
    lgf = cx.sb("lgf", [128, 4]); lgb = cx.sb("lgb", [128, 4])
    lgfc = cx.sb("lgfc", [128, 1]); lgbc = cx.sb("lgbc", [128, 1])
    cx.dma("sp", lgf[:, :], dr["ret_ld_f"][l, :].partition_broadcast(128), w=["lgf"])
    cx.dma("sp", lgb[:, :], dr["ret_ld_b"][l, :].partition_broadcast(128), w=["lgb"])
    for h in range(4):
        cx.dma("sp", lgfc[32 * h:32 * h + 32, :], dr["ret_ld_f"][l, h:h + 1].partition_broadcast(32), w=["lgfc"])
        cx.dma("sp", lgbc[32 * h:32 * h + 32, :], dr["ret_ld_b"][l, h:h + 1].partition_broadcast(32), w=["lgbc"])
    kdf = cx.sb("kdf", [128, 4]); kdb = cx.sb("kdb", [128, 4])
    KDF = cx.sb("KDF", [128, 128]); KDB = cx.sb("KDB", [128, 128])
    cx.act(kdf[:, :], lgf[:, :], AF.Exp, scale=cs(cst, "c127mj"), r=["lgf", "cst"], w=["kdf"])
    cx.act(kdb[:, :], lgb[:, :], AF.Exp, scale=cs(cst, "cj"), r=["lgb", "cst"], w=["kdb"])
    for h in range(4):
        cx.ts(KDF[:, 32 * h:32 * h + 32], cs(cst, "ones")[:, 0:32], kdf[:, h:h + 1], ALU.mult, r=["kdf", "cst"], w=["KDF"])
        cx.ts(KDB[:, 32 * h:32 * h + 32], cs(cst, "ones")[:, 0:32], kdb[:, h:h + 1], ALU.mult, r=["kdb", "cst"], w=["KDB"])
    cdf = cx.sb("cdf", [128, 1]); cdb = cx.sb("cdb", [128, 1])
    cx.act(cdf[:, :], lgfc[:, :], AF.Exp, scale=128.0, r=["lgfc"], w=["cdf"])
    cx.act(cdb[:, :], lgbc[:, :], AF.Exp, scale=128.0, r=["lgbc"], w=["cdb"])
    qdf4 = cx.sb("qdf4", [128, 4, 128]); qdb4 = cx.sb("qdb4", [128, 4, 128])
    for c in range(4):
        cx.act(qdf4[:, c, :], cs(cst, "ip1"), AF.Exp, scale=lgfc[:, 0:1], r=["lgfc", "cst"], w=["qdf4"])
        cx.act(qdb4[:, c, :], cs(cst, "m128i"), AF.Exp, scale=lgbc[:, 0:1], r=["lgbc", "cst"], w=["qdb4"])
    maskT = cx.sb("maskT", [128, 4, 128]); mtmp = cx.sb("mtmp", [128, 128])
    for h in range(4):
        cx.act(mtmp[:, :], cs(cst, "D1"), AF.Exp, scale=lgf[:, h:h + 1], r=["lgf", "cst"], w=["mtmp"])
        cx.tt(maskT[:, h, :], mtmp[:, :], cs(cst, "U"), ALU.mult, r=["mtmp", "cst"], w=["maskT"])
        cx.tt(maskT[:, h, :], maskT[:, h, :], cs(cst, "I2"), ALU.add, r=["maskT", "cst"], w=["maskT"])
        cx.act(mtmp[:, :], cs(cst, "D2"), AF.Exp, scale=lgb[:, h:h + 1], r=["lgb", "cst"], w=["mtmp"])
        cx.tt(mtmp[:, :], mtmp[:, :], cs(cst, "Lo"), ALU.mult, r=["mtmp", "cst"], w=["mtmp"])
        cx.tt(maskT[:, h, :], maskT[:, h, :], mtmp[:, :], ALU.add, r=["maskT", "mtmp"], w=["maskT"])
    for (tag, T) in segs:
        sfx = "_" + tag
        NCH = T // 128
        rq = cx.sb("rq" + tag, [128, T], BF16); rk = cx.sb("rk" + tag, [128, T], BF16)
        rqh = cx.sb("rqh" + tag, [128, 4, T], BF16)
        rv = cx.sb("rv" + tag, [128, NCH, 256], BF16); rg = cx.sb("rg" + tag, [128, NCH, 256], BF16)
        cx.dma("sp", rq[:, :], dr["rqT" + sfx][:, :], r=["D:rope" + sfx + "16"], w=["rq"])
        cx.dma("sp", rk[:, :], dr["rkT" + sfx][:, :], r=["D:rope" + sfx + "17"], w=["rk"])
        cx.dma("sp", rv[:, :, :], dr["rv" + sfx].rearrange("(n p) e -> p n e", p=128), r=["D:rv" + sfx], w=["rv"])
        cx.dma("sp", rg[:, :, :], dr["rg" + sfx].rearrange("(n p) e -> p n e", p=128), r=["D:rg" + sfx], w=["rg"])
        for h in range(4):
            cx.ts(rqh[:, h, :], rq[:, :], cs(cst, "hm")[:, h:h + 1], ALU.mult, r=["rq", "cst"], w=["rqh"], eng="pool")
        SF = cx.sb("SF" + tag, [128, NCH, 256]); SB = cx.sb("SB" + tag, [128, NCH, 256])
        SFb = cx.sb("SFb" + tag, [128, NCH, 256], BF16); SBb = cx.sb("SBb" + tag, [128, NCH, 256], BF16)
        KV = cx.sb("KV" + tag, [128, NCH, 2, 256])
        if tag == "l":
            Tall = cx.sb("Tall", [128, 9, 2, 256]); expo = cx.sb("expo", [128, 2, 9]); coef = cx.sb("coef", [128, 2, 9])
            cx.dma("sp", Tall[:, 0:8, :, :], dr["gt"].rearrange("(s a p) e -> p s a e", s=NCORE, a=2), w=["Tall"])
            cx.dma("sp", Tall[:, 8, :, :], dr["Tst_c"].rearrange("a p e -> p a e"), w=["Tall"])
            cx.dma("sp", expo[:, :, :], dr["expo"][:, :, :], w=["expo"])
            cx.act(coef[:, 0, :], expo[:, 0, :], AF.Exp, scale=lgfc[:, 0:1], r=["expo", "lgfc"], w=["coef"])
            cx.act(coef[:, 1, :], expo[:, 1, :], AF.Exp, scale=lgbc[:, 0:1], r=["expo", "lgbc"], w=["coef"])
            for a, (St, n0, key) in enumerate(((SF, 0, "SF0"), (SB, NCH - 1, "SB%d" % (NCH - 1)))):
                cx.ts(St[:, n0, :], Tall[:, 0, a, :], coef[:, a, 0:1], ALU.mult, r=["Tall", "coef"], w=[key])
                for s in range(1, 9):
                    cx.stt(St[:, n0, :], Tall[:, s, a, :], coef[:, a, s:s + 1], St[:, n0, :], ALU.mult, ALU.add,
                           r=["Tall", "coef", key], w=[key])
        else:
            cx.memset(SF[:, 0, :], 0.0, w=["SF0"])
            cx.memset(SB[:, NCH - 1, :], 0.0, w=["SB%d" % (NCH - 1)])
        kfb = [cx.sb("kfb%d" % i + tag, [128, 256], BF16) for i in range(2)]
        for n in range(NCH):
            b = n % 2
            csl = slice(n * 128, (n + 1) * 128)
            cx.tr(psb[0][:, 0:128], rk[:, csl], ident[:, :], r=["rk", "ident"], w=["psb0"])
            cx.tt(kfb[b][:, 0:128], psb[0][:, 0:128], KDF[:, :], ALU.mult, r=["psb0", "KDF"], w=["kfb%d" % b])
            cx.tt(kfb[b][:, 128:256], psb[0][:, 0:128], KDB[:, :], ALU.mult, r=["psb0", "KDB"], w=["kfb%d" % b])
            cx.mm(ps[0][:, 0:256], kfb[b][:, 0:128], rv[:, n, :], True, True, r=["kfb%d" % b, "rv"], w=["ps0"])
            cx.mm(ps[0][:, 256:512], kfb[b][:, 128:256], rv[:, n, :], True, True, r=["kfb%d" % b, "rv"], w=["ps0"])
            cx.copy(KV[:, n, :, :], ps[0][:, :].rearrange("p (a e) -> p a e", a=2), r=["ps0", "ps0"], w=["KV%d" % n], eng="act")
        for n in range(NCH - 1):
            cx.stt(SF[:, n + 1, :], SF[:, n, :], cdf[:, 0:1], KV[:, n, 0, :], ALU.mult, ALU.add,
                   r=["SF%d" % n, "cdf", "KV%d" % n], w=["SF%d" % (n + 1)])
        for n in range(NCH - 1, 0, -1):
            cx.stt(SB[:, n - 1, :], SB[:, n, :], cdb[:, 0:1], KV[:, n, 1, :], ALU.mult, ALU.add,
                   r=["SB%d" % n, "cdb", "KV%d" % n], w=["SB%d" % (n - 1)])
        allSF = ["SF%d" % n for n in range(NCH)]; allSB = ["SB%d" % n for n in range(NCH)]
        cx.copy(SFb[:, :, :], SF[:, :, :], r=allSF, w=["SFb"], eng="pool")
        cx.copy(SBb[:, :, :], SB[:, :, :], r=allSB, w=["SBb"], eng="pool")
        sT = [cx.sb("sT%d" % i + tag, [128, 4, 128], BF16) for i in range(2)]
        Qf = [cx.sb("Qf%d" % i + tag, [128, 4, 128], BF16) for i in range(2)]
        Qb = [cx.sb("Qb%d" % i + tag, [128, 4, 128], BF16) for i in range(2)]
        osb = cx.sb("osb" + tag, [128, 4, 64]); osq = cx.sb("osq" + tag, [128, 4, 64])
        st = cx.sb("st" + tag, [128, 4, 4])
        ysb = [cx.sb("ysb%d" % i + tag, [128, 256], BF16) for i in range(2)]
        yT = cx.sb("yT" + tag, [128, 2, T], BF16)
        for n in range(NCH):
            b = n % 2
            csl = slice(n * 128, (n + 1) * 128)
            for h in range(4):
                cx.mm(ps[1][:, h * 128:(h + 1) * 128], rk[:, csl], rqh[:, h, csl], True, True, r=["rk", "rqh"], w=["ps1"])
            cx.tt(sT[b][:, :, :], ps[1][:, :].rearrange("p (h i) -> p h i", h=4), maskT[:, :, :], ALU.mult,
                  r=["ps1", "maskT"], w=["sT%d" % b])
            cx.tt(Qf[b][:, :, :], rqh[:, :, csl], qdf4[:, :, :], ALU.mult, r=["rqh", "qdf4"], w=["Qf%d" % b], eng="pool")
            cx.tt(Qb[b][:, :, :], rqh[:, :, csl], qdb4[:, :, :], ALU.mult, r=["rqh", "qdb4"], w=["Qb%d" % b], eng="pool")
            for h in range(4):
                o = ps[2][:, h * 64:(h + 1) * 64]
                es = slice(h * 64, (h + 1) * 64)
                cx.mm(o, sT[b][:, h, :], rv[:, n, es], True, False, r=["sT%d" % b, "rv"], w=["ps2"])
                cx.mm(o, Qf[b][:, h, :], SFb[:, n, es], False, False, r=["Qf%d" % b, "SFb"], w=["ps2"])
                cx.mm(o, Qb[b][:, h, :], SBb[:, n, es], False, True, r=["Qb%d" % b, "SBb"], w=["ps2"])
            p2 = ["ps2"]
            cx.copy(osb[:, :, :], ps[2][:, 0:256].rearrange("p (h e) -> p h e", h=4), r=p2, w=["osb"], eng="act")
            cx.red(st[:, 0, :], osb[:, :, :], ALU.add, r=["osb"], w=["st0"])
            cx.tt(osq[:, :, :], osb[:, :, :], osb[:, :, :], ALU.mult, r=["osb"], w=["osq"], eng="pool")
            cx.red(st[:, 1, :], osq[:, :, :], ALU.add, r=["osq"], w=["st1"])
            cx.ts(st[:, 2, :], st[:, 0, :], 1.0 / 64, ALU.mult, r=["st0"], w=["st2"])
            cx.tt(st[:, 3, :], st[:, 2, :], st[:, 2, :], ALU.mult, r=["st2"], w=["st3"])
            cx.stt(st[:, 3, :], st[:, 1, :], 1.0 / 64, st[:, 3, :], ALU.mult, ALU.subtract, r=["st1", "st3"], w=["st3"])
            cx.act(st[:, 3, :], st[:, 3, :], AF.Sqrt, bias=EPS, r=["st3"], w=["st3"])
            cx.recip(st[:, 3, :], st[:, 3, :], r=["st3"], w=["st3"])
            for h in range(4):
                cx.ts(osb[:, h, :], osb[:, h, :], st[:, 2, h:h + 1], ALU.subtract, s2=st[:, 3, h:h + 1], op1=ALU.mult,
                      r=["osb", "st2", "st3"], w=["osb"])
            cx.tt(ysb[b][:, :], osb[:, :, :].rearrange("p h e -> p (h e)"), rg[:, n, :], ALU.mult, r=["osb", "rg"], w=["ysb%d" % b])
            for c2 in range(2):
                cx.tr(psb[1][:, c2 * 128:(c2 + 1) * 128], ysb[b][:, c2 * 128:(c2 + 1) * 128], ident[:, :],
                      r=["ysb%d" % b, "ident"], w=["psb1"])
            cx.copy(yT[:, :, csl], psb[1][:, 0:256].rearrange("p (c t) -> p c t", c=2), r=["psb1"], w=["yT"], eng="act")
        for c2 in range(2):
            cx.dma("sp", dr["ysT" + sfx][6 + c2, :, :], yT[:, c2, :], r=["yT"], w=["D:ys3" + sfx])
    cx.end()


def phase_merge(cx, l, dr, segs, wl=None):
    wl = l if wl is None else wl
    cx.begin(nf=8, nb=0)
    ps = cx.ps
    cst = cx.sb("cst", [128, CSTW])
    cx.dma("sp", cst[:, :], dr["cst"][:, :], w=["cst"])
    WG = cx.sb("WG", [128, 8, 4096], BF16)
    WB = cx.sb("WB", [128, 8, 1024], BF16)
    WO = cx.sb("WO", [128, 8, 1024], BF16)
    for kc in range(8):
        cx.dma("pool", WG[:, kc, :], dr["w_gate"][wl, kc * 128:(kc + 1) * 128, :], w=["WG%d" % kc])
        cx.dma("pool", WB[:, kc, :], dr["w_branch"][wl, kc // 2, (kc % 2) * 128:(kc % 2 + 1) * 128, :], w=["WB%d" % kc])
        cx.dma("pool", WO[:, kc, :], dr["w_o"][wl, kc * 128:(kc + 1) * 128, :], w=["WO%d" % kc])
    braw = cx.sb("braw", [32, 128]); bgt = cx.sb("bgt", [128, 32])
    cx.dma("sp", braw[:, :], dr["b_gate"][l, :].rearrange("(a p) -> a p", p=128), w=["braw"])
    cx.tr(ps[7][:, 0:32], braw[:, :], cs(cst, "I1")[0:32, 0:32], r=["braw", "cst"], w=["ps7"])
    cx.copy(bgt[:, :], ps[7][:, 0:32], r=["ps7"], w=["bgt"])
    g1b = cx.sb("g1b", [128, D])
    hT = [cx.sb("mhT%d" % i, [128, 8, 512], BF16) for i in range(2)]
    yT = [cx.sb("myT%d" % i, [128, 8, 512], BF16) for i in range(2)]
    mT = cx.sb("mT", [128, 8, 512], BF16)
    sg = [cx.sb("sg%d" % i, [128, 512]) for i in range(2)]
    macc = cx.sb("macc", [128, 512]); mtmp = cx.sb("mtmp", [128, 512])
    xt = [cx.sb("mxt%d" % i, [128, D]) for i in range(2)]
    gi = 0
    xi = 0
    for (tag, T, xin, xout) in segs:
        sfx = "_" + tag
        seg = 0 if tag == "l" else 1
        cx.dma("sp", g1b[:, :], dr["modv"][l, seg, 2, :].partition_broadcast(128), r=["D:modv%d" % l], w=["g1b"])
        G = min(512, T)
        for g in range(T // G):
            t0 = g * G
            b = gi % 2
            gi += 1
            cx.dma("sp", hT[b][:, :, 0:G], dr["hT" + sfx][:, :, t0:t0 + G], r=["D:hT" + sfx], w=["mhT%d" % b])
            cx.dma("sp", yT[b][:, :, 0:G], dr["ysT" + sfx][:, :, t0:t0 + G].rearrange("a p t -> p a t"),
                   r=["D:ys0" + sfx, "D:ys1" + sfx, "D:ys2" + sfx, "D:ys3" + sfx], w=["myT%d" % b])
            for nn in range(8):
                for i in range(4):
                    pa = ps[(i % 2) * 2]
                    pb = ps[(i % 2) * 2 + 1]
                    ka = "ps%d" % ((i % 2) * 2)
                    kb = "ps%d" % ((i % 2) * 2 + 1)
                    col = i * 1024 + nn * 128
                    for kc in range(8):
                        cx.mm(pa[:, 0:G], WG[:, kc, col:col + 128], hT[b][:, kc, 0:G], kc == 0, kc == 7,
                              r=["WG%d" % kc, "mhT%d" % b], w=[ka])
                    for c2 in range(2):
                        cx.mm(pb[:, 0:G], WB[:, i * 2 + c2, nn * 128:(nn + 1) * 128], yT[b][:, i * 2 + c2, 0:G], c2 == 0, c2 == 1,
                              r=["WB%d" % (i * 2 + c2), "myT%d" % b], w=[kb])
                    s = sg[i % 2]
                    ks = "sg%d" % (i % 2)
                    cx.act(s[:, 0:G], pa[:, 0:G], AF.Sigmoid, bias=bgt[:, i * 8 + nn:i * 8 + nn + 1], r=[ka, "bgt"], w=[ks])
                    if i == 0:
                        cx.tt(macc[:, 0:G], pb[:, 0:G], s[:, 0:G], ALU.mult, r=[kb, ks], w=["macc"])
                    elif i < 3:
                        cx.tt(mtmp[:, 0:G], pb[:, 0:G], s[:, 0:G], ALU.mult, r=[kb, ks], w=["mtmp"])
                        cx.tt(macc[:, 0:G], macc[:, 0:G], mtmp[:, 0:G], ALU.add, r=["macc", "mtmp"], w=["macc"], eng="pool")
                    else:
                        cx.tt(mtmp[:, 0:G], pb[:, 0:G], s[:, 0:G], ALU.mult, r=[kb, ks], w=["mtmp"])
                        cx.tt(mT[:, nn, 0:G], macc[:, 0:G], mtmp[:, 0:G], ALU.add, r=["macc", "mtmp"], w=["mT"], eng="pool")
            for j in range(G // 128):
                xb = xi % 2
                xi += 1
                rows = slice(t0 + j * 128, t0 + (j + 1) * 128)
                cx.dma("sp", xt[xb][:, :], xin[rows, :], w=["mxt%d" % xb])
                for nh in range(2):
                    po = ps[4 + nh]
                    for kc in range(8):
                        cx.mm(po[:, :], mT[:, kc, j * 128:(j + 1) * 128], WO[:, kc, nh * 512:(nh + 1) * 512], kc == 0, kc == 7,
                              r=["mT", "WO%d" % kc], w=["ps%d" % (4 + nh)])
                    hs = slice(nh * 512, (nh + 1) * 512)
                    cx.tt(mtmp[:, :], po[:, :], g1b[:, hs], ALU.mult, r=["ps%d" % (4 + nh), "g1b"], w=["mtmp"])
                    cx.tt(xt[xb][:, hs], xt[xb][:, hs], mtmp[:, :], ALU.add, r=["mxt%d" % xb, "mtmp"], w=["mxt%d" % xb], eng="pool")
                cx.dma("sp", xout[rows, :], xt[xb][:, :], r=["mxt%d" % xb], w=["D:xmid" + sfx])
    cx.end()


def phase_attn(cx, l, dr, segs, lam_init):
    NB = 2
    NSB = 3
    cx.begin(nf=0, nb=0)
    psS = [cx.psum("psS%d" % i, [128, NB * 512]) for i in range(NSB)]
    psO = [cx.psum("psO%d" % i, [128, 512]) for i in range(2)]
    NKT = max(s[2] for s in segs)
    cst = cx.sb("cst", [128, CSTW])
    ident = cx.sb("ident", [128, 128], BF16)
    cx.dma("sp", cst[:, :], dr["cst"][:, :], w=["cst"])
    cx.dma("sp", ident[:, :], dr["ident"][:, :], w=["ident"])
    kT = cx.sb("kTall", [128, 2, NKT * 128], BF16)
    Va = cx.sb("Vaug", [128, NKT, 4, 65], BF16)
    vst = [cx.sb("vst%d" % i, [128, 10, 256], BF16) for i in range(2)]
    kTk = []
    for c in range(2):
        cx.dma("sp", kT[:, c, 0:TC], dr["kT_c"][c, :, :], w=["kTc%d" % c])
        kTk.append("kTc%d" % c)
        if NKT > TC // 128:
            for r_ in range(NCORE):
                cx.dma("sp" if (r_ % 2 == 0) else "act", kT[:, c, TC + r_ * TL:TC + (r_ + 1) * TL],
                       dr["gk"][(r_ * 2 + c) * 128:(r_ * 2 + c + 1) * 128, :], w=["kT%d_%d" % (c, r_)])
                kTk.append("kT%d_%d" % (c, r_))
    cx.memset(Va[:, :, :, 64:65], 1.0, w=["Vones"], eng="pool")
    chunks = [(0, TC // 128, dr["V_c"], 0)]
    k0 = TC // 128
    while k0 < NKT:
        k1 = min(NKT, k0 + 10)
        chunks.append((k0, k1, dr["gv"], (k0 - TC // 128) * 128))
        k0 = k1
    nst = len(chunks)
    for i, (k0, k1, src, row0) in enumerate(chunks):
        b = i % 2
        cx.dma("sp", vst[b][:, 0:k1 - k0, :], src[row0:row0 + (k1 - k0) * 128, :].rearrange("(k p) e -> p k e", p=128), w=["vst%d" % b])
        cx.copy(Va[:, k0:k1, :, 0:64], vst[b][:, 0:k1 - k0, :].rearrange("p k (h e) -> p k h e", h=4), r=["vst%d" % b],
                w=["Va%d" % i], eng=("pool" if i % 2 == 0 else "dve"))
    Vak = ["Va%d" % i for i in range(nst)] + ["Vones"]
    lq = cx.sb("lq", [128, 4, 32]); lp = cx.sb("lp", [128, 2, 32]); ls = cx.sb("ls", [128, 4])
    for i, nm in enumerate(("lam_q1", "lam_k1", "lam_q2", "lam_k2")):
        cx.dma("sp", lq[:, i, :], dr[nm][l, :].partition_broadcast(128), w=["lq"])
    cx.tt(lp[:, 0, :], lq[:, 0, :], lq[:, 1, :], ALU.mult, r=["lq"], w=["lp"])
    cx.tt(lp[:, 1, :], lq[:, 2, :], lq[:, 3, :], ALU.mult, r=["lq"], w=["lp"])
    cx.red(ls[:, 0:2], lp[:, :, :], ALU.add, r=["lp"], w=["ls"])
    cx.act(ls[:, 0:2], ls[:, 0:2], AF.Exp, r=["ls"], w=["ls"])
    cx.tt(ls[:, 2:3], ls[:, 1:2], ls[:, 0:1], ALU.subtract, r=["ls"], w=["ls2"])
    cx.ts(ls[:, 3:4], ls[:, 2:3], -lam_init, ALU.add, r=["ls2"], w=["nlam"])
    nlam = ls[:, 3:4]
    dgb = cx.sb("dgb", [128, 4, 64])
    for h in range(4):
        cx.dma("sp", dgb[:, h, :], dr["diff_g"][l, :].partition_broadcast(128), w=["dgb"])
    cx.ts(dgb[:, :, :], dgb[:, :, :], 1.0 - lam_init, ALU.mult, r=["dgb"], w=["dgb"])
    qg = [cx.sb("qg%d" % i, [128, 2, 512], BF16) for i in range(2)]
    qm = [cx.sb("qm%d" % i, [128, 8, 512], BF16) for i in range(2)]
    pT = [cx.sb("pT%d" % i, [128, NB, 512], BF16) for i in range(NSB)]
    oT = cx.sb("oT", [65, 2, 512])
    oatt = cx.sb("oatt", [128, 4, 4, 64]); osq = cx.sb("aosq", [128, 4, 64])
    rr = cx.sb("rr", [128, 4]); ast = cx.sb("ast", [128, 2, 4])
    ysb = [cx.sb("aysb%d" % i, [128, 256]) for i in range(2)]
    yT = cx.sb("ayT", [128, 2, 512], BF16)
    gi = 0
    for (tag, T, nkt) in segs:
        sfx = "_" + tag
        G = min(512, T)
        nt = G // 128
        assert nkt % NB == 0
        for g in range(T // G):
            t0 = g * G
            b = gi % 2
            gi += 1
            cx.dma("sp", qg[b][:, :, 0:G], dr["qT" + sfx][:, :, t0:t0 + G].rearrange("c p t -> p c t"),
                   r=["D:rope" + sfx + "10", "D:rope" + sfx + "11"], w=["qg%d" % b])
            qb_ = b
            for c in range(2):
                for bl in range(4):
                    cx.ts(qm[qb_][:, c * 4 + bl, 0:G], qg[b][:, c, 0:G], cs(cst, "bm8")[:, bl:bl + 1], ALU.mult,
                          r=["qg%d" % b, "cst"], w=["qm%d_%d" % (qb_, c * 4 + bl)])
            items = [(h, m, kb) for h in range(4) for m in range(2) for kb in range(nkt // NB)]
            LA = 2

            def emit_S(i):
                h, m, kb = items[i]
                c = h // 2
                qi = c * 4 + (h % 2) * 2 + m
                sb_ = i % NSB
                for j in range(NB):
                    kt = kb * NB + j
                    kk = "kTc%d" % c if kt < TC // 128 else "kT%d_%d" % (c, (kt * 128 - TC) // TL)
                    cx.mm(psS[sb_][:, j * 512:j * 512 + G], kT[:, c, kt * 128:(kt + 1) * 128], qm[qb_][:, qi, 0:G], True, True,
                          r=[kk, "qm%d_%d" % (qb_, qi)], w=["psS%d" % sb_])
                cx.act(pT[sb_][:, :, 0:G], psS[sb_][:, :].rearrange("p (j n) -> p j n", j=NB)[:, :, 0:G], AF.Exp, scale=QSCALE,
                       r=["psS%d" % sb_], w=["pT%d" % sb_])

            def emit_O(i):
                h, m, kb = items[i]
                sb_ = i % NSB
                for j in range(NB):
                    kt = kb * NB + j
                    vk = "Va0" if kt < TC // 128 else "Va%d" % (1 + (kt - TC // 128) // 10)
                    cx.mm(psO[m][0:65, 0:G], Va[:, kt, h, :], pT[sb_][:, j, 0:G], kt == 0, kt == nkt - 1,
                          r=[vk, "Vones", "pT%d" % sb_], w=["psO%d" % m])
                if kb == nkt // NB - 1:
                    cx.copy(oT[:, m, 0:G], psO[m][0:65, 0:G], r=["psO%d" % m], w=["oT%d" % m])
                    if m == 1:
                        head_epilogue(h)

            def head_epilogue(h):
                for j in range(nt):
                    for m in range(2):
                        cx.tr(psO[0][:, m * 65:m * 65 + 65], oT[0:65, m, j * 128:(j + 1) * 128], cs(cst, "I1")[0:65, 0:65],
                              r=["oT%d" % m, "cst"], w=["psO0"])
                    cx.recip(rr[:, 0:1], psO[0][:, 64:65], r=["psO0"], w=["rr0"])
                    cx.recip(rr[:, 1:2], psO[0][:, 129:130], r=["psO0"], w=["rr1"])
                    cx.tt(rr[:, 2:3], rr[:, 1:2], nlam, ALU.mult, r=["rr1", "nlam"], w=["rr2"])
                    cx.ts(oatt[:, j, h, :], psO[0][:, 0:64], rr[:, 0:1], ALU.mult, r=["psO0", "rr0"], w=["oatt%d" % j])
                    cx.stt(oatt[:, j, h, :], psO[0][:, 65:129], rr[:, 2:3], oatt[:, j, h, :], ALU.mult, ALU.add,
                           r=["psO0", "rr2", "oatt%d" % j], w=["oatt%d" % j])

            if ATT_ROW:
                items = [(h, kt) for h in range(4) for kt in range(nkt)]

                def emit_S(i):
                    h, kt = items[i]
                    c = h // 2
                    sb_ = i % NSB
                    kk = "kTc%d" % c if kt < TC // 128 else "kT%d_%d" % (c, (kt * 128 - TC) // TL)
                    for m in range(2):
                        blk = (h % 2) * 2 + m
                        rs = slice(32 * blk, 32 * blk + 32)
                        cx.mm(psS[sb_][:, m * 512:m * 512 + G], kT[rs, c, kt * 128:(kt + 1) * 128], qg[b][rs, c, 0:G], True, True,
                              r=[kk, "qg%d" % b], w=["psS%d" % sb_], tile_position=(32 * blk, 0))
                    cx.act(pT[sb_][:, :, 0:G], psS[sb_][:, :].rearrange("p (j n) -> p j n", j=NB)[:, :, 0:G], AF.Exp, scale=QSCALE,
                           r=["psS%d" % sb_], w=["pT%d" % sb_])

                def emit_O(i):
                    h, kt = items[i]
                    sb_ = i % NSB
                    vk = "Va0" if kt < TC // 128 else "Va%d" % (1 + (kt - TC // 128) // 10)
                    for m in range(2):
                        cx.mm(psO[m][0:65, 0:G], Va[:, kt, h, :], pT[sb_][:, m, 0:G], kt == 0, kt == nkt - 1,
                              r=[vk, "Vones", "pT%d" % sb_], w=["psO%d" % m])
                    if kt == nkt - 1:
                        for m in range(2):
                            cx.copy(oT[:, m, 0:G], psO[m][0:65, 0:G], r=["psO%d" % m], w=["oT%d" % m])
                        head_epilogue(h)
            n_it = len(items)
            for i in range(n_it + LA):
                if i < n_it:
                    emit_S(i)
                if i >= LA:
                    emit_O(i - LA)
            for j in range(nt):
                yb = j % 2
                cx.tt(osq[:, :, :], oatt[:, j, :, :], oatt[:, j, :, :], ALU.mult, r=["oatt%d" % j], w=["aosq"], eng="pool")
                cx.red(ast[:, 0, :], osq[:, :, :], ALU.add, r=["aosq"], w=["ast0"])
                cx.act(ast[:, 1, :], ast[:, 0, :], AF.Sqrt, scale=1.0 / 64, bias=EPS, r=["ast0"], w=["ast1"])
                cx.recip(ast[:, 1, :], ast[:, 1, :], r=["ast1"], w=["ast1"])
                for h in range(4):
                    cx.stt(oatt[:, j, h, :], oatt[:, j, h, :], ast[:, 1, h:h + 1], dgb[:, h, :], ALU.mult, ALU.mult,
                           r=["oatt%d" % j, "ast1", "dgb"], w=["oatt%d" % j])
                cx.copy(ysb[yb][:, :], oatt[:, j, :, :].rearrange("p h e -> p (h e)"), r=["oatt%d" % j], w=["aysb%d" % yb], eng="pool")
                for c2 in range(2):
                    cx.tr(psO[1][:, c2 * 128:(c2 + 1) * 128], ysb[yb][:, c2 * 128:(c2 + 1) * 128], cs(cst, "I1"),
                          r=["aysb%d" % yb, "cst"], w=["psO1"])
                cx.copy(yT[:, :, j * 128:(j + 1) * 128], psO[1][:, 0:256].rearrange("p (c t) -> p c t", c=2), r=["psO1"], w=["ayT"])
            for c2 in range(2):
                cx.dma("sp", dr["ysT" + sfx][4 + c2, :, t0:t0 + G], yT[:, c2, 0:G], r=["ayT"], w=["D:ys2" + sfx])
    cx.end()


def phase_moe(cx, l, dr, segs, final, wl=None):
    wl = l if wl is None else wl
    cx.begin(nf=6, nb=2)
    ps, psb = cx.ps, cx.psb
    NT = sum(s[1] for s in segs) // 128
    TT = NT * 128
    cst = cx.sb("cst", [128, CSTW])
    ident = cx.sb("ident", [128, 128], BF16)
    cx.dma("sp", cst[:, :], dr["cst"][:, :], w=["cst"])
    cx.dma("sp", ident[:, :], dr["ident"][:, :], w=["ident"])
    h2T = cx.sb("h2T", [128, 8, TT], BF16)
    acc = cx.sb("eacc", [128, NT, D])
    wt = cx.sb("wt", [128, NT, 16])
    WR = cx.sb("WRt", [128, 8, 16], BF16)
    cx.dma("pool", WR[:, :, :], dr["w_router"].rearrange("(k p) e -> p k e", p=128), w=["WRt"])
    brb = cx.sb("brb", [128, 16])
    cx.dma("sp", brb[:, :], dr["b_router"][0, :].partition_broadcast(128), w=["brb"])
    gsb = cx.sb("gsb2", [128, D]); shb = cx.sb("shb2", [128, D]); g2b = cx.sb("g2b2", [128, D])
    xt = [cx.sb("ext%d" % i, [128, D]) for i in range(2)]
    junk = cx.sb("ejunk", [128, D], BF16)
    t1 = cx.sb("et1", [128, D])
    hb = [cx.sb("ehb%d" % i, [128, D], BF16) for i in range(2)]
    ssq = cx.sb("essq", [128, 2]); rstd = cx.sb("erstd", [128, 2])
    rt = cx.sb("rt", [128, 8, 16])
    W1 = [cx.sb("W1_%d" % i, [128, 8, DFF], BF16) for i in range(2)]
    W3 = [cx.sb("W3_%d" % i, [128, 8, DFF], BF16) for i in range(2)]
    W2 = [cx.sb("W2_%d" % i, [128, 4, D], BF16) for i in range(2)]

    def load_expert(e):
        b = e % 2
        cx.dma("pool", W1[b][:, :, :], dr["w1_e"][wl, e].rearrange("(k p) f -> p k f", p=128), w=["W1_%d" % b])
        cx.dma("pool", W3[b][:, :, :], dr["w3_e"][wl, e].rearrange("(k p) f -> p k f", p=128), w=["W3_%d" % b])
        cx.dma("pool", W2[b][:, :, :], dr["w2_e"][wl, e].rearrange("(k p) n -> p k n", p=128), w=["W2_%d" % b])
    load_expert(0)
    load_expert(1)
    ti = 0
    tiles = []
    for (tag, T, xmid, xout) in segs:
        seg = 0 if tag == "l" else 1
        cx.dma("sp", gsb[:, :], dr["modv"][l, seg, 3, :].partition_broadcast(128), r=["D:modv%d" % l], w=["gsb2"])
        cx.dma("sp", shb[:, :], dr["modv"][l, seg, 4, :].partition_broadcast(128), r=["D:modv%d" % l], w=["shb2"])
        for j in range(T // 128):
            b = ti % 2
            kx = "ext%d" % b
            rows = slice(j * 128, (j + 1) * 128)
            tiles.append((tag, seg, xmid, xout, rows))
            cx.dma("sp", xt[b][:, :], xmid[rows, :], r=["D:xmid_" + tag], w=[kx])
            cx.act(junk[:, :], xt[b][:, :], AF.Square, accum_out=ssq[:, b:b + 1], r=[kx], w=["ejunk", "essq%d" % b])
            cx.act(rstd[:, b:b + 1], ssq[:, b:b + 1], AF.Sqrt, scale=1.0 / D, bias=EPS, r=["essq%d" % b], w=["ers%d" % b])
            cx.recip(rstd[:, b:b + 1], rstd[:, b:b + 1], r=["ers%d" % b], w=["ers%d" % b])
            cx.stt(t1[:, :], xt[b][:, :], rstd[:, b:b + 1], gsb[:, :], ALU.mult, ALU.mult, r=[kx, "ers%d" % b, "gsb2"], w=["et1"])
            cx.tt(hb[b][:, :], t1[:, :], shb[:, :], ALU.add, r=["et1", "shb2"], w=["ehb%d" % b], eng="pool")
            for kc in range(8):
                cx.tr(psb[0][:, kc * 128:(kc + 1) * 128], hb[b][:, kc * 128:(kc + 1) * 128], ident[:, :],
                      r=["ehb%d" % b, "ident"], w=["psb0"])
            cx.copy(h2T[:, :, ti * 128:(ti + 1) * 128], psb[0][:, :].rearrange("p (k t) -> p k t", k=8),
                    r=["psb0"], w=["h2T%d" % ti], eng="act")
            for kc in range(8):
                cx.mm(ps[0][:, 0:16], h2T[:, kc, ti * 128:(ti + 1) * 128], WR[:, kc, :], kc == 0, kc == 7,
                      r=["h2T%d" % ti, "WRt"], w=["ps0"])
            s_ = rt[:, 0, :]; sbv = rt[:, 1, :]; tmp = rt[:, 2, :]; sb2 = rt[:, 3, :]; sbm = rt[:, 4, :]
            msk = rt[:, 5, :]; sel = rt[:, 6, :]
            g4 = rt[:, 7, 0:4]; g4b = rt[:, 7, 4:8]; gm = rt[:, 7, 8:12]; e1 = rt[:, 7, 12:13]; e2 = rt[:, 7, 13:14]
            den = rt[:, 7, 14:15]
            cx.act(s_, ps[0][:, 0:16], AF.Sigmoid, r=["ps0"], w=["r_s"])
            cx.tt(sbv, s_, brb[:, :], ALU.add, r=["r_s", "brb"], w=["r_sb"])
            v4 = lambda a: a.rearrange("p (g e) -> p g e", g=4)
            cx.red(g4, v4(sbv), ALU.max, r=["r_sb"], w=["r_g4"])
            for g_ in range(4):
                cx.ts(tmp[:, g_ * 4:(g_ + 1) * 4], sbv[:, g_ * 4:(g_ + 1) * 4], g4[:, g_:g_ + 1], ALU.is_equal,
                      r=["r_sb", "r_g4"], w=["r_tmp"])
            cx.stt(sb2, tmp, -1.0e9, sbv, ALU.mult, ALU.add, r=["r_tmp", "r_sb"], w=["r_sb2"])
            cx.red(g4b, v4(sb2), ALU.max, r=["r_sb2"], w=["r_g4b"])
            cx.tt(g4, g4, g4b, ALU.add, r=["r_g4", "r_g4b"], w=["r_g4"])
            cx.red(e1, g4, ALU.max, r=["r_g4"], w=["r_e1"])
            cx.ts(gm, g4, e1, ALU.is_equal, s2=-1.0, op1=ALU.add, r=["r_g4", "r_e1"], w=["r_gm"])
            for g_ in range(4):
                cx.ts(tmp[:, g_ * 4:(g_ + 1) * 4], cs(cst, "ones")[:, 0:4], gm[:, g_:g_ + 1], ALU.mult,
                      r=["r_gm", "cst"], w=["r_tmp"])
            cx.stt(sbm, tmp, 1.0e9, sbv, ALU.mult, ALU.add, r=["r_tmp", "r_sb"], w=["r_sbm"])
            cx.red(e1, sbm, ALU.max, r=["r_sbm"], w=["r_e1"])
            cx.ts(msk, sbm, e1, ALU.is_equal, r=["r_sbm", "r_e1"], w=["r_msk"])
            cx.stt(sb2, msk, -1.0e9, sbm, ALU.mult, ALU.add, r=["r_msk", "r_sbm"], w=["r_sb2"])
            cx.red(e2, sb2, ALU.max, r=["r_sb2"], w=["r_e2"])
            cx.ts(sel, sb2, e2, ALU.is_equal, r=["r_sb2", "r_e2"], w=["r_sel"])
            cx.tt(sel, sel, msk, ALU.add, r=["r_sel", "r_msk"], w=["r_sel"])
            cx.tt(sel, sel, s_, ALU.mult, r=["r_sel", "r_s"], w=["r_sel"])
            cx.red(den, sel, ALU.add, r=["r_sel"], w=["r_den"])
            cx.recip(den, den, r=["r_den"], w=["r_den"])
            cx.ts(wt[:, ti, :], sel, den, ALU.mult, r=["r_sel", "r_den"], w=["wt%d" % ti])
            ti += 1
    uT = [cx.sb("uT%d" % i, [128, 4, 512], BF16) for i in range(2)]
    s1 = [cx.sb("s1_%d" % i, [128, 512]) for i in range(2)]
    groups = []
    t = 0
    while t < NT:
        n = min(4, NT - t)
        groups.append((t, n))
        t += n
    ui = 0
    for e in range(NEXP):
        b = e % 2
        if e >= 2:
            load_expert(e)
        for (tg, ntl) in groups:
            G = ntl * 128
            gsl = slice(tg * 128, tg * 128 + G)
            hk = ["h2T%d" % i for i in range(tg, tg + ntl)]
            ub = ui % 2
            ui += 1
            for fc in range(4):
                fs = slice(fc * 128, (fc + 1) * 128)
                pa = ps[(fc % 2) * 2]; pb = ps[(fc % 2) * 2 + 1]
                ka = "ps%d" % ((fc % 2) * 2); kb = "ps%d" % ((fc % 2) * 2 + 1)
                for kc in range(8):
                    cx.mm(pa[:, 0:G], W1[b][:, kc, fs], h2T[:, kc, gsl], kc == 0, kc == 7, r=hk + ["W1_%d" % b], w=[ka])
                for kc in range(8):
                    cx.mm(pb[:, 0:G], W3[b][:, kc, fs], h2T[:, kc, gsl], kc == 0, kc == 7, r=hk + ["W3_%d" % b], w=[kb])
                sb_ = s1[fc % 2]
                cx.act(sb_[:, 0:G], pa[:, 0:G], AF.Silu, r=[ka], w=["s1_%d" % (fc % 2)])
                cx.tt(uT[ub][:, fc, 0:G], pb[:, 0:G], sb_[:, 0:G], ALU.mult, r=[kb, "s1_%d" % (fc % 2)], w=["uT%d" % ub])
            for j in range(ntl):
                tix = tg + j
                for nh in range(2):
                    po = ps[4 + nh]
                    for fc in range(4):
                        cx.mm(po[:, :], uT[ub][:, fc, j * 128:(j + 1) * 128], W2[b][:, fc, nh * 512:(nh + 1) * 512], fc == 0, fc == 3,
                              r=["uT%d" % ub, "W2_%d" % b], w=["ps%d" % (4 + nh)])
                    a = acc[:, tix, nh * 512:(nh + 1) * 512]
                    ka2 = "eacc%d_%d" % (tix, nh)
                    if e == 0:
                        cx.ts(a, po[:, :], wt[:, tix, e:e + 1], ALU.mult, r=["ps%d" % (4 + nh), "wt%d" % tix], w=[ka2])
                    else:
                        cx.stt(a, po[:, :], wt[:, tix, e:e + 1], a, ALU.mult, ALU.add, r=["ps%d" % (4 + nh), "wt%d" % tix, ka2], w=[ka2])
    if final:
        gfb = cx.sb("gfb", [128, D])
        cx.dma("sp", gfb[:, :], dr["g_final"][0, :].partition_broadcast(128), w=["gfb"])
    cur = None
    for ti, (tag, seg, xmid, xout, rows) in enumerate(tiles):
        if cur != seg:
            cx.dma("sp", g2b[:, :], dr["modv"][l, seg, 5, :].partition_broadcast(128), r=["D:modv%d" % l], w=["g2b2"])
            cur = seg
        b = ti % 2
        kx = "ext%d" % b
        cx.dma("sp", xt[b][:, :], xmid[rows, :], r=["D:xmid_" + tag], w=[kx])
        cx.tt(t1[:, :], acc[:, ti, :], g2b[:, :], ALU.mult, r=["eacc%d_0" % ti, "eacc%d_1" % ti, "g2b2"], w=["et1"], eng="pool")
        cx.tt(xt[b][:, :], xt[b][:, :], t1[:, :], ALU.add, r=[kx, "et1"], w=[kx])
        if final:
            cx.act(junk[:, :], xt[b][:, :], AF.Square, accum_out=ssq[:, b:b + 1], r=[kx], w=["ejunk", "essq%d" % b])
            cx.act(rstd[:, b:b + 1], ssq[:, b:b + 1], AF.Sqrt, scale=1.0 / D, bias=EPS, r=["essq%d" % b], w=["ers%d" % b])
            cx.recip(rstd[:, b:b + 1], rstd[:, b:b + 1], r=["ers%d" % b], w=["ers%d" % b])
            cx.stt(xt[b][:, :], xt[b][:, :], rstd[:, b:b + 1], gfb[:, :], ALU.mult, ALU.mult, r=[kx, "ers%d" % b, "gfb"], w=[kx])
        cx.dma("sp", xout[rows, :], xt[b][:, :], r=[kx], w=["D:xout_" + tag])
    cx.end()


def phase_moe_sparse(cx, l, dr, segs, final, wl=None):
    wl = l if wl is None else wl
    C = MOE_CAP
    cx.begin(nf=6, nb=2)
    ps, psb = cx.ps, cx.psb
    NT = sum(s[1] for s in segs) // 128
    cst = cx.sb("cst", [128, CSTW])
    ident = cx.sb("ident", [128, 128], BF16)
    cx.dma("sp", cst[:, :], dr["cst"][:, :], w=["cst"])
    cx.dma("sp", ident[:, :], dr["ident"][:, :], w=["ident"])
    Xg = dr["Xg"]
    Yg = dr["Yg"]
    bcreg = {}

    def bc(h):
        if "r" not in bcreg:
            bcreg["r"] = h.to_reg(NSLOT - 1)
        return bcreg["r"]
    W1 = [cx.sb("W1_%d" % i, [128, 8, DFF], BF16) for i in range(2)]
    W3 = [cx.sb("W3_%d" % i, [128, 8, DFF], BF16) for i in range(2)]
    W2 = [cx.sb("W2_%d" % i, [128, 4, D], BF16) for i in range(2)]

    def load_expert(e):
        b = e % 2
        cx.dma("pool", W1[b][:, :, :], dr["w1_e"][wl, e].rearrange("(k p) f -> p k f", p=128), w=["W1_%d" % b])
        cx.dma("pool", W3[b][:, :, :], dr["w3_e"][wl, e].rearrange("(k p) f -> p k f", p=128), w=["W3_%d" % b])
        cx.dma("pool", W2[b][:, :, :], dr["w2_e"][wl, e].rearrange("(k p) n -> p k n", p=128), w=["W2_%d" % b])
    load_expert(0)
    load_expert(1)
    zt = cx.sb("zt", [128, 4, D], BF16)
    cx.memset(zt[:, :, :], 0.0, w=["zt"], eng="pool")
    for i in range(NSLOT // 512):
        cx.dma("sp", Xg[i * 512:(i + 1) * 512, :].rearrange("(b p) d -> p b d", p=128), zt[:, :, :], r=["zt"], w=["D:XgZ%d" % i])
    WR = cx.sb("WRt", [128, 8, 16], BF16)
    cx.dma("pool", WR[:, :, :], dr["w_router"].rearrange("(k p) e -> p k e", p=128), w=["WRt"])
    brb = cx.sb("brb", [128, 16])
    cx.dma("sp", brb[:, :], dr["b_router"][0, :].partition_broadcast(128), w=["brb"])
    gsb = cx.sb("gsb2", [128, D]); shb = cx.sb("shb2", [128, D]); g2b = cx.sb("g2b2", [128, D])
    xt = [cx.sb("ext%d" % i, [128, D]) for i in range(2)]
    junk = cx.sb("ejunk", [128, D], BF16)
    t1s = [cx.sb("et1_%d" % i, [128, D]) for i in range(2)]
    hb = [cx.sb("ehb%d" % i, [128, D], BF16) for i in range(2)]
    hTt = [cx.sb("ehT%d" % i, [128, 8, 128], BF16) for i in range(2)]
    ssq = cx.sb("essq", [128, 2]); rstd = cx.sb("erstd", [128, 2])
    slf = cx.sb("slf", [128, NT, 2]); sli = cx.sb("sli", [128, NT, 2], mybir.dt.int32); wts = cx.sb("wts", [128, NT, 2])
    H2d = dr["H2d"]
    lg = cx.sb("lgall", [128, NT, 16])
    ti = 0
    tiles = []
    for (tag, T, xmid, xout) in segs:
        seg = 0 if tag == "l" else 1
        cx.dma("sp", gsb[:, :], dr["modv"][l, seg, 3, :].partition_broadcast(128), r=["D:modv%d" % l], w=["gsb2"])
        cx.dma("sp", shb[:, :], dr["modv"][l, seg, 4, :].partition_broadcast(128), r=["D:modv%d" % l], w=["shb2"])
        for j in range(T // 128):
            b = ti % 2
            kx = "ext%d" % b
            t1 = t1s[b]
            rows = slice(j * 128, (j + 1) * 128)
            tiles.append((tag, seg, xmid, xout, rows))
            cx.dma("sp", xt[b][:, :], xmid[rows, :], r=["D:xmid_" + tag], w=[kx])
            cx.act(junk[:, :], xt[b][:, :], AF.Square, accum_out=ssq[:, b:b + 1], r=[kx], w=["ejunk", "essq%d" % b])
            cx.act(rstd[:, b:b + 1], ssq[:, b:b + 1], AF.Sqrt, scale=1.0 / D, bias=EPS, r=["essq%d" % b], w=["ers%d" % b])
            cx.recip(rstd[:, b:b + 1], rstd[:, b:b + 1], r=["ers%d" % b], w=["ers%d" % b])
            cx.stt(t1[:, :], xt[b][:, :], rstd[:, b:b + 1], gsb[:, :], ALU.mult, ALU.mult, r=[kx, "ers%d" % b, "gsb2"], w=["et1_%d" % b])
            cx.tt(hb[b][:, :], t1[:, :], shb[:, :], ALU.add, r=["et1_%d" % b, "shb2"], w=["ehb%d" % b], eng="pool")
            cx.dma("sp", H2d[ti * 128:(ti + 1) * 128, :], hb[b][:, :], r=["ehb%d" % b], w=["D:H2d%d" % ti])
            for kc in range(8):
                cx.tr(psb[0][:, kc * 128:(kc + 1) * 128], hb[b][:, kc * 128:(kc + 1) * 128], ident[:, :],
                      r=["ehb%d" % b, "ident"], w=["psb0"])
            cx.copy(hTt[b][:, :, :], psb[0][:, :].rearrange("p (k t) -> p k t", k=8), r=["psb0"], w=["ehT%d" % b], eng="act")
            for kc in range(8):
                cx.mm(ps[0][:, 0:16], hTt[b][:, kc, :], WR[:, kc, :], kc == 0, kc == 7, r=["ehT%d" % b, "WRt"], w=["ps0"])
            cx.copy(lg[:, ti, :], ps[0][:, 0:16], r=["ps0"], w=["lg%d" % ti])
            ti += 1
    RT = lambda n: cx.sb("R" + n, [128, NT, 16])
    S_ = RT("s"); sbv = RT("sb"); tmp = RT("tmp"); sb2 = RT("sb2"); sbm = RT("sbm"); msk = RT("msk"); m2 = RT("m2"); sel = RT("sel")
    pos = RT("pos"); offs = RT("offs"); wv = RT("wv")
    g4 = cx.sb("Rg4", [128, NT, 4]); g4b = cx.sb("Rg4b", [128, NT, 4]); gm = cx.sb("Rgm", [128, NT, 4])
    e1 = cx.sb("Re1", [128, NT]); e2 = cx.sb("Re2", [128, NT]); den = cx.sb("Rden", [128, NT])
    f2 = lambda a: a[:, :, :].rearrange("p t e -> p (t e)")
    v4 = lambda a: a[:, :, :].rearrange("p t (g e) -> p t g e", g=4)
    bt = lambda a: a[:, :].unsqueeze(2).to_broadcast([128, NT, 16])
    bg = lambda a: a[:, :, :].unsqueeze(3).to_broadcast([128, NT, 4, 4])
    b16 = lambda a: a.unsqueeze(1).to_broadcast([128, NT, 16])
    lgk = ["lg%d" % t_ for t_ in range(NT)]
    cx.act(f2(S_), f2(lg), AF.Sigmoid, r=lgk, w=["Rs"])
    cx.tt(sbv[:, :, :], S_[:, :, :], b16(brb[:, :]), ALU.add, r=["Rs", "brb"], w=["Rsb"])
    cx.red(g4[:, :, :], v4(sbv), ALU.max, r=["Rsb"], w=["Rg4"])
    cx.tt(v4(tmp), v4(sbv), bg(g4), ALU.is_equal, r=["Rsb", "Rg4"], w=["Rtmp"])
    cx.stt(f2(sb2), f2(tmp), -1.0e9, f2(sbv), ALU.mult, ALU.add, r=["Rtmp", "Rsb"], w=["Rsb2"])
    cx.red(g4b[:, :, :], v4(sb2), ALU.max, r=["Rsb2"], w=["Rg4b"])
    cx.tt(g4[:, :, :], g4[:, :, :], g4b[:, :, :], ALU.add, r=["Rg4", "Rg4b"], w=["Rg4"])
    cx.red(e1[:, :], g4[:, :, :], ALU.max, r=["Rg4"], w=["Re1"])
    cx.tt(gm[:, :, :], g4[:, :, :], e1[:, :].unsqueeze(2).to_broadcast([128, NT, 4]), ALU.is_equal, r=["Rg4", "Re1"], w=["Rgm"])
    cx.ts(gm[:, :, :], gm[:, :, :], -1.0, ALU.add, s2=1.0e9, op1=ALU.mult, r=["Rgm"], w=["Rgm"])
    cx.tt(v4(sbm), v4(sbv), bg(gm), ALU.add, r=["Rsb", "Rgm"], w=["Rsbm"])
    cx.red(e1[:, :], sbm[:, :, :], ALU.max, r=["Rsbm"], w=["Re1"])
    cx.tt(msk[:, :, :], sbm[:, :, :], bt(e1), ALU.is_equal, r=["Rsbm", "Re1"], w=["Rmsk"])
    cx.stt(f2(sb2), f2(msk), -1.0e9, f2(sbm), ALU.mult, ALU.add, r=["Rmsk", "Rsbm"], w=["Rsb2"])
    cx.red(e2[:, :], sb2[:, :, :], ALU.max, r=["Rsb2"], w=["Re2"])
    cx.tt(m2[:, :, :], sb2[:, :, :], bt(e2), ALU.is_equal, r=["Rsb2", "Re2"], w=["Rm2"])
    cx.tt(sel[:, :, :], m2[:, :, :], msk[:, :, :], ALU.add, r=["Rm2", "Rmsk"], w=["Rsel"])
    cx.mm(ps[1][:, 0:NT * 16], cs(cst, "Ltri"), f2(sel), True, True, r=["cst", "Rsel"], w=["ps1"])
    cx.mm(ps[2][:, 0:NT * 16], cs(cst, "ones"), f2(sel), True, True, r=["cst", "Rsel"], w=["ps2"])
    cx.copy(f2(tmp), ps[2][:, 0:NT * 16], r=["ps2"], w=["Rtmp"])
    cx.memset(offs[:, 0, :], 0.0, w=["Roffs"])
    for t_ in range(1, NT):
        cx.tt(offs[:, t_, :], offs[:, t_ - 1, :], tmp[:, t_ - 1, :], ALU.add, r=["Roffs", "Rtmp"], w=["Roffs"])
    cx.tt(f2(pos), ps[1][:, 0:NT * 16], f2(offs), ALU.add, r=["ps1", "Roffs"], w=["Rpos"])
    cx.ts(f2(tmp), f2(pos), float(C) - 0.5, ALU.is_lt, r=["Rpos"], w=["Rtmp"])
    cx.tt(pos[:, :, :], pos[:, :, :], b16(cs(cst, "eoff")), ALU.add, r=["Rpos", "cst"], w=["Rpos"])
    cx.stt(f2(pos), f2(tmp), -1.0e6, f2(pos), ALU.mult, ALU.add, r=["Rtmp", "Rpos"], w=["Rpos"])
    cx.ts(f2(pos), f2(pos), 1.0e6, ALU.add, r=["Rpos"], w=["Rpos"])
    cx.tt(wv[:, :, :], sel[:, :, :], S_[:, :, :], ALU.mult, r=["Rsel", "Rs"], w=["Rwv"])
    cx.red(den[:, :], wv[:, :, :], ALU.add, r=["Rwv"], w=["Rden"])
    cx.recip(den[:, :], den[:, :], r=["Rden"], w=["Rden"])
    cx.tt(wv[:, :, :], wv[:, :, :], bt(den), ALU.mult, r=["Rwv", "Rden"], w=["Rwv"])
    cx.tt(wv[:, :, :], wv[:, :, :], tmp[:, :, :], ALU.mult, r=["Rwv", "Rtmp"], w=["Rwv"])
    for q, mk, kk in ((0, msk, "Rmsk"), (1, m2, "Rm2")):
        cx.tt(sbm[:, :, :], mk[:, :, :], pos[:, :, :], ALU.mult, r=[kk, "Rpos"], w=["Rsbm"])
        cx.red(slf[:, :, q], sbm[:, :, :], ALU.add, r=["Rsbm"], w=["slf%d" % q])
        cx.tt(sbm[:, :, :], mk[:, :, :], wv[:, :, :], ALU.mult, r=[kk, "Rwv"], w=["Rsbm"])
        cx.red(wts[:, :, q], sbm[:, :, :], ALU.add, r=["Rsbm"], w=["wtsq%d" % q])
    cx.copy(sli[:, :, :], slf[:, :, :], r=["slf0", "slf1"], w=["sli"])
    zk = ["D:XgZ%d" % i_ for i_ in range(NSLOT // 512)]
    for ti in range(NT):
        b = ti % 2
        cx.dma("sp", hb[b][:, :], H2d[ti * 128:(ti + 1) * 128, :], r=["D:H2d%d" % ti], w=["ehb%d" % b])
        for q in range(2):
            idx = sli[:, ti, q:q + 1]
            src = hb[b][:, :]
            cx.S.add("pool", lambda h, idx=idx, src=src: h.indirect_dma_start(
                out=Xg[:, :], out_offset=bass.IndirectOffsetOnAxis(ap=idx, axis=0), in_=src, in_offset=None,
                bounds_check=bc(h), oob_is_err=False), r=["sli", "ehb%d" % b] + zk, w=["D:Xg%d_%d" % (ti, q)], dma=True)
    dummy = cx.sb("dummy", [128, 4])
    cx.memset(dummy[:, 0:1], 0.0, w=["XgAll"], eng="pool")
    cx.S.ops[-1].deps.update({cx.S.lastw[k]: True for k in ["D:Xg%d_%d" % (t_, q) for t_ in range(NT) for q in range(2)]})
    NBLK = C // 128
    NPC = (C + 511) // 512
    PW = C // NPC
    xg = [cx.sb("xg%d" % i, [128, NBLK, D], BF16) for i in range(2)]
    xT = [cx.sb("xTe%d" % i, [128, 8, C], BF16) for i in range(2)]
    uT = cx.sb("uTe", [128, 4, C], BF16)
    s1 = [cx.sb("s1_%d" % i, [128, 512]) for i in range(2)]
    yb = [cx.sb("ybe%d" % i, [128, D]) for i in range(2)]
    yi = 0

    def load_xg(e):
        cx.dma("sp", xg[e % 2][:, :, :], Xg[e * C:(e + 1) * C, :].rearrange("(j p) d -> p j d", p=128), r=["XgAll"], w=["xg%d" % (e % 2)])
    load_xg(0)
    for e in range(NEXP):
        b = e % 2
        if e >= 2:
            load_expert(e)
        if e + 1 < NEXP:
            load_xg(e + 1)
        for j in range(NBLK):
            pbk = psb[j % 2]
            kp = "psb%d" % (j % 2)
            for kc in range(8):
                cx.tr(pbk[:, kc * 128:(kc + 1) * 128], xg[b][:, j, kc * 128:(kc + 1) * 128], ident[:, :],
                      r=["xg%d" % b, "ident"], w=[kp])
            cx.copy(xT[b][:, :, j * 128:(j + 1) * 128], pbk[:, :].rearrange("p (k t) -> p k t", k=8), r=[kp], w=["xTe%d" % b],
                    eng=("act" if j % 2 == 0 else "dve"))
        it = 0
        for fc in range(4):
            fs = slice(fc * 128, (fc + 1) * 128)
            for pc in range(NPC):
                cs_ = slice(pc * PW, (pc + 1) * PW)
                pa = ps[(it % 2) * 2]; pb = ps[(it % 2) * 2 + 1]
                ka = "ps%d" % ((it % 2) * 2); kb = "ps%d" % ((it % 2) * 2 + 1)
                for kc in range(8):
                    cx.mm(pa[:, 0:PW], W1[b][:, kc, fs], xT[b][:, kc, cs_], kc == 0, kc == 7, r=["xTe%d" % b, "W1_%d" % b], w=[ka])
                for kc in range(8):
                    cx.mm(pb[:, 0:PW], W3[b][:, kc, fs], xT[b][:, kc, cs_], kc == 0, kc == 7, r=["xTe%d" % b, "W3_%d" % b], w=[kb])
                sb_ = s1[it % 2]
                cx.act(sb_[:, 0:PW], pa[:, 0:PW], AF.Silu, r=[ka], w=["s1_%d" % (it % 2)])
                cx.tt(uT[:, fc, cs_], pb[:, 0:PW], sb_[:, 0:PW], ALU.mult, r=[kb, "s1_%d" % (it % 2)], w=["uTe"])
                it += 1
        for j in range(NBLK):
            y = yb[yi % 2]
            ky = "ybe%d" % (yi % 2)
            yi += 1
            for nh in range(2):
                po = ps[4 + nh]
                for fc in range(4):
                    cx.mm(po[:, :], uT[:, fc, j * 128:(j + 1) * 128], W2[b][:, fc, nh * 512:(nh + 1) * 512], fc == 0, fc == 3,
                          r=["uTe", "W2_%d" % b], w=["ps%d" % (4 + nh)])
                cx.copy(y[:, nh * 512:(nh + 1) * 512], po[:, :], r=["ps%d" % (4 + nh)], w=[ky], eng=("act" if nh == 0 else "dve"))
            cx.dma("sp", Yg[e * C + j * 128:e * C + (j + 1) * 128, :], y[:, :], r=[ky], w=["D:Yg%d_%d" % (e, j)])
    if final:
        gfb = cx.sb("gfb", [128, D])
        cx.dma("sp", gfb[:, :], dr["g_final"][0, :].partition_broadcast(128), w=["gfb"])
    cx.memset(dummy[:, 1:2], 0.0, w=["YgAll"], eng="pool")
    cx.S.ops[-1].deps.update({cx.S.lastw[k]: True for k in ["D:Yg%d_%d" % (e_, j_) for e_ in range(NEXP) for j_ in range(C // 128)]})
    yg = [[cx.sb("yg%d_%d" % (i, q), [128, D]) for q in range(2)] for i in range(2)]
    for i in range(2):
        for q in range(2):
            cx.memset(yg[i][q][:, :], 0.0, w=["yg%d_%d" % (i, q)], eng="pool")
    cur = None
    for ti, (tag, seg, xmid, xout, rows) in enumerate(tiles):
        if cur != seg:
            cx.dma("sp", g2b[:, :], dr["modv"][l, seg, 5, :].partition_broadcast(128), r=["D:modv%d" % l], w=["g2b2"])
            cur = seg
        b = ti % 2
        kx = "ext%d" % b
        t1 = t1s[b]
        kt1 = "et1_%d" % b
        cx.dma("sp", xt[b][:, :], xmid[rows, :], r=["D:xmid_" + tag], w=[kx])
        for q in range(2):
            dst = yg[b][q][:, :]
            idx = sli[:, ti, q:q + 1]
            cx.S.add("pool", lambda h, idx=idx, dst=dst: h.indirect_dma_start(
                out=dst, out_offset=None, in_=Yg[:, :], in_offset=bass.IndirectOffsetOnAxis(ap=idx, axis=0),
                bounds_check=bc(h), oob_is_err=False), r=["sli", "YgAll"], w=["yg%d_%d" % (b, q)], dma=True)
        cx.ts(t1[:, :], yg[b][0][:, :], wts[:, ti, 0:1], ALU.mult, r=["yg%d_0" % b, "wtsq0"], w=[kt1])
        cx.stt(t1[:, :], yg[b][1][:, :], wts[:, ti, 1:2], t1[:, :], ALU.mult, ALU.add, r=["yg%d_1" % b, "wtsq1", kt1], w=[kt1])
        cx.tt(t1[:, :], t1[:, :], g2b[:, :], ALU.mult, r=[kt1, "g2b2"], w=[kt1])
        cx.tt(xt[b][:, :], xt[b][:, :], t1[:, :], ALU.add, r=[kx, kt1], w=[kx])
        if final:
            cx.act(junk[:, :], xt[b][:, :], AF.Square, accum_out=ssq[:, b:b + 1], r=[kx], w=["ejunk", "essq%d" % b])
            cx.act(rstd[:, b:b + 1], ssq[:, b:b + 1], AF.Sqrt, scale=1.0 / D, bias=EPS, r=["essq%d" % b], w=["ers%d" % b])
            cx.recip(rstd[:, b:b + 1], rstd[:, b:b + 1], r=["ers%d" % b], w=["ers%d" % b])
            cx.stt(xt[b][:, :], xt[b][:, :], rstd[:, b:b + 1], gfb[:, :], ALU.mult, ALU.mult, r=[kx, "ers%d" % b, "gfb"], w=[kx])
        cx.dma("sp", xout[rows, :], xt[b][:, :], r=[kx], w=["D:xout_" + tag])
    cx.end()


def make_expo(core):
    BIG = 1.0e7
    e = np.full((2, 9), BIG, np.float32)
    for c2 in range(NCORE):
        if c2 < core:
            e[0, c2] = TL * (core - 1 - c2)
        if c2 > core:
            e[1, c2] = TL * (c2 - core - 1)
    e[0, 8] = TL * core
    e[1, 8] = TL * (NCORE - 1 - core)
    return np.broadcast_to(e[None], (128, 2, 9)).copy()


def make_expo(core):
    BIG = 1.0e7
    e = np.full((2, 9), BIG, np.float32)
    for c2 in range(NCORE):
        if c2 < core:
            e[0, c2] = TL * (core - 1 - c2)
        if c2 > core:
            e[1, c2] = TL * (c2 - core - 1)
    e[0, 8] = TL * core
    e[1, 8] = TL * (NCORE - 1 - core)
    return np.broadcast_to(e[None], (128, 2, 9)).copy()


def make_sel(core):
    s = np.zeros((128, 2, NCORE), np.float32)
    if core > 0:
        s[:, 0, core - 1] = 1.0
    if core < NCORE - 1:
        s[:, 1, core + 1] = 1.0
    return s


def phase_exchange(cx, l, dr):
    cx.begin(nf=0, nb=0)
    hin = dr["hin"]
    for a, (src, c2) in enumerate(((dr["uT_l"], 0), (dr["uT_l"], 1), (dr["tT_l"], 0), (dr["tT_l"], 1))):
        cx.dma("sp", hin[a * 128:(a + 1) * 128, 0:16], src[c2, :, 0:16], r=["D:uT_l", "D:tT_l"], w=["D:hin"])
        cx.dma("sp", hin[a * 128:(a + 1) * 128, 16:32], src[c2, :, TL - 16:TL], r=["D:uT_l", "D:tT_l"], w=["D:hin"])
    grp = [list(range(NCORE))]

    def cc(src, dst, rk, wk):
        cx.S.add("pool", lambda h: h.collective_compute("AllGather", ALU.bypass, replica_groups=grp, ins=[src], outs=[dst]),
                 r=rk, w=wk, cc=True)
    cc(dr["kT_l"].rearrange("c p t -> (c p) t").opt(), dr["gk"].opt(), ["D:rope_l12", "D:rope_l13"], ["D:gk"])
    cc(dr["V_l"].opt(), dr["gv"].opt(), ["D:V_l"], ["D:gv"])
    cc(dr["Tst_l"].rearrange("a p e -> (a p) e").opt(), dr["gt"].opt(), ["D:Tst_l"], ["D:gt"])
    cc(hin.opt(), dr["hg"].opt(), ["D:hin"], ["D:hg"])
    cx.end()


A_OUT = (("hT", lambda T: [128, 8, T], BF16), ("uT", lambda T: [2, 128, T], F32), ("tT", lambda T: [2, 128, T], F32),
         ("bgT", lambda T: [2, 128, T], BF16), ("qT", lambda T: [2, 128, T], BF16), ("kT", lambda T: [2, 128, T], BF16),
         ("rqT", lambda T: [128, T], BF16), ("rkT", lambda T: [128, T], BF16), ("V", lambda T: [T, 256], BF16),
         ("rv", lambda T: [T, 256], BF16), ("rg", lambda T: [T, 256], BF16), ("Tst", lambda T: [2, 128, 256], F32))
SEGT = (("l", TL), ("c", TC))
NKALL = (SEQ + TC) // 128
EXT_IN = (("c", [1, D]), ("c_ctx", [1, D]), ("w_mod", [2, D, 6 * D]), ("b_mod", [2, 6 * D]), ("g_norm1", [2, D]), ("g_norm2", [2, D]),
          ("w_in", [2, D, INC]), ("conv_a_w", [2, 31, 256]), ("conv_a_b", [2, 256]), ("conv_a_g", [2, 256]),
          ("conv_a_beta", [2, 256]), ("conv_b_w", [2, 3, 256]), ("lam_q1", [2, 32]), ("lam_k1", [2, 32]), ("lam_q2", [2, 32]),
          ("lam_k2", [2, 32]), ("diff_g", [2, 64]), ("ret_ld_f", [2, 4]), ("ret_ld_b", [2, 4]), ("w_gate", [2, D, 4096]),
          ("b_gate", [2, 4096]), ("w_branch", [2, 4, 256, D]), ("w_o", [2, D, D]), ("w_router", [D, 16]), ("b_router", [1, 16]),
          ("w1_e", [2, NEXP, D, DFF]), ("w3_e", [2, NEXP, D, DFF]), ("w2_e", [2, NEXP, DFF, D]), ("g_final", [1, D]))


SPARSE_MOE = True


def moe_phase(cx, l, dr, segs, final, wl=None):
    if SPARSE_MOE:
        return phase_moe_sparse(cx, l, dr, segs, final, wl=wl)
    return phase_moe(cx, l, dr, segs, final, wl=wl)


def lam_init_of(l):
    return 0.8 - 0.6 * math.exp(-0.3 * l)


class Launch:
    def __init__(self):
        self.nc = bass.Bass("TRN2", target_bir_lowering=False)
        self.dr = {}
        self.ins = []
        self.outs = []

    def t(self, name, shape, dt=F32, kind=None):
        if kind is None:
            self.dr[name] = self.nc.dram_tensor(name, list(shape), dt).ap()
        else:
            self.dr[name] = self.nc.dram_tensor(name, list(shape), dt, kind=kind).ap()
        if kind == "ExternalInput":
            self.ins.append(name)
        elif kind == "ExternalOutput":
            self.outs.append(name)


def build_fused():
    L = Launch()
    L.t("cst", [128, CSTW], F32, "ExternalInput")
    L.t("ident", [128, 128], BF16, "ExternalInput")
    for n_, s_ in EXT_IN:
        L.t(n_, s_, F32, "ExternalInput")
    for n_, s_ in (("cosT", [128, TL]), ("sinT", [128, TL]), ("x_l", [TL, D]), ("x_c", [TC, D]), ("expo", [128, 2, 9]),
                   ("sel", [128, 2, NCORE])):
        L.t(n_, s_, F32, "ExternalInput")
    L.t("out", [TL, D], F32, "ExternalOutput")
    L.t("modv", [2, 2, 6, D])
    L.t("x1_l", [TL, D])
    L.t("x1_c", [TC, D])
    L.t("Xg", [NSLOT, D], BF16)
    L.t("Yg", [NSLOT, D], F32)
    L.t("H2d", [TL + TC, D], BF16)
    drl = []
    for l in range(DEPTH):
        d_ = dict(L.dr)
        for tag, T in SEGT:
            for nm, shp, dt in A_OUT:
                L.t("%s_%s%d" % (nm, tag, l), shp(T), dt)
                d_[nm + "_" + tag] = L.dr["%s_%s%d" % (nm, tag, l)]
            L.t("ysT_%s%d" % (tag, l), [8, 128, T], BF16)
            L.t("xmid_%s%d" % (tag, l), [T, D])
            d_["ysT_" + tag] = L.dr["ysT_%s%d" % (tag, l)]
            d_["xmid_" + tag] = L.dr["xmid_%s%d" % (tag, l)]
        for nm, shp, dt in (("gk", [NCORE * 256, TL], BF16), ("gv", [NCORE * TL, 256], BF16), ("gt", [NCORE * 256, 256], F32),
                            ("hg", [NCORE * 512, 32], F32), ("hin", [512, 32], F32)):
            L.t("%s%d" % (nm, l), shp, dt)
            d_[nm] = L.dr["%s%d" % (nm, l)]
        drl.append(d_)
    for d_ in drl:
        for k in ("modv", "x1_l", "x1_c", "Xg", "Yg", "H2d"):
            d_[k] = L.dr[k]
    with ExitStack() as st:
        S = Sched(L.nc, st)
        cx = Ctx(L.nc, S)
        phase_mods(cx, 0, drl[0])
        phase_mods(cx, 1, drl[0])
        d0 = drl[0]
        phase_a(cx, 0, d0, [("l", TL, L.dr["x_l"]), ("c", TC, L.dr["x_c"])])
        phase_exchange(cx, 0, d0)
        phase_conv(cx, 0, d0, [("l", TL), ("c", TC)])
        phase_attn(cx, 0, d0, [("l", TL, NKALL), ("c", TC, TC // 128)], lam_init_of(0))
        phase_ret(cx, 0, d0, [("l", TL), ("c", TC)])
        phase_merge(cx, 0, d0, [("l", TL, L.dr["x_l"], d0["xmid_l"]), ("c", TC, L.dr["x_c"], d0["xmid_c"])])
        moe_phase(cx, 0, d0, [("l", TL, d0["xmid_l"], L.dr["x1_l"]), ("c", TC, d0["xmid_c"], L.dr["x1_c"])], False)
        d1 = drl[1]
        phase_a(cx, 1, d1, [("l", TL, L.dr["x1_l"]), ("c", TC, L.dr["x1_c"])])
        phase_exchange(cx, 1, d1)
        phase_conv(cx, 1, d1, [("l", TL)])
        phase_attn(cx, 1, d1, [("l", TL, NKALL)], lam_init_of(1))
        phase_ret(cx, 1, d1, [("l", TL)])
        phase_merge(cx, 1, d1, [("l", TL, L.dr["x1_l"], d1["xmid_l"])])
        moe_phase(cx, 1, d1, [("l", TL, d1["xmid_l"], L.dr["out"])], True)
    return L


def kernel_fused(**inp):
    f32 = lambda a: np.ascontiguousarray(np.asarray(a, dtype=np.float32))
    x = f32(inp["x"])[0]
    ctx = f32(inp["ctx"])[0]
    base = dict(cst=make_cst(), ident=np.eye(128, dtype=np.float32).astype(NPBF), x_c=ctx)
    for n_, s_ in EXT_IN:
        base[n_] = f32(inp[n_]).reshape(s_)
    L = build_fused()
    maps = []
    for c in range(NCORE):
        m = dict(base)
        cosT, sinT = rope_tables(c)
        m.update(cosT=cosT, sinT=sinT, x_l=x[c * TL:(c + 1) * TL], expo=make_expo(c), sel=make_sel(c))
        maps.append({k: m[k] for k in L.ins})
    res = run_bass_kernel_spmd(L.nc, maps, core_ids=list(range(NCORE)))
    out = np.concatenate([np.asarray(res.results[c]["out"]) for c in range(NCORE)], axis=0)
    return out.reshape(1, SEQ, D).astype(np.float32)


WSLICE = ("w_in", "w_gate", "w_branch", "w_o", "w1_e", "w3_e", "w2_e")
GATH = (("gk", [NCORE * 256, TL], BF16), ("gv", [NCORE * TL, 256], BF16), ("gt", [NCORE * 256, 256], F32),
        ("hg", [NCORE * 512, 32], F32))


def build_stage(stage):
    L = Launch()
    L.t("cst", [128, CSTW], F32, "ExternalInput")
    L.t("ident", [128, 128], BF16, "ExternalInput")
    for n_, s_ in EXT_IN:
        if stage > 1 and n_ in ("w_mod", "b_mod", "c", "c_ctx"):
            continue
        if stage == 1 and n_ in ("w_gate", "w_branch", "w_o", "w1_e", "w3_e", "w2_e"):
            continue
        if stage == 3 and n_ == "w_in":
            continue
        shp = [1] + list(s_[1:]) if n_ in WSLICE else s_
        L.t(n_, shp, F32, "ExternalInput")
    for n_, s_ in (("cosT", [128, TL]), ("sinT", [128, TL]), ("expo", [128, 2, 9]), ("sel", [128, 2, NCORE])):
        L.t(n_, s_, F32, "ExternalInput")
    io = "ExternalInput"
    if stage == 1:
        L.t("x_l", [TL, D], F32, io)
        L.t("x_c", [TC, D], F32, io)
        L.t("modv", [2, 2, 6, D], F32, "ExternalOutput")
    else:
        L.t("modv", [2, 2, 6, D], F32, io)

    def a_tensors(prefix, kind, tags):
        d_ = {}
        for tag, T in SEGT:
            if tag not in tags:
                continue
            for nm, shp, dt in A_OUT:
                L.t(prefix + nm + "_" + tag, shp(T), dt, kind)
                d_[nm + "_" + tag] = L.dr[prefix + nm + "_" + tag]
        return d_
    with ExitStack() as st:
        S = Sched(L.nc, st)
        cx = Ctx(L.nc, S)
        if stage == 1:
            dA = dict(L.dr)
            dA.update(a_tensors("", "ExternalOutput", ("l", "c")))
            phase_mods(cx, 0, dA)
            phase_mods(cx, 1, dA)
            phase_a(cx, 0, dA, [("l", TL, L.dr["x_l"]), ("c", TC, L.dr["x_c"])], wl=0)
        else:
            l = stage - 2
            tags = ("l", "c")
            for nm, shp, dt in GATH:
                L.t(nm, shp, dt, io)
            L.t("Xg", [NSLOT, D], BF16)
            L.t("Yg", [NSLOT, D], F32)
            L.t("H2d", [TL + TC, D], BF16)
            dB = dict(L.dr)
            dB.update(a_tensors("b_", io, tags))
            segs = [("l", TL), ("c", TC)] if l == 0 else [("l", TL)]
            for tag, T in segs:
                L.t("ysT_" + tag, [8, 128, T], BF16)
                L.t("xmid_" + tag, [T, D])
                dB["ysT_" + tag] = L.dr["ysT_" + tag]
                dB["xmid_" + tag] = L.dr["xmid_" + tag]
            if l == 0:
                L.t("x_l", [TL, D], F32, io)
                L.t("x_c", [TC, D], F32, io)
                L.t("x1_l", [TL, D], F32, "ExternalOutput")
                L.t("x1_c", [TC, D])
                xin_l, xin_c, xo_l, xo_c = L.dr["x_l"], L.dr["x_c"], L.dr["x1_l"], L.dr["x1_c"]
            else:
                L.t("x1_l", [TL, D], F32, io)
                L.t("out", [TL, D], F32, "ExternalOutput")
                xin_l, xo_l = L.dr["x1_l"], L.dr["out"]
            phase_conv(cx, l, dB, segs)
            phase_attn(cx, l, dB, [("l", TL, NKALL)] + ([("c", TC, TC // 128)] if l == 0 else []), lam_init_of(l))
            phase_ret(cx, l, dB, segs)
            if l == 0:
                phase_merge(cx, l, dB, [("l", TL, xin_l, dB["xmid_l"]), ("c", TC, xin_c, dB["xmid_c"])], wl=0)
                moe_phase(cx, l, dB, [("l", TL, dB["xmid_l"], xo_l), ("c", TC, dB["xmid_c"], xo_c)], False, wl=0)
                dA = dict(L.dr)
                dA.update(a_tensors("", "ExternalOutput", ("l", "c")))
                phase_a(cx, 1, dA, [("l", TL, xo_l), ("c", TC, xo_c)], wl=0)
            else:
                phase_merge(cx, l, dB, [("l", TL, xin_l, dB["xmid_l"])], wl=0)
                moe_phase(cx, l, dB, [("l", TL, dB["xmid_l"], xo_l)], True, wl=0)
    return L


def host_gather(oA):
    gk = np.concatenate([np.asarray(o["kT_l"]).reshape(256, TL) for o in oA], axis=0)
    gv = np.concatenate([np.asarray(o["V_l"]) for o in oA], axis=0)
    gt = np.concatenate([np.asarray(o["Tst_l"]).reshape(256, 256) for o in oA], axis=0)
    hs = []
    for o in oA:
        u = np.asarray(o["uT_l"])
        t = np.asarray(o["tT_l"])
        h = np.concatenate([np.concatenate([a[c2][:, 0:16], a[c2][:, TL - 16:TL]], axis=1) for a in (u, t) for c2 in range(2)], axis=0)
        hs.append(h)
    hg = np.concatenate(hs, axis=0).astype(np.float32)
    return dict(gk=gk, gv=gv, gt=gt, hg=hg)


def kernel_unfused(**inp):
    f32 = lambda a: np.ascontiguousarray(np.asarray(a, dtype=np.float32))
    x = f32(inp["x"])[0]
    ctx = f32(inp["ctx"])[0]
    ropes = [rope_tables(c) for c in range(NCORE)]
    full = {n_: f32(inp[n_]).reshape(s_) for n_, s_ in EXT_IN}
    cst = make_cst()
    ident = np.eye(128, dtype=np.float32).astype(NPBF)

    def run(L, extra, wl):
        maps = []
        for c in range(NCORE):
            m = dict(cst=cst, ident=ident, cosT=ropes[c][0], sinT=ropes[c][1], expo=make_expo(c), sel=make_sel(c),
                     x_l=x[c * TL:(c + 1) * TL], x_c=ctx)
            for k, v in full.items():
                m[k] = v[wl[k]:wl[k] + 1] if k in WSLICE else v
            m.update(extra[c])
            maps.append({k: m[k] for k in L.ins})
        res = run_bass_kernel_spmd(L.nc, maps, core_ids=list(range(NCORE)))
        return [{k: np.asarray(r[k]) for k in L.outs} for r in res.results]

    o1 = run(build_stage(1), [dict() for _ in range(NCORE)], dict.fromkeys(WSLICE, 0))
    modv = o1[0]["modv"]

    def b_extra(oA):
        g = host_gather(oA)
        ex = []
        for c in range(NCORE):
            e = dict(g)
            e["modv"] = modv
            for k, v in oA[c].items():
                if k != "modv" and k != "x1_l":
                    e["b_" + k] = v
            ex.append(e)
        return ex
    wl2 = dict.fromkeys(WSLICE, 0)
    wl2["w_in"] = 1
    o2 = run(build_stage(2), b_extra(o1), wl2)
    ex3 = b_extra(o2)
    for c in range(NCORE):
        ex3[c]["x1_l"] = o2[c]["x1_l"]
    o3 = run(build_stage(3), ex3, dict.fromkeys(WSLICE, 1))
    out = np.concatenate([o3[c]["out"] for c in range(NCORE)], axis=0)
    return out.reshape(1, SEQ, D).astype(np.float32)


FUSED = False


def kernel(**inp):
    return kernel_fused(**inp) if FUSED else kernel_unfused(**inp)
```

```python
import math
from contextlib import ExitStack
import numpy as np
import ml_dtypes
import concourse.bass as bass
import concourse.mybir as mybir
from concourse.bass_utils import run_bass_kernel_spmd

F32 = mybir.dt.float32
BF16 = mybir.dt.bfloat16
AF = mybir.ActivationFunctionType
ALU = mybir.AluOpType
AX = mybir.AxisListType
NPBF = ml_dtypes.bfloat16

NCORE = 8
D = 1024
SEQ = 16384
TL = SEQ // NCORE
TC = 256
DEPTH = 2
INC = 2816
EPS = 1e-6
NEXP = 16
DFF = 512
QSCALE = 32 ** -0.5
ATT_ROW = False
MOE_CAP = 768
NSLOT = NEXP * MOE_CAP


class Op:
    __slots__ = ("id", "eng", "fn", "deps", "dma", "n", "signal", "val", "cc")


class Sched:
    ENGS = (("sp", "sync"), ("act", "scalar"), ("dve", "vector"), ("pool", "gpsimd"), ("pe", "tensor"))

    def __init__(self, nc, stack):
        self.nc = nc
        self.ops = []
        self.phase_start = 0
        self.lastw = {}
        self.readers = {}
        self.K = dict(sp=12, pool=8, act=4)
        self.dma_list = {e: [] for e in self.K}
        self.sems = {e: stack.enter_context(nc.semaphore("sm_" + e)) for e in ("pe", "act", "dve", "pool")}
        self.dsems = {e: [stack.enter_context(nc.semaphore("sd_%s%d" % (e, i))) for i in range(k)]
                      for e, k in self.K.items()}
        self.ccsems = [stack.enter_context(nc.semaphore("sc_%d" % i)) for i in range(12)]
        self.ncc = 0
        self.cc_list = []
        self.cnt = {e: 0 for e in self.sems}
        self.waited = {e: {} for e, _ in self.ENGS}

    def add(self, eng, fn, r=(), w=(), dma=False, cc=False):
        i = len(self.ops)
        deps = {}
        for k in r:
            j = self.lastw.get(k)
            if j is not None:
                deps[j] = True
        for k in w:
            j = self.lastw.get(k)
            if j is not None and j not in deps:
                deps[j] = False
            rd = self.readers.get(k)
            if rd:
                for j in rd.values():
                    if isinstance(j, list):
                        for jj in j:
                            deps.setdefault(jj, False)
                    else:
                        deps.setdefault(j, False)
        n = None
        if dma:
            lst = self.dma_list[eng]
            n = len(lst)
            if n >= self.K[eng]:
                deps.setdefault(lst[n - self.K[eng]], False)
            lst.append(i)
        op = Op()
        op.id, op.eng, op.fn, op.deps, op.dma, op.n, op.signal, op.val = i, eng, fn, deps, dma, n, False, 0
        op.cc = None
        if cc:
            op.dma = True
            op.cc = self.ncc
            self.ncc += 1
            self.cc_list.append(i)
            dma = True
        self.ops.append(op)
        for k in r:
            rd = self.readers.setdefault(k, {})
            if dma:
                rd.setdefault("dma", []).append(i)
            else:
                rd[eng] = i
        for k in w:
            self.lastw[k] = i
            self.readers[k] = {}
        return i

    def _needed(self, op, dj, raw):
        if dj.dma:
            return True
        if dj.eng == op.eng:
            if op.dma:
                return True
            return raw and op.eng != "pe"
        return True

    def end_phase(self, name=None):
        nc = self.nc
        ps = self.phase_start
        for e in self.K:
            lst = [i for i in self.dma_list[e][-self.K[e]:] if i >= ps]
            if e == "pool":
                lst = lst + [i for i in self.cc_list if i >= ps]
            if lst:
                i = self.add(e, lambda h: h.nop())
                for j in lst:
                    self.ops[i].deps[j] = True
        ops = self.ops
        for op in ops[ps:]:
            latest = {}
            for j, raw in op.deps.items():
                if j < ps:
                    continue
                dj = ops[j]
                if not dj.dma and self._needed(op, dj, raw):
                    if latest.get(dj.eng, -1) < j:
                        latest[dj.eng] = j
            for j in latest.values():
                ops[j].signal = True
        for op in ops[ps:]:
            if op.signal and not op.dma:
                self.cnt[op.eng] += 1
                op.val = self.cnt[op.eng]
        with nc.Block() as block:
            for e, bn in self.ENGS:
                ops_e = [op for op in ops[ps:] if op.eng == e]
                if not ops_e:
                    continue

                def body(h, ops_e=ops_e, e=e):
                    self._emit(e, h, ops_e, ps)
                getattr(block, bn)(body)
        self.phase_start = len(ops)
        self.lastw = {k: v for k, v in self.lastw.items() if isinstance(k, str) and k.startswith("D:")}
        self.readers = {k: {} for k in self.lastw}
        for op in ops[:self.phase_start]:
            op.fn = None

    def _emit(self, e, h, ops_e, ps):
        ops = self.ops
        waited = self.waited[e]
        for op in ops_e:
            want = {}
            for j, raw in op.deps.items():
                if j < ps:
                    continue
                dj = ops[j]
                if not self._needed(op, dj, raw):
                    continue
                if dj.cc is not None:
                    key = ("cc", dj.cc)
                    sem = self.ccsems[dj.cc]
                    val = 1
                elif dj.dma:
                    K = self.K[dj.eng]
                    key = (dj.eng, dj.n % K)
                    sem = self.dsems[dj.eng][dj.n % K]
                    val = 16 * (dj.n // K + 1)
                else:
                    key = dj.eng
                    sem = self.sems[dj.eng]
                    if key in want and want[key][2] > j:
                        continue
                    want[key] = (sem, dj.val, j)
                    continue
                if key not in want or want[key][1] < val:
                    want[key] = (sem, val, j)
            for key, (sem, val, _j) in want.items():
                if waited.get(key, 0) >= val:
                    continue
                h.wait_ge(sem, val)
                waited[key] = val
            inst = op.fn(h)
            if op.cc is not None:
                inst.then_inc(self.ccsems[op.cc])
            elif op.dma:
                inst.then_inc(self.dsems[e][op.n % self.K[e]], 16)
            elif op.signal:
                inst.then_inc(self.sems[e], 1)


class Ctx:
    def __init__(self, nc, S):
        self.nc = nc
        self.S = S
        self.stack = None
        self.uid = 0

    def psum(self, name, shape, dt=F32):
        self.uid += 1
        return self.stack.enter_context(self.nc.psum_tensor("%s_%d" % (name, self.uid), list(shape), dt))

    def begin(self, nf=8, nb=0):
        self.stack = ExitStack()
        self.ps = [self.stack.enter_context(self.nc.psum_tensor("ps%d_%d" % (i, self.uid), [128, 512], F32))
                   for i in range(nf)]
        self.psb = [self.stack.enter_context(self.nc.psum_tensor("psb%d_%d" % (i, self.uid), [128, 1024], BF16))
                    for i in range(nb)]
        self.uid += 1

    def end(self):
        self.S.end_phase()
        self.stack.close()
        self.stack = None

    def sb(self, name, shape, dt=F32):
        self.uid += 1
        return self.stack.enter_context(self.nc.sbuf_tensor("%s_%d" % (name, self.uid), list(shape), dt))

    def dma(self, eng, out, in_, r=(), w=(), **kw):
        return self.S.add(eng, lambda h: h.dma_start(out=out, in_=in_, **kw), r=r, w=w, dma=True)

    def mm(self, out, lhsT, rhs, start, stop, r=(), w=(), **kw):
        return self.S.add("pe", lambda h: h.matmul(out, lhsT, rhs, start=start, stop=stop, **kw), r=r, w=w)

    def tr(self, out, in_, ident, r=(), w=()):
        return self.S.add("pe", lambda h: h.transpose(out, in_, ident), r=r, w=w)

    def act(self, out, in_, func, r=(), w=(), eng="act", **kw):
        return self.S.add(eng, lambda h: h.activation(out=out, in_=in_, func=func, **kw), r=r, w=w)

    def tt(self, out, in0, in1, op, r=(), w=(), eng="dve"):
        return self.S.add(eng, lambda h: h.tensor_tensor(out=out, in0=in0, in1=in1, op=op), r=r, w=w)

    def ts(self, out, in0, s1, op0, s2=None, op1=None, r=(), w=(), eng="dve", **kw):
        if op1 is None:
            return self.S.add(eng, lambda h: h.tensor_scalar(out=out, in0=in0, scalar1=s1, scalar2=None, op0=op0, **kw),
                              r=r, w=w)
        return self.S.add(eng, lambda h: h.tensor_scalar(out=out, in0=in0, scalar1=s1, scalar2=s2, op0=op0, op1=op1, **kw),
                          r=r, w=w)

    def stt(self, out, in0, scalar, in1, op0, op1, r=(), w=()):
        return self.S.add("dve", lambda h: h.scalar_tensor_tensor(out=out, in0=in0, scalar=scalar, in1=in1,
                                                                    op0=op0, op1=op1), r=r, w=w)

    def copy(self, out, in_, r=(), w=(), eng="dve"):
        if eng == "act":
            return self.S.add("act", lambda h: h.activation(out=out, in_=in_, func=AF.Copy), r=r, w=w)
        return self.S.add(eng, lambda h: h.tensor_copy(out=out, in_=in_), r=r, w=w)

    def memset(self, ap, val, w=(), eng="dve"):
        return self.S.add(eng, lambda h: h.memset(ap, val), w=w)

    def red(self, out, in_, op, r=(), w=(), axis=None):
        ax = AX.X if axis is None else axis
        return self.S.add("dve", lambda h: h.tensor_reduce(out=out, in_=in_, axis=ax, op=op), r=r, w=w)

    def recip(self, out, in_, r=(), w=()):
        return self.S.add("dve", lambda h: h.reciprocal(out=out, in_=in_), r=r, w=w)


def phase_mods(cx, l, dr):
    cx.begin()
    cv = cx.sb("cv", [128, 8, 2])
    sc = cx.sb("sc", [128, 8, 2])
    acc = cx.sb("macc", [2, 6144])
    bm = cx.sb("mbm", [2, 6144])
    g1b = cx.sb("g1b", [2, 1024])
    g2b = cx.sb("g2b", [2, 1024])
    mv = cx.sb("mv", [2, 6, 1024])
    wm = [cx.sb("wm%d" % i, [128, 6144]) for i in range(2)]
    for s, src in enumerate((dr["c"], dr["c_ctx"])):
        for kc in range(8):
            cx.dma("sp", cv[:, kc, s:s + 1], src[0:1, kc * 128:(kc + 1) * 128].rearrange("o p -> p o"), w=["cv"])
    cx.dma("sp", bm[:, :], dr["b_mod"][l, :].partition_broadcast(2), w=["bm"])
    cx.dma("sp", g1b[:, :], dr["g_norm1"][l, :].partition_broadcast(2), w=["g1b"])
    cx.dma("sp", g2b[:, :], dr["g_norm2"][l, :].partition_broadcast(2), w=["g2b"])
    cx.act(sc[:, :, :], cv[:, :, :], AF.Silu, r=["cv"], w=["sc"])
    for kc in range(8):
        b = kc % 2
        cx.dma("sp", wm[b][:, :], dr["w_mod"][l, kc * 128:(kc + 1) * 128, :], w=["wm%d" % b])
        for n in range(12):
            p = cx.ps[n % 4]
            cx.mm(p[0:2, :], sc[:, kc, :], wm[b][:, n * 512:(n + 1) * 512], True, True,
                  r=["sc", "wm%d" % b], w=["ps%d" % (n % 4)])
            a = acc[:, n * 512:(n + 1) * 512]
            if kc == 0:
                cx.tt(a, p[0:2, :], bm[:, n * 512:(n + 1) * 512], ALU.add, r=["ps%d" % (n % 4), "bm"], w=["macc%d" % n])
            else:
                cx.tt(a, p[0:2, :], a, ALU.add, r=["ps%d" % (n % 4), "macc%d" % n], w=["macc%d" % n])
    allacc = ["macc%d" % n for n in range(12)]
    cx.stt(mv[:, 0, :], acc[:, 1024:2048], 1.0, g1b[:, :], ALU.add, ALU.mult, r=allacc + ["g1b"], w=["mv0"])
    cx.copy(mv[:, 1, :], acc[:, 0:1024], r=allacc, w=["mv1"])
    cx.copy(mv[:, 2, :], acc[:, 2048:3072], r=allacc, w=["mv2"])
    cx.stt(mv[:, 3, :], acc[:, 4096:5120], 1.0, g2b[:, :], ALU.add, ALU.mult, r=allacc + ["g2b"], w=["mv3"])
    cx.copy(mv[:, 4, :], acc[:, 3072:4096], r=allacc, w=["mv4"])
    cx.copy(mv[:, 5, :], acc[:, 5120:6144], r=allacc, w=["mv5"])
    cx.dma("sp", dr["modv"][l, :, :, :], mv[:, :, :], r=["mv%d" % i for i in range(6)], w=["D:modv%d" % l])
    cx.end()


CST = {}
_off = 0
for _n, _w in (("c127mj", 1), ("cj", 1), ("ef_l", 16), ("eb_l", 16), ("ef_c", 2), ("eb_c", 2), ("ip1", 128),
               ("m128i", 128), ("D1", 128), ("D2", 128), ("U", 128), ("Lo", 128), ("I2", 128), ("ones", 128), ("I1", 128), ("hm", 4), ("bm8", 8), ("Ltri", 128), ("eoff", 16)):
    CST[_n] = (_off, _off + _w)
    _off += _w
CSTW = _off


def make_cst():
    c = np.zeros((128, CSTW), np.float32)
    p = np.arange(128, dtype=np.float32)
    i = np.arange(128, dtype=np.float32)

    def put(n, v):
        a, b = CST[n]
        c[:, a:b] = v
    put("c127mj", (127 - p)[:, None])
    put("cj", p[:, None])
    put("ef_l", (128.0 * (15 - np.arange(16)))[None, :])
    put("eb_l", (128.0 * np.arange(16))[None, :])
    put("ef_c", (128.0 * (1 - np.arange(2)))[None, :])
    put("eb_c", (128.0 * np.arange(2))[None, :])
    put("ip1", (i + 1)[None, :])
    put("m128i", (128 - i)[None, :])
    dd = i[None, :] - p[:, None]
    put("D1", np.maximum(dd, 0))
    put("D2", np.maximum(-dd, 0))
    put("U", (dd > 0).astype(np.float32))
    put("Lo", (dd < 0).astype(np.float32))
    put("I2", 2.0 * (dd == 0))
    put("ones", 1.0)
    put("I1", (dd == 0).astype(np.float32))
    put("hm", (p[:, None] // 32 == np.arange(4)[None, :]).astype(np.float32))
    put("bm8", np.tile((p[:, None] // 32 == np.arange(4)[None, :]).astype(np.float32), (1, 2)))
    put("Ltri", (dd > 0).astype(np.float32))
    put("eoff", (float(MOE_CAP) * np.arange(16))[None, :])
    return c


def rope_tables(core):
    t = np.arange(core * TL, (core + 1) * TL)
    row = (t // 64).astype(np.float32)
    col = (t % 64).astype(np.float32)
    inv = (np.float32(10000.0) ** (-np.arange(8, dtype=np.float32) / np.float32(8))).astype(np.float32)
    ang = np.concatenate([row[:, None] * inv[None, :], col[:, None] * inv[None, :]], axis=1).astype(np.float32)
    cos = np.cos(ang).astype(np.float32).T
    sin = np.sin(ang).astype(np.float32).T
    return np.tile(cos, (8, 1)).copy(), np.tile(sin, (8, 1)).copy()


def cs(cst, name):
    a, b = CST[name]
    return cst[:, a:b]


def phase_a(cx, l, dr, segs, wl=None):
    wl = l if wl is None else wl
    cx.begin(nf=6, nb=2)
    ps, psb = cx.ps, cx.psb
    W = cx.sb("W", [128, 8, INC], BF16)
    WR = cx.sb("WR", [128, 8, 768], BF16)
    cst = cx.sb("cst", [128, CSTW])
    ident = cx.sb("ident", [128, 128], BF16)
    cx.dma("sp", cst[:, :], dr["cst"][:, :], w=["cst"])
    cx.dma("sp", ident[:, :], dr["ident"][:, :], w=["ident"])
    for kc in range(8):
        cx.dma("pool", W[:, kc, :], dr["w_in"][wl, kc * 128:(kc + 1) * 128, :], w=["W%d" % kc])
    for kc in range(8):
        cx.ts(W[:, kc, 2176:2304], W[:, kc, 2176:2304], QSCALE, ALU.mult, r=["W%d" % kc], w=["W%d" % kc], eng="pool")
        for (s0, n, o0) in ((1280, 512, 0), (2048, 256, 512)):
            src = W[:, kc, s0:s0 + n].rearrange("p (b t d) -> p b t d", t=2, d=16)
            dst = WR[:, kc, o0:o0 + n].rearrange("p (b t d) -> p b t d", t=2, d=16)
            cx.ts(dst[:, :, 0, :], src[:, :, 1, :], -1.0, ALU.mult, r=["W%d" % kc], w=["WR%d" % kc], eng="pool")
            cx.copy(dst[:, :, 1, :], src[:, :, 0, :], r=["W%d" % kc], w=["WR%d" % kc], eng="pool")
    Wk = ["W%d" % kc for kc in range(8)]
    WRk = ["WR%d" % kc for kc in range(8)]
    lgf = cx.sb("lgf", [128, 4]); lgb = cx.sb("lgb", [128, 4])
    lgfc = cx.sb("lgfc", [128, 1]); lgbc = cx.sb("lgbc", [128, 1])
    cx.dma("sp", lgf[:, :], dr["ret_ld_f"][l, :].partition_broadcast(128), w=["lgf"])
    cx.dma("sp", lgb[:, :], dr["ret_ld_b"][l, :].partition_broadcast(128), w=["lgb"])
    for h in range(4):
        cx.dma("sp", lgfc[32 * h:32 * h + 32, :], dr["ret_ld_f"][l, h:h + 1].partition_broadcast(32), w=["lgfc"])
        cx.dma("sp", lgbc[32 * h:32 * h + 32, :], dr["ret_ld_b"][l, h:h + 1].partition_broadcast(32), w=["lgbc"])
    kdf = cx.sb("kdf", [128, 4]); kdb = cx.sb("kdb", [128, 4])
    KDF = cx.sb("KDF", [128, 128]); KDB = cx.sb("KDB", [128, 128])
    cx.act(kdf[:, :], lgf[:, :], AF.Exp, scale=cs(cst, "c127mj"), r=["lgf", "cst"], w=["kdf"])
    cx.act(kdb[:, :], lgb[:, :], AF.Exp, scale=cs(cst, "cj"), r=["lgb", "cst"], w=["kdb"])
    for h in range(4):
        cx.ts(KDF[:, 32 * h:32 * h + 32], cs(cst, "ones")[:, 0:32], kdf[:, h:h + 1], ALU.mult, r=["kdf", "cst"], w=["KDF"])
        cx.ts(KDB[:, 32 * h:32 * h + 32], cs(cst, "ones")[:, 0:32], kdb[:, h:h + 1], ALU.mult, r=["kdb", "cst"], w=["KDB"])
    pw = {}
    for tag, nch in (("l", 16), ("c", 2)):
        pf = cx.sb("pwf" + tag, [128, nch]); pb = cx.sb("pwb" + tag, [128, nch])
        cx.act(pf[:, :], cs(cst, "ef_" + tag), AF.Exp, scale=lgfc[:, 0:1], r=["lgfc", "cst"], w=["pwf" + tag])
        cx.act(pb[:, :], cs(cst, "eb_" + tag), AF.Exp, scale=lgbc[:, 0:1], r=["lgbc", "cst"], w=["pwb" + tag])
        pw[tag] = (pf, pb)
    Cl = cx.sb("Cl", [128, TL]); Sl = cx.sb("Sl", [128, TL])
    cx.dma("sp", Cl[:, :], dr["cosT"][:, :], w=["Cl"])
    cx.dma("sp", Sl[:, :], dr["sinT"][:, :], w=["Sl"])
    xt = [cx.sb("xt%d" % i, [128, D]) for i in range(2)]
    junk = cx.sb("junk", [128, D], BF16)
    t1 = [cx.sb("t1_%d" % i, [128, D]) for i in range(2)]
    hb = [cx.sb("hb%d" % i, [128, D], BF16) for i in range(2)]
    ssq = cx.sb("ssq", [128, 4]); rstd = cx.sb("rstd", [128, 4])
    hTs = [cx.sb("hT%d" % i, [128, 8, 512], BF16) for i in range(2)]
    gcount = [0]
    gsb = cx.sb("gsb", [128, D]); shb = cx.sb("shb", [128, D])
    ev = [cx.sb("ev%d" % i, [128, 512]) for i in range(4)]
    evb = [cx.sb("evb%d" % i, [128, 512], BF16) for i in range(4)]
    rk_sb = cx.sb("rk_sb", [128, 512], BF16)
    vt = [cx.sb("vt%d" % i, [128, 512], BF16) for i in range(2)]
    rgt = [cx.sb("rgt%d" % i, [128, 256], BF16) for i in range(2)]
    kfb = [cx.sb("kfb%d" % i, [128, 256], BF16) for i in range(2)]
    Tst = cx.sb("Tst", [128, 2, 256])
    evi = [0]

    def nxt():
        evi[0] = (evi[0] + 1) % 4
        return evi[0]

    xi = 0
    for (tag, T, xd) in segs:
        sfx = "_" + tag
        seg = 0 if tag == "l" else 1
        cx.dma("sp", gsb[:, :], dr["modv"][l, seg, 0, :].partition_broadcast(128), r=["D:modv%d" % l], w=["gsb"])
        cx.dma("sp", shb[:, :], dr["modv"][l, seg, 1, :].partition_broadcast(128), r=["D:modv%d" % l], w=["shb"])
        G = min(512, T)
        for g in range(T // G):
            t0 = g * G
            nt = G // 128
            hT = hTs[gcount[0] % 2]
            kh = "hT%d" % (gcount[0] % 2)
            gcount[0] += 1
            for j in range(nt):
                b = xi % 2
                xi += 1
                kx = "xt%d" % b
                cx.dma("sp", xt[b][:, :], xd[t0 + j * 128:t0 + (j + 1) * 128, :], w=[kx])
                cx.act(junk[:, :], xt[b][:, :], AF.Square, accum_out=ssq[:, j:j + 1], r=[kx], w=["junk", "ssq%d" % j])
                cx.act(rstd[:, j:j + 1], ssq[:, j:j + 1], AF.Sqrt, scale=1.0 / D, bias=EPS, r=["ssq%d" % j], w=["rs%d" % j])
                cx.recip(rstd[:, j:j + 1], rstd[:, j:j + 1], r=["rs%d" % j], w=["rs%d" % j])
                cx.stt(t1[b][:, :], xt[b][:, :], rstd[:, j:j + 1], gsb[:, :], ALU.mult, ALU.mult,
                       r=[kx, "rs%d" % j, "gsb"], w=["t1_%d" % b])
                cx.tt(hb[b][:, :], t1[b][:, :], shb[:, :], ALU.add, r=["t1_%d" % b, "shb"], w=["hb%d" % b], eng="pool")
                for kc in range(8):
                    cx.tr(psb[0][:, kc * 128:(kc + 1) * 128], hb[b][:, kc * 128:(kc + 1) * 128], ident[:, :],
                          r=["hb%d" % b, "ident"], w=["psb0"])
                cx.copy(hT[:, :, j * 128:(j + 1) * 128], psb[0][:, :].rearrange("p (k t) -> p k t", k=8),
                        r=["psb0"], w=[kh], eng="act")
            cx.dma("sp", dr["hT" + sfx][:, :, t0:t0 + G], hT[:, :, 0:G], r=[kh], w=["D:hT" + sfx])

            def proj(cc, bank, rot=False):
                Wt = WR if rot else W
                for kc in range(8):
                    cx.mm(ps[bank][:, 0:G], Wt[:, kc, cc * 128:(cc + 1) * 128], hT[:, kc, 0:G], kc == 0, kc == 7,
                          r=[kh, (WRk if rot else Wk)[kc]], w=["ps%d" % bank])

            Cg = Cl[:, t0:t0 + G] if tag == "l" else None
            Sg = Sl[:, t0:t0 + G] if tag == "l" else None
            for c2 in range(2):
                e = nxt()
                proj(2 + c2, 0)
                cx.act(ev[e][:, 0:G], ps[0][:, 0:G], AF.Sigmoid, r=["ps0"], w=["ev%d" % e])
                proj(0 + c2, 1)
                cx.tt(ev[e][:, 0:G], ps[1][:, 0:G], ev[e][:, 0:G], ALU.mult, r=["ps1", "ev%d" % e], w=["ev%d" % e])
                cx.dma("sp", dr["uT" + sfx][c2, :, t0:t0 + G], ev[e][:, 0:G], r=["ev%d" % e], w=["D:uT" + sfx])
            for c2 in range(2):
                e = nxt()
                proj(4 + c2, 0)
                cx.copy(evb[e][:, 0:G], ps[0][:, 0:G], r=["ps0"], w=["evb%d" % e], eng="act")
                cx.dma("sp", dr["bgT" + sfx][c2, :, t0:t0 + G], evb[e][:, 0:G], r=["evb%d" % e], w=["D:bgT" + sfx])
                proj(6 + c2, 1)
                cx.copy(ev[e][:, 0:G], ps[1][:, 0:G], r=["ps1"], w=["ev%d" % e], eng="act")
                proj(8 + c2, 0)
                cx.tt(ev[e][:, 0:G], ps[0][:, 0:G], ev[e][:, 0:G], ALU.mult, r=["ps0", "ev%d" % e], w=["ev%d" % e])
                cx.dma("sp", dr["tT" + sfx][c2, :, t0:t0 + G], ev[e][:, 0:G], r=["ev%d" % e], w=["D:tT" + sfx])
            for (cc, ro, dst, keep) in ((10, 0, dr["qT" + sfx][0], None), (11, 1, dr["qT" + sfx][1], None),
                                        (12, 2, dr["kT" + sfx][0], None), (13, 3, dr["kT" + sfx][1], None),
                                        (16, 4, dr["rqT" + sfx], None), (17, 5, dr["rkT" + sfx], rk_sb)):
                e = nxt()
                ob = keep if keep is not None else evb[e]
                okey = "rk_sb" if keep is not None else "evb%d" % e
                proj(cc, 0)
                if tag == "l":
                    proj(ro, 2, rot=True)
                    e2 = nxt()
                    cx.tt(ev[e][:, 0:G], ps[0][:, 0:G], Cg, ALU.mult, r=["ps0", "Cl"], w=["ev%d" % e])
                    cx.tt(ev[e2][:, 0:G], ps[2][:, 0:G], Sg, ALU.mult, r=["ps2", "Sl"], w=["ev%d" % e2])
                    cx.tt(ob[:, 0:G], ev[e][:, 0:G], ev[e2][:, 0:G], ALU.add, r=["ev%d" % e, "ev%d" % e2], w=[okey], eng="pool")
                else:
                    cx.copy(ob[:, 0:G], ps[0][:, 0:G], r=["ps0"], w=[okey], eng="act")
                cx.dma("sp", dst[:, t0:t0 + G], ob[:, 0:G], r=[okey], w=["D:rope" + sfx + str(cc)])
            pf, pb = pw[tag]
            for j in range(nt):
                n = (t0 // 128) + j
                b = j % 2
                tsl = slice(j * 128, (j + 1) * 128)
                for kc in range(8):
                    cx.mm(ps[3][:, 0:256], hT[:, kc, tsl], W[:, kc, 1792:2048], kc == 0, kc == 7, r=[kh, Wk[kc]], w=["ps3"])
                for kc in range(8):
                    cx.mm(ps[3][:, 256:512], hT[:, kc, tsl], W[:, kc, 2304:2560], kc == 0, kc == 7, r=[kh, Wk[kc]], w=["ps3"])
                for kc in range(8):
                    cx.mm(ps[4][:, 0:256], hT[:, kc, tsl], W[:, kc, 2560:2816], kc == 0, kc == 7, r=[kh, Wk[kc]], w=["ps4"])
                cx.copy(vt[b][:, :], ps[3][:, :], r=["ps3", "ps3"], w=["vt%d" % b])
                cx.act(rgt[b][:, :], ps[4][:, 0:256], AF.Silu, r=["ps4"], w=["rgt%d" % b])
                rows = slice(t0 + j * 128, t0 + (j + 1) * 128)
                cx.dma("sp", dr["V" + sfx][rows, :], vt[b][:, 0:256], r=["vt%d" % b], w=["D:V" + sfx])
                cx.dma("sp", dr["rv" + sfx][rows, :], vt[b][:, 256:512], r=["vt%d" % b], w=["D:rv" + sfx])
                cx.dma("sp", dr["rg" + sfx][rows, :], rgt[b][:, :], r=["rgt%d" % b], w=["D:rg" + sfx])
                cx.tr(psb[1][:, 0:128], rk_sb[:, tsl], ident[:, :], r=["rk_sb", "ident"], w=["psb1"])
                cx.tt(kfb[b][:, 0:128], psb[1][:, 0:128], KDF[:, :], ALU.mult, r=["psb1", "KDF"], w=["kfb%d" % b])
                cx.tt(kfb[b][:, 128:256], psb[1][:, 0:128], KDB[:, :], ALU.mult, r=["psb1", "KDB"], w=["kfb%d" % b])
                cx.mm(ps[5][:, 0:256], kfb[b][:, 0:128], vt[b][:, 256:512], True, True, r=["kfb%d" % b, "vt%d" % b], w=["ps5"])
                cx.mm(ps[5][:, 256:512], kfb[b][:, 128:256], vt[b][:, 256:512], True, True, r=["kfb%d" % b, "vt%d" % b], w=["ps5"])
                if n == 0:
                    cx.ts(Tst[:, 0, :], ps[5][:, 0:256], pf[:, n:n + 1], ALU.mult, r=["ps5", "pwf" + tag], w=["Tf"])
                    cx.ts(Tst[:, 1, :], ps[5][:, 256:512], pb[:, n:n + 1], ALU.mult, r=["ps5", "pwb" + tag], w=["Tb"])
                else:
                    cx.stt(Tst[:, 0, :], ps[5][:, 0:256], pf[:, n:n + 1], Tst[:, 0, :], ALU.mult, ALU.add,
                           r=["ps5", "pwf" + tag, "Tf"], w=["Tf"])
                    cx.stt(Tst[:, 1, :], ps[5][:, 256:512], pb[:, n:n + 1], Tst[:, 1, :], ALU.mult, ALU.add,
                           r=["ps5", "pwb" + tag, "Tb"], w=["Tb"])
        cx.dma("sp", dr["Tst" + sfx].rearrange("a p e -> p a e"), Tst[:, :, :], r=["Tf", "Tb"], w=["D:Tst" + sfx])
    cx.end()


def phase_conv(cx, l, dr, segs):
    cx.begin(nf=8, nb=0)
    ps = cx.ps
    cst = cx.sb("cst", [128, CSTW])
    cx.dma("sp", cst[:, :], dr["cst"][:, :], w=["cst"])
    praw = cx.sb("praw", [40, 256])
    cx.memset(praw[:, :], 0.0, w=["praw"])
    cx.dma("sp", praw[0:31, :], dr["conv_a_w"][l, :, :], w=["praw"])
    cx.dma("sp", praw[31:32, :], dr["conv_a_b"][l:l + 1, :], w=["praw"])
    cx.dma("sp", praw[32:33, :], dr["conv_a_g"][l:l + 1, :], w=["praw"])
    cx.dma("sp", praw[33:34, :], dr["conv_a_beta"][l:l + 1, :], w=["praw"])
    cx.dma("sp", praw[34:37, :], dr["conv_b_w"][l, :, :], w=["praw"])
    par = cx.sb("par", [128, 2, 40])
    for c2 in range(2):
        cx.tr(ps[7][:, 0:40], praw[0:40, c2 * 128:(c2 + 1) * 128], cs(cst, "I1")[0:40, 0:40], r=["praw", "cst"], w=["ps7"])
        cx.copy(par[:, c2, :], ps[7][:, 0:40], r=["ps7"], w=["par"])
    onesm = cx.sb("onesm", [128, 128])
    cx.memset(onesm[:, :], 1.0 / 256.0, w=["onesm"])
    HG = cx.sb("HG", [128, NCORE, 4, 32]); selt = cx.sb("selt", [128, 2, NCORE])
    cx.dma("sp", HG[:, :, :, :], dr["hg"].rearrange("(r a p) w -> p r a w", r=NCORE, a=4), w=["HG"])
    cx.dma("sp", selt[:, :, :], dr["sel"][:, :, :], w=["selt"])
    for (tag, T) in segs:
        sfx = "_" + tag
        ue = cx.sb("ue" + tag, [128, 2, T + 32])
        te = cx.sb("te" + tag, [128, 2, T + 32])
        bg = cx.sb("bg" + tag, [128, 2, T], BF16)
        acc = cx.sb("acc" + tag, [128, 2, T])
        sq = cx.sb("sq" + tag, [128, 2, T])
        yb = cx.sb("yb" + tag, [128, 2, T], BF16)
        for c2 in range(2):
            cx.dma("sp", ue[:, c2, 16:16 + T], dr["uT" + sfx][c2, :, :], r=["D:uT" + sfx], w=["ue%d" % c2])
            cx.dma("sp", te[:, c2, 16:16 + T], dr["tT" + sfx][c2, :, :], r=["D:tT" + sfx], w=["te%d" % c2])
            cx.dma("sp", bg[:, c2, :], dr["bgT" + sfx][c2, :, :], r=["D:bgT" + sfx], w=["bg%d" % c2])
            if tag == "l":
                for a, (buf, k) in enumerate(((ue, "ue%d" % c2), (te, "te%d" % c2))):
                    ai = a * 2 + c2
                    for side, dst, src in ((0, slice(0, 16), slice(16, 32)), (1, slice(16 + T, 32 + T), slice(0, 16))):
                        for r_ in range(NCORE):
                            if r_ == 0:
                                cx.ts(buf[:, c2, dst], HG[:, r_, ai, src], selt[:, side, r_:r_ + 1], ALU.mult,
                                      r=["HG", "selt"], w=[k])
                            else:
                                cx.stt(buf[:, c2, dst], HG[:, r_, ai, src], selt[:, side, r_:r_ + 1], buf[:, c2, dst],
                                       ALU.mult, ALU.add, r=["HG", "selt", k], w=[k])
            else:
                for buf, k in ((ue, "ue%d" % c2), (te, "te%d" % c2)):
                    cx.memset(buf[:, c2, 0:16], 0.0, w=[k], eng="pool")
                    cx.memset(buf[:, c2, 16 + T:32 + T], 0.0, w=[k], eng="pool")
        for c2 in range(2):
            ka = "acc%d" % c2
            cx.ts(acc[:, c2, :], ue[:, c2, 1:1 + T], par[:, c2, 0:1], ALU.mult, s2=par[:, c2, 31:32], op1=ALU.add,
                  r=["ue%d" % c2, "par"], w=[ka])
            for k in range(1, 31):
                cx.stt(acc[:, c2, :], ue[:, c2, k + 1:k + 1 + T], par[:, c2, k:k + 1], acc[:, c2, :], ALU.mult, ALU.add,
                       r=["ue%d" % c2, "par", ka], w=[ka])
            cx.tt(sq[:, c2, :], acc[:, c2, :], acc[:, c2, :], ALU.mult, r=[ka], w=["sq%d" % c2], eng="pool")
        G = min(512, T)
        mm2 = cx.sb("m2" + tag, [128, G]); var = cx.sb("var" + tag, [128, G]); dd = cx.sb("dd" + tag, [128, G])
        for g in range(T // G):
            gs = slice(g * G, (g + 1) * G)
            for c2 in range(2):
                cx.mm(ps[0][:, 0:G], onesm[:, :], acc[:, c2, gs], c2 == 0, c2 == 1, r=["onesm", "acc%d" % c2], w=["ps0"])
            for c2 in range(2):
                cx.mm(ps[1][:, 0:G], onesm[:, :], sq[:, c2, gs], c2 == 0, c2 == 1, r=["onesm", "sq%d" % c2], w=["ps1"])
            cx.act(mm2[:, :], ps[0][:, 0:G], AF.Square, r=["ps0"], w=["mm2"])
            cx.tt(var[:, :], ps[1][:, 0:G], mm2[:, :], ALU.subtract, r=["ps1", "mm2"], w=["var"])
            cx.act(var[:, :], var[:, :], AF.Ln, bias=EPS, r=["var"], w=["var"])
            cx.act(var[:, :], var[:, :], AF.Exp, scale=-0.5, r=["var"], w=["var"])
            for c2 in range(2):
                cx.tt(dd[:, :], acc[:, c2, gs], ps[0][:, 0:G], ALU.subtract, r=["acc%d" % c2, "ps0"], w=["dd"])
                cx.tt(dd[:, :], dd[:, :], var[:, :], ALU.mult, r=["dd", "var"], w=["dd"])
                cx.act(yb[:, c2, gs], dd[:, :], AF.Silu, scale=par[:, c2, 32:33], bias=par[:, c2, 33:34],
                       r=["dd", "par"], w=["yb%d" % c2])
        for c2 in range(2):
            cx.dma("sp", dr["ysT" + sfx][0 + c2, :, :], yb[:, c2, :], r=["yb%d" % c2], w=["D:ys0" + sfx])
        for c2 in range(2):
            ka = "acc%d" % c2
            cx.ts(acc[:, c2, :], te[:, c2, 15:15 + T], par[:, c2, 34:35], ALU.mult, r=["te%d" % c2, "par"], w=[ka])
            for k in (1, 2):
                cx.stt(acc[:, c2, :], te[:, c2, 15 + k:15 + k + T], par[:, c2, 34 + k:35 + k], acc[:, c2, :], ALU.mult, ALU.add,
                       r=["te%d" % c2, "par", ka], w=[ka])
            cx.tt(yb[:, c2, :], acc[:, c2, :], bg[:, c2, :], ALU.mult, r=[ka, "bg%d" % c2], w=["yb%d" % c2])
            cx.dma("sp", dr["ysT" + sfx][2 + c2, :, :], yb[:, c2, :], r=["yb%d" % c2], w=["D:ys1" + sfx])
    cx.end()


def phase_ret(cx, l, dr, segs):
    cx.begin(nf=6, nb=2)
    ps, psb = cx.ps, cx.psb
    cst = cx.sb("cst", [128, CSTW])
    ident = cx.sb("ident", [128, 128], BF16)
    cx.dma("sp", cst[:, :], dr["cst"][:, :], w=["cst"])
    cx.dma("sp", ident[:, :], dr["ident"][:, :], w=["ident"])
    lgf = cx.sb("lgf", [128, 4]); lgb = cx.sb("lgb", [128, 4])
    lgfc = cx.sb("lgfc", [128, 1]); lgbc = cx.sb("lgbc", [128, 1])
    cx.dma("sp", lgf[:, :], dr["ret_ld_f"][l, :].partition_broadcast(128), w=["lgf"])
    cx.dma("sp", lgb[:, :], dr["ret_ld_b"][l, :].partition_broadcast(128), w=["lgb"])
    for h in range(4):
        cx.dma("sp", lgfc[32 * h:32 * h + 32, :], dr["ret_ld_f"][l, h:h + 1].partition_broadcast(32), w=["lgfc"])
        cx.dma("sp", lgbc[32 * h:32 * h + 32, :], dr["ret_ld_b"][l, h:h + 1].partition_broadcast(32), w=["lgbc"])
    kdf = cx.sb("kdf", [128, 4]); kdb = cx.sb("kdb", [128, 4])
    KDF = cx.sb("KDF", [128, 128]); KDB = cx.sb("KDB", [128, 128])
    cx.act(kdf[:, :], lgf[:, :], AF.Exp, scale=cs(cst, "c127mj"), r=["lgf", "cst"], w=["kdf"])
    cx.act(kdb[:, :], lgb[:, :], AF.Exp, scale=cs(cst, "cj"), r=["lgb", "cst"], w=["kdb"])
    for h in range(4):
        cx.ts(KDF[:, 32 * h:32 * h + 32], cs(cst, "ones")[:, 0:32], kdf[:, h:h + 1], ALU.mult, r=["kdf", "cst"], w=["KDF"])
        cx.ts(KDB[:, 32 * h:32 * h + 32], cs(cst, "ones")[:, 0:32], kdb[:, h:h + 1], ALU.mult, r=["kdb", "cst"], w=["KDB"])
    cdf = cx.sb("cdf", [128, 1]); cdb = cx.sb("cdb", [128, 1])
    cx.act(cdf[:, :], lgfc[:, :], AF.Exp, scale=128.0, r=["lgfc"], w=["cdf"])
    cx.act(cdb[:, :], lgbc[:, :], AF.Exp, scale=128.0, r=["lgbc"], w=["cdb"])
    qdf4 = cx.sb("qdf4", [128, 4, 128]); qdb4 = cx.sb("qdb4", [128, 4, 128])
    for c in range(4):
        cx.act(qdf4[:, c, :], cs(cst, "ip1"), AF.Exp, scale=lgfc[:, 0:1], r=["lgfc", "cst"], w=["qdf4"])
        cx.act(qdb4[:, c, :], cs(cst, "m128i"), AF.Exp, scale=lgbc[:, 0:1], r=["lgbc", "cst"], w=["qdb4"])
    maskT = cx.sb("maskT", [128, 4, 128]); mtmp = cx.sb("mtmp", [128, 128])
    for h in range(4):
        cx.act(mtmp[:, :], cs(cst, "D1"), AF.Exp, scale=lgf[:, h:h + 1], r=["lgf", "cst"], w=["mtmp"])
        cx.tt(maskT[:, h, :], mtmp[:, :], cs(cst, "U"), ALU.mult, r=["mtmp", "cst"], w=["maskT"])
        cx.tt(maskT[:, h, :], maskT[:, h, :], cs(cst, "I2"), ALU.add, r=["maskT", "cst"], w=["maskT"])
        cx.act(mtmp[:, :], cs(cst, "D2"), AF.Exp, scale=lgb[:, h:h + 1], r=["lgb", "cst"], w=["mtmp"])
        cx.tt(mtmp[:, :], mtmp[:, :], cs(cst, "Lo"), ALU.mult, r=["mtmp", "cst"], w=["mtmp"])
        cx.tt(maskT[:, h, :], maskT[:, h, :], mtmp[:, :], ALU.add, r=["maskT", "mtmp"], w=["maskT"])
    for (tag, T) in segs:
        sfx = "_" + tag
        NCH = T // 128
        rq = cx.sb("rq" + tag, [128, T], BF16); rk = cx.sb("rk" + tag, [128, T], BF16)
        rqh = cx.sb("rqh" + tag, [128, 4, T], BF16)
        rv = cx.sb("rv" + tag, [128, NCH, 256], BF16); rg = cx.sb("rg" + tag, [128, NCH, 256], BF16)
        cx.dma("sp", rq[:, :], dr["rqT" + sfx][:, :], r=["D:rope" + sfx + "16"], w=["rq"])
        cx.dma("sp", rk[:, :], dr["rkT" + sfx][:, :], r=["D:rope" + sfx + "17"], w=["rk"])
        cx.dma("sp", rv[:, :, :], dr["rv" + sfx].rearrange("(n p) e -> p n e", p=128), r=["D:rv" + sfx], w=["rv"])
        cx.dma("sp", rg[:, :, :], dr["rg" + sfx].rearrange("(n p) e -> p n e", p=128), r=["D:rg" + sfx], w=["rg"])
        for h in range(4):
            cx.ts(rqh[:, h, :], rq[:, :], cs(cst, "hm")[:, h:h + 1], ALU.mult, r=["rq", "cst"], w=["rqh"], eng="pool")
        SF = cx.sb("SF" + tag, [128, NCH, 256]); SB = cx.sb("SB" + tag, [128, NCH, 256])
        SFb = cx.sb("SFb" + tag, [128, NCH, 256], BF16); SBb = cx.sb("SBb" + tag, [128, NCH, 256], BF16)
        KV = cx.sb("KV" + tag, [128, NCH, 2, 256])
        if tag == "l":
            Tall = cx.sb("Tall", [128, 9, 2, 256]); expo = cx.sb("expo", [128, 2, 9]); coef = cx.sb("coef", [128, 2, 9])
            cx.dma("sp", Tall[:, 0:8, :, :], dr["gt"].rearrange("(s a p) e -> p s a e", s=NCORE, a=2), w=["Tall"])
            cx.dma("sp", Tall[:, 8, :, :], dr["Tst_c"].rearrange("a p e -> p a e"), w=["Tall"])
            cx.dma("sp", expo[:, :, :], dr["expo"][:, :, :], w=["expo"])
            cx.act(coef[:, 0, :], expo[:, 0, :], AF.Exp, scale=lgfc[:, 0:1], r=["expo", "lgfc"], w=["coef"])
            cx.act(coef[:, 1, :], expo[:, 1, :], AF.Exp, scale=lgbc[:, 0:1], r=["expo", "lgbc"], w=["coef"])
            for a, (St, n0, key) in enumerate(((SF, 0, "SF0"), (SB, NCH - 1, "SB%d" % (NCH - 1)))):
                cx.ts(St[:, n0, :], Tall[:, 0, a, :], coef[:, a, 0:1], ALU.mult, r=["Tall", "coef"], w=[key])
                for s in range(1, 9):
                    cx.stt(St[:, n0, :], Tall[:, s, a, :], coef[:, a, s:s + 1], St[:, n0, :], ALU.mult, ALU.add,
                           r=["Tall", "coef", key], w=[key])
        else:
            cx.memset(SF[:, 0, :], 0.0, w=["SF0"])
            cx.memset(SB[:, NCH - 1, :], 0.0, w=["SB%d" % (NCH - 1)])
        kfb = [cx.sb("kfb%d" % i + tag, [128, 256], BF16) for i in range(2)]
        for n in range(NCH):
            b = n % 2
            csl = slice(n * 128, (n + 1) * 128)
            cx.tr(psb[0][:, 0:128], rk[:, csl], ident[:, :], r=["rk", "ident"], w=["psb0"])
            cx.tt(kfb[b][:, 0:128], psb[0][:, 0:128], KDF[:, :], ALU.mult, r=["psb0", "KDF"], w=["kfb%d" % b])
            cx.tt(kfb[b][:, 128:256], psb[0][:, 0:128], KDB[:, :], ALU.mult, r=["psb0", "KDB"], w=["kfb%d" % b])
            cx.mm(ps[0][:, 0:256], kfb[b][:, 0:128], rv[:, n, :], True, True, r=["kfb%d" % b, "rv"], w=["ps0"])
            cx.mm(ps[0][:, 256:512], kfb[b][:, 128:256], rv[:, n, :], True, True, r=["kfb%d" % b, "rv"], w=["ps0"])
            cx.copy(KV[:, n, :, :], ps[0][:, :].rearrange("p (a e) -> p a e", a=2), r=["ps0", "ps0"], w=["KV%d" % n], eng="act")
        for n in range(NCH - 1):
            cx.stt(SF[:, n + 1, :], SF[:, n, :], cdf[:, 0:1], KV[:, n, 0, :], ALU.mult, ALU.add,
                   r=["SF%d" % n, "cdf", "KV%d" % n], w=["SF%d" % (n + 1)])
        for n in range(NCH - 1, 0, -1):
            cx.stt(SB[:, n - 1, :], SB[:, n, :], cdb[:, 0:1], KV[:, n, 1, :], ALU.mult, ALU.add,
                   r=["SB%d" % n, "cdb", "KV%d" % n], w=["SB%d" % (n - 1)])
        allSF = ["SF%d" % n for n in range(NCH)]; allSB = ["SB%d" % n for n in range(NCH)]
        cx.copy(SFb[:, :, :], SF[:, :, :], r=allSF, w=["SFb"], eng="pool")
        cx.copy(SBb[:, :, :], SB[:, :, :], r=allSB, w=["SBb"], eng="pool")
        sT = [cx.sb("sT%d" % i + tag, [128, 4, 128], BF16) for i in range(2)]
        Qf = [cx.sb("Qf%d" % i + tag, [128, 4, 128], BF16) for i in range(2)]
        Qb = [cx.sb("Qb%d" % i + tag, [128, 4, 128], BF16) for i in range(2)]
        osbs = [cx.sb("osb%d" % i + tag, [128, 4, 64]) for i in range(2)]
        osqs = [cx.sb("osq%d" % i + tag, [128, 4, 64]) for i in range(2)]
        sts = [cx.sb("st%d" % i + tag, [128, 4, 4]) for i in range(2)]
        ysb = [cx.sb("ysb%d" % i + tag, [128, 256], BF16) for i in range(2)]
        yT = cx.sb("yT" + tag, [128, 2, T], BF16)
        for n in range(NCH):
            b = n % 2
            osb = osbs[b]; osq = osqs[b]; st = sts[b]
            pS = ps[1 + 2 * b]; kS = "ps%d" % (1 + 2 * b)
            pO = ps[2 + 2 * b]; kO = "ps%d" % (2 + 2 * b)
            KB = lambda nm: "%s_%d" % (nm, b)
            csl = slice(n * 128, (n + 1) * 128)
            for h in range(4):
                cx.mm(pS[:, h * 128:(h + 1) * 128], rk[:, csl], rqh[:, h, csl], True, True, r=["rk", "rqh"], w=[kS])
            cx.tt(sT[b][:, :, :], pS[:, :].rearrange("p (h i) -> p h i", h=4), maskT[:, :, :], ALU.mult,
                  r=[kS, "maskT"], w=["sT%d" % b])
            cx.tt(Qf[b][:, :, :], rqh[:, :, csl], qdf4[:, :, :], ALU.mult, r=["rqh", "qdf4"], w=["Qf%d" % b], eng="pool")
            cx.tt(Qb[b][:, :, :], rqh[:, :, csl], qdb4[:, :, :], ALU.mult, r=["rqh", "qdb4"], w=["Qb%d" % b], eng="pool")
            for h in range(4):
                o = pO[:, h * 64:(h + 1) * 64]
                es = slice(h * 64, (h + 1) * 64)
                cx.mm(o, sT[b][:, h, :], rv[:, n, es], True, False, r=["sT%d" % b, "rv"], w=[kO])
                cx.mm(o, Qf[b][:, h, :], SFb[:, n, es], False, False, r=["Qf%d" % b, "SFb"], w=[kO])
                cx.mm(o, Qb[b][:, h, :], SBb[:, n, es], False, True, r=["Qb%d" % b, "SBb"], w=[kO])
            p2 = [kO]
            cx.copy(osb[:, :, :], pO[:, 0:256].rearrange("p (h e) -> p h e", h=4), r=p2, w=[KB("osb")], eng="act")
            cx.red(st[:, 0, :], osb[:, :, :], ALU.add, r=[KB("osb")], w=[KB("st0")])
            cx.tt(osq[:, :, :], osb[:, :, :], osb[:, :, :], ALU.mult, r=[KB("osb")], w=[KB("osq")], eng="pool")
            cx.red(st[:, 1, :], osq[:, :, :], ALU.add, r=[KB("osq")], w=[KB("st1")])
            cx.ts(st[:, 2, :], st[:, 0, :], 1.0 / 64, ALU.mult, r=[KB("st0")], w=[KB("st2")])
            cx.tt(st[:, 3, :], st[:, 2, :], st[:, 2, :], ALU.mult, r=[KB("st2")], w=[KB("st3")])
            cx.stt(st[:, 3, :], st[:, 1, :], 1.0 / 64, st[:, 3, :], ALU.mult, ALU.subtract, r=[KB("st1"), KB("st3")], w=[KB("st3")])
            cx.act(st[:, 3, :], st[:, 3, :], AF.Sqrt, bias=EPS, r=[KB("st3")], w=[KB("st3")])
            cx.recip(st[:, 3, :], st[:, 3, :], r=[KB("st3")], w=[KB("st3")])
            for h in range(4):
                cx.ts(osb[:, h, :], osb[:, h, :], st[:, 2, h:h + 1], ALU.subtract, s2=st[:, 3, h:h + 1], op1=ALU.mult,
                      r=[KB("osb"), KB("st2"), KB("st3")], w=[KB("osb")])
            cx.tt(ysb[b][:, :], osb[:, :, :].rearrange("p h e -> p (h e)"), rg[:, n, :], ALU.mult, r=[KB("osb"), "rg"], w=["ysb%d" % b])
            for c2 in range(2):
                cx.tr(psb[1][:, c2 * 128:(c2 + 1) * 128], ysb[b][:, c2 * 128:(c2 + 1) * 128], ident[:, :],
                      r=["ysb%d" % b, "ident"], w=["psb1"])
            cx.copy(yT[:, :, csl], psb[1][:, 0:256].rearrange("p (c t) -> p c t", c=2), r=["psb1"], w=["yT"], eng="act")
        for c2 in range(2):
            cx.dma("sp", dr["ysT" + sfx][6 + c2, :, :], yT[:, c2, :], r=["yT"], w=["D:ys3" + sfx])
    cx.end()


def phase_merge(cx, l, dr, segs, wl=None):
    wl = l if wl is None else wl
    cx.begin(nf=8, nb=0)
    ps = cx.ps
    cst = cx.sb("cst", [128, CSTW])
    cx.dma("sp", cst[:, :], dr["cst"][:, :], w=["cst"])
    WG = cx.sb("WG", [128, 8, 4096], BF16)
    WB = cx.sb("WB", [128, 8, 1024], BF16)
    WO = cx.sb("WO", [128, 8, 1024], BF16)
    for kc in range(8):
        cx.dma("pool", WG[:, kc, :], dr["w_gate"][wl, kc * 128:(kc + 1) * 128, :], w=["WG%d" % kc])
        cx.dma("pool", WB[:, kc, :], dr["w_branch"][wl, kc // 2, (kc % 2) * 128:(kc % 2 + 1) * 128, :], w=["WB%d" % kc])
        cx.dma("pool", WO[:, kc, :], dr["w_o"][wl, kc * 128:(kc + 1) * 128, :], w=["WO%d" % kc])
    braw = cx.sb("braw", [32, 128]); bgt = cx.sb("bgt", [128, 32])
    cx.dma("sp", braw[:, :], dr["b_gate"][l, :].rearrange("(a p) -> a p", p=128), w=["braw"])
    cx.tr(ps[7][:, 0:32], braw[:, :], cs(cst, "I1")[0:32, 0:32], r=["braw", "cst"], w=["ps7"])
    cx.copy(bgt[:, :], ps[7][:, 0:32], r=["ps7"], w=["bgt"])
    g1b = cx.sb("g1b", [128, D])
    hT = [cx.sb("mhT%d" % i, [128, 8, 512], BF16) for i in range(2)]
    yT = [cx.sb("myT%d" % i, [128, 8, 512], BF16) for i in range(2)]
    mT = cx.sb("mT", [128, 8, 512], BF16)
    sg = [cx.sb("sg%d" % i, [128, 512]) for i in range(2)]
    macc = cx.sb("macc", [128, 512]); mtmp = cx.sb("mtmp", [128, 512])
    xt = [cx.sb("mxt%d" % i, [128, D]) for i in range(2)]
    gi = 0
    xi = 0
    for (tag, T, xin, xout) in segs:
        sfx = "_" + tag
        seg = 0 if tag == "l" else 1
        cx.dma("sp", g1b[:, :], dr["modv"][l, seg, 2, :].partition_broadcast(128), r=["D:modv%d" % l], w=["g1b"])
        G = min(512, T)
        for g in range(T // G):
            t0 = g * G
            b = gi % 2
            gi += 1
            cx.dma("sp", hT[b][:, :, 0:G], dr["hT" + sfx][:, :, t0:t0 + G], r=["D:hT" + sfx], w=["mhT%d" % b])
            cx.dma("sp", yT[b][:, :, 0:G], dr["ysT" + sfx][:, :, t0:t0 + G].rearrange("a p t -> p a t"),
                   r=["D:ys0" + sfx, "D:ys1" + sfx, "D:ys2" + sfx, "D:ys3" + sfx], w=["myT%d" % b])
            for nn in range(8):
                for i in range(4):
                    pa = ps[(i % 2) * 2]
                    pb = ps[(i % 2) * 2 + 1]
                    ka = "ps%d" % ((i % 2) * 2)
                    kb = "ps%d" % ((i % 2) * 2 + 1)
                    col = i * 1024 + nn * 128
                    for kc in range(8):
                        cx.mm(pa[:, 0:G], WG[:, kc, col:col + 128], hT[b][:, kc, 0:G], kc == 0, kc == 7,
                              r=["WG%d" % kc, "mhT%d" % b], w=[ka])
                    for c2 in range(2):
                        cx.mm(pb[:, 0:G], WB[:, i * 2 + c2, nn * 128:(nn + 1) * 128], yT[b][:, i * 2 + c2, 0:G], c2 == 0, c2 == 1,
                              r=["WB%d" % (i * 2 + c2), "myT%d" % b], w=[kb])
                    s = sg[i % 2]
                    ks = "sg%d" % (i % 2)
                    cx.act(s[:, 0:G], pa[:, 0:G], AF.Sigmoid, bias=bgt[:, i * 8 + nn:i * 8 + nn + 1], r=[ka, "bgt"], w=[ks])
                    if i == 0:
                        cx.tt(macc[:, 0:G], pb[:, 0:G], s[:, 0:G], ALU.mult, r=[kb, ks], w=["macc"])
                    elif i < 3:
                        cx.tt(mtmp[:, 0:G], pb[:, 0:G], s[:, 0:G], ALU.mult, r=[kb, ks], w=["mtmp"])
                        cx.tt(macc[:, 0:G], macc[:, 0:G], mtmp[:, 0:G], ALU.add, r=["macc", "mtmp"], w=["macc"], eng="pool")
                    else:
                        cx.tt(mtmp[:, 0:G], pb[:, 0:G], s[:, 0:G], ALU.mult, r=[kb, ks], w=["mtmp"])
                        cx.tt(mT[:, nn, 0:G], macc[:, 0:G], mtmp[:, 0:G], ALU.add, r=["macc", "mtmp"], w=["mT"], eng="pool")
            for j in range(G // 128):
                xb = xi % 2
                xi += 1
                rows = slice(t0 + j * 128, t0 + (j + 1) * 128)
                cx.dma("sp", xt[xb][:, :], xin[rows, :], w=["mxt%d" % xb])
                for nh in range(2):
                    po = ps[4 + nh]
                    for kc in range(8):
                        cx.mm(po[:, :], mT[:, kc, j * 128:(j + 1) * 128], WO[:, kc, nh * 512:(nh + 1) * 512], kc == 0, kc == 7,
                              r=["mT", "WO%d" % kc], w=["ps%d" % (4 + nh)])
                    hs = slice(nh * 512, (nh + 1) * 512)
                    cx.tt(mtmp[:, :], po[:, :], g1b[:, hs], ALU.mult, r=["ps%d" % (4 + nh), "g1b"], w=["mtmp"])
                    cx.tt(xt[xb][:, hs], xt[xb][:, hs], mtmp[:, :], ALU.add, r=["mxt%d" % xb, "mtmp"], w=["mxt%d" % xb], eng="pool")
                cx.dma("sp", xout[rows, :], xt[xb][:, :], r=["mxt%d" % xb], w=["D:xmid" + sfx])
    cx.end()


def phase_attn(cx, l, dr, segs, lam_init):
    NB = 2
    NSB = 3
    cx.begin(nf=0, nb=0)
    psS = [cx.psum("psS%d" % i, [128, NB * 512]) for i in range(NSB)]
    psO = [cx.psum("psO%d" % i, [128, 512]) for i in range(2)]
    NKT = max(s[2] for s in segs)
    cst = cx.sb("cst", [128, CSTW])
    ident = cx.sb("ident", [128, 128], BF16)
    cx.dma("sp", cst[:, :], dr["cst"][:, :], w=["cst"])
    cx.dma("sp", ident[:, :], dr["ident"][:, :], w=["ident"])
    kT = cx.sb("kTall", [128, 2, NKT * 128], BF16)
    Va = cx.sb("Vaug", [128, NKT, 4, 65], BF16)
    vst = [cx.sb("vst%d" % i, [128, 10, 256], BF16) for i in range(2)]
    kTk = []
    for c in range(2):
        cx.dma("sp", kT[:, c, 0:TC], dr["kT_c"][c, :, :], w=["kTc%d" % c])
        kTk.append("kTc%d" % c)
        if NKT > TC // 128:
            for r_ in range(NCORE):
                cx.dma("sp" if (r_ % 2 == 0) else "act", kT[:, c, TC + r_ * TL:TC + (r_ + 1) * TL],
                       dr["gk"][(r_ * 2 + c) * 128:(r_ * 2 + c + 1) * 128, :], w=["kT%d_%d" % (c, r_)])
                kTk.append("kT%d_%d" % (c, r_))
    cx.memset(Va[:, :, :, 64:65], 1.0, w=["Vones"], eng="pool")
    chunks = [(0, TC // 128, dr["V_c"], 0)]
    k0 = TC // 128
    while k0 < NKT:
        k1 = min(NKT, k0 + 10)
        chunks.append((k0, k1, dr["gv"], (k0 - TC // 128) * 128))
        k0 = k1
    nst = len(chunks)
    for i, (k0, k1, src, row0) in enumerate(chunks):
        b = i % 2
        cx.dma("sp", vst[b][:, 0:k1 - k0, :], src[row0:row0 + (k1 - k0) * 128, :].rearrange("(k p) e -> p k e", p=128), w=["vst%d" % b])
        cx.copy(Va[:, k0:k1, :, 0:64], vst[b][:, 0:k1 - k0, :].rearrange("p k (h e) -> p k h e", h=4), r=["vst%d" % b],
                w=["Va%d" % i], eng=("pool" if i % 2 == 0 else "dve"))
    Vak = ["Va%d" % i for i in range(nst)] + ["Vones"]
    lq = cx.sb("lq", [128, 4, 32]); lp = cx.sb("lp", [128, 2, 32]); ls = cx.sb("ls", [128, 4])
    for i, nm in enumerate(("lam_q1", "lam_k1", "lam_q2", "lam_k2")):
        cx.dma("sp", lq[:, i, :], dr[nm][l, :].partition_broadcast(128), w=["lq"])
    cx.tt(lp[:, 0, :], lq[:, 0, :], lq[:, 1, :], ALU.mult, r=["lq"], w=["lp"])
    cx.tt(lp[:, 1, :], lq[:, 2, :], lq[:, 3, :], ALU.mult, r=["lq"], w=["lp"])
    cx.red(ls[:, 0:2], lp[:, :, :], ALU.add, r=["lp"], w=["ls"])
    cx.act(ls[:, 0:2], ls[:, 0:2], AF.Exp, r=["ls"], w=["ls"])
    cx.tt(ls[:, 2:3], ls[:, 1:2], ls[:, 0:1], ALU.subtract, r=["ls"], w=["ls2"])
    cx.ts(ls[:, 3:4], ls[:, 2:3], -lam_init, ALU.add, r=["ls2"], w=["nlam"])
    nlam = ls[:, 3:4]
    dgb = cx.sb("dgb", [128, 4, 64])
    for h in range(4):
        cx.dma("sp", dgb[:, h, :], dr["diff_g"][l, :].partition_broadcast(128), w=["dgb"])
    cx.ts(dgb[:, :, :], dgb[:, :, :], 1.0 - lam_init, ALU.mult, r=["dgb"], w=["dgb"])
    qg = [cx.sb("qg%d" % i, [128, 2, 512], BF16) for i in range(2)]
    qm = [cx.sb("qm%d" % i, [128, 8, 512], BF16) for i in range(2)]
    pT = [cx.sb("pT%d" % i, [128, NB, 512], BF16) for i in range(NSB)]
    oT = cx.sb("oT", [65, 2, 512])
    oatt = cx.sb("oatt", [128, 4, 4, 64]); osq = cx.sb("aosq", [128, 4, 64])
    rr = cx.sb("rr", [128, 4]); ast = cx.sb("ast", [128, 2, 4])
    ysb = [cx.sb("aysb%d" % i, [128, 256]) for i in range(2)]
    yT = cx.sb("ayT", [128, 2, 512], BF16)
    gi = 0
    for (tag, T, nkt) in segs:
        sfx = "_" + tag
        G = min(512, T)
        nt = G // 128
        assert nkt % NB == 0
        for g in range(T // G):
            t0 = g * G
            b = gi % 2
            gi += 1
            cx.dma("sp", qg[b][:, :, 0:G], dr["qT" + sfx][:, :, t0:t0 + G].rearrange("c p t -> p c t"),
                   r=["D:rope" + sfx + "10", "D:rope" + sfx + "11"], w=["qg%d" % b])
            qb_ = b
            for c in range(2):
                for bl in range(4):
                    cx.ts(qm[qb_][:, c * 4 + bl, 0:G], qg[b][:, c, 0:G], cs(cst, "bm8")[:, bl:bl + 1], ALU.mult,
                          r=["qg%d" % b, "cst"], w=["qm%d_%d" % (qb_, c * 4 + bl)])
            items = [(h, m, kb) for h in range(4) for m in range(2) for kb in range(nkt // NB)]
            LA = 2

            def emit_S(i):
                h, m, kb = items[i]
                c = h // 2
                qi = c * 4 + (h % 2) * 2 + m
                sb_ = i % NSB
                for j in range(NB):
                    kt = kb * NB + j
                    kk = "kTc%d" % c if kt < TC // 128 else "kT%d_%d" % (c, (kt * 128 - TC) // TL)
                    cx.mm(psS[sb_][:, j * 512:j * 512 + G], kT[:, c, kt * 128:(kt + 1) * 128], qm[qb_][:, qi, 0:G], True, True,
                          r=[kk, "qm%d_%d" % (qb_, qi)], w=["psS%d" % sb_])
                cx.act(pT[sb_][:, :, 0:G], psS[sb_][:, :].rearrange("p (j n) -> p j n", j=NB)[:, :, 0:G], AF.Exp, scale=QSCALE,
                       r=["psS%d" % sb_], w=["pT%d" % sb_])

            def emit_O(i):
                h, m, kb = items[i]
                sb_ = i % NSB
                for j in range(NB):
                    kt = kb * NB + j
                    vk = "Va0" if kt < TC // 128 else "Va%d" % (1 + (kt - TC // 128) // 10)
                    cx.mm(psO[m][0:65, 0:G], Va[:, kt, h, :], pT[sb_][:, j, 0:G], kt == 0, kt == nkt - 1,
                          r=[vk, "Vones", "pT%d" % sb_], w=["psO%d" % m])
                if kb == nkt // NB - 1:
                    cx.copy(oT[:, m, 0:G], psO[m][0:65, 0:G], r=["psO%d" % m], w=["oT%d" % m])
                    if m == 1:
                        head_epilogue(h)

            def head_epilogue(h):
                for j in range(nt):
                    for m in range(2):
                        cx.tr(psO[0][:, m * 65:m * 65 + 65], oT[0:65, m, j * 128:(j + 1) * 128], cs(cst, "I1")[0:65, 0:65],
                              r=["oT%d" % m, "cst"], w=["psO0"])
                    cx.recip(rr[:, 0:1], psO[0][:, 64:65], r=["psO0"], w=["rr0"])
                    cx.recip(rr[:, 1:2], psO[0][:, 129:130], r=["psO0"], w=["rr1"])
                    cx.tt(rr[:, 2:3], rr[:, 1:2], nlam, ALU.mult, r=["rr1", "nlam"], w=["rr2"])
                    cx.ts(oatt[:, j, h, :], psO[0][:, 0:64], rr[:, 0:1], ALU.mult, r=["psO0", "rr0"], w=["oatt%d" % j])
                    cx.stt(oatt[:, j, h, :], psO[0][:, 65:129], rr[:, 2:3], oatt[:, j, h, :], ALU.mult, ALU.add,
                           r=["psO0", "rr2", "oatt%d" % j], w=["oatt%d" % j])

            if ATT_ROW:
                items = [(h, kt) for h in range(4) for kt in range(nkt)]

                def emit_S(i):
                    h, kt = items[i]
                    c = h // 2
                    sb_ = i % NSB
                    kk = "kTc%d" % c if kt < TC // 128 else "kT%d_%d" % (c, (kt * 128 - TC) // TL)
                    for m in range(2):
                        blk = (h % 2) * 2 + m
                        rs = slice(32 * blk, 32 * blk + 32)
                        cx.mm(psS[sb_][:, m * 512:m * 512 + G], kT[rs, c, kt * 128:(kt + 1) * 128], qg[b][rs, c, 0:G], True, True,
                              r=[kk, "qg%d" % b], w=["psS%d" % sb_], tile_position=(32 * blk, 0))
                    cx.act(pT[sb_][:, :, 0:G], psS[sb_][:, :].rearrange("p (j n) -> p j n", j=NB)[:, :, 0:G], AF.Exp, scale=QSCALE,
                           r=["psS%d" % sb_], w=["pT%d" % sb_])

                def emit_O(i):
                    h, kt = items[i]
                    sb_ = i % NSB
                    vk = "Va0" if kt < TC // 128 else "Va%d" % (1 + (kt - TC // 128) // 10)
                    for m in range(2):
                        cx.mm(psO[m][0:65, 0:G], Va[:, kt, h, :], pT[sb_][:, m, 0:G], kt == 0, kt == nkt - 1,
                              r=[vk, "Vones", "pT%d" % sb_], w=["psO%d" % m])
                    if kt == nkt - 1:
                        for m in range(2):
                            cx.copy(oT[:, m, 0:G], psO[m][0:65, 0:G], r=["psO%d" % m], w=["oT%d" % m])
                        head_epilogue(h)
            n_it = len(items)
            for i in range(n_it + LA):
                if i < n_it:
                    emit_S(i)
                if i >= LA:
                    emit_O(i - LA)
            for j in range(nt):
                yb = j % 2
                cx.tt(osq[:, :, :], oatt[:, j, :, :], oatt[:, j, :, :], ALU.mult, r=["oatt%d" % j], w=["aosq"], eng="pool")
                cx.red(ast[:, 0, :], osq[:, :, :], ALU.add, r=["aosq"], w=["ast0"])
                cx.act(ast[:, 1, :], ast[:, 0, :], AF.Sqrt, scale=1.0 / 64, bias=EPS, r=["ast0"], w=["ast1"])
                cx.recip(ast[:, 1, :], ast[:, 1, :], r=["ast1"], w=["ast1"])
                for h in range(4):
                    cx.stt(oatt[:, j, h, :], oatt[:, j, h, :], ast[:, 1, h:h + 1], dgb[:, h, :], ALU.mult, ALU.mult,
                           r=["oatt%d" % j, "ast1", "dgb"], w=["oatt%d" % j])
                cx.copy(ysb[yb][:, :], oatt[:, j, :, :].rearrange("p h e -> p (h e)"), r=["oatt%d" % j], w=["aysb%d" % yb], eng="pool")
                for c2 in range(2):
                    cx.tr(psO[1][:, c2 * 128:(c2 + 1) * 128], ysb[yb][:, c2 * 128:(c2 + 1) * 128], cs(cst, "I1"),
                          r=["aysb%d" % yb, "cst"], w=["psO1"])
                cx.copy(yT[:, :, j * 128:(j + 1) * 128], psO[1][:, 0:256].rearrange("p (c t) -> p c t", c=2), r=["psO1"], w=["ayT"])
            for c2 in range(2):
                cx.dma("sp", dr["ysT" + sfx][4 + c2, :, t0:t0 + G], yT[:, c2, 0:G], r=["ayT"], w=["D:ys2" + sfx])
    cx.end()


def phase_moe(cx, l, dr, segs, final, wl=None):
    wl = l if wl is None else wl
    cx.begin(nf=6, nb=2)
    ps, psb = cx.ps, cx.psb
    NT = sum(s[1] for s in segs) // 128
    TT = NT * 128
    cst = cx.sb("cst", [128, CSTW])
    ident = cx.sb("ident", [128, 128], BF16)
    cx.dma("sp", cst[:, :], dr["cst"][:, :], w=["cst"])
    cx.dma("sp", ident[:, :], dr["ident"][:, :], w=["ident"])
    h2T = cx.sb("h2T", [128, 8, TT], BF16)
    acc = cx.sb("eacc", [128, NT, D])
    wt = cx.sb("wt", [128, NT, 16])
    WR = cx.sb("WRt", [128, 8, 16], BF16)
    cx.dma("pool", WR[:, :, :], dr["w_router"].rearrange("(k p) e -> p k e", p=128), w=["WRt"])
    brb = cx.sb("brb", [128, 16])
    cx.dma("sp", brb[:, :], dr["b_router"][0, :].partition_broadcast(128), w=["brb"])
    gsb = cx.sb("gsb2", [128, D]); shb = cx.sb("shb2", [128, D]); g2b = cx.sb("g2b2", [128, D])
    xt = [cx.sb("ext%d" % i, [128, D]) for i in range(2)]
    junk = cx.sb("ejunk", [128, D], BF16)
    t1 = cx.sb("et1", [128, D])
    hb = [cx.sb("ehb%d" % i, [128, D], BF16) for i in range(2)]
    ssq = cx.sb("essq", [128, 2]); rstd = cx.sb("erstd", [128, 2])
    rt = cx.sb("rt", [128, 8, 16])
    W1 = [cx.sb("W1_%d" % i, [128, 8, DFF], BF16) for i in range(2)]
    W3 = [cx.sb("W3_%d" % i, [128, 8, DFF], BF16) for i in range(2)]
    W2 = [cx.sb("W2_%d" % i, [128, 4, D], BF16) for i in range(2)]

    def load_expert(e):
        b = e % 2
        cx.dma("pool", W1[b][:, :, :], dr["w1_e"][wl, e].rearrange("(k p) f -> p k f", p=128), w=["W1_%d" % b])
        cx.dma("pool", W3[b][:, :, :], dr["w3_e"][wl, e].rearrange("(k p) f -> p k f", p=128), w=["W3_%d" % b])
        cx.dma("pool", W2[b][:, :, :], dr["w2_e"][wl, e].rearrange("(k p) n -> p k n", p=128), w=["W2_%d" % b])
    load_expert(0)
    load_expert(1)
    ti = 0
    tiles = []
    for (tag, T, xmid, xout) in segs:
        seg = 0 if tag == "l" else 1
        cx.dma("sp", gsb[:, :], dr["modv"][l, seg, 3, :].partition_broadcast(128), r=["D:modv%d" % l], w=["gsb2"])
        cx.dma("sp", shb[:, :], dr["modv"][l, seg, 4, :].partition_broadcast(128), r=["D:modv%d" % l], w=["shb2"])
        for j in range(T // 128):
            b = ti % 2
            kx = "ext%d" % b
            rows = slice(j * 128, (j + 1) * 128)
            tiles.append((tag, seg, xmid, xout, rows))
            cx.dma("sp", xt[b][:, :], xmid[rows, :], r=["D:xmid_" + tag], w=[kx])
            cx.act(junk[:, :], xt[b][:, :], AF.Square, accum_out=ssq[:, b:b + 1], r=[kx], w=["ejunk", "essq%d" % b])
            cx.act(rstd[:, b:b + 1], ssq[:, b:b + 1], AF.Sqrt, scale=1.0 / D, bias=EPS, r=["essq%d" % b], w=["ers%d" % b])
            cx.recip(rstd[:, b:b + 1], rstd[:, b:b + 1], r=["ers%d" % b], w=["ers%d" % b])
            cx.stt(t1[:, :], xt[b][:, :], rstd[:, b:b + 1], gsb[:, :], ALU.mult, ALU.mult, r=[kx, "ers%d" % b, "gsb2"], w=["et1"])
            cx.tt(hb[b][:, :], t1[:, :], shb[:, :], ALU.add, r=["et1", "shb2"], w=["ehb%d" % b], eng="pool")
            for kc in range(8):
                cx.tr(psb[0][:, kc * 128:(kc + 1) * 128], hb[b][:, kc * 128:(kc + 1) * 128], ident[:, :],
                      r=["ehb%d" % b, "ident"], w=["psb0"])
            cx.copy(h2T[:, :, ti * 128:(ti + 1) * 128], psb[0][:, :].rearrange("p (k t) -> p k t", k=8),
                    r=["psb0"], w=["h2T%d" % ti], eng="act")
            for kc in range(8):
                cx.mm(ps[0][:, 0:16], h2T[:, kc, ti * 128:(ti + 1) * 128], WR[:, kc, :], kc == 0, kc == 7,
                      r=["h2T%d" % ti, "WRt"], w=["ps0"])
            s_ = rt[:, 0, :]; sbv = rt[:, 1, :]; tmp = rt[:, 2, :]; sb2 = rt[:, 3, :]; sbm = rt[:, 4, :]
            msk = rt[:, 5, :]; sel = rt[:, 6, :]
            g4 = rt[:, 7, 0:4]; g4b = rt[:, 7, 4:8]; gm = rt[:, 7, 8:12]; e1 = rt[:, 7, 12:13]; e2 = rt[:, 7, 13:14]
            den = rt[:, 7, 14:15]
            cx.act(s_, ps[0][:, 0:16], AF.Sigmoid, r=["ps0"], w=["r_s"])
            cx.tt(sbv, s_, brb[:, :], ALU.add, r=["r_s", "brb"], w=["r_sb"])
            v4 = lambda a: a.rearrange("p (g e) -> p g e", g=4)
            cx.red(g4, v4(sbv), ALU.max, r=["r_sb"], w=["r_g4"])
            for g_ in range(4):
                cx.ts(tmp[:, g_ * 4:(g_ + 1) * 4], sbv[:, g_ * 4:(g_ + 1) * 4], g4[:, g_:g_ + 1], ALU.is_equal,
                      r=["r_sb", "r_g4"], w=["r_tmp"])
            cx.stt(sb2, tmp, -1.0e9, sbv, ALU.mult, ALU.add, r=["r_tmp", "r_sb"], w=["r_sb2"])
            cx.red(g4b, v4(sb2), ALU.max, r=["r_sb2"], w=["r_g4b"])
            cx.tt(g4, g4, g4b, ALU.add, r=["r_g4", "r_g4b"], w=["r_g4"])
            cx.red(e1, g4, ALU.max, r=["r_g4"], w=["r_e1"])
            cx.ts(gm, g4, e1, ALU.is_equal, s2=-1.0, op1=ALU.add, r=["r_g4", "r_e1"], w=["r_gm"])
            for g_ in range(4):
                cx.ts(tmp[:, g_ * 4:(g_ + 1) * 4], cs(cst, "ones")[:, 0:4], gm[:, g_:g_ + 1], ALU.mult,
                      r=["r_gm", "cst"], w=["r_tmp"])
            cx.stt(sbm, tmp, 1.0e9, sbv, ALU.mult, ALU.add, r=["r_tmp", "r_sb"], w=["r_sbm"])
            cx.red(e1, sbm, ALU.max, r=["r_sbm"], w=["r_e1"])
            cx.ts(msk, sbm, e1, ALU.is_equal, r=["r_sbm", "r_e1"], w=["r_msk"])
            cx.stt(sb2, msk, -1.0e9, sbm, ALU.mult, ALU.add, r=["r_msk", "r_sbm"], w=["r_sb2"])
            cx.red(e2, sb2, ALU.max, r=["r_sb2"], w=["r_e2"])
            cx.ts(sel, sb2, e2, ALU.is_equal, r=["r_sb2", "r_e2"], w=["r_sel"])
            cx.tt(sel, sel, msk, ALU.add, r=["r_sel", "r_msk"], w=["r_sel"])
            cx.tt(sel, sel, s_, ALU.mult, r=["r_sel", "r_s"], w=["r_sel"])
            cx.red(den, sel, ALU.add, r=["r_sel"], w=["r_den"])
            cx.recip(den, den, r=["r_den"], w=["r_den"])
            cx.ts(wt[:, ti, :], sel, den, ALU.mult, r=["r_sel", "r_den"], w=["wt%d" % ti])
            ti += 1
    uT = [cx.sb("uT%d" % i, [128, 4, 512], BF16) for i in range(2)]
    s1 = [cx.sb("s1_%d" % i, [128, 512]) for i in range(2)]
    groups = []
    t = 0
    while t < NT:
        n = min(4, NT - t)
        groups.append((t, n))
        t += n
    ui = 0
    for e in range(NEXP):
        b = e % 2
        if e >= 2:
            load_expert(e)
        for (tg, ntl) in groups:
            G = ntl * 128
            gsl = slice(tg * 128, tg * 128 + G)
            hk = ["h2T%d" % i for i in range(tg, tg + ntl)]
            ub = ui % 2
            ui += 1
            for fc in range(4):
                fs = slice(fc * 128, (fc + 1) * 128)
                pa = ps[(fc % 2) * 2]; pb = ps[(fc % 2) * 2 + 1]
                ka = "ps%d" % ((fc % 2) * 2); kb = "ps%d" % ((fc % 2) * 2 + 1)
                for kc in range(8):
                    cx.mm(pa[:, 0:G], W1[b][:, kc, fs], h2T[:, kc, gsl], kc == 0, kc == 7, r=hk + ["W1_%d" % b], w=[ka])
                for kc in range(8):
                    cx.mm(pb[:, 0:G], W3[b][:, kc, fs], h2T[:, kc, gsl], kc == 0, kc == 7, r=hk + ["W3_%d" % b], w=[kb])
                sb_ = s1[fc % 2]
                cx.act(sb_[:, 0:G], pa[:, 0:G], AF.Silu, r=[ka], w=["s1_%d" % (fc % 2)])
                cx.tt(uT[ub][:, fc, 0:G], pb[:, 0:G], sb_[:, 0:G], ALU.mult, r=[kb, "s1_%d" % (fc % 2)], w=["uT%d" % ub])
            for j in range(ntl):
                tix = tg + j
                for nh in range(2):
                    po = ps[4 + nh]
                    for fc in range(4):
                        cx.mm(po[:, :], uT[ub][:, fc, j * 128:(j + 1) * 128], W2[b][:, fc, nh * 512:(nh + 1) * 512], fc == 0, fc == 3,
                              r=["uT%d" % ub, "W2_%d" % b], w=["ps%d" % (4 + nh)])
                    a = acc[:, tix, nh * 512:(nh + 1) * 512]
                    ka2 = "eacc%d_%d" % (tix, nh)
                    if e == 0:
                        cx.ts(a, po[:, :], wt[:, tix, e:e + 1], ALU.mult, r=["ps%d" % (4 + nh), "wt%d" % tix], w=[ka2])
                    else:
                        cx.stt(a, po[:, :], wt[:, tix, e:e + 1], a, ALU.mult, ALU.add, r=["ps%d" % (4 + nh), "wt%d" % tix, ka2], w=[ka2])
    if final:
        gfb = cx.sb("gfb", [128, D])
        cx.dma("sp", gfb[:, :], dr["g_final"][0, :].partition_broadcast(128), w=["gfb"])
    cur = None
    for ti, (tag, seg, xmid, xout, rows) in enumerate(tiles):
        if cur != seg:
            cx.dma("sp", g2b[:, :], dr["modv"][l, seg, 5, :].partition_broadcast(128), r=["D:modv%d" % l], w=["g2b2"])
            cur = seg
        b = ti % 2
        kx = "ext%d" % b
        cx.dma("sp", xt[b][:, :], xmid[rows, :], r=["D:xmid_" + tag], w=[kx])
        cx.tt(t1[:, :], acc[:, ti, :], g2b[:, :], ALU.mult, r=["eacc%d_0" % ti, "eacc%d_1" % ti, "g2b2"], w=["et1"], eng="pool")
        cx.tt(xt[b][:, :], xt[b][:, :], t1[:, :], ALU.add, r=[kx, "et1"], w=[kx])
        if final:
            cx.act(junk[:, :], xt[b][:, :], AF.Square, accum_out=ssq[:, b:b + 1], r=[kx], w=["ejunk", "essq%d" % b])
            cx.act(rstd[:, b:b + 1], ssq[:, b:b + 1], AF.Sqrt, scale=1.0 / D, bias=EPS, r=["essq%d" % b], w=["ers%d" % b])
            cx.recip(rstd[:, b:b + 1], rstd[:, b:b + 1], r=["ers%d" % b], w=["ers%d" % b])
            cx.stt(xt[b][:, :], xt[b][:, :], rstd[:, b:b + 1], gfb[:, :], ALU.mult, ALU.mult, r=[kx, "ers%d" % b, "gfb"], w=[kx])
        cx.dma("sp", xout[rows, :], xt[b][:, :], r=[kx], w=["D:xout_" + tag])
    cx.end()


def phase_moe_sparse(cx, l, dr, segs, final, wl=None):
    wl = l if wl is None else wl
    C = MOE_CAP
    cx.begin(nf=6, nb=2)
    ps, psb = cx.ps, cx.psb
    NT = sum(s[1] for s in segs) // 128
    cst = cx.sb("cst", [128, CSTW])
    ident = cx.sb("ident", [128, 128], BF16)
    cx.dma("sp", cst[:, :], dr["cst"][:, :], w=["cst"])
    cx.dma("sp", ident[:, :], dr["ident"][:, :], w=["ident"])
    Xg = dr["Xg"]
    Yg = dr["Yg"]
    bcreg = {}

    def bc(h):
        if "r" not in bcreg:
            bcreg["r"] = h.to_reg(NSLOT - 1)
        return bcreg["r"]
    W1 = [cx.sb("W1_%d" % i, [128, 8, DFF], BF16) for i in range(2)]
    W3 = [cx.sb("W3_%d" % i, [128, 8, DFF], BF16) for i in range(2)]
    W2 = [cx.sb("W2_%d" % i, [128, 4, D], BF16) for i in range(2)]

    def load_expert(e):
        b = e % 2
        cx.dma("pool", W1[b][:, :, :], dr["w1_e"][wl, e].rearrange("(k p) f -> p k f", p=128), w=["W1_%d" % b])
        cx.dma("pool", W3[b][:, :, :], dr["w3_e"][wl, e].rearrange("(k p) f -> p k f", p=128), w=["W3_%d" % b])
        cx.dma("pool", W2[b][:, :, :], dr["w2_e"][wl, e].rearrange("(k p) n -> p k n", p=128), w=["W2_%d" % b])
    load_expert(0)
    load_expert(1)
    zt = cx.sb("zt", [128, 4, D], BF16)
    cx.memset(zt[:, :, :], 0.0, w=["zt"], eng="pool")
    for i in range(NSLOT // 512):
        cx.dma("sp", Xg[i * 512:(i + 1) * 512, :].rearrange("(b p) d -> p b d", p=128), zt[:, :, :], r=["zt"], w=["D:XgZ%d" % i])
    WR = cx.sb("WRt", [128, 8, 16], BF16)
    cx.dma("pool", WR[:, :, :], dr["w_router"].rearrange("(k p) e -> p k e", p=128), w=["WRt"])
    brb = cx.sb("brb", [128, 16])
    cx.dma("sp", brb[:, :], dr["b_router"][0, :].partition_broadcast(128), w=["brb"])
    gsb = cx.sb("gsb2", [128, D]); shb = cx.sb("shb2", [128, D]); g2b = cx.sb("g2b2", [128, D])
    xt = [cx.sb("ext%d" % i, [128, D]) for i in range(2)]
    junk = cx.sb("ejunk", [128, D], BF16)
    t1s = [cx.sb("et1_%d" % i, [128, D]) for i in range(2)]
    hb = [cx.sb("ehb%d" % i, [128, D], BF16) for i in range(2)]
    hTt = [cx.sb("ehT%d" % i, [128, 8, 128], BF16) for i in range(2)]
    ssq = cx.sb("essq", [128, 2]); rstd = cx.sb("erstd", [128, 2])
    slf = cx.sb("slf", [128, NT, 2]); sli = cx.sb("sli", [128, NT, 2], mybir.dt.int32); wts = cx.sb("wts", [128, NT, 2])
    H2d = dr["H2d"]
    lg = cx.sb("lgall", [128, NT, 16])
    ti = 0
    tiles = []
    for (tag, T, xmid, xout) in segs:
        seg = 0 if tag == "l" else 1
        cx.dma("sp", gsb[:, :], dr["modv"][l, seg, 3, :].partition_broadcast(128), r=["D:modv%d" % l], w=["gsb2"])
        cx.dma("sp", shb[:, :], dr["modv"][l, seg, 4, :].partition_broadcast(128), r=["D:modv%d" % l], w=["shb2"])
        for j in range(T // 128):
            b = ti % 2
            kx = "ext%d" % b
            t1 = t1s[b]
            rows = slice(j * 128, (j + 1) * 128)
            tiles.append((tag, seg, xmid, xout, rows))
            cx.dma("sp", xt[b][:, :], xmid[rows, :], r=["D:xmid_" + tag], w=[kx])
            cx.act(junk[:, :], xt[b][:, :], AF.Square, accum_out=ssq[:, b:b + 1], r=[kx], w=["ejunk", "essq%d" % b])
            cx.act(rstd[:, b:b + 1], ssq[:, b:b + 1], AF.Sqrt, scale=1.0 / D, bias=EPS, r=["essq%d" % b], w=["ers%d" % b])
            cx.recip(rstd[:, b:b + 1], rstd[:, b:b + 1], r=["ers%d" % b], w=["ers%d" % b])
            cx.stt(t1[:, :], xt[b][:, :], rstd[:, b:b + 1], gsb[:, :], ALU.mult, ALU.mult, r=[kx, "ers%d" % b, "gsb2"], w=["et1_%d" % b])
            cx.tt(hb[b][:, :], t1[:, :], shb[:, :], ALU.add, r=["et1_%d" % b, "shb2"], w=["ehb%d" % b], eng="pool")
            cx.dma("sp", H2d[ti * 128:(ti + 1) * 128, :], hb[b][:, :], r=["ehb%d" % b], w=["D:H2d%d" % ti])
            for kc in range(8):
                cx.tr(psb[0][:, kc * 128:(kc + 1) * 128], hb[b][:, kc * 128:(kc + 1) * 128], ident[:, :],
                      r=["ehb%d" % b, "ident"], w=["psb0"])
            cx.copy(hTt[b][:, :, :], psb[0][:, :].rearrange("p (k t) -> p k t", k=8), r=["psb0"], w=["ehT%d" % b], eng="act")
            for kc in range(8):
                cx.mm(ps[0][:, 0:16], hTt[b][:, kc, :], WR[:, kc, :], kc == 0, kc == 7, r=["ehT%d" % b, "WRt"], w=["ps0"])
            cx.copy(lg[:, ti, :], ps[0][:, 0:16], r=["ps0"], w=["lg%d" % ti])
            ti += 1
    RT = lambda n: cx.sb("R" + n, [128, NT, 16])
    S_ = RT("s"); sbv = RT("sb"); tmp = RT("tmp"); sb2 = RT("sb2"); sbm = RT("sbm"); msk = RT("msk"); m2 = RT("m2"); sel = RT("sel")
    pos = RT("pos"); offs = RT("offs"); wv = RT("wv")
    g4 = cx.sb("Rg4", [128, NT, 4]); g4b = cx.sb("Rg4b", [128, NT, 4]); gm = cx.sb("Rgm", [128, NT, 4])
    e1 = cx.sb("Re1", [128, NT]); e2 = cx.sb("Re2", [128, NT]); den = cx.sb("Rden", [128, NT])
    f2 = lambda a: a[:, :, :].rearrange("p t e -> p (t e)")
    v4 = lambda a: a[:, :, :].rearrange("p t (g e) -> p t g e", g=4)
    bt = lambda a: a[:, :].unsqueeze(2).to_broadcast([128, NT, 16])
    bg = lambda a: a[:, :, :].unsqueeze(3).to_broadcast([128, NT, 4, 4])
    b16 = lambda a: a.unsqueeze(1).to_broadcast([128, NT, 16])
    lgk = ["lg%d" % t_ for t_ in range(NT)]
    cx.act(f2(S_), f2(lg), AF.Sigmoid, r=lgk, w=["Rs"])
    cx.tt(sbv[:, :, :], S_[:, :, :], b16(brb[:, :]), ALU.add, r=["Rs", "brb"], w=["Rsb"])
    cx.red(g4[:, :, :], v4(sbv), ALU.max, r=["Rsb"], w=["Rg4"])
    cx.tt(v4(tmp), v4(sbv), bg(g4), ALU.is_equal, r=["Rsb", "Rg4"], w=["Rtmp"])
    cx.stt(f2(sb2), f2(tmp), -1.0e9, f2(sbv), ALU.mult, ALU.add, r=["Rtmp", "Rsb"], w=["Rsb2"])
    cx.red(g4b[:, :, :], v4(sb2), ALU.max, r=["Rsb2"], w=["Rg4b"])
    cx.tt(g4[:, :, :], g4[:, :, :], g4b[:, :, :], ALU.add, r=["Rg4", "Rg4b"], w=["Rg4"])
    cx.red(e1[:, :], g4[:, :, :], ALU.max, r=["Rg4"], w=["Re1"])
    cx.tt(gm[:, :, :], g4[:, :, :], e1[:, :].unsqueeze(2).to_broadcast([128, NT, 4]), ALU.is_equal, r=["Rg4", "Re1"], w=["Rgm"])
    cx.ts(gm[:, :, :], gm[:, :, :], -1.0, ALU.add, s2=1.0e9, op1=ALU.mult, r=["Rgm"], w=["Rgm"])
    cx.tt(v4(sbm), v4(sbv), bg(gm), ALU.add, r=["Rsb", "Rgm"], w=["Rsbm"])
    cx.red(e1[:, :], sbm[:, :, :], ALU.max, r=["Rsbm"], w=["Re1"])
    cx.tt(msk[:, :, :], sbm[:, :, :], bt(e1), ALU.is_equal, r=["Rsbm", "Re1"], w=["Rmsk"])
    cx.stt(f2(sb2), f2(msk), -1.0e9, f2(sbm), ALU.mult, ALU.add, r=["Rmsk", "Rsbm"], w=["Rsb2"])
    cx.red(e2[:, :], sb2[:, :, :], ALU.max, r=["Rsb2"], w=["Re2"])
    cx.tt(m2[:, :, :], sb2[:, :, :], bt(e2), ALU.is_equal, r=["Rsb2", "Re2"], w=["Rm2"])
    cx.tt(sel[:, :, :], m2[:, :, :], msk[:, :, :], ALU.add, r=["Rm2", "Rmsk"], w=["Rsel"])
    cx.mm(ps[1][:, 0:NT * 16], cs(cst, "Ltri"), f2(sel), True, True, r=["cst", "Rsel"], w=["ps1"])
    cx.mm(ps[2][:, 0:NT * 16], cs(cst, "ones"), f2(sel), True, True, r=["cst", "Rsel"], w=["ps2"])
    cx.copy(f2(tmp), ps[2][:, 0:NT * 16], r=["ps2"], w=["Rtmp"])
    cx.memset(offs[:, 0, :], 0.0, w=["Roffs"])
    for t_ in range(1, NT):
        cx.tt(offs[:, t_, :], offs[:, t_ - 1, :], tmp[:, t_ - 1, :], ALU.add, r=["Roffs", "Rtmp"], w=["Roffs"])
    cx.tt(f2(pos), ps[1][:, 0:NT * 16], f2(offs), ALU.add, r=["ps1", "Roffs"], w=["Rpos"])
    cx.ts(f2(tmp), f2(pos), float(C) - 0.5, ALU.is_lt, r=["Rpos"], w=["Rtmp"])
    cx.tt(pos[:, :, :], pos[:, :, :], b16(cs(cst, "eoff")), ALU.add, r=["Rpos", "cst"], w=["Rpos"])
    cx.stt(f2(pos), f2(tmp), -1.0e6, f2(pos), ALU.mult, ALU.add, r=["Rtmp", "Rpos"], w=["Rpos"])
    cx.ts(f2(pos), f2(pos), 1.0e6, ALU.add, r=["Rpos"], w=["Rpos"])
    cx.tt(wv[:, :, :], sel[:, :, :], S_[:, :, :], ALU.mult, r=["Rsel", "Rs"], w=["Rwv"])
    cx.red(den[:, :], wv[:, :, :], ALU.add, r=["Rwv"], w=["Rden"])
    cx.recip(den[:, :], den[:, :], r=["Rden"], w=["Rden"])
    cx.tt(wv[:, :, :], wv[:, :, :], bt(den), ALU.mult, r=["Rwv", "Rden"], w=["Rwv"])
    cx.tt(wv[:, :, :], wv[:, :, :], tmp[:, :, :], ALU.mult, r=["Rwv", "Rtmp"], w=["Rwv"])
    for q, mk, kk in ((0, msk, "Rmsk"), (1, m2, "Rm2")):
        cx.tt(sbm[:, :, :], mk[:, :, :], pos[:, :, :], ALU.mult, r=[kk, "Rpos"], w=["Rsbm"])
        cx.red(slf[:, :, q], sbm[:, :, :], ALU.add, r=["Rsbm"], w=["slf%d" % q])
        cx.tt(sbm[:, :, :], mk[:, :, :], wv[:, :, :], ALU.mult, r=[kk, "Rwv"], w=["Rsbm"])
        cx.red(wts[:, :, q], sbm[:, :, :], ALU.add, r=["Rsbm"], w=["wtsq%d" % q])
    cx.copy(sli[:, :, :], slf[:, :, :], r=["slf0", "slf1"], w=["sli"])
    zk = ["D:XgZ%d" % i_ for i_ in range(NSLOT // 512)]
    for ti in range(NT):
        b = ti % 2
        cx.dma("sp", hb[b][:, :], H2d[ti * 128:(ti + 1) * 128, :], r=["D:H2d%d" % ti], w=["ehb%d" % b])
        for q in range(2):
            idx = sli[:, ti, q:q + 1]
            src = hb[b][:, :]
            cx.S.add("pool", lambda h, idx=idx, src=src: h.indirect_dma_start(
                out=Xg[:, :], out_offset=bass.IndirectOffsetOnAxis(ap=idx, axis=0), in_=src, in_offset=None,
                bounds_check=bc(h), oob_is_err=False), r=["sli", "ehb%d" % b] + zk, w=["D:Xg%d_%d" % (ti, q)], dma=True)
    dummy = cx.sb("dummy", [128, 4])
    cx.memset(dummy[:, 0:1], 0.0, w=["XgAll"], eng="pool")
    cx.S.ops[-1].deps.update({cx.S.lastw[k]: True for k in ["D:Xg%d_%d" % (t_, q) for t_ in range(NT) for q in range(2)]})
    NBLK = C // 128
    NPC = (C + 511) // 512
    PW = C // NPC
    xg = [cx.sb("xg%d" % i, [128, NBLK, D], BF16) for i in range(2)]
    xT = [cx.sb("xTe%d" % i, [128, 8, C], BF16) for i in range(2)]
    uT = cx.sb("uTe", [128, 4, C], BF16)
    s1 = [cx.sb("s1_%d" % i, [128, 512]) for i in range(2)]
    yb = [cx.sb("ybe%d" % i, [128, D]) for i in range(2)]
    yi = 0

    def load_xg(e):
        cx.dma("sp", xg[e % 2][:, :, :], Xg[e * C:(e + 1) * C, :].rearrange("(j p) d -> p j d", p=128), r=["XgAll"], w=["xg%d" % (e % 2)])
    load_xg(0)
    for e in range(NEXP):
        b = e % 2
        if e >= 2:
            load_expert(e)
        if e + 1 < NEXP:
            load_xg(e + 1)
        for j in range(NBLK):
            pbk = psb[j % 2]
            kp = "psb%d" % (j % 2)
            for kc in range(8):
                cx.tr(pbk[:, kc * 128:(kc + 1) * 128], xg[b][:, j, kc * 128:(kc + 1) * 128], ident[:, :],
                      r=["xg%d" % b, "ident"], w=[kp])
            cx.copy(xT[b][:, :, j * 128:(j + 1) * 128], pbk[:, :].rearrange("p (k t) -> p k t", k=8), r=[kp], w=["xTe%d" % b],
                    eng=("act" if j % 2 == 0 else "dve"))
        it = 0
        for fc in range(4):
            fs = slice(fc * 128, (fc + 1) * 128)
            for pc in range(NPC):
                cs_ = slice(pc * PW, (pc + 1) * PW)
                pa = ps[(it % 2) * 2]; pb = ps[(it % 2) * 2 + 1]
                ka = "ps%d" % ((it % 2) * 2); kb = "ps%d" % ((it % 2) * 2 + 1)
                for kc in range(8):
                    cx.mm(pa[:, 0:PW], W1[b][:, kc, fs], xT[b][:, kc, cs_], kc == 0, kc == 7, r=["xTe%d" % b, "W1_%d" % b], w=[ka])
                for kc in range(8):
                    cx.mm(pb[:, 0:PW], W3[b][:, kc, fs], xT[b][:, kc, cs_], kc == 0, kc == 7, r=["xTe%d" % b, "W3_%d" % b], w=[kb])
                sb_ = s1[it % 2]
                cx.act(sb_[:, 0:PW], pa[:, 0:PW], AF.Silu, r=[ka], w=["s1_%d" % (it % 2)])
                cx.tt(uT[:, fc, cs_], pb[:, 0:PW], sb_[:, 0:PW], ALU.mult, r=[kb, "s1_%d" % (it % 2)], w=["uTe"])
                it += 1
        for j in range(NBLK):
            y = yb[yi % 2]
            ky = "ybe%d" % (yi % 2)
            yi += 1
            for nh in range(2):
                po = ps[4 + nh]
                for fc in range(4):
                    cx.mm(po[:, :], uT[:, fc, j * 128:(j + 1) * 128], W2[b][:, fc, nh * 512:(nh + 1) * 512], fc == 0, fc == 3,
                          r=["uTe", "W2_%d" % b], w=["ps%d" % (4 + nh)])
                cx.copy(y[:, nh * 512:(nh + 1) * 512], po[:, :], r=["ps%d" % (4 + nh)], w=[ky], eng=("act" if nh == 0 else "dve"))
            cx.dma("sp", Yg[e * C + j * 128:e * C + (j + 1) * 128, :], y[:, :], r=[ky], w=["D:Yg%d_%d" % (e, j)])
    if final:
        gfb = cx.sb("gfb", [128, D])
        cx.dma("sp", gfb[:, :], dr["g_final"][0, :].partition_broadcast(128), w=["gfb"])
    cx.memset(dummy[:, 1:2], 0.0, w=["YgAll"], eng="pool")
    cx.S.ops[-1].deps.update({cx.S.lastw[k]: True for k in ["D:Yg%d_%d" % (e_, j_) for e_ in range(NEXP) for j_ in range(C // 128)]})
    yg = [[cx.sb("yg%d_%d" % (i, q), [128, D]) for q in range(2)] for i in range(2)]
    for i in range(2):
        for q in range(2):
            cx.memset(yg[i][q][:, :], 0.0, w=["yg%d_%d" % (i, q)], eng="pool")
    cur = None
    for ti, (tag, seg, xmid, xout, rows) in enumerate(tiles):
        if cur != seg:
            cx.dma("sp", g2b[:, :], dr["modv"][l, seg, 5, :].partition_broadcast(128), r=["D:modv%d" % l], w=["g2b2"])
            cur = seg
        b = ti % 2
        kx = "ext%d" % b
        t1 = t1s[b]
        kt1 = "et1_%d" % b
        cx.dma("sp", xt[b][:, :], xmid[rows, :], r=["D:xmid_" + tag], w=[kx])
        for q in range(2):
            dst = yg[b][q][:, :]
            idx = sli[:, ti, q:q + 1]
            cx.S.add("pool", lambda h, idx=idx, dst=dst: h.indirect_dma_start(
                out=dst, out_offset=None, in_=Yg[:, :], in_offset=bass.IndirectOffsetOnAxis(ap=idx, axis=0),
                bounds_check=bc(h), oob_is_err=False), r=["sli", "YgAll"], w=["yg%d_%d" % (b, q)], dma=True)
        cx.ts(t1[:, :], yg[b][0][:, :], wts[:, ti, 0:1], ALU.mult, r=["yg%d_0" % b, "wtsq0"], w=[kt1])
        cx.stt(t1[:, :], yg[b][1][:, :], wts[:, ti, 1:2], t1[:, :], ALU.mult, ALU.add, r=["yg%d_1" % b, "wtsq1", kt1], w=[kt1])
        cx.tt(t1[:, :], t1[:, :], g2b[:, :], ALU.mult, r=[kt1, "g2b2"], w=[kt1])
        cx.tt(xt[b][:, :], xt[b][:, :], t1[:, :], ALU.add, r=[kx, kt1], w=[kx])
        if final:
            cx.act(junk[:, :], xt[b][:, :], AF.Square, accum_out=ssq[:, b:b + 1], r=[kx], w=["ejunk", "essq%d" % b])
            cx.act(rstd[:, b:b + 1], ssq[:, b:b + 1], AF.Sqrt, scale=1.0 / D, bias=EPS, r=["essq%d" % b], w=["ers%d" % b])
            cx.recip(rstd[:, b:b + 1], rstd[:, b:b + 1], r=["ers%d" % b], w=["ers%d" % b])
            cx.stt(xt[b][:, :], xt[b][:, :], rstd[:, b:b + 1], gfb[:, :], ALU.mult, ALU.mult, r=[kx, "ers%d" % b, "gfb"], w=[kx])
        cx.dma("sp", xout[rows, :], xt[b][:, :], r=[kx], w=["D:xout_" + tag])
    cx.end()


def make_expo(core):
    BIG = 1.0e7
    e = np.full((2, 9), BIG, np.float32)
    for c2 in range(NCORE):
        if c2 < core:
            e[0, c2] = TL * (core - 1 - c2)
        if c2 > core:
            e[1, c2] = TL * (c2 - core - 1)
    e[0, 8] = TL * core
    e[1, 8] = TL * (NCORE - 1 - core)
    return np.broadcast_to(e[None], (128, 2, 9)).copy()


def make_expo(core):
    BIG = 1.0e7
    e = np.full((2, 9), BIG, np.float32)
    for c2 in range(NCORE):
        if c2 < core:
            e[0, c2] = TL * (core - 1 - c2)
        if c2 > core:
            e[1, c2] = TL * (c2 - core - 1)
    e[0, 8] = TL * core
    e[1, 8] = TL * (NCORE - 1 - core)
    return np.broadcast_to(e[None], (128, 2, 9)).copy()


def make_sel(core):
    s = np.zeros((128, 2, NCORE), np.float32)
    if core > 0:
        s[:, 0, core - 1] = 1.0
    if core < NCORE - 1:
        s[:, 1, core + 1] = 1.0
    return s


def phase_exchange(cx, l, dr):
    cx.begin(nf=0, nb=0)
    hin = dr["hin"]
    for a, (src, c2) in enumerate(((dr["uT_l"], 0), (dr["uT_l"], 1), (dr["tT_l"], 0), (dr["tT_l"], 1))):
        cx.dma("sp", hin[a * 128:(a + 1) * 128, 0:16], src[c2, :, 0:16], r=["D:uT_l", "D:tT_l"], w=["D:hin"])
        cx.dma("sp", hin[a * 128:(a + 1) * 128, 16:32], src[c2, :, TL - 16:TL], r=["D:uT_l", "D:tT_l"], w=["D:hin"])
    grp = [list(range(NCORE))]

    def cc(src, dst, rk, wk):
        cx.S.add("pool", lambda h: h.collective_compute("AllGather", ALU.bypass, replica_groups=grp, ins=[src], outs=[dst]),
                 r=rk, w=wk, cc=True)
    cc(dr["kT_l"].rearrange("c p t -> (c p) t").opt(), dr["gk"].opt(), ["D:rope_l12", "D:rope_l13"], ["D:gk"])
    cc(dr["V_l"].opt(), dr["gv"].opt(), ["D:V_l"], ["D:gv"])
    cc(dr["Tst_l"].rearrange("a p e -> (a p) e").opt(), dr["gt"].opt(), ["D:Tst_l"], ["D:gt"])
    cc(hin.opt(), dr["hg"].opt(), ["D:hin"], ["D:hg"])
    cx.end()


A_OUT = (("hT", lambda T: [128, 8, T], BF16), ("uT", lambda T: [2, 128, T], F32), ("tT", lambda T: [2, 128, T], F32),
         ("bgT", lambda T: [2, 128, T], BF16), ("qT", lambda T: [2, 128, T], BF16), ("kT", lambda T: [2, 128, T], BF16),
         ("rqT", lambda T: [128, T], BF16), ("rkT", lambda T: [128, T], BF16), ("V", lambda T: [T, 256], BF16),
         ("rv", lambda T: [T, 256], BF16), ("rg", lambda T: [T, 256], BF16), ("Tst", lambda T: [2, 128, 256], F32))
SEGT = (("l", TL), ("c", TC))
NKALL = (SEQ + TC) // 128
EXT_IN = (("c", [1, D]), ("c_ctx", [1, D]), ("w_mod", [2, D, 6 * D]), ("b_mod", [2, 6 * D]), ("g_norm1", [2, D]), ("g_norm2", [2, D]),
          ("w_in", [2, D, INC]), ("conv_a_w", [2, 31, 256]), ("conv_a_b", [2, 256]), ("conv_a_g", [2, 256]),
          ("conv_a_beta", [2, 256]), ("conv_b_w", [2, 3, 256]), ("lam_q1", [2, 32]), ("lam_k1", [2, 32]), ("lam_q2", [2, 32]),
          ("lam_k2", [2, 32]), ("diff_g", [2, 64]), ("ret_ld_f", [2, 4]), ("ret_ld_b", [2, 4]), ("w_gate", [2, D, 4096]),
          ("b_gate", [2, 4096]), ("w_branch", [2, 4, 256, D]), ("w_o", [2, D, D]), ("w_router", [D, 16]), ("b_router", [1, 16]),
          ("w1_e", [2, NEXP, D, DFF]), ("w3_e", [2, NEXP, D, DFF]), ("w2_e", [2, NEXP, DFF, D]), ("g_final", [1, D]))


SPARSE_MOE = True


def moe_phase(cx, l, dr, segs, final, wl=None):
    if SPARSE_MOE:
        return phase_moe_sparse(cx, l, dr, segs, final, wl=wl)
    return phase_moe(cx, l, dr, segs, final, wl=wl)


def lam_init_of(l):
    return 0.8 - 0.6 * math.exp(-0.3 * l)


class Launch:
    def __init__(self):
        self.nc = bass.Bass("TRN2", target_bir_lowering=False)
        self.dr = {}
        self.ins = []
        self.outs = []

    def t(self, name, shape, dt=F32, kind=None):
        if kind is None:
            self.dr[name] = self.nc.dram_tensor(name, list(shape), dt).ap()
        else:
            self.dr[name] = self.nc.dram_tensor(name, list(shape), dt, kind=kind).ap()
        if kind == "ExternalInput":
            self.ins.append(name)
        elif kind == "ExternalOutput":
            self.outs.append(name)


def build_fused():
    L = Launch()
    L.t("cst", [128, CSTW], F32, "ExternalInput")
    L.t("ident", [128, 128], BF16, "ExternalInput")
    for n_, s_ in EXT_IN:
        L.t(n_, s_, F32, "ExternalInput")
    for n_, s_ in (("cosT", [128, TL]), ("sinT", [128, TL]), ("x_l", [TL, D]), ("x_c", [TC, D]), ("expo", [128, 2, 9]),
                   ("sel", [128, 2, NCORE])):
        L.t(n_, s_, F32, "ExternalInput")
    L.t("out", [TL, D], F32, "ExternalOutput")
    L.t("modv", [2, 2, 6, D])
    L.t("x1_l", [TL, D])
    L.t("x1_c", [TC, D])
    L.t("Xg", [NSLOT, D], BF16)
    L.t("Yg", [NSLOT, D], F32)
    L.t("H2d", [TL + TC, D], BF16)
    drl = []
    for l in range(DEPTH):
        d_ = dict(L.dr)
        for tag, T in SEGT:
            for nm, shp, dt in A_OUT:
                L.t("%s_%s%d" % (nm, tag, l), shp(T), dt)
                d_[nm + "_" + tag] = L.dr["%s_%s%d" % (nm, tag, l)]
            L.t("ysT_%s%d" % (tag, l), [8, 128, T], BF16)
            L.t("xmid_%s%d" % (tag, l), [T, D])
            d_["ysT_" + tag] = L.dr["ysT_%s%d" % (tag, l)]
            d_["xmid_" + tag] = L.dr["xmid_%s%d" % (tag, l)]
        for nm, shp, dt in (("gk", [NCORE * 256, TL], BF16), ("gv", [NCORE * TL, 256], BF16), ("gt", [NCORE * 256, 256], F32),
                            ("hg", [NCORE * 512, 32], F32), ("hin", [512, 32], F32)):
            L.t("%s%d" % (nm, l), shp, dt)
            d_[nm] = L.dr["%s%d" % (nm, l)]
        drl.append(d_)
    for d_ in drl:
        for k in ("modv", "x1_l", "x1_c", "Xg", "Yg", "H2d"):
            d_[k] = L.dr[k]
    with ExitStack() as st:
        S = Sched(L.nc, st)
        cx = Ctx(L.nc, S)
        phase_mods(cx, 0, drl[0])
        phase_mods(cx, 1, drl[0])
        d0 = drl[0]
        phase_a(cx, 0, d0, [("l", TL, L.dr["x_l"]), ("c", TC, L.dr["x_c"])])
        phase_exchange(cx, 0, d0)
        phase_conv(cx, 0, d0, [("l", TL), ("c", TC)])
        phase_attn(cx, 0, d0, [("l", TL, NKALL), ("c", TC, TC // 128)], lam_init_of(0))
        phase_ret(cx, 0, d0, [("l", TL), ("c", TC)])
        phase_merge(cx, 0, d0, [("l", TL, L.dr["x_l"], d0["xmid_l"]), ("c", TC, L.dr["x_c"], d0["xmid_c"])])
        moe_phase(cx, 0, d0, [("l", TL, d0["xmid_l"], L.dr["x1_l"]), ("c", TC, d0["xmid_c"], L.dr["x1_c"])], False)
        d1 = drl[1]
        phase_a(cx, 1, d1, [("l", TL, L.dr["x1_l"]), ("c", TC, L.dr["x1_c"])])
        phase_exchange(cx, 1, d1)
        phase_conv(cx, 1, d1, [("l", TL)])
        phase_attn(cx, 1, d1, [("l", TL, NKALL)], lam_init_of(1))
        phase_ret(cx, 1, d1, [("l", TL)])
        phase_merge(cx, 1, d1, [("l", TL, L.dr["x1_l"], d1["xmid_l"])])
        moe_phase(cx, 1, d1, [("l", TL, d1["xmid_l"], L.dr["out"])], True)
    return L


def kernel_fused(**inp):
    f32 = lambda a: np.ascontiguousarray(np.asarray(a, dtype=np.float32))
    x = f32(inp["x"])[0]
    ctx = f32(inp["ctx"])[0]
    base = dict(cst=make_cst(), ident=np.eye(128, dtype=np.float32).astype(NPBF), x_c=ctx)
    for n_, s_ in EXT_IN:
        base[n_] = f32(inp[n_]).reshape(s_)
    L = build_fused()
    maps = []
    for c in range(NCORE):
        m = dict(base)
        cosT, sinT = rope_tables(c)
        m.update(cosT=cosT, sinT=sinT, x_l=x[c * TL:(c + 1) * TL], expo=make_expo(c), sel=make_sel(c))
        maps.append({k: m[k] for k in L.ins})
    res = run_bass_kernel_spmd(L.nc, maps, core_ids=list(range(NCORE)))
    out = np.concatenate([np.asarray(res.results[c]["out"]) for c in range(NCORE)], axis=0)
    return out.reshape(1, SEQ, D).astype(np.float32)


WSLICE = ("w_in", "w_gate", "w_branch", "w_o", "w1_e", "w3_e", "w2_e")
GATH = (("gk", [NCORE * 256, TL], BF16), ("gv", [NCORE * TL, 256], BF16), ("gt", [NCORE * 256, 256], F32),
        ("hg", [NCORE * 512, 32], F32))


def build_stage(stage):
    L = Launch()
    L.t("cst", [128, CSTW], F32, "ExternalInput")
    L.t("ident", [128, 128], BF16, "ExternalInput")
    for n_, s_ in EXT_IN:
        if stage > 1 and n_ in ("w_mod", "b_mod", "c", "c_ctx"):
            continue
        if stage == 1 and n_ in ("w_gate", "w_branch", "w_o", "w1_e", "w3_e", "w2_e"):
            continue
        if stage == 3 and n_ == "w_in":
            continue
        shp = [1] + list(s_[1:]) if n_ in WSLICE else s_
        L.t(n_, shp, F32, "ExternalInput")
    for n_, s_ in (("cosT", [128, TL]), ("sinT", [128, TL]), ("expo", [128, 2, 9]), ("sel", [128, 2, NCORE])):
        L.t(n_, s_, F32, "ExternalInput")
    io = "ExternalInput"
    if stage == 1:
        L.t("x_l", [TL, D], F32, io)
        L.t("x_c", [TC, D], F32, io)
        L.t("modv", [2, 2, 6, D], F32, "ExternalOutput")
    else:
        L.t("modv", [2, 2, 6, D], F32, io)

    def a_tensors(prefix, kind, tags):
        d_ = {}
        for tag, T in SEGT:
            if tag not in tags:
                continue
            for nm, shp, dt in A_OUT:
                L.t(prefix + nm + "_" + tag, shp(T), dt, kind)
                d_[nm + "_" + tag] = L.dr[prefix + nm + "_" + tag]
        return d_
    with ExitStack() as st:
        S = Sched(L.nc, st)
        cx = Ctx(L.nc, S)
        if stage == 1:
            dA = dict(L.dr)
            dA.update(a_tensors("", "ExternalOutput", ("l", "c")))
            phase_mods(cx, 0, dA)
            phase_mods(cx, 1, dA)
            phase_a(cx, 0, dA, [("l", TL, L.dr["x_l"]), ("c", TC, L.dr["x_c"])], wl=0)
        else:
            l = stage - 2
            tags = ("l", "c")
            for nm, shp, dt in GATH:
                L.t(nm, shp, dt, io)
            L.t("Xg", [NSLOT, D], BF16)
            L.t("Yg", [NSLOT, D], F32)
            L.t("H2d", [TL + TC, D], BF16)
            dB = dict(L.dr)
            dB.update(a_tensors("b_", io, tags))
            segs = [("l", TL), ("c", TC)] if l == 0 else [("l", TL)]
            for tag, T in segs:
                L.t("ysT_" + tag, [8, 128, T], BF16)
                L.t("xmid_" + tag, [T, D])
                dB["ysT_" + tag] = L.dr["ysT_" + tag]
                dB["xmid_" + tag] = L.dr["xmid_" + tag]
            if l == 0:
                L.t("x_l", [TL, D], F32, io)
                L.t("x_c", [TC, D], F32, io)
                L.t("x1_l", [TL, D], F32, "ExternalOutput")
                L.t("x1_c", [TC, D])
                xin_l, xin_c, xo_l, xo_c = L.dr["x_l"], L.dr["x_c"], L.dr["x1_l"], L.dr["x1_c"]
            else:
                L.t("x1_l", [TL, D], F32, io)
                L.t("out", [TL, D], F32, "ExternalOutput")
                xin_l, xo_l = L.dr["x1_l"], L.dr["out"]
            phase_conv(cx, l, dB, segs)
            phase_attn(cx, l, dB, [("l", TL, NKALL)] + ([("c", TC, TC // 128)] if l == 0 else []), lam_init_of(l))
            phase_ret(cx, l, dB, segs)
            if l == 0:
                phase_merge(cx, l, dB, [("l", TL, xin_l, dB["xmid_l"]), ("c", TC, xin_c, dB["xmid_c"])], wl=0)
                moe_phase(cx, l, dB, [("l", TL, dB["xmid_l"], xo_l), ("c", TC, dB["xmid_c"], xo_c)], False, wl=0)
                dA = dict(L.dr)
                dA.update(a_tensors("", "ExternalOutput", ("l", "c")))
                phase_a(cx, 1, dA, [("l", TL, xo_l), ("c", TC, xo_c)], wl=0)
            else:
                phase_merge(cx, l, dB, [("l", TL, xin_l, dB["xmid_l"])], wl=0)
                moe_phase(cx, l, dB, [("l", TL, dB["xmid_l"], xo_l)], True, wl=0)
    return L


def host_gather(oA):
    gk = np.concatenate([np.asarray(o["kT_l"]).reshape(256, TL) for o in oA], axis=0)
    gv = np.concatenate([np.asarray(o["V_l"]) for o in oA], axis=0)
    gt = np.concatenate([np.asarray(o["Tst_l"]).reshape(256, 256) for o in oA], axis=0)
    hs = []
    for o in oA:
        u = np.asarray(o["uT_l"])
        t = np.asarray(o["tT_l"])
        h = np.concatenate([np.concatenate([a[c2][:, 0:16], a[c2][:, TL - 16:TL]], axis=1) for a in (u, t) for c2 in range(2)], axis=0)
        hs.append(h)
    hg = np.concatenate(hs, axis=0).astype(np.float32)
    return dict(gk=gk, gv=gv, gt=gt, hg=hg)


def kernel_unfused(**inp):
    f32 = lambda a: np.ascontiguousarray(np.asarray(a, dtype=np.float32))
    x = f32(inp["x"])[0]
    ctx = f32(inp["ctx"])[0]
    ropes = [rope_tables(c) for c in range(NCORE)]
    full = {n_: f32(inp[n_]).reshape(s_) for n_, s_ in EXT_IN}
    cst = make_cst()
    ident = np.eye(128, dtype=np.float32).astype(NPBF)

    def run(L, extra, wl):
        maps = []
        for c in range(NCORE):
            m = dict(cst=cst, ident=ident, cosT=ropes[c][0], sinT=ropes[c][1], expo=make_expo(c), sel=make_sel(c),
                     x_l=x[c * TL:(c + 1) * TL], x_c=ctx)
            for k, v in full.items():
                m[k] = v[wl[k]:wl[k] + 1] if k in WSLICE else v
            m.update(extra[c])
            maps.append({k: m[k] for k in L.ins})
        res = run_bass_kernel_spmd(L.nc, maps, core_ids=list(range(NCORE)))
        return [{k: np.asarray(r[k]) for k in L.outs} for r in res.results]

    o1 = run(build_stage(1), [dict() for _ in range(NCORE)], dict.fromkeys(WSLICE, 0))
    modv = o1[0]["modv"]

    def b_extra(oA):
        g = host_gather(oA)
        ex = []
        for c in range(NCORE):
            e = dict(g)
            e["modv"] = modv
            for k, v in oA[c].items():
                if k != "modv" and k != "x1_l":
                    e["b_" + k] = v
            ex.append(e)
        return ex
    wl2 = dict.fromkeys(WSLICE, 0)
    wl2["w_in"] = 1
    o2 = run(build_stage(2), b_extra(o1), wl2)
    ex3 = b_extra(o2)
    for c in range(NCORE):
        ex3[c]["x1_l"] = o2[c]["x1_l"]
    o3 = run(build_stage(3), ex3, dict.fromkeys(WSLICE, 1))
    out = np.concatenate([o3[c]["out"] for c in range(NCORE)], axis=0)
    return out.reshape(1, SEQ, D).astype(np.float32)


FUSED = False


def kernel(**inp):
    return kernel_fused(**inp) if FUSED else kernel_unfused(**inp)
```
